# Optimizing a Trainium2 kernel written in Bass

```python
import jax, jax.numpy as jnp
from jax import lax
import numpy as np

D_MODEL = 1024
BATCH = 2
SEQ = 8192
DEPTH = 2

GRID_W = 64
MLSTM_HEADS = 4
MLSTM_HD = 128
MLSTM_W = MLSTM_HEADS * MLSTM_HD
MLSTM_CHUNK = 64
CONV_W = 5
ATTN_HEADS = 8
KV_HEADS = 2
ATTN_HD = 64
ATTN_W = ATTN_HEADS * ATTN_HD
KV_W = KV_HEADS * ATTN_HD
Q_BLOCK = 128
ROPE_BASE = 10000.0
N_GROUPS = 4
EXPERTS_PER_GROUP = 8
N_EXPERTS = N_GROUPS * EXPERTS_PER_GROUP
TOP_K = 2
EXPERT_HIDDEN = 512
NORM_EPS = 1e-6
SPLIT_SIZES = (MLSTM_W, MLSTM_W, MLSTM_W, MLSTM_W, 4 * MLSTM_HEADS, ATTN_W, KV_W, KV_W, D_MODEL, D_MODEL)
IN_W = sum(SPLIT_SIZES)

kernel_name = 'hybrid_mlstm_gqa_hmoe_encoder'


def rms_norm(x, g):
    xf = x.astype(jnp.float32)
    y = xf * lax.rsqrt(jnp.mean(xf * xf, axis=-1, keepdims=True) + NORM_EPS)
    return (y * g.astype(jnp.float32)).astype(x.dtype)


def rope_tables(seq):
    rows = seq // GRID_W
    row = jnp.repeat(jnp.arange(rows, dtype=jnp.float32), GRID_W)
    col = jnp.tile(jnp.arange(GRID_W, dtype=jnp.float32), rows)
    half = ATTN_HD // 2
    inv_freq = ROPE_BASE ** (-jnp.arange(0, half, 2, dtype=jnp.float32) / half)
    ang_r = row[:, None] * inv_freq
    ang_c = col[:, None] * inv_freq
    return (jnp.cos(ang_r), jnp.sin(ang_r), jnp.cos(ang_c), jnp.sin(ang_c))


def rotate(x, cos, sin):
    x1, x2 = jnp.split(x, 2, axis=-1)
    return jnp.concatenate([x1 * cos - x2 * sin, x2 * cos + x1 * sin], axis=-1)


def axial_rope(x, tables):
    cr, sr, cc, sc = [t[:, None, :] for t in tables]
    xr, xc = jnp.split(x, 2, axis=-1)
    return jnp.concatenate([rotate(xr, cr, sr), rotate(xc, cc, sc)], axis=-1)


def bidirectional_gqa(q, k, v, q_g, k_g, tables):
    B, S = q.shape[0], q.shape[1]
    G = ATTN_HEADS // KV_HEADS
    q = axial_rope(rms_norm(q, q_g).astype(jnp.float32), tables)
    k = axial_rope(rms_norm(k, k_g).astype(jnp.float32), tables)
    nb = S // Q_BLOCK
    qb = q.reshape(B, nb, Q_BLOCK, KV_HEADS, G, ATTN_HD).transpose(1, 0, 3, 4, 2, 5)
    kt = k.transpose(0, 2, 1, 3)
    vt = v.transpose(0, 2, 1, 3)
    scale = ATTN_HD ** -0.5

    def block(q_blk):
        s = jnp.einsum('bkgqd,bksd->bkgqs', q_blk, kt) * scale
        p = jax.nn.softmax(s.astype(jnp.float32), axis=-1)
        return jnp.einsum('bkgqs,bksd->bkgqd', p.astype(vt.dtype), vt)

    o = lax.map(block, qb)
    return o.transpose(1, 0, 4, 2, 3, 5).reshape(B, S, ATTN_W)


def mlstm_one_direction(q, k, v, log_i, log_f):
    B, H, S, dh = q.shape
    L = MLSTM_CHUNK
    nc = S // L
    q = q.reshape(B, H, nc, L, dh)
    k = k.reshape(B, H, nc, L, dh)
    v = v.reshape(B, H, nc, L, dh)
    li = log_i.reshape(B, H, nc, L)
    lf = log_f.reshape(B, H, nc, L)
    b = jnp.cumsum(lf, axis=-1)
    b_last = b[..., -1]
    a = b_last[..., None] - b + li
    a_max = jnp.max(a, axis=-1)
    w = jnp.exp(a - a_max[..., None])
    c_chunk = jnp.einsum('bhcl,bhcld,bhcle->bhcde', w, v, k)
    n_chunk = jnp.einsum('bhcl,bhcle->bhce', w, k)

    def step(carry, inp):
        c_st, n_st, m_st = carry
        c_in, n_in, bl, am = inp
        m_new = jnp.maximum(bl + m_st, am)
        decay = jnp.exp(bl + m_st - m_new)
        inject = jnp.exp(am - m_new)
        c_new = decay[..., None, None] * c_st + inject[..., None, None] * c_in
        n_new = decay[..., None] * n_st + inject[..., None] * n_in
        return (c_new, n_new, m_new), (c_st, n_st, m_st)

    init = (jnp.zeros((B, H, dh, dh), jnp.float32), jnp.zeros((B, H, dh), jnp.float32), jnp.zeros((B, H), jnp.float32))
    xs = (jnp.moveaxis(c_chunk, 2, 0), jnp.moveaxis(n_chunk, 2, 0), jnp.moveaxis(b_last, 2, 0), jnp.moveaxis(a_max, 2, 0))
    _, (c_prev, n_prev, m_prev) = lax.scan(step, init, xs)
    c_prev = jnp.moveaxis(c_prev, 0, 2)
    n_prev = jnp.moveaxis(n_prev, 0, 2)
    m_prev = jnp.moveaxis(m_prev, 0, 2)

    lower = jnp.tril(jnp.ones((L, L), dtype=bool))
    d = jnp.where(lower, b[..., :, None] - b[..., None, :] + li[..., None, :], -jnp.inf)
    m_inter = b + m_prev[..., None]
    m_out = jnp.maximum(m_inter, jnp.max(d, axis=-1))
    dw = jnp.exp(d - m_out[..., None])
    inter_w = jnp.exp(m_inter - m_out)
    s = jnp.einsum('bhcjd,bhcld->bhcjl', q, k) * dw
    num = jnp.einsum('bhcjl,bhcld->bhcjd', s, v) + inter_w[..., None] * jnp.einsum('bhcde,bhcje->bhcjd', c_prev, q)
    den = jnp.sum(s, axis=-1) + inter_w * jnp.einsum('bhce,bhcje->bhcj', n_prev, q)
    h = num / jnp.maximum(jnp.abs(den), jnp.exp(-m_out))[..., None]
    return h.reshape(B, H, S, dh)


def mlstm_mixer(q_pre, k_pre, v, o_pre, gates, conv_w, conv_b, norm_g):
    B, S = q_pre.shape[0], q_pre.shape[1]
    H, dh = MLSTM_HEADS, MLSTM_HD
    qk = jnp.concatenate([q_pre, k_pre], axis=-1)
    qk = lax.conv_general_dilated(qk, conv_w, window_strides=(1,), padding=[(CONV_W // 2, CONV_W // 2)],
                                  dimension_numbers=('NWC', 'WIO', 'NWC'), feature_group_count=2 * MLSTM_W) + conv_b
    qk = jax.nn.silu(qk)
    q, k = jnp.split(qk, 2, axis=-1)

    def heads(t):
        return t.reshape(B, S, H, dh).transpose(0, 2, 1, 3).astype(jnp.float32)

    q = heads(q) * (MLSTM_HD ** -0.5)
    k = heads(k)
    v = heads(v)
    g = gates.astype(jnp.float32).reshape(B, S, 4, H).transpose(2, 0, 3, 1)
    h_fwd = mlstm_one_direction(q, k, v, g[0], jax.nn.log_sigmoid(g[1]))

    def flip(t):
        return jnp.flip(t, axis=2)

    h_bwd = flip(mlstm_one_direction(flip(q), flip(k), flip(v), flip(g[2]), flip(jax.nn.log_sigmoid(g[3]))))
    h = (h_fwd + h_bwd).transpose(0, 2, 1, 3)
    h = rms_norm(h, norm_g.reshape(H, dh))
    return h.reshape(B, S, MLSTM_W).astype(o_pre.dtype) * jax.nn.sigmoid(o_pre)


def hierarchical_moe(h, w_rg, b_rg, w_re, b_re, w_gate, w_up, w_down):
    B, S, D = h.shape
    t = h.reshape(B * S, D)
    tf = t.astype(jnp.float32)
    p_group = jax.nn.softmax(tf @ w_rg.astype(jnp.float32) + b_rg.astype(jnp.float32), axis=-1)
    p_top, g_idx = lax.top_k(p_group, 1)
    e_logits = (tf @ w_re.astype(jnp.float32) + b_re.astype(jnp.float32)).reshape(-1, N_GROUPS, EXPERTS_PER_GROUP)
    e_logits = jnp.take_along_axis(e_logits, g_idx[:, :, None], axis=1)[:, 0]
    p_exp = jax.nn.softmax(e_logits, axis=-1)
    w_top, e_idx = lax.top_k(p_exp, TOP_K)
    w_top = w_top / jnp.sum(w_top, axis=-1, keepdims=True) * p_top
    expert_id = g_idx * EXPERTS_PER_GROUP + e_idx
    combine = jnp.sum(jax.nn.one_hot(expert_id, N_EXPERTS, dtype=jnp.float32) * w_top[..., None], axis=1).astype(t.dtype)
    out = jnp.zeros_like(t)
    for g in range(N_GROUPS):
        sl = slice(g * EXPERTS_PER_GROUP, (g + 1) * EXPERTS_PER_GROUP)
        a = jnp.einsum('td,edf->tef', t, w_gate[sl])
        u = jnp.einsum('td,edf->tef', t, w_up[sl])
        act = jax.nn.silu(a) * u * combine[:, sl, None]
        out = out + jnp.einsum('tef,efd->td', act, w_down[sl])
    return out.reshape(B, S, D)


def setup_inputs(seed: int = 0) -> dict:
    key = jax.random.key(seed)
    ks = iter(jax.random.split(key, 32))

    def normal(shape, scale):
        return scale * jax.random.normal(next(ks), shape, jnp.float32)

    D, H, F = D_MODEL, MLSTM_HEADS, EXPERT_HIDDEN
    x = normal((BATCH, SEQ, D), 1.0)
    c = normal((BATCH, D), 1.0)
    w_ada = normal((DEPTH, D, 6 * D), 0.5 * D ** -0.5)
    b_ada = normal((DEPTH, 6 * D), 0.02)
    norm1_g = 1.0 + normal((DEPTH, D), 0.02)
    w_in = normal((DEPTH, D, IN_W), D ** -0.5)
    b_in = normal((DEPTH, IN_W), 0.02)
    f_off = 4 * MLSTM_W
    f_bias = jnp.linspace(3.0, 6.0, H, dtype=jnp.float32)
    b_in = b_in.at[:, f_off + H:f_off + 2 * H].add(f_bias).at[:, f_off + 3 * H:f_off + 4 * H].add(f_bias)
    conv_w = normal((DEPTH, CONV_W, 1, 2 * MLSTM_W), CONV_W ** -0.5)
    conv_b = normal((DEPTH, 2 * MLSTM_W), 0.02)
    mlstm_norm_g = 1.0 + normal((DEPTH, MLSTM_W), 0.02)
    q_norm_g = 1.0 + normal((DEPTH, ATTN_HD), 0.02)
    k_norm_g = 1.0 + normal((DEPTH, ATTN_HD), 0.02)
    w_branch_m = normal((DEPTH, MLSTM_W, D), MLSTM_W ** -0.5)
    w_branch_a = normal((DEPTH, ATTN_W, D), ATTN_W ** -0.5)
    w_out = normal((DEPTH, D, D), D ** -0.5)
    norm2_g = 1.0 + normal((DEPTH, D), 0.02)
    w_router_group = normal((DEPTH, D, N_GROUPS), D ** -0.5)
    b_router_group = normal((DEPTH, N_GROUPS), 0.01)
    w_router_expert = normal((DEPTH, D, N_EXPERTS), D ** -0.5)
    b_router_expert = normal((DEPTH, N_EXPERTS), 0.01)
    w_gate = normal((DEPTH, N_EXPERTS, D, F), D ** -0.5)
    w_up = normal((DEPTH, N_EXPERTS, D, F), D ** -0.5)
    w_down = normal((DEPTH, N_EXPERTS, F, D), F ** -0.5)
    final_norm_g = 1.0 + normal((D,), 0.02)
    return {'x': x, 'c': c, 'w_ada': w_ada, 'b_ada': b_ada, 'norm1_g': norm1_g, 'w_in': w_in, 'b_in': b_in,
            'conv_w': conv_w, 'conv_b': conv_b, 'mlstm_norm_g': mlstm_norm_g, 'q_norm_g': q_norm_g,
            'k_norm_g': k_norm_g, 'w_branch_m': w_branch_m, 'w_branch_a': w_branch_a, 'w_out': w_out,
            'norm2_g': norm2_g, 'w_router_group': w_router_group, 'b_router_group': b_router_group,
            'w_router_expert': w_router_expert, 'b_router_expert': b_router_expert, 'w_gate': w_gate,
            'w_up': w_up, 'w_down': w_down, 'final_norm_g': final_norm_g}


def reference(x, c, w_ada, b_ada, norm1_g, w_in, b_in, conv_w, conv_b, mlstm_norm_g, q_norm_g, k_norm_g,
              w_branch_m, w_branch_a, w_out, norm2_g, w_router_group, b_router_group, w_router_expert,
              b_router_expert, w_gate, w_up, w_down, final_norm_g):
    B, S, D = x.shape
    tables = rope_tables(S)
    offsets = np.cumsum(SPLIT_SIZES)[:-1].tolist()
    cond = jax.nn.silu(c)
    for l in range(DEPTH):
        mod = cond @ w_ada[l] + b_ada[l]
        shift1, scale1, gate1, shift2, scale2, gate2 = [m[:, None, :] for m in jnp.split(mod, 6, axis=-1)]
        h = rms_norm(x, norm1_g[l]) * (1 + scale1) + shift1
        proj = h @ w_in[l] + b_in[l]
        mq, mk, mv, mo, mgates, aq, ak, av, gm, ga = jnp.split(proj, offsets, axis=-1)
        y_m = mlstm_mixer(mq, mk, mv, mo, mgates, conv_w[l], conv_b[l], mlstm_norm_g[l])
        y_a = bidirectional_gqa(aq.reshape(B, S, ATTN_HEADS, ATTN_HD), ak.reshape(B, S, KV_HEADS, ATTN_HD),
                                av.reshape(B, S, KV_HEADS, ATTN_HD), q_norm_g[l], k_norm_g[l], tables)
        merged = jax.nn.sigmoid(gm) * (y_m @ w_branch_m[l]) + jax.nn.sigmoid(ga) * (y_a.astype(x.dtype) @ w_branch_a[l])
        x = x + gate1 * (merged @ w_out[l])
        h2 = rms_norm(x, norm2_g[l]) * (1 + scale2) + shift2
        x = x + gate2 * hierarchical_moe(h2, w_router_group[l], b_router_group[l], w_router_expert[l],
                                         b_router_expert[l], w_gate[l], w_up[l], w_down[l])
    return rms_norm(x, final_norm_g)
```

```python
import numpy as np
import ml_dtypes
from contextlib import ExitStack
import concourse.bass as bass
import concourse.mybir as mybir
from concourse.bass_utils import run_bass_kernel_spmd

F32 = mybir.dt.float32
BF16 = mybir.dt.bfloat16
AF = mybir.ActivationFunctionType
ALU = mybir.AluOpType
AX = mybir.AxisListType

ENGS = ("tensor", "vector", "scalar", "gpsimd", "sync")
GEN = 30000


class Prog:
    def __init__(self, nc, n_dma_slots=8, same_engine_sync=True):
        self.nc = nc
        self.ops = {e: [] for e in ENGS}
        self.last_writer = {}
        self.readers = {}
        self.n_dma_slots = n_dma_slots
        self.dma_count = {e: 0 for e in ENGS}
        self.same_engine_sync = same_engine_sync
        self.pending_dma = []

    def op(self, eng, fn, reads=(), writes=(), dma=False, nosync_same=False):
        idx = len(self.ops[eng])
        deps = set()
        for k in reads:
            w = self.last_writer.get(k)
            if w is not None:
                deps.add(w)
        for k in writes:
            w = self.last_writer.get(k)
            if w is not None:
                deps.add(w)
            for r in self.readers.get(k, ()):
                deps.add(r)
        me = (eng, idx)
        deps.discard(me)
        slot = None
        if dma:
            slot = self.dma_count[eng] % self.n_dma_slots
            self.dma_count[eng] += 1
            self.pending_dma.append(me)
        elif nosync_same or not self.same_engine_sync:
            deps = {d for d in deps if d[0] != eng or self.ops[d[0]][d[1]]["dma"]}
        rec = dict(eng=eng, fn=fn, deps=deps, dma=dma, slot=slot, signal=False)
        self.ops[eng].append(rec)
        for d in deps:
            self.ops[d[0]][d[1]]["signal"] = True
        for k in reads:
            self.readers.setdefault(k, []).append(me)
        for k in writes:
            self.last_writer[k] = me
            self.readers[k] = []
        return me

    def mm(self, fn, reads=(), writes=()):
        return self.op("tensor", fn, reads, writes, nosync_same=True)

    def dve(self, fn, reads=(), writes=()):
        return self.op("vector", fn, reads, writes)

    def act(self, fn, reads=(), writes=()):
        return self.op("scalar", fn, reads, writes)

    def pool(self, fn, reads=(), writes=()):
        return self.op("gpsimd", fn, reads, writes)

    def dma(self, fn, reads=(), writes=(), q="sync"):
        return self.op(q, fn, reads, writes, dma=True)

    def barrier(self):
        lasts = []
        for e in ENGS:
            for i in range(len(self.ops[e]) - 1, -1, -1):
                r = self.ops[e][i]
                if r["fn"] is not None and not r["dma"]:
                    lasts.append((e, i))
                    break
        deps = set(lasts) | set(self.pending_dma)
        self.pending_dma = []
        for d in deps:
            self.ops[d[0]][d[1]]["signal"] = True
        for e in ENGS:
            self.ops[e].append(dict(eng=e, fn=None, deps={d for d in deps}, dma=False, slot=None, signal=False))

    def emit(self):
        nc = self.nc
        ngen = {}
        final_slot_counts = {}
        for e in ENGS:
            c = 0
            slot_counts = [0] * self.n_dma_slots
            for r in self.ops[e]:
                if r["dma"]:
                    slot_counts[r["slot"]] += 1
                    r["slot_prev"] = slot_counts[r["slot"]] - 1
                    r["sig"] = ("dma", e, r["slot"], 16 * slot_counts[r["slot"]])
                elif r["signal"]:
                    g, v = divmod(c, GEN)
                    r["sig"] = ("eng", e, g, v + 1)
                    c += 1
                else:
                    r["sig"] = None
            ngen[e] = (c + GEN - 1) // GEN if c else 0
            final_slot_counts[e] = slot_counts
        with ExitStack() as st:
            sems = {}
            for e in ENGS:
                for g in range(ngen[e]):
                    sems[("eng", e, g)] = st.enter_context(nc.semaphore(f"s_{e}_{g}"))
                if self.dma_count[e]:
                    for s in range(self.n_dma_slots):
                        sems[("dma", e, s)] = st.enter_context(nc.semaphore(f"d_{e}_{s}"))
            block = st.enter_context(nc.Block())

            def make_body(e):
                def body(eng):
                    waited = {}
                    for r in self.ops[e]:
                        need = {}
                        for d in r["deps"]:
                            if d[0] == e and r["fn"] is None and not self.ops[d[0]][d[1]]["dma"]:
                                continue
                            sig = self.ops[d[0]][d[1]]["sig"]
                            key = sig[:3]
                            need[key] = max(need.get(key, 0), sig[3])
                        if r["dma"] and r["slot_prev"] > 0:
                            key = ("dma", e, r["slot"])
                            need[key] = max(need.get(key, 0), 16 * r["slot_prev"])
                        for key, v in need.items():
                            if key[0] == "eng":
                                best = waited.get((key[0], key[1]), (-1, 0))
                                if (key[2], v) <= best:
                                    continue
                                waited[(key[0], key[1])] = (key[2], v)
                            else:
                                if waited.get(key, 0) >= v:
                                    continue
                                waited[key] = v
                            eng.wait_ge(sems[key], v)
                        if r["fn"] is None:
                            continue
                        ins = r["fn"](eng)
                        if r["sig"] is not None:
                            ins.then_inc(sems[r["sig"][:3]], 16 if r["dma"] else 1)
                    if e == "sync":
                        for q in ENGS:
                            if self.dma_count[q]:
                                for s in range(self.n_dma_slots):
                                    cnt = final_slot_counts[q][s]
                                    if cnt:
                                        eng.wait_ge(sems[("dma", q, s)], 16 * cnt)
                return body

            for e in ENGS:
                getattr(block, e)(make_body(e))


def mm_group(P, out_ap, pairs, reads, writes):
    n = len(pairs)

    def fn(e):
        ins = None
        for i, (l, r) in enumerate(pairs):
            ins = e.matmul(out_ap, lhsT=l, rhs=r, start=(i == 0), stop=(i == n - 1))
        return ins
    return P.mm(fn, reads, writes)


def dma(P, out_ap, in_ap, reads=(), writes=(), q="sync"):
    return P.dma(lambda e: e.dma_start(out=out_ap, in_=in_ap), reads, writes, q=q)


def act(P, out_ap, in_ap, func, reads, writes, bias=None, scale=None):
    kw = {}
    if bias is not None:
        kw["bias"] = bias
    if scale is not None:
        kw["scale"] = scale
    return P.act(lambda e: e.activation(out=out_ap, in_=in_ap, func=func, **kw), reads, writes)


def tt(P, out_ap, a, b, op, reads, writes, eng="vector"):
    return P.op(eng, lambda e: e.tensor_tensor(out=out_ap, in0=a, in1=b, op=op), reads, writes)


def ts(P, out_ap, a, s1, op0, reads, writes, s2=None, op1=None, eng="vector"):
    if op1 is None:
        return P.op(eng, lambda e: e.tensor_scalar(out=out_ap, in0=a, scalar1=s1, scalar2=None, op0=op0), reads, writes)
    return P.op(eng, lambda e: e.tensor_scalar(out=out_ap, in0=a, scalar1=s1, scalar2=s2, op0=op0, op1=op1), reads, writes)


def stt(P, out_ap, a, s, b, op0, op1, reads, writes):
    return P.dve(lambda e: e.scalar_tensor_tensor(out=out_ap, in0=a, scalar=s, in1=b, op0=op0, op1=op1), reads, writes)


def cp(P, out_ap, in_ap, reads, writes, eng="vector"):
    if eng == "scalar":
        return P.op(eng, lambda e: e.activation(out=out_ap, in_=in_ap, func=AF.Identity), reads, writes)
    return P.op(eng, lambda e: e.tensor_copy(out=out_ap, in_=in_ap), reads, writes)


def red(P, out_ap, in_ap, op, reads, writes):
    return P.dve(lambda e: e.tensor_reduce(out=out_ap, in_=in_ap, axis=AX.X, op=op), reads, writes)


D = 1024
NK = 8
TB = 512
EPS = 1e-6


def emit_mod(P, nc, st, pb, cvec, w_ada, b_ada, col_chunks, name="mod"):
    T = lambda n, s, d: st.enter_context(nc.sbuf_tensor(n, s, d))
    ncol = len(col_chunks)
    cs = T(name + "_cs", [128, NK], F32)
    css = T(name + "_css", [128, NK], F32)
    nch = max(col_chunks) + 1
    bsb = T(name + "_b", [128, nch], F32)
    mod = T(name, [128, nch], F32)
    dma(P, cs[:], cvec[:, :], writes=[name + "cs"])
    dma(P, bsb[:], b_ada[:, :], writes=[name + "b"])
    act(P, css[:], cs[:], AF.Silu, [name + "cs"], [name + "css"])
    wv = w_ada.rearrange("(k p) c -> p k c", p=128)
    with ExitStack() as st2:
        wa = [st2.enter_context(nc.sbuf_tensor(f"{name}_wa{i}", [128, NK, 768], F32)) for i in range(2)]
        pieces = []
        cur = []
        for j in col_chunks:
            if cur and (j != cur[-1] + 1 or len(cur) == 6):
                pieces.append(cur)
                cur = []
            cur.append(j)
        if cur:
            pieces.append(cur)
        for pi, piece in enumerate(pieces):
            buf = wa[pi % 2]
            key = (name + "wa", pi % 2)
            c0 = piece[0] * 128
            n = len(piece) * 128
            for kh in range(2):
                dma(P, buf[:, kh * 4:(kh + 1) * 4, 0:n], wv[:, kh * 4:(kh + 1) * 4, c0:c0 + n], writes=[key],
                    q=("sync" if kh == 0 else "gpsimd"))
            for jj, j in enumerate(piece):
                pairs = [(buf[:, k, jj * 128:(jj + 1) * 128], css[:, k:k + 1]) for k in range(NK)]
                mm_group(P, pb[0][:, j:j + 1], pairs, [key, name + "css"], ["pb0"])
        for j in col_chunks:
            tt(P, mod[:, j:j + 1], pb[0][:, j:j + 1], bsb[:, j:j + 1], ALU.add, ["pb0", name + "b"], [name])
        P.barrier()
    return mod


def emit_norm(P, nc, W, xk, a_t, shift_t, outs, xkeys, okeys, pbank, pkey, n=TB, tag="n"):
    sq, rt, rstd, tmp = W["sq"], W["rt"], W["rstd"], W["tmp"]
    for k in range(NK):
        act(P, sq[:, k, 0:n], xk(k), AF.Square, xkeys, [("sq", k)])
    pairs = [(W["ones_bf"][:], sq[:, k, 0:n]) for k in range(NK)]
    mm_group(P, pbank[:, 0:n], pairs, [("sq", k) for k in range(NK)] + ["ones"], [pkey])
    act(P, rt[:, 0:n], pbank[:, 0:n], AF.Sqrt, [pkey, "eps"], ["rt"], bias=W["eps"][:, 0:1], scale=1.0 / D)
    P.dve(lambda e: e.reciprocal(out=rstd[:, 0:n], in_=rt[:, 0:n]), ["rt"], ["rstd"])
    for k in range(NK):
        tb_ = tmp[k % 2]
        tt(P, tb_[:, 0:n], xk(k), rstd[:, 0:n], ALU.mult, xkeys + ["rstd"], [("ntmp", k % 2)])
        for oi, ofn in enumerate(outs):
            if shift_t is not None:
                act(P, ofn(k), tb_[:, 0:n], AF.Identity, [("ntmp", k % 2)], [okeys[oi](k)],
                    bias=shift_t[:, k:k + 1], scale=a_t[:, k:k + 1])
            else:
                act(P, ofn(k), tb_[:, 0:n], AF.Identity, [("ntmp", k % 2)], [okeys[oi](k)],
                    scale=a_t[:, k:k + 1])


NT_B = 2048
NE = 32
FH = 512


def build_B(last, n_experts=NE):
    nc = bass.Bass("TRN2", target_bir_lowering=False)

    def din(name, shape, dt=F32):
        return nc.dram_tensor(name, shape, dt, kind="ExternalInput").ap()
    xT = din("xT", [D, NT_B])
    ymT = din("ymT", [512, NT_B], BF16)
    yaT = din("yaT", [512, NT_B], BF16)
    cvec = din("cvec", [128, NK])
    w_ada = din("w_ada", [D, 6 * D])
    b_ada = din("b_ada", [128, 48])
    g1 = din("g1", [128, NK])
    g2 = din("g2", [128, NK])
    gf = din("gf", [128, NK])
    w_g = din("w_g", [D, 2 * D])
    b_g = din("b_g", [128, 16])
    w_bm = din("w_bm", [512, D])
    w_ba = din("w_ba", [512, D])
    w_o = din("w_o", [D, D])
    w_r = din("w_r", [D, 36])
    b_r = din("b_r", [128, 36])
    w_gate = din("w_gate", [NE, D, FH])
    w_up = din("w_up", [NE, D, FH])
    w_down = din("w_down", [NE, FH, D])
    esel = din("esel", [32, 32 * 128])
    ident = din("ident", [128, 128])
    outT = nc.dram_tensor("outT", [D, NT_B], F32, kind="ExternalOutput").ap()
    NTB = NT_B // TB

    with ExitStack() as st:
        T = lambda n, s, d: st.enter_context(nc.sbuf_tensor(n, s, d))
        P = Prog(nc)
        pb = [st.enter_context(nc.psum_tensor(f"pb{i}", [128, 512], F32)) for i in range(8)]
        pk = [f"pb{i}" for i in range(8)]
        x1T = T("x1T", [128, NK, NT_B], F32)
        W = dict(ones_bf=T("ones_bf", [128, 128], BF16), eps=T("eps", [128, 1], F32),
                 sq=T("sq", [128, NK, TB], BF16), rt=T("rt", [128, TB], F32), rstd=T("rstd", [128, TB], F32),
                 tmp=[T("ntmp0", [128, TB], F32), T("ntmp1", [128, TB], F32)])
        identf = T("identf", [128, 128], F32)
        g1s, g2s, gfs = T("g1s", [128, NK], F32), T("g2s", [128, NK], F32), T("gfs", [128, NK], F32)
        a1, a2 = T("a1", [128, NK], F32), T("a2", [128, NK], F32)
        bgs = T("bgs", [128, 16], F32)
        brs = T("brs", [128, 36], F32)
        wr = T("wr", [128, NK, 36], F32)
        P.pool(lambda e: e.memset(W["ones_bf"][:], 1.0), [], ["ones"])
        P.pool(lambda e: e.memset(W["eps"][:], EPS), [], ["eps"])
        dma(P, identf[:], ident[:, :], writes=["ident"])
        dma(P, g1s[:], g1[:, :], writes=["g1"])
        dma(P, g2s[:], g2[:, :], writes=["g2"])
        dma(P, gfs[:], gf[:, :], writes=["gf"])
        dma(P, bgs[:], b_g[:, :], writes=["bg"])
        dma(P, brs[:], b_r[:, :], writes=["br"])
        dma(P, wr[:], w_r.rearrange("(k p) c -> p k c", p=128), writes=["wr"])
        xv = xT.rearrange("(k p) t -> p k t", p=128)
        ymv = ymT.rearrange("(k p) t -> p k t", p=128)
        yav = yaT.rearrange("(k p) t -> p k t", p=128)
        for tb in range(NTB):
            for kh in range(2):
                dma(P, x1T[:, kh * 4:(kh + 1) * 4, tb * TB:(tb + 1) * TB], xv[:, kh * 4:(kh + 1) * 4, tb * TB:(tb + 1) * TB],
                    writes=[("x1T", k, tb) for k in range(kh * 4, kh * 4 + 4)])

        mod = emit_mod(P, nc, st, pb, cvec, w_ada, b_ada, list(range(48)))
        stt(P, a1[:], mod[:, 8:16], 1.0, g1s[:], ALU.add, ALU.mult, ["mod", "g1"], ["a1"])
        stt(P, a2[:], mod[:, 32:40], 1.0, g2s[:], ALU.add, ALU.mult, ["mod", "g2"], ["a2"])
        shift1, gate1, shift2, gate2 = mod[:, 0:8], mod[:, 16:24], mod[:, 24:32], mod[:, 40:48]

        with ExitStack() as s1:
            T1 = lambda n, s, d: s1.enter_context(nc.sbuf_tensor(n, s, d))
            wg_ = T1("wg_", [128, NK, 2 * D], BF16)
            wbm = T1("wbm", [128, 4, D], BF16)
            wba = T1("wba", [128, 4, D], BF16)
            wo = T1("wo", [128, NK, D], BF16)
            wgv = w_g.rearrange("(k p) c -> p k c", p=128)
            for k in range(NK):
                dma(P, wg_[:, k, :], wgv[:, k, :], writes=[("wg_", k)], q="gpsimd")
            dma(P, wbm[:], w_bm.rearrange("(k p) c -> p k c", p=128), writes=["wbm"], q="gpsimd")
            dma(P, wba[:], w_ba.rearrange("(k p) c -> p k c", p=128), writes=["wba"], q="gpsimd")
            wov = w_o.rearrange("(k p) c -> p k c", p=128)
            for k in range(0, NK, 2):
                dma(P, wo[:, k:k + 2, :], wov[:, k:k + 2, :], writes=[("wo", k), ("wo", k + 1)], q="gpsimd")
            h1 = T1("h1", [128, NK, TB], BF16)
            ymb = [T1(f"ymb{i}", [128, 4, TB], BF16) for i in range(2)]
            yab = [T1(f"yab{i}", [128, 4, TB], BF16) for i in range(2)]
            merged = T1("merged", [128, NK, TB], BF16)
            sg = [[T1(f"sg{i}{j}", [128, TB], F32) for j in range(2)] for i in range(2)]
            t12 = [[T1(f"t12{i}{j}", [128, TB], F32) for j in range(2)] for i in range(2)]
            for tb in range(NTB):
                tsl = slice(tb * TB, (tb + 1) * TB)
                b = tb % 2
                dma(P, ymb[b][:], ymv[:, :, tsl], writes=[("ymb", b)])
                dma(P, yab[b][:], yav[:, :, tsl], writes=[("yab", b)])
                emit_norm(P, nc, W, lambda k: x1T[:, k, tsl], a1, shift1,
                          [lambda k: h1[:, k, :]], [("x1T", k_, tb) for k_ in range(NK)] + ["a1", "mod"], [lambda k: ("h1", k)],
                          pb[0], "pb0")
                for dc in range(NK):
                    par = dc % 2
                    base = 4 * par
                    csl = slice(dc * 128, (dc + 1) * 128)
                    hk = [("h1", k) for k in range(NK)]
                    mm_group(P, pb[base][:], [(wg_[:, k, csl], h1[:, k, :]) for k in range(NK)],
                             hk + [("wg_", k) for k in range(NK)], [pk[base]])
                    mm_group(P, pb[base + 1][:], [(wg_[:, k, D + dc * 128:D + (dc + 1) * 128], h1[:, k, :]) for k in range(NK)],
                             hk + [("wg_", k) for k in range(NK)], [pk[base + 1]])
                    mm_group(P, pb[base + 2][:], [(wbm[:, k, csl], ymb[b][:, k, :]) for k in range(4)],
                             ["wbm", ("ymb", b)], [pk[base + 2]])
                    mm_group(P, pb[base + 3][:], [(wba[:, k, csl], yab[b][:, k, :]) for k in range(4)],
                             ["wba", ("yab", b)], [pk[base + 3]])
                    act(P, sg[par][0][:], pb[base][:], AF.Sigmoid, [pk[base], "bg"], [("sg", par, 0)], bias=bgs[:, dc:dc + 1])
                    act(P, sg[par][1][:], pb[base + 1][:], AF.Sigmoid, [pk[base + 1], "bg"], [("sg", par, 1)], bias=bgs[:, 8 + dc:9 + dc])
                    tt(P, t12[par][0][:], sg[par][0][:], pb[base + 2][:], ALU.mult, [("sg", par, 0), pk[base + 2]], [("t12", par, 0)])
                    tt(P, t12[par][1][:], sg[par][1][:], pb[base + 3][:], ALU.mult, [("sg", par, 1), pk[base + 3]], [("t12", par, 1)])
                    tt(P, merged[:, dc, :], t12[par][0][:], t12[par][1][:], ALU.add, [("t12", par, 0), ("t12", par, 1)],
                       [("merged", dc)], eng="gpsimd")
                for dc in range(NK):
                    bank = dc % 2
                    csl = slice(dc * 128, (dc + 1) * 128)
                    mm_group(P, pb[bank][:], [(wo[:, k, csl], merged[:, k, :]) for k in range(NK)],
                             [("merged", k) for k in range(NK)] + [("wo", k) for k in range(NK)], [pk[bank]])
                    stt(P, x1T[:, dc, tsl], pb[bank][:], gate1[:, dc:dc + 1], x1T[:, dc, tsl], ALU.mult, ALU.add,
                        [pk[bank], "mod", ("x1T", dc, tb)], [("x1T", dc, tb)])
            P.barrier()

        s23 = st.enter_context(ExitStack())
        h2T = s23.enter_context(nc.sbuf_tensor("h2T", [128, NK, NT_B], BF16))
        combT = s23.enter_context(nc.sbuf_tensor("combT", [32, NT_B], F32))
        with ExitStack() as s2:
            T2 = lambda n, s, d: s2.enter_context(nc.sbuf_tensor(n, s, d))
            h2f = T2("h2f", [128, NK, TB], F32)
            R = {n: T2("r_" + n, [128, s], F32) for n, s in
                 [("lg", 36), ("gmax", 1), ("ngmax", 1), ("eg", 4), ("ssum", 1), ("ptop", 1), ("mg", 4), ("pen", 4),
                  ("lem", 32), ("e1", 1), ("m1", 32), ("lem2", 32), ("e2", 1), ("m2", 32), ("d", 1), ("s2", 1),
                  ("w2", 1), ("w1", 1), ("comb", 32), ("comb2", 32)]}
            for tb in range(NTB):
                tsl = slice(tb * TB, (tb + 1) * TB)
                emit_norm(P, nc, W, lambda k: x1T[:, k, tsl], a2, shift2,
                          [lambda k: h2T[:, k, tsl], lambda k: h2f[:, k, :]],
                          [("x1T", k_, tb) for k_ in range(NK)] + ["a2", "mod"],
                          [lambda k: ("h2T", k, tb), lambda k: ("h2f", k)], pb[0], "pb0")
                for sub in range(4):
                    ssl = slice(sub * 128, (sub + 1) * 128)
                    bank = 1 + sub % 2
                    mm_group(P, pb[bank][:, 0:36], [(h2f[:, k, ssl], wr[:, k, :]) for k in range(NK)],
                             [("h2f", k) for k in range(NK)] + ["wr"], [pk[bank]])
                    tt(P, R["lg"][:], pb[bank][:, 0:36], brs[:], ALU.add, [pk[bank], "br"], ["r_lg"])
                    red(P, R["gmax"][:], R["lg"][:, 0:4], ALU.max, ["r_lg"], ["r_gmax"])
                    ts(P, R["ngmax"][:], R["gmax"][:], -1.0, ALU.mult, ["r_gmax"], ["r_ngmax"])
                    act(P, R["eg"][:], R["lg"][:, 0:4], AF.Exp, ["r_lg", "r_ngmax"], ["r_eg"], bias=R["ngmax"][:, 0:1])
                    red(P, R["ssum"][:], R["eg"][:], ALU.add, ["r_eg"], ["r_ssum"])
                    P.dve(lambda e: e.reciprocal(out=R["ptop"][:], in_=R["ssum"][:]), ["r_ssum"], ["r_ptop"])
                    ts(P, R["mg"][:], R["lg"][:, 0:4], R["gmax"][:, 0:1], ALU.is_equal, ["r_lg", "r_gmax"], ["r_mg"])
                    ts(P, R["pen"][:], R["mg"][:], -1.0, ALU.add, ["r_mg"], ["r_pen"], s2=1e30, op1=ALU.mult)
                    for g in range(4):
                        ts(P, R["lem"][:, g * 8:(g + 1) * 8], R["lg"][:, 4 + g * 8:12 + g * 8], R["pen"][:, g:g + 1], ALU.add,
                           ["r_lg", "r_pen"], [("r_lem", g)])
                    lemk = [("r_lem", g) for g in range(4)]
                    red(P, R["e1"][:], R["lem"][:], ALU.max, lemk, ["r_e1"])
                    ts(P, R["m1"][:], R["lem"][:], R["e1"][:, 0:1], ALU.is_equal, lemk + ["r_e1"], ["r_m1"])
                    stt(P, R["lem2"][:], R["m1"][:], -1e30, R["lem"][:], ALU.mult, ALU.add, lemk + ["r_m1"], ["r_lem2"])
                    red(P, R["e2"][:], R["lem2"][:], ALU.max, ["r_lem2"], ["r_e2"])
                    ts(P, R["m2"][:], R["lem2"][:], R["e2"][:, 0:1], ALU.is_equal, ["r_lem2", "r_e2"], ["r_m2"])
                    tt(P, R["d"][:], R["e2"][:], R["e1"][:], ALU.subtract, ["r_e1", "r_e2"], ["r_d"])
                    act(P, R["s2"][:], R["d"][:], AF.Sigmoid, ["r_d"], ["r_s2"])
                    tt(P, R["w2"][:], R["ptop"][:], R["s2"][:], ALU.mult, ["r_ptop", "r_s2"], ["r_w2"])
                    tt(P, R["w1"][:], R["ptop"][:], R["w2"][:], ALU.subtract, ["r_ptop", "r_w2"], ["r_w1"])
                    ts(P, R["comb"][:], R["m1"][:], R["w1"][:, 0:1], ALU.mult, ["r_m1", "r_w1"], ["r_comb"])
                    stt(P, R["comb2"][:], R["m2"][:], R["w2"][:, 0:1], R["comb"][:], ALU.mult, ALU.add,
                        ["r_m2", "r_w2", "r_comb"], ["r_comb2"])
                    P.mm(lambda e: e.transpose(pb[3][0:32, 0:128], R["comb2"][:], identf[:]), ["r_comb2", "ident"], [pk[3]])
                    tok = slice(tb * TB + sub * 128, tb * TB + (sub + 1) * 128)
                    act(P, combT[:, tok], pb[3][0:32, 0:128], AF.Identity, [pk[3]], [("combT", tb, sub)])
            P.barrier()

        with ExitStack() as s3:
            T3 = lambda n, s, d: s3.enter_context(nc.sbuf_tensor(n, s, d))
            eselT = T3("eselT", [32, 32 * 128], F32)
            dma(P, eselT[:], esel[:, :], writes=["esel"])
            wgt = [T3(f"wgt{i}", [128, NK, FH], BF16) for i in range(2)]
            wut = [T3(f"wut{i}", [128, NK, FH], BF16) for i in range(2)]
            wdt = [T3(f"wdt{i}", [128, 4, D], BF16) for i in range(2)]
            actT = [T3(f"actT{i}", [128, 4, TB], BF16) for i in range(2)]
            sl = [T3(f"sl{i}", [128, TB], F32) for i in range(2)]
            pr = [T3(f"pr{i}", [128, TB], F32) for i in range(2)]
            it = 0
            for e_ in range(n_experts):
                wb = e_ % 2
                gv = w_gate[e_].rearrange("(k p) f -> p k f", p=128)
                uv = w_up[e_].rearrange("(k p) f -> p k f", p=128)
                dv = w_down[e_].rearrange("(k p) c -> p k c", p=128)
                for kh in range(2):
                    ksl = slice(kh * 4, kh * 4 + 4)
                    dma(P, wgt[wb][:, ksl, :], gv[:, ksl, :], writes=[("wgt", wb, kh)], q="gpsimd")
                    dma(P, wut[wb][:, ksl, :], uv[:, ksl, :], writes=[("wut", wb, kh)], q="gpsimd")
                for kh in range(2):
                    ksl = slice(kh * 2, kh * 2 + 2)
                    dma(P, wdt[wb][:, ksl, :], dv[:, ksl, :], writes=[("wdt", wb, kh)], q="gpsimd")
                for tb in range(NTB):
                    tsl = slice(tb * TB, (tb + 1) * TB)
                    ab = it % 2
                    it += 1
                    h2k = [("h2T", k, tb) for k in range(NK)]
                    mm_group(P, pb[6][:], [(eselT[:, e_ * 128:(e_ + 1) * 128], combT[:, tsl])],
                             ["esel"] + [("combT", tb, s_) for s_ in range(4)], [pk[6]])
                    for fc in range(4):
                        fsl = slice(fc * 128, (fc + 1) * 128)
                        pa, pu = pb[2 * (fc % 2)], pb[2 * (fc % 2) + 1]
                        ka, ku = pk[2 * (fc % 2)], pk[2 * (fc % 2) + 1]
                        mm_group(P, pa[:], [(wgt[wb][:, k, fsl], h2T[:, k, tsl]) for k in range(NK)],
                                 h2k + [("wgt", wb, 0), ("wgt", wb, 1)], [ka])
                        mm_group(P, pu[:], [(wut[wb][:, k, fsl], h2T[:, k, tsl]) for k in range(NK)],
                                 h2k + [("wut", wb, 0), ("wut", wb, 1)], [ku])
                        act(P, sl[fc % 2][:], pa[:], AF.Silu, [ka], [("sl", fc % 2)])
                        tt(P, pr[fc % 2][:], sl[fc % 2][:], pu[:], ALU.mult, [("sl", fc % 2), ku], [("pr", fc % 2)])
                        tt(P, actT[ab][:, fc, :], pr[fc % 2][:], pb[6][:], ALU.mult, [("pr", fc % 2), pk[6]], [("actT", ab, fc)])
                    for dc in range(NK):
                        csl = slice(dc * 128, (dc + 1) * 128)
                        po, ko = pb[4 + dc % 2], pk[4 + dc % 2]
                        mm_group(P, po[:], [(wdt[wb][:, fc, csl], actT[ab][:, fc, :]) for fc in range(4)],
                                 [("actT", ab, fc) for fc in range(4)] + [("wdt", wb, 0), ("wdt", wb, 1)], [ko])
                        stt(P, x1T[:, dc, tsl], po[:], gate2[:, dc:dc + 1], x1T[:, dc, tsl], ALU.mult, ALU.add,
                            [ko, "mod", ("x1T", dc, tb)], [("x1T", dc, tb)])
            P.barrier()

        s23.close()
        ov = outT.rearrange("(k p) t -> p k t", p=128)
        if last:
            with ExitStack() as s4:
                T4 = lambda n, s, d: s4.enter_context(nc.sbuf_tensor(n, s, d))
                ob = [T4(f"ob{i}", [128, NK, TB], F32) for i in range(2)]
                for tb in range(NTB):
                    tsl = slice(tb * TB, (tb + 1) * TB)
                    o = ob[tb % 2]
                    emit_norm(P, nc, W, lambda k: x1T[:, k, tsl], gfs, None,
                              [lambda k: o[:, k, :]], [("x1T", k_, tb) for k_ in range(NK)] + ["gf"], [lambda k: ("ob", tb % 2, k)],
                              pb[0], "pb0")
                    dma(P, ov[:, :, tsl], o[:], reads=[("ob", tb % 2, k) for k in range(NK)])
                P.barrier()
        else:
            for tb in range(NTB):
                tsl = slice(tb * TB, (tb + 1) * TB)
                dma(P, ov[:, :, tsl], x1T[:, :, tsl], reads=[("x1T", k, tb) for k in range(NK)])
        P.emit()
    return nc


S_LEN = 8192
TA = 256
NCH = S_LEN // 128
MSCALE = 128.0 ** -0.5
ASCALE = 64.0 ** -0.5
NT_T = 324


def build_A(att_qblocks=16, do_mlstm=True):
    nc = bass.Bass("TRN2", target_bir_lowering=False)

    def din(name, shape, dt=F32):
        return nc.dram_tensor(name, shape, dt, kind="ExternalInput").ap()
    xT = din("xT", [D, S_LEN])
    cvec = din("cvec", [128, NK])
    w_ada = din("w_ada", [D, 2 * D])
    b_ada = din("b_ada", [128, 16])
    g1 = din("g1", [128, NK])
    w_F = din("w_F", [D, 512])
    b_F = din("b_F", [128, 4])
    w_T = din("w_T", [D, NT_T])
    b_T = din("b_T", [128, NT_T])
    cw = din("cw", [128, 10])
    cb = din("cb", [128, 2])
    gmr = din("gmr", [128, 128])
    gqk = din("gqk", [128, 2])
    cosT = din("cosT", [128, S_LEN])
    sinT = din("sinT", [128, S_LEN])
    ident = din("ident", [128, 128])
    masks = din("masks", [128, 256])
    rT = din("rT", [128, 128])
    oblk = din("oblk", [128, 128])
    ymT = nc.dram_tensor("ymT", [128, S_LEN], BF16, kind="ExternalOutput").ap()
    yaT = nc.dram_tensor("yaT", [128, S_LEN], BF16, kind="ExternalOutput").ap()
    NB = S_LEN // TA

    with ExitStack() as st:
        T = lambda n, s, d: st.enter_context(nc.sbuf_tensor(n, s, d))
        P = Prog(nc)
        pb = [st.enter_context(nc.psum_tensor(f"pb{i}", [128, 512], F32)) for i in range(8)]
        pk = [f"pb{i}" for i in range(8)]
        QmT = T("QmT", [128, S_LEN], BF16)
        KmT = T("KmT", [128, S_LEN], BF16)
        Vaug = T("Vaug", [128, NCH, 129], BF16)
        osig = T("osig", [128, NCH, 128], BF16)
        G = T("G", [128, NCH, 4], F32)
        QaT = T("QaT", [128, S_LEN], BF16)
        KTa = T("KTa", [128, S_LEN], BF16)
        KTb = T("KTb", [128, S_LEN], BF16)
        Va = T("Va", [128, NCH, 65], BF16)
        W = dict(ones_bf=T("ones_bf", [128, 128], BF16), eps=T("eps", [128, 1], F32),
                 sq=T("sq", [128, NK, TA], BF16), rt=T("rt", [128, TA], F32), rstd=T("rstd", [128, TA], F32),
                 tmp=[T("ntmp0", [128, TA], F32), T("ntmp1", [128, TA], F32)])
        identf = T("identf", [128, 128], F32)
        identb = T("identb", [128, 128], BF16)
        mk = T("mk", [128, 256], F32)
        onesf = T("onesf", [128, 128], F32)
        one1 = T("one1", [128, 1], F32)
        rTs = T("rTs", [128, 128], F32)
        oblkb = T("oblkb", [128, 128], BF16)
        oblkf = T("oblkf", [128, 128], F32)
        gmrs = T("gmrs", [128, 128], F32)
        gqks = T("gqks", [128, 2], F32)
        bFs = T("bFs", [128, 4], F32)
        bTs = T("bTs", [128, NT_T], F32)
        cws = T("cws", [128, 10], F32)
        cbs = T("cbs", [128, 2], F32)
        g1s = T("g1s", [128, NK], F32)
        a1 = T("a1", [128, NK], F32)
        P.pool(lambda e: e.memset(W["ones_bf"][:], 1.0), [], ["ones"])
        P.pool(lambda e: e.memset(W["eps"][:], EPS), [], ["eps"])
        P.pool(lambda e: e.memset(onesf[:], 1.0), [], ["onesf"])
        P.pool(lambda e: e.memset(one1[:], 1.0), [], ["one1"])
        P.pool(lambda e: e.memset(Vaug[:, :, 128:129], 1.0), [], ["Vaug1"])
        P.pool(lambda e: e.memset(Va[:, :, 64:65], 1.0), [], ["Va1"])
        P.pool(lambda e: e.memset(KTa[64:128, :], 0.0), [], ["KTa0"])
        P.pool(lambda e: e.memset(KTb[0:64, :], 0.0), [], ["KTb0"])
        for t_, d_, k_ in [(identf, ident, "ident"), (mk, masks, "mk"), (rTs, rT, "rT"), (oblkf, oblk, "oblkf"),
                           (gmrs, gmr, "gmr"), (gqks, gqk, "gqk"), (bFs, b_F, "bF"), (bTs, b_T, "bT"),
                           (cws, cw, "cw"), (cbs, cb, "cb"), (g1s, g1, "g1")]:
            dma(P, t_[:], d_[:, :], writes=[k_])
        cp(P, identb[:], identf[:], ["ident"], ["identb"])
        cp(P, oblkb[:], oblkf[:], ["oblkf"], ["oblkb"])

        mod = emit_mod(P, nc, st, pb, cvec, w_ada, b_ada, list(range(16)))
        stt(P, a1[:], mod[:, 8:16], 1.0, g1s[:], ALU.add, ALU.mult, ["mod", "g1"], ["a1"])
        shift1 = mod[:, 0:8]

        with ExitStack() as s1:
            T1 = lambda n, s, d: s1.enter_context(nc.sbuf_tensor(n, s, d))
            wF = T1("wF", [128, NK, 512], BF16)
            wT = T1("wT", [128, NK, NT_T], BF16)
            dma(P, wF[:], w_F.rearrange("(k p) c -> p k c", p=128), writes=["wF"], q="gpsimd")
            dma(P, wT[:], w_T.rearrange("(k p) c -> p k c", p=128), writes=["wT"], q="gpsimd")
            xb = [T1(f"xb{i}", [128, NK, TA], F32) for i in range(2)]
            hT = T1("hT", [128, NK, TA], BF16)
            ring = [T1(f"ring{i}", [128, 4, TA], F32) for i in range(2)]
            acc = [T1(f"acc{i}", [128, TA], F32) for i in range(2)]
            sact = [T1(f"sact{i}", [128, TA], F32) for i in range(2)]
            cs_ = [T1(f"cosb{i}", [128, TA], F32) for i in range(2)]
            sn_ = [T1(f"sinb{i}", [128, TA], F32) for i in range(2)]
            qf = T1("qf", [128, TA], F32)
            qsq = T1("qsq", [128, TA], BF16)
            qrt = T1("qrt", [128, TA], F32)
            qrs = T1("qrs", [128, TA], F32)
            qu = T1("qu", [128, TA], F32)
            qt1 = T1("qt1", [128, TA], F32)
            qt2 = T1("qt2", [128, TA], F32)
            tmpT = [T1(f"tmpT{i}", [128, NT_T], F32) for i in range(2)]
            xv = xT.rearrange("(k p) t -> p k t", p=128)

            def load_x(tb):
                tsl = slice(tb * TA, (tb + 1) * TA)
                for kh in range(2):
                    dma(P, xb[tb % 2][:, kh * 4:(kh + 1) * 4, :], xv[:, kh * 4:(kh + 1) * 4, tsl],
                        writes=[("xb", tb % 2, k) for k in range(kh * 4, kh * 4 + 4)])

            def conv_block(j):
                tsl = slice(j * TA, (j + 1) * TA)
                for qk in range(2):
                    cur = ring[qk][:, j % 4, :]
                    a = acc[qk]
                    ak = ("acc", qk)
                    rk = lambda jj: ("ring", qk, jj % 4)
                    wcol = lambda k: cws[:, qk * 5 + k:qk * 5 + k + 1]
                    ts(P, a[:], cur, wcol(2), ALU.mult, [rk(j), "cw"], [ak])
                    for k in (0, 1, 3, 4):
                        s_ = k - 2
                        if s_ < 0:
                            stt(P, a[:, -s_:TA], ring[qk][:, j % 4, 0:TA + s_], wcol(k), a[:, -s_:TA], ALU.mult, ALU.add,
                                [rk(j), "cw", ak], [ak])
                            if j > 0:
                                stt(P, a[:, 0:-s_], ring[qk][:, (j - 1) % 4, TA + s_:TA], wcol(k), a[:, 0:-s_], ALU.mult, ALU.add,
                                    [rk(j - 1), "cw", ak], [ak])
                        else:
                            stt(P, a[:, 0:TA - s_], ring[qk][:, j % 4, s_:TA], wcol(k), a[:, 0:TA - s_], ALU.mult, ALU.add,
                                [rk(j), "cw", ak], [ak])
                            if j < NB - 1:
                                stt(P, a[:, TA - s_:TA], ring[qk][:, (j + 1) % 4, 0:s_], wcol(k), a[:, TA - s_:TA], ALU.mult, ALU.add,
                                    [rk(j + 1), "cw", ak], [ak])
                    dest = (QmT if qk == 0 else KmT)
                    act(P, dest[:, tsl], a[:], AF.Silu, [ak, "cb"], [("QKm", qk, j)], bias=cbs[:, qk:qk + 1])

            def rope_norm(pf, pkey, bcol, gcol, dest, dkey, tb):
                tsl = slice(tb * TA, (tb + 1) * TA)
                cb_, sb_ = cs_[tb % 2], sn_[tb % 2]
                act(P, qf[:], pf[:, 0:TA], AF.Identity, [pkey, "bF"], ["qf"], bias=bFs[:, bcol:bcol + 1])
                act(P, qsq[:], qf[:], AF.Square, ["qf"], ["qsq"])
                mm_group(P, pb[5][:, 0:TA], [(oblkb[:], qsq[:])], ["qsq", "oblkb"], [pk[5]])
                act(P, qrt[:], pb[5][:, 0:TA], AF.Sqrt, [pk[5], "eps"], ["qrt"], bias=W["eps"][:, 0:1], scale=1.0 / 64)
                P.dve(lambda e: e.reciprocal(out=qrs[:], in_=qrt[:]), ["qrt"], ["qrs"])
                ts(P, qu[:], qf[:], gqks[:, gcol:gcol + 1], ALU.mult, ["qf", "gqk"], ["qu"])
                mm_group(P, pb[6][:, 0:TA], [(rTs[:], qu[:])], ["qu", "rT"], [pk[6]])
                tt(P, qt1[:], qu[:], cb_[:], ALU.mult, ["qu", ("cos", tb % 2)], ["qt1"])
                tt(P, qt2[:], pb[6][:, 0:TA], sb_[:], ALU.mult, [pk[6], ("sin", tb % 2)], ["qt2"])
                tt(P, qt1[:], qt1[:], qt2[:], ALU.add, ["qt1", "qt2"], ["qt1"], eng="gpsimd")
                if dest is None:
                    tt(P, KTa[0:64, tsl], qt1[0:64, :], qrs[0:64, :], ALU.mult, ["qt1", "qrs"], [("KTa", tb)])
                    tt(P, KTb[64:128, tsl], qt1[64:128, :], qrs[64:128, :], ALU.mult, ["qt1", "qrs"], [("KTb", tb)])
                else:
                    tt(P, dest[:, tsl], qt1[:], qrs[:], ALU.mult, ["qt1", "qrs"], [(dkey, tb)])

            load_x(0)
            for tb in range(NB):
                tsl = slice(tb * TA, (tb + 1) * TA)
                if tb + 1 < NB:
                    load_x(tb + 1)
                dma(P, cs_[tb % 2][:], cosT[:, tsl], writes=[("cos", tb % 2)])
                dma(P, sn_[tb % 2][:], sinT[:, tsl], writes=[("sin", tb % 2)])
                x_ = xb[tb % 2]
                emit_norm(P, nc, W, lambda k: x_[:, k, :], a1, shift1, [lambda k: hT[:, k, :]],
                          [("xb", tb % 2, k_) for k_ in range(NK)] + ["a1", "mod"], [lambda k: ("hT", k)],
                          pb[0], "pb0", n=TA)
                hk = [("hT", k) for k in range(NK)]
                for fc in range(4):
                    bank = 1 + fc % 2
                    mm_group(P, pb[bank][:, 0:TA], [(wF[:, k, fc * 128:(fc + 1) * 128], hT[:, k, :]) for k in range(NK)],
                             hk + ["wF"], [pk[bank]])
                    if fc < 2:
                        act(P, ring[fc][:, tb % 4, :], pb[bank][:, 0:TA], AF.Identity, [pk[bank], "bF"], [("ring", fc, tb % 4)],
                            bias=bFs[:, fc:fc + 1])
                    elif fc == 2:
                        rope_norm(pb[bank], pk[bank], 2, 0, QaT, "QaT", tb)
                    else:
                        rope_norm(pb[bank], pk[bank], 3, 1, None, "KT", tb)
                for sub in range(TA // 128):
                    ch = tb * (TA // 128) + sub
                    bank = 3 + sub % 2
                    mm_group(P, pb[bank][:, 0:NT_T], [(hT[:, k, sub * 128:(sub + 1) * 128], wT[:, k, :]) for k in range(NK)],
                             hk + ["wT"], [pk[bank]])
                    tm = tmpT[sub % 2]
                    tk = ("tmpT", sub % 2)
                    tt(P, tm[:], pb[bank][:, 0:NT_T], bTs[:], ALU.add, [pk[bank], "bT"], [tk])
                    cp(P, Vaug[:, ch, 0:128], tm[:, 0:128], [tk], [("Vaug", ch)], eng="gpsimd")
                    act(P, osig[:, ch, :], tm[:, 128:256], AF.Sigmoid, [tk], [("osig", ch)])
                    cp(P, G[:, ch, :], tm[:, 256:260], [tk], [("G", ch)], eng="gpsimd")
                    cp(P, Va[:, ch, 0:64], tm[:, 260:324], [tk], [("Va", ch)], eng="gpsimd")
                if tb >= 1:
                    conv_block(tb - 1)
            conv_block(NB - 1)
            P.barrier()

        if do_mlstm:
          with ExitStack() as s2:
            T2 = lambda n, s, d: s2.enter_context(nc.sbuf_tensor(n, s, d))
            hfwd = T2("hfwd", [128, NCH, 128], F32)
            Gk = [("G", ch) for ch in range(NCH)]
            ge = T2("ge", [128, NCH, 2], F32)
            lfn = T2("lfn", [128, NCH, 2], F32)
            dirs = []
            for d_ in range(2):
                dd = {n: T2(f"{n}{d_}", [128, NCH], F32) for n in ("b", "imb", "w", "ws", "flo", "ebl")}
                dirs.append(dd)
            Cst = T2("Cst", [128, 129], F32)
            Cbf = T2("Cbf", [128, 129], BF16)
            Ktok = [T2(f"Ktok{i}", [128, 128], BF16) for i in range(2)]
            Vw = [T2(f"Vw{i}", [128, 129], BF16) for i in range(2)]
            Sp = [T2(f"Sp{i}", [128, 128], BF16) for i in range(2)]
            den = [T2(f"den{i}", [128, 1], F32) for i in range(2)]
            rden = [T2(f"rden{i}", [128, 1], F32) for i in range(2)]
            hs = [T2(f"hs{i}", [128, 128], F32) for i in range(2)]
            hsq = [T2(f"hsq{i}", [128, 128], F32) for i in range(2)]
            ss = [T2(f"ss{i}", [128, 1], F32) for i in range(2)]
            srt = [T2(f"srt{i}", [128, 1], F32) for i in range(2)]
            srn = [T2(f"srn{i}", [128, 1], F32) for i in range(2)]
            yt = [T2(f"yt{i}", [128, 128], F32) for i in range(2)]
            y2 = [T2(f"y2{i}", [128, 128], BF16) for i in range(2)]
            ymb = [T2(f"ymb{i}", [128, 512], BF16) for i in range(2)]
            for d_ in range(2):
                fcol = 1 + 2 * d_
                act(P, ge[:, :, d_], G[:, :, fcol], AF.Exp, Gk, [("ge", d_)], scale=-1.0)
                act(P, lfn[:, :, d_], ge[:, :, d_], AF.Ln, [("ge", d_), "one1"], [("lfn", d_)], bias=one1[:, 0:1])
            for d_ in range(2):
                dd = dirs[d_]
                icol = 2 * d_
                mslice = mk[:, d_ * 128:(d_ + 1) * 128]
                mm_group(P, pb[0][:, 0:NCH], [(mslice, lfn[:, :, d_])], [("lfn", d_), "mk"], [pk[0]])
                mm_group(P, pb[1][:, 0:NCH], [(onesf[:], lfn[:, :, d_])], [("lfn", d_), "onesf"], [pk[1]])
                cp(P, dd["b"][:], pb[0][:, 0:NCH], [pk[0]], [("mb", d_)])
                tt(P, dd["imb"][:], G[:, :, icol], dd["b"][:], ALU.add, Gk + [("mb", d_)], [("imb", d_)])
                act(P, dd["w"][:], dd["imb"][:], AF.Exp, [("imb", d_)], [("w", d_)])
                ts(P, dd["ws"][:], dd["w"][:], MSCALE, ALU.mult, [("w", d_)], [("ws", d_)])
                act(P, dd["flo"][:], dd["b"][:], AF.Exp, [("mb", d_)], [("flo", d_)])
                act(P, dd["ebl"][:], pb[1][:, 0:NCH], AF.Exp, [pk[1]], [("ebl", d_)], scale=-1.0)
            it = 0
            for d_ in range(2):
                dd = dirs[d_]
                mslice = mk[:, d_ * 128:(d_ + 1) * 128]
                P.dve(lambda e: e.memset(Cst[:], 0.0), [], ["Cst"])
                P.dve(lambda e: e.memset(Cbf[:], 0.0), [], ["Cbf"])
                order = range(NCH) if d_ == 0 else range(NCH - 1, -1, -1)
                for c in order:
                    p2 = it % 2
                    it += 1
                    csl = slice(c * 128, (c + 1) * 128)
                    tb_q = c // (TA // 128)
                    qk_keys = [("QKm", 0, tb_q), ("QKm", 1, tb_q)]
                    P.mm(lambda e, o=pb[2][:].bitcast(BF16)[:, 0:128], i_=KmT[:, csl]: e.transpose(o, i_, identb[:]),
                         [("QKm", 1, tb_q), "identb"], [pk[2]])
                    cp(P, Ktok[p2][:], pb[2][:].bitcast(BF16)[:, 0:128], [pk[2]], [("Ktok", p2)], eng="scalar")
                    ts(P, Vw[p2][:], Vaug[:, c, :], dd["w"][:, c:c + 1], ALU.mult, [("Vaug", c), "Vaug1", ("w", d_)], [("Vw", p2)],
                       eng="gpsimd")
                    sb_ = 3 + p2
                    mm_group(P, pb[sb_][:, 0:128], [(KmT[:, csl], QmT[:, csl])], qk_keys, [pk[sb_]])
                    stt(P, Sp[p2][:], pb[sb_][:, 0:128], dd["ws"][:, c:c + 1], mslice, ALU.mult, ALU.mult,
                        [pk[sb_], ("ws", d_), "mk"], [("Sp", p2)])
                    ob_ = 5 + p2
                    mm_group(P, pb[ob_][:, 0:129], [(QmT[:, csl], Cbf[:]), (Sp[p2][:], Vaug[:, c, :])],
                             qk_keys + ["Cbf", ("Sp", p2), ("Vaug", c), "Vaug1"], [pk[ob_]])
                    act(P, den[p2][:], pb[ob_][:, 128:129], AF.Abs, [pk[ob_]], [("den", p2)])
                    tt(P, den[p2][:], den[p2][:], dd["flo"][:, c:c + 1], ALU.max, [("den", p2), ("flo", d_)], [("den", p2)])
                    P.dve(lambda e, o=rden[p2][:], i_=den[p2][:]: e.reciprocal(out=o, in_=i_), [("den", p2)], [("rden", p2)])
                    if d_ == 0:
                        ts(P, hfwd[:, c, :], pb[ob_][:, 0:128], rden[p2][:, 0:1], ALU.mult, [pk[ob_], ("rden", p2)], [("hfwd", c)])
                    else:
                        stt(P, hs[p2][:], pb[ob_][:, 0:128], rden[p2][:, 0:1], hfwd[:, c, :], ALU.mult, ALU.add,
                            [pk[ob_], ("rden", p2), ("hfwd", c)], [("hs", p2)])
                        tt(P, hsq[p2][:], hs[p2][:], hs[p2][:], ALU.mult, [("hs", p2)], [("hsq", p2)], eng="gpsimd")
                        red(P, ss[p2][:], hsq[p2][:], ALU.add, [("hsq", p2)], [("ss", p2)])
                        act(P, srt[p2][:], ss[p2][:], AF.Sqrt, [("ss", p2), "eps"], [("srt", p2)], bias=W["eps"][:, 0:1], scale=1.0 / 128)
                        P.dve(lambda e, o=srn[p2][:], i_=srt[p2][:]: e.reciprocal(out=o, in_=i_), [("srt", p2)], [("srn", p2)])
                        stt(P, yt[p2][:], hs[p2][:], srn[p2][:, 0:1], gmrs[:], ALU.mult, ALU.mult,
                            [("hs", p2), ("srn", p2), "gmr"], [("yt", p2)])
                        tt(P, y2[p2][:], yt[p2][:], osig[:, c, :], ALU.mult, [("yt", p2), ("osig", c)], [("y2", p2)], eng="gpsimd")
                        P.mm(lambda e, o=pb[7][:].bitcast(BF16)[:, 0:128], i_=y2[p2][:]: e.transpose(o, i_, identb[:]),
                             [("y2", p2), "identb"], [pk[7]])
                        grp = c // 4
                        yb = ymb[grp % 2]
                        cp(P, yb[:, (c % 4) * 128:(c % 4 + 1) * 128], pb[7][:].bitcast(BF16)[:, 0:128], [pk[7]],
                           [("ymb", grp % 2, c % 4)], eng="scalar")
                        if c % 4 == 0:
                            dma(P, ymT[:, grp * 512:(grp + 1) * 512], yb[:], reads=[("ymb", grp % 2, q_) for q_ in range(4)])
                    mm_group(P, pb[2][:, 256:385], [(Ktok[p2][:], Vw[p2][:])], [("Ktok", p2), ("Vw", p2)], [pk[2] + "u"])
                    ts(P, Cst[:], Cst[:], dd["ebl"][:, c:c + 1], ALU.mult, ["Cst", ("ebl", d_)], ["Cst"])
                    stt(P, Cst[:], pb[2][:, 256:385], dd["ebl"][:, c:c + 1], Cst[:], ALU.mult, ALU.add,
                        [pk[2] + "u", ("ebl", d_), "Cst"], ["Cst"])
                    act(P, Cbf[:], Cst[:], AF.Identity, ["Cst"], ["Cbf"], scale=MSCALE)
            P.barrier()

        with ExitStack() as s3:
            T3 = lambda n, s, d: s3.enter_context(nc.sbuf_tensor(n, s, d))
            pT = [T3(f"pT{i}", [128, 512], BF16) for i in range(3)]
            osb = [T3(f"osb{i}", [64, 512], F32) for i in range(2)]
            rec = T3("rec", [128, 512], F32)
            yab = [T3(f"yab{i}", [64, 512], BF16) for i in range(2)]
            NTQ = 512 // TA
            jobs = [(qb, h) for qb in range(att_qblocks) for h in range(2)]
            steps = [(ji, kc) for ji in range(len(jobs)) for kc in range(NCH)]
            LOOK = 2

            def emit_qk(i):
                ji, kc = steps[i]
                qb, h = jobs[ji]
                hsl = slice(h * 64, (h + 1) * 64)
                qsl = slice(qb * 512, (qb + 1) * 512)
                ksl = slice(kc * 128, (kc + 1) * 128)
                sb_ = i % 3
                KT_ = KTa if h == 0 else KTb
                mm_group(P, pb[sb_][:], [(KT_[:, ksl], QaT[:, qsl])],
                         [("KTa" if h == 0 else "KTb", kc // (TA // 128)), "KTa0", "KTb0"]
                         + [("QaT", qb * NTQ + i_) for i_ in range(NTQ)], [pk[sb_]])
                act(P, pT[sb_][:], pb[sb_][:], AF.Exp, [pk[sb_]], [("pT", sb_)], scale=ASCALE)

            def emit_pv(i):
                ji, kc = steps[i]
                qb, h = jobs[ji]
                hsl = slice(h * 64, (h + 1) * 64)
                qsl = slice(qb * 512, (qb + 1) * 512)
                sb_ = i % 3
                ob_ = 3 + ji % 2
                P.mm(lambda e, o=pb[ob_][0:65, :], l_=Va[:, kc, :], r_=pT[sb_][:], a_=(kc == 0), z_=(kc == NCH - 1):
                     e.matmul(o, lhsT=l_, rhs=r_, start=a_, stop=z_),
                     [("Va", kc), "Va1", ("pT", sb_)], [pk[ob_]])
                if kc == NCH - 1:
                    jb = ji % 2
                    P.dve(lambda e, o=rec[64:65, :], i_=pb[ob_][64:65, :]: e.reciprocal(out=o, in_=i_), [pk[ob_]], ["rec"])
                    mm_group(P, pb[5][0:64, :], [(onesf[64:65, 0:64], rec[64:65, :])], ["rec", "onesf"], [pk[5]])
                    cp(P, osb[jb][:], pb[ob_][0:64, :], [pk[ob_]], [("osb", jb)], eng="gpsimd" if False else "vector")
                    tt(P, yab[jb][:], osb[jb][:], pb[5][0:64, :], ALU.mult, [("osb", jb), pk[5]], [("yab", jb)])
                    dma(P, yaT[hsl, qsl], yab[jb][:], reads=[("yab", jb)])

            for i in range(len(steps) + LOOK):
                if i < len(steps):
                    emit_qk(i)
                if i - LOOK >= 0:
                    emit_pv(i - LOOK)
            P.barrier()
        P.emit()
    return nc


OFF = dict(mq=0, mk=512, mv=1024, mo=1536, gates=2048, aq=2064, ak=2576, av=2704, gm=2832, ga=3856, end=4880)


def _pk(v, n):
    return np.ascontiguousarray(np.asarray(v, np.float32).reshape(n, 128).T)


def _consts():
    esel = np.zeros((32, 32, 128), np.float32)
    for e in range(32):
        esel[e, e, :] = 1.0
    return dict(esel=esel.reshape(32, 32 * 128), ident=np.eye(128, dtype=np.float32))


def prep_B(inp, l, b, r, xT_b, ymT_b, yaT_b):
    tok = slice(r * NT_B, (r + 1) * NT_B)
    w_in = inp["w_in"][l]
    b_in = inp["b_in"][l]
    m = dict(
        xT=np.ascontiguousarray(xT_b[:, tok]),
        ymT=np.ascontiguousarray(ymT_b[:, tok]),
        yaT=np.ascontiguousarray(yaT_b[:, tok]),
        cvec=_pk(inp["c"][b], 8),
        w_ada=np.ascontiguousarray(inp["w_ada"][l]),
        b_ada=_pk(inp["b_ada"][l], 48),
        g1=_pk(inp["norm1_g"][l], 8), g2=_pk(inp["norm2_g"][l], 8), gf=_pk(inp["final_norm_g"], 8),
        w_g=np.ascontiguousarray(w_in[:, OFF["gm"]:OFF["end"]]),
        b_g=_pk(b_in[OFF["gm"]:OFF["end"]], 16),
        w_bm=np.ascontiguousarray(inp["w_branch_m"][l]),
        w_ba=np.ascontiguousarray(inp["w_branch_a"][l]),
        w_o=np.ascontiguousarray(inp["w_out"][l]),
        w_r=np.ascontiguousarray(np.concatenate([inp["w_router_group"][l], inp["w_router_expert"][l]], axis=1)),
        b_r=np.ascontiguousarray(np.broadcast_to(
            np.concatenate([inp["b_router_group"][l], inp["b_router_expert"][l]])[None, :], (128, 36))),
        w_gate=np.ascontiguousarray(inp["w_gate"][l]),
        w_up=np.ascontiguousarray(inp["w_up"][l]),
        w_down=np.ascontiguousarray(inp["w_down"][l]),
    )
    m.update(_consts())
    return m


def _rope_consts():
    rows = S_LEN // 64
    row = np.repeat(np.arange(rows, dtype=np.float32), 64)
    col = np.tile(np.arange(64, dtype=np.float32), rows)
    half = 32
    inv_freq = (np.float32(10000.0) ** (-np.arange(0, half, 2, dtype=np.float32) / np.float32(half))).astype(np.float32)
    ang_r = (row[:, None] * inv_freq).astype(np.float32)
    ang_c = (col[:, None] * inv_freq).astype(np.float32)
    cosT = np.zeros((64, S_LEN), np.float32)
    sinT = np.zeros((64, S_LEN), np.float32)
    for i in range(64):
        ang = ang_r if i < 32 else ang_c
        cosT[i] = np.cos(ang[:, i % 16])
        sinT[i] = np.sin(ang[:, i % 16])
    R = np.zeros((64, 64), np.float32)
    for i in range(64):
        if i % 32 < 16:
            R[i, i + 16] = -1.0
        else:
            R[i, i - 16] = 1.0
    R2 = np.zeros((128, 128), np.float32)
    R2[:64, :64] = R
    R2[64:, 64:] = R
    oblk = np.zeros((128, 128), np.float32)
    oblk[:64, :64] = 1.0
    oblk[64:, 64:] = 1.0
    masks = np.concatenate([np.triu(np.ones((128, 128), np.float32)), np.tril(np.ones((128, 128), np.float32))], axis=1)
    return dict(cosT=np.ascontiguousarray(np.tile(cosT, (2, 1))), sinT=np.ascontiguousarray(np.tile(sinT, (2, 1))),
                rT=np.ascontiguousarray(R2.T), oblk=oblk, masks=np.ascontiguousarray(masks),
                ident=np.eye(128, dtype=np.float32))


_ROPE = None


def prep_A(inp, l, b, r, xT_b):
    global _ROPE
    if _ROPE is None:
        _ROPE = _rope_consts()
    w_in = inp["w_in"][l]
    b_in = inp["b_in"][l]
    kv = r // 2
    fcols = np.concatenate([np.arange(OFF["mq"] + r * 128, OFF["mq"] + (r + 1) * 128),
                            np.arange(OFF["mk"] + r * 128, OFF["mk"] + (r + 1) * 128),
                            np.arange(OFF["aq"] + r * 128, OFF["aq"] + (r + 1) * 128),
                            np.arange(OFF["ak"] + kv * 64, OFF["ak"] + (kv + 1) * 64),
                            np.arange(OFF["ak"] + kv * 64, OFF["ak"] + (kv + 1) * 64)])
    tcols = np.concatenate([np.arange(OFF["mv"] + r * 128, OFF["mv"] + (r + 1) * 128),
                            np.arange(OFF["mo"] + r * 128, OFF["mo"] + (r + 1) * 128),
                            OFF["gates"] + np.arange(4) * 4 + r,
                            np.arange(OFF["av"] + kv * 64, OFF["av"] + (kv + 1) * 64)])
    cwl = inp["conv_w"][l][:, 0, :]
    cw = np.zeros((128, 10), np.float32)
    cb = np.zeros((128, 2), np.float32)
    for qk in range(2):
        ch = slice(qk * 512 + r * 128, qk * 512 + (r + 1) * 128)
        cw[:, qk * 5:(qk + 1) * 5] = cwl[:, ch].T
        cb[:, qk] = inp["conv_b"][l][ch]
    m = dict(
        xT=xT_b,
        cvec=_pk(inp["c"][b], 8),
        w_ada=np.ascontiguousarray(inp["w_ada"][l][:, 0:2 * D]),
        b_ada=_pk(inp["b_ada"][l][0:2 * D], 16),
        g1=_pk(inp["norm1_g"][l], 8),
        w_F=np.ascontiguousarray(w_in[:, fcols]),
        b_F=_pk(b_in[fcols], 4),
        w_T=np.ascontiguousarray(w_in[:, tcols]),
        b_T=np.ascontiguousarray(np.broadcast_to(b_in[tcols][None, :], (128, NT_T))),
        cw=cw, cb=cb,
        gmr=np.ascontiguousarray(np.broadcast_to(inp["mlstm_norm_g"][l][r * 128:(r + 1) * 128][None, :], (128, 128))),
        gqk=np.ascontiguousarray(np.stack([np.tile(inp["q_norm_g"][l], 2), np.tile(inp["k_norm_g"][l], 2)], axis=1)),
    )
    m.update(_ROPE)
    return m


def kernel(**inputs):
    inp = {k: np.asarray(v) for k, v in inputs.items()}
    cores = list(range(8))
    xT = [np.ascontiguousarray(inp["x"][b].T) for b in range(2)]
    for l in range(2):
        ncA = build_A()
        resA = run_bass_kernel_spmd(ncA, [prep_A(inp, l, c // 4, c % 4, xT[c // 4]) for c in cores], core_ids=cores)
        ymT = [np.concatenate([resA.results[b * 4 + r]["ymT"] for r in range(4)], axis=0) for b in range(2)]
        yaT = [np.concatenate([resA.results[b * 4 + r]["yaT"] for r in range(4)], axis=0) for b in range(2)]
        del resA
        ncB = build_B(last=(l == 1))
        resB = run_bass_kernel_spmd(ncB, [prep_B(inp, l, c // 4, c % 4, xT[c // 4], ymT[c // 4], yaT[c // 4]) for c in cores],
                                    core_ids=cores)
        xT = [np.concatenate([resB.results[b * 4 + r]["outT"] for r in range(4)], axis=1) for b in range(2)]
        del resB
    return np.ascontiguousarray(np.stack([xT[b].T for b in range(2)])).astype(np.float32)
```

```python
import numpy as np
import ml_dtypes
from contextlib import ExitStack
import concourse.bass as bass
import concourse.mybir as mybir
from concourse.bass_utils import run_bass_kernel_spmd

F32 = mybir.dt.float32
BF16 = mybir.dt.bfloat16
AF = mybir.ActivationFunctionType
ALU = mybir.AluOpType
AX = mybir.AxisListType

ENGS = ("tensor", "vector", "scalar", "gpsimd", "sync")
GEN = 30000


class Prog:
    def __init__(self, nc, n_dma_slots=8, same_engine_sync=True):
        self.nc = nc
        self.ops = {e: [] for e in ENGS}
        self.last_writer = {}
        self.readers = {}
        self.n_dma_slots = n_dma_slots
        self.dma_count = {e: 0 for e in ENGS}
        self.same_engine_sync = same_engine_sync
        self.pending_dma = []

    def op(self, eng, fn, reads=(), writes=(), dma=False, nosync_same=False):
        idx = len(self.ops[eng])
        deps = set()
        for k in reads:
            w = self.last_writer.get(k)
            if w is not None:
                deps.add(w)
        for k in writes:
            w = self.last_writer.get(k)
            if w is not None:
                deps.add(w)
            for r in self.readers.get(k, ()):
                deps.add(r)
        me = (eng, idx)
        deps.discard(me)
        slot = None
        if dma:
            slot = self.dma_count[eng] % self.n_dma_slots
            self.dma_count[eng] += 1
            self.pending_dma.append(me)
        elif nosync_same or not self.same_engine_sync:
            deps = {d for d in deps if d[0] != eng or self.ops[d[0]][d[1]]["dma"]}
        rec = dict(eng=eng, fn=fn, deps=deps, dma=dma, slot=slot, signal=False)
        self.ops[eng].append(rec)
        for d in deps:
            self.ops[d[0]][d[1]]["signal"] = True
        for k in reads:
            self.readers.setdefault(k, []).append(me)
        for k in writes:
            self.last_writer[k] = me
            self.readers[k] = []
        return me

    def mm(self, fn, reads=(), writes=()):
        return self.op("tensor", fn, reads, writes, nosync_same=True)

    def dve(self, fn, reads=(), writes=()):
        return self.op("vector", fn, reads, writes)

    def act(self, fn, reads=(), writes=()):
        return self.op("scalar", fn, reads, writes)

    def pool(self, fn, reads=(), writes=()):
        return self.op("gpsimd", fn, reads, writes)

    def dma(self, fn, reads=(), writes=(), q="sync"):
        return self.op(q, fn, reads, writes, dma=True)

    def barrier(self):
        lasts = []
        for e in ENGS:
            for i in range(len(self.ops[e]) - 1, -1, -1):
                r = self.ops[e][i]
                if r["fn"] is not None and not r["dma"]:
                    lasts.append((e, i))
                    break
        deps = set(lasts) | set(self.pending_dma)
        self.pending_dma = []
        for d in deps:
            self.ops[d[0]][d[1]]["signal"] = True
        for e in ENGS:
            self.ops[e].append(dict(eng=e, fn=None, deps={d for d in deps}, dma=False, slot=None, signal=False))

    def emit(self):
        nc = self.nc
        ngen = {}
        final_slot_counts = {}
        for e in ENGS:
            c = 0
            slot_counts = [0] * self.n_dma_slots
            for r in self.ops[e]:
                if r["dma"]:
                    slot_counts[r["slot"]] += 1
                    r["slot_prev"] = slot_counts[r["slot"]] - 1
                    r["sig"] = ("dma", e, r["slot"], 16 * slot_counts[r["slot"]])
                elif r["signal"]:
                    g, v = divmod(c, GEN)
                    r["sig"] = ("eng", e, g, v + 1)
                    c += 1
                else:
                    r["sig"] = None
            ngen[e] = (c + GEN - 1) // GEN if c else 0
            final_slot_counts[e] = slot_counts
        with ExitStack() as st:
            sems = {}
            for e in ENGS:
                for g in range(ngen[e]):
                    sems[("eng", e, g)] = st.enter_context(nc.semaphore(f"s_{e}_{g}"))
                if self.dma_count[e]:
                    for s in range(self.n_dma_slots):
                        sems[("dma", e, s)] = st.enter_context(nc.semaphore(f"d_{e}_{s}"))
            block = st.enter_context(nc.Block())

            def make_body(e):
                def body(eng):
                    waited = {}
                    for r in self.ops[e]:
                        need = {}
                        for d in r["deps"]:
                            if d[0] == e and r["fn"] is None and not self.ops[d[0]][d[1]]["dma"]:
                                continue
                            sig = self.ops[d[0]][d[1]]["sig"]
                            key = sig[:3]
                            need[key] = max(need.get(key, 0), sig[3])
                        if r["dma"] and r["slot_prev"] > 0:
                            key = ("dma", e, r["slot"])
                            need[key] = max(need.get(key, 0), 16 * r["slot_prev"])
                        for key, v in need.items():
                            if key[0] == "eng":
                                best = waited.get((key[0], key[1]), (-1, 0))
                                if (key[2], v) <= best:
                                    continue
                                waited[(key[0], key[1])] = (key[2], v)
                            else:
                                if waited.get(key, 0) >= v:
                                    continue
                                waited[key] = v
                            eng.wait_ge(sems[key], v)
                        if r["fn"] is None:
                            continue
                        ins = r["fn"](eng)
                        if r["sig"] is not None:
                            ins.then_inc(sems[r["sig"][:3]], 16 if r["dma"] else 1)
                    if e == "sync":
                        for q in ENGS:
                            if self.dma_count[q]:
                                for s in range(self.n_dma_slots):
                                    cnt = final_slot_counts[q][s]
                                    if cnt:
                                        eng.wait_ge(sems[("dma", q, s)], 16 * cnt)
                return body

            for e in ENGS:
                getattr(block, e)(make_body(e))


def mm_group(P, out_ap, pairs, reads, writes):
    n = len(pairs)

    def fn(e):
        ins = None
        for i, (l, r) in enumerate(pairs):
            ins = e.matmul(out_ap, lhsT=l, rhs=r, start=(i == 0), stop=(i == n - 1))
        return ins
    return P.mm(fn, reads, writes)


def dma(P, out_ap, in_ap, reads=(), writes=(), q="sync"):
    return P.dma(lambda e: e.dma_start(out=out_ap, in_=in_ap), reads, writes, q=q)


def act(P, out_ap, in_ap, func, reads, writes, bias=None, scale=None):
    kw = {}
    if bias is not None:
        kw["bias"] = bias
    if scale is not None:
        kw["scale"] = scale
    return P.act(lambda e: e.activation(out=out_ap, in_=in_ap, func=func, **kw), reads, writes)


def tt(P, out_ap, a, b, op, reads, writes, eng="vector"):
    return P.op(eng, lambda e: e.tensor_tensor(out=out_ap, in0=a, in1=b, op=op), reads, writes)


def ts(P, out_ap, a, s1, op0, reads, writes, s2=None, op1=None, eng="vector"):
    if op1 is None:
        return P.op(eng, lambda e: e.tensor_scalar(out=out_ap, in0=a, scalar1=s1, scalar2=None, op0=op0), reads, writes)
    return P.op(eng, lambda e: e.tensor_scalar(out=out_ap, in0=a, scalar1=s1, scalar2=s2, op0=op0, op1=op1), reads, writes)


def stt(P, out_ap, a, s, b, op0, op1, reads, writes):
    return P.dve(lambda e: e.scalar_tensor_tensor(out=out_ap, in0=a, scalar=s, in1=b, op0=op0, op1=op1), reads, writes)


def cp(P, out_ap, in_ap, reads, writes, eng="vector"):
    if eng == "scalar":
        return P.op(eng, lambda e: e.activation(out=out_ap, in_=in_ap, func=AF.Identity), reads, writes)
    return P.op(eng, lambda e: e.tensor_copy(out=out_ap, in_=in_ap), reads, writes)


def red(P, out_ap, in_ap, op, reads, writes):
    return P.dve(lambda e: e.tensor_reduce(out=out_ap, in_=in_ap, axis=AX.X, op=op), reads, writes)


D = 1024
NK = 8
TB = 512
EPS = 1e-6


def emit_mod(P, nc, st, pb, cvec, w_ada, b_ada, col_chunks, name="mod"):
    T = lambda n, s, d: st.enter_context(nc.sbuf_tensor(n, s, d))
    ncol = len(col_chunks)
    cs = T(name + "_cs", [128, NK], F32)
    css = T(name + "_css", [128, NK], F32)
    nch = max(col_chunks) + 1
    bsb = T(name + "_b", [128, nch], F32)
    mod = T(name, [128, nch], F32)
    dma(P, cs[:], cvec[:, :], writes=[name + "cs"])
    dma(P, bsb[:], b_ada[:, :], writes=[name + "b"])
    act(P, css[:], cs[:], AF.Silu, [name + "cs"], [name + "css"])
    wv = w_ada.rearrange("(k p) c -> p k c", p=128)
    with ExitStack() as st2:
        wa = [st2.enter_context(nc.sbuf_tensor(f"{name}_wa{i}", [128, NK, 768], F32)) for i in range(2)]
        pieces = []
        cur = []
        for j in col_chunks:
            if cur and (j != cur[-1] + 1 or len(cur) == 6):
                pieces.append(cur)
                cur = []
            cur.append(j)
        if cur:
            pieces.append(cur)
        for pi, piece in enumerate(pieces):
            buf = wa[pi % 2]
            key = (name + "wa", pi % 2)
            c0 = piece[0] * 128
            n = len(piece) * 128
            for kh in range(2):
                dma(P, buf[:, kh * 4:(kh + 1) * 4, 0:n], wv[:, kh * 4:(kh + 1) * 4, c0:c0 + n], writes=[key],
                    q=("sync" if kh == 0 else "gpsimd"))
            for jj, j in enumerate(piece):
                pairs = [(buf[:, k, jj * 128:(jj + 1) * 128], css[:, k:k + 1]) for k in range(NK)]
                mm_group(P, pb[0][:, j:j + 1], pairs, [key, name + "css"], ["pb0"])
        for j in col_chunks:
            tt(P, mod[:, j:j + 1], pb[0][:, j:j + 1], bsb[:, j:j + 1], ALU.add, ["pb0", name + "b"], [name])
        P.barrier()
    return mod


def emit_norm(P, nc, W, xk, a_t, shift_t, outs, xkeys, okeys, pbank, pkey, n=TB, tag="n", xfull=None, part="ab"):
    sq, rt, rstd, tmp = W["sq"], W["rt"], W["rstd"], W["tmp"]
    sqk = [(tag + "sq", k) for k in range(NK)]
    if "a" in part:
        if xfull is not None:
            act(P, sq[:, :, 0:n], xfull, AF.Square, xkeys, sqk)
        else:
            for k in range(NK):
                act(P, sq[:, k, 0:n], xk(k), AF.Square, xkeys, [sqk[k]])
    if "b" not in part:
        return
    pairs = [(W["ones_bf"][:], sq[:, k, 0:n]) for k in range(NK)]
    mm_group(P, pbank[:, 0:n], pairs, sqk + ["ones"], [pkey])
    act(P, rt[:, 0:n], pbank[:, 0:n], AF.Sqrt, [pkey, "eps"], [tag + "rt"], bias=W["eps"][:, 0:1], scale=1.0 / D)
    P.dve(lambda e: e.reciprocal(out=rstd[:, 0:n], in_=rt[:, 0:n]), [tag + "rt"], [tag + "rstd"])
    for k in range(NK):
        tb_ = tmp[k % 2]
        tk_ = (tag + "ntmp", k % 2)
        tt(P, tb_[:, 0:n], xk(k), rstd[:, 0:n], ALU.mult, xkeys + [tag + "rstd"], [tk_])
        for oi, ofn in enumerate(outs):
            if shift_t is not None:
                act(P, ofn(k), tb_[:, 0:n], AF.Identity, [tk_], [okeys[oi](k)],
                    bias=shift_t[:, k:k + 1], scale=a_t[:, k:k + 1])
            else:
                act(P, ofn(k), tb_[:, 0:n], AF.Identity, [tk_], [okeys[oi](k)],
                    scale=a_t[:, k:k + 1])


NT_B = 2048
NE = 32
FH = 512


def build_B(last, n_experts=NE):
    nc = bass.Bass("TRN2", target_bir_lowering=False)

    def din(name, shape, dt=F32):
        return nc.dram_tensor(name, shape, dt, kind="ExternalInput").ap()
    xT = din("xT", [D, NT_B])
    ymT = din("ymT", [512, NT_B], BF16)
    yaT = din("yaT", [512, NT_B], BF16)
    cvec = din("cvec", [128, NK])
    w_ada = din("w_ada", [D, 6 * D])
    b_ada = din("b_ada", [128, 48])
    g1 = din("g1", [128, NK])
    g2 = din("g2", [128, NK])
    gf = din("gf", [128, NK])
    w_g = din("w_g", [D, 2 * D])
    b_g = din("b_g", [128, 16])
    w_bm = din("w_bm", [512, D])
    w_ba = din("w_ba", [512, D])
    w_o = din("w_o", [D, D])
    w_r = din("w_r", [D, 36])
    b_r = din("b_r", [128, 36])
    w_gate = din("w_gate", [NE, D, FH])
    w_up = din("w_up", [NE, D, FH])
    w_down = din("w_down", [NE, FH, D])
    esel = din("esel", [32, 32 * 128])
    ident = din("ident", [128, 128])
    outT = nc.dram_tensor("outT", [D, NT_B], F32, kind="ExternalOutput").ap()
    NTB = NT_B // TB

    with ExitStack() as st:
        T = lambda n, s, d: st.enter_context(nc.sbuf_tensor(n, s, d))
        P = Prog(nc, same_engine_sync=SES_B)
        pb = [st.enter_context(nc.psum_tensor(f"pb{i}", [128, 512], F32)) for i in range(8)]
        pk = [f"pb{i}" for i in range(8)]
        x1T = T("x1T", [128, NK, NT_B], F32)
        W = dict(ones_bf=T("ones_bf", [128, 128], BF16), eps=T("eps", [128, 1], F32),
                 sq=T("sq", [128, NK, TB], BF16), rt=T("rt", [128, TB], F32), rstd=T("rstd", [128, TB], F32),
                 tmp=[T("ntmp0", [128, TB], F32), T("ntmp1", [128, TB], F32)])
        identf = T("identf", [128, 128], F32)
        g1s, g2s, gfs = T("g1s", [128, NK], F32), T("g2s", [128, NK], F32), T("gfs", [128, NK], F32)
        a1, a2 = T("a1", [128, NK], F32), T("a2", [128, NK], F32)
        bgs = T("bgs", [128, 16], F32)
        brs = T("brs", [128, 36], F32)
        wr = T("wr", [128, NK, 36], F32)
        P.pool(lambda e: e.memset(W["ones_bf"][:], 1.0), [], ["ones"])
        P.pool(lambda e: e.memset(W["eps"][:], EPS), [], ["eps"])
        dma(P, identf[:], ident[:, :], writes=["ident"])
        dma(P, g1s[:], g1[:, :], writes=["g1"])
        dma(P, g2s[:], g2[:, :], writes=["g2"])
        dma(P, gfs[:], gf[:, :], writes=["gf"])
        dma(P, bgs[:], b_g[:, :], writes=["bg"])
        dma(P, brs[:], b_r[:, :], writes=["br"])
        dma(P, wr[:], w_r.rearrange("(k p) c -> p k c", p=128), writes=["wr"])
        xv = xT.rearrange("(k p) t -> p k t", p=128)
        ymv = ymT.rearrange("(k p) t -> p k t", p=128)
        yav = yaT.rearrange("(k p) t -> p k t", p=128)
        for tb in range(NTB):
            for kh in range(2):
                dma(P, x1T[:, kh * 4:(kh + 1) * 4, tb * TB:(tb + 1) * TB], xv[:, kh * 4:(kh + 1) * 4, tb * TB:(tb + 1) * TB],
                    writes=[("x1T", k, tb) for k in range(kh * 4, kh * 4 + 4)])

        mod = emit_mod(P, nc, st, pb, cvec, w_ada, b_ada, list(range(48)))
        stt(P, a1[:], mod[:, 8:16], 1.0, g1s[:], ALU.add, ALU.mult, ["mod", "g1"], ["a1"])
        stt(P, a2[:], mod[:, 32:40], 1.0, g2s[:], ALU.add, ALU.mult, ["mod", "g2"], ["a2"])
        shift1, gate1, shift2, gate2 = mod[:, 0:8], mod[:, 16:24], mod[:, 24:32], mod[:, 40:48]

        with ExitStack() as s1:
            T1 = lambda n, s, d: s1.enter_context(nc.sbuf_tensor(n, s, d))
            wg_ = T1("wg_", [128, NK, 2 * D], BF16)
            wbm = T1("wbm", [128, 4, D], BF16)
            wba = T1("wba", [128, 4, D], BF16)
            wo = T1("wo", [128, NK, D], BF16)
            wgv = w_g.rearrange("(k p) c -> p k c", p=128)
            for k in range(NK):
                dma(P, wg_[:, k, :], wgv[:, k, :], writes=[("wg_", k)], q="gpsimd")
            dma(P, wbm[:], w_bm.rearrange("(k p) c -> p k c", p=128), writes=["wbm"], q="gpsimd")
            dma(P, wba[:], w_ba.rearrange("(k p) c -> p k c", p=128), writes=["wba"], q="gpsimd")
            wov = w_o.rearrange("(k p) c -> p k c", p=128)
            for k in range(0, NK, 2):
                dma(P, wo[:, k:k + 2, :], wov[:, k:k + 2, :], writes=[("wo", k), ("wo", k + 1)], q="gpsimd")
            h1 = T1("h1", [128, NK, TB], BF16)
            ymb = [T1(f"ymb{i}", [128, 4, TB], BF16) for i in range(2)]
            yab = [T1(f"yab{i}", [128, 4, TB], BF16) for i in range(2)]
            merged = T1("merged", [128, NK, TB], BF16)
            sg = [[T1(f"sg{i}{j}", [128, TB], F32) for j in range(2)] for i in range(2)]
            t12 = [[T1(f"t12{i}{j}", [128, TB], F32) for j in range(2)] for i in range(2)]
            for tb in range(NTB):
                tsl = slice(tb * TB, (tb + 1) * TB)
                b = tb % 2
                dma(P, ymb[b][:], ymv[:, :, tsl], writes=[("ymb", b)])
                dma(P, yab[b][:], yav[:, :, tsl], writes=[("yab", b)])
                emit_norm(P, nc, W, lambda k: x1T[:, k, tsl], a1, shift1,
                          [lambda k: h1[:, k, :]], [("x1T", k_, tb) for k_ in range(NK)] + ["a1", "mod"], [lambda k: ("h1", k)],
                          pb[0], "pb0")
                for dc in range(NK):
                    par = dc % 2
                    base = 4 * par
                    csl = slice(dc * 128, (dc + 1) * 128)
                    hk = [("h1", k) for k in range(NK)]
                    mm_group(P, pb[base][:], [(wg_[:, k, csl], h1[:, k, :]) for k in range(NK)],
                             hk + [("wg_", k) for k in range(NK)], [pk[base]])
                    mm_group(P, pb[base + 1][:], [(wg_[:, k, D + dc * 128:D + (dc + 1) * 128], h1[:, k, :]) for k in range(NK)],
                             hk + [("wg_", k) for k in range(NK)], [pk[base + 1]])
                    mm_group(P, pb[base + 2][:], [(wbm[:, k, csl], ymb[b][:, k, :]) for k in range(4)],
                             ["wbm", ("ymb", b)], [pk[base + 2]])
                    mm_group(P, pb[base + 3][:], [(wba[:, k, csl], yab[b][:, k, :]) for k in range(4)],
                             ["wba", ("yab", b)], [pk[base + 3]])
                    act(P, sg[par][0][:], pb[base][:], AF.Sigmoid, [pk[base], "bg"], [("sg", par, 0)], bias=bgs[:, dc:dc + 1])
                    act(P, sg[par][1][:], pb[base + 1][:], AF.Sigmoid, [pk[base + 1], "bg"], [("sg", par, 1)], bias=bgs[:, 8 + dc:9 + dc])
                    tt(P, t12[par][0][:], sg[par][0][:], pb[base + 2][:], ALU.mult, [("sg", par, 0), pk[base + 2]], [("t12", par, 0)])
                    tt(P, t12[par][1][:], sg[par][1][:], pb[base + 3][:], ALU.mult, [("sg", par, 1), pk[base + 3]], [("t12", par, 1)])
                    tt(P, merged[:, dc, :], t12[par][0][:], t12[par][1][:], ALU.add, [("t12", par, 0), ("t12", par, 1)],
                       [("merged", dc)], eng="gpsimd")
                for dc in range(NK):
                    bank = dc % 2
                    csl = slice(dc * 128, (dc + 1) * 128)
                    mm_group(P, pb[bank][:], [(wo[:, k, csl], merged[:, k, :]) for k in range(NK)],
                             [("merged", k) for k in range(NK)] + [("wo", k) for k in range(NK)], [pk[bank]])
                    stt(P, x1T[:, dc, tsl], pb[bank][:], gate1[:, dc:dc + 1], x1T[:, dc, tsl], ALU.mult, ALU.add,
                        [pk[bank], "mod", ("x1T", dc, tb)], [("x1T", dc, tb)])
            P.barrier()

        s23 = st.enter_context(ExitStack())
        h2T = s23.enter_context(nc.sbuf_tensor("h2T", [128, NK, NT_B], BF16))
        combT = s23.enter_context(nc.sbuf_tensor("combT", [32, NT_B], F32))
        with ExitStack() as s2:
            T2 = lambda n, s, d: s2.enter_context(nc.sbuf_tensor(n, s, d))
            h2f = T2("h2f", [128, NK, TB], F32)
            R = {n: T2("r_" + n, [128, s], F32) for n, s in
                 [("lg", 36), ("gmax", 1), ("ngmax", 1), ("eg", 4), ("ssum", 1), ("ptop", 1), ("mg", 4), ("pen", 4),
                  ("lem", 32), ("e1", 1), ("m1", 32), ("lem2", 32), ("e2", 1), ("m2", 32), ("d", 1), ("s2", 1),
                  ("w2", 1), ("w1", 1), ("comb", 32), ("comb2", 32)]}
            for tb in range(NTB):
                tsl = slice(tb * TB, (tb + 1) * TB)
                emit_norm(P, nc, W, lambda k: x1T[:, k, tsl], a2, shift2,
                          [lambda k: h2T[:, k, tsl], lambda k: h2f[:, k, :]],
                          [("x1T", k_, tb) for k_ in range(NK)] + ["a2", "mod"],
                          [lambda k: ("h2T", k, tb), lambda k: ("h2f", k)], pb[0], "pb0")
                for sub in range(4):
                    ssl = slice(sub * 128, (sub + 1) * 128)
                    bank = 1 + sub % 2
                    mm_group(P, pb[bank][:, 0:36], [(h2f[:, k, ssl], wr[:, k, :]) for k in range(NK)],
                             [("h2f", k) for k in range(NK)] + ["wr"], [pk[bank]])
                    tt(P, R["lg"][:], pb[bank][:, 0:36], brs[:], ALU.add, [pk[bank], "br"], ["r_lg"])
                    red(P, R["gmax"][:], R["lg"][:, 0:4], ALU.max, ["r_lg"], ["r_gmax"])
                    ts(P, R["ngmax"][:], R["gmax"][:], -1.0, ALU.mult, ["r_gmax"], ["r_ngmax"])
                    act(P, R["eg"][:], R["lg"][:, 0:4], AF.Exp, ["r_lg", "r_ngmax"], ["r_eg"], bias=R["ngmax"][:, 0:1])
                    red(P, R["ssum"][:], R["eg"][:], ALU.add, ["r_eg"], ["r_ssum"])
                    P.dve(lambda e: e.reciprocal(out=R["ptop"][:], in_=R["ssum"][:]), ["r_ssum"], ["r_ptop"])
                    ts(P, R["mg"][:], R["lg"][:, 0:4], R["gmax"][:, 0:1], ALU.is_equal, ["r_lg", "r_gmax"], ["r_mg"])
                    ts(P, R["pen"][:], R["mg"][:], -1.0, ALU.add, ["r_mg"], ["r_pen"], s2=1e30, op1=ALU.mult)
                    for g in range(4):
                        ts(P, R["lem"][:, g * 8:(g + 1) * 8], R["lg"][:, 4 + g * 8:12 + g * 8], R["pen"][:, g:g + 1], ALU.add,
                           ["r_lg", "r_pen"], [("r_lem", g)])
                    lemk = [("r_lem", g) for g in range(4)]
                    red(P, R["e1"][:], R["lem"][:], ALU.max, lemk, ["r_e1"])
                    ts(P, R["m1"][:], R["lem"][:], R["e1"][:, 0:1], ALU.is_equal, lemk + ["r_e1"], ["r_m1"])
                    stt(P, R["lem2"][:], R["m1"][:], -1e30, R["lem"][:], ALU.mult, ALU.add, lemk + ["r_m1"], ["r_lem2"])
                    red(P, R["e2"][:], R["lem2"][:], ALU.max, ["r_lem2"], ["r_e2"])
                    ts(P, R["m2"][:], R["lem2"][:], R["e2"][:, 0:1], ALU.is_equal, ["r_lem2", "r_e2"], ["r_m2"])
                    tt(P, R["d"][:], R["e2"][:], R["e1"][:], ALU.subtract, ["r_e1", "r_e2"], ["r_d"])
                    act(P, R["s2"][:], R["d"][:], AF.Sigmoid, ["r_d"], ["r_s2"])
                    tt(P, R["w2"][:], R["ptop"][:], R["s2"][:], ALU.mult, ["r_ptop", "r_s2"], ["r_w2"])
                    tt(P, R["w1"][:], R["ptop"][:], R["w2"][:], ALU.subtract, ["r_ptop", "r_w2"], ["r_w1"])
                    ts(P, R["comb"][:], R["m1"][:], R["w1"][:, 0:1], ALU.mult, ["r_m1", "r_w1"], ["r_comb"])
                    stt(P, R["comb2"][:], R["m2"][:], R["w2"][:, 0:1], R["comb"][:], ALU.mult, ALU.add,
                        ["r_m2", "r_w2", "r_comb"], ["r_comb2"])
                    P.mm(lambda e: e.transpose(pb[3][0:32, 0:128], R["comb2"][:], identf[:]), ["r_comb2", "ident"], [pk[3]])
                    tok = slice(tb * TB + sub * 128, tb * TB + (sub + 1) * 128)
                    act(P, combT[:, tok], pb[3][0:32, 0:128], AF.Identity, [pk[3]], [("combT", tb, sub)])
            P.barrier()

        with ExitStack() as s3:
            T3 = lambda n, s, d: s3.enter_context(nc.sbuf_tensor(n, s, d))
            eselT = T3("eselT", [32, 32 * 128], F32)
            dma(P, eselT[:], esel[:, :], writes=["esel"])
            wgt = [T3(f"wgt{i}", [128, NK, FH], BF16) for i in range(2)]
            wut = [T3(f"wut{i}", [128, NK, FH], BF16) for i in range(2)]
            wdt = [T3(f"wdt{i}", [128, 4, D], BF16) for i in range(2)]
            actT = [T3(f"actT{i}", [128, 4, TB], BF16) for i in range(2)]
            sl = [T3(f"sl{i}", [128, TB], F32) for i in range(2)]
            pr = [T3(f"pr{i}", [128, TB], F32) for i in range(2)]
            it = 0
            for e_ in range(n_experts):
                wb = e_ % 2
                gv = w_gate[e_].rearrange("(k p) f -> p k f", p=128)
                uv = w_up[e_].rearrange("(k p) f -> p k f", p=128)
                dv = w_down[e_].rearrange("(k p) c -> p k c", p=128)
                for kh in range(2):
                    ksl = slice(kh * 4, kh * 4 + 4)
                    dma(P, wgt[wb][:, ksl, :], gv[:, ksl, :], writes=[("wgt", wb, kh)], q="gpsimd")
                    dma(P, wut[wb][:, ksl, :], uv[:, ksl, :], writes=[("wut", wb, kh)], q="gpsimd")
                for kh in range(2):
                    ksl = slice(kh * 2, kh * 2 + 2)
                    dma(P, wdt[wb][:, ksl, :], dv[:, ksl, :], writes=[("wdt", wb, kh)], q="gpsimd")
                for tb in range(NTB):
                    tsl = slice(tb * TB, (tb + 1) * TB)
                    ab = it % 2
                    it += 1
                    h2k = [("h2T", k, tb) for k in range(NK)]
                    mm_group(P, pb[6][:], [(eselT[:, e_ * 128:(e_ + 1) * 128], combT[:, tsl])],
                             ["esel"] + [("combT", tb, s_) for s_ in range(4)], [pk[6]])
                    for fc in range(4):
                        fsl = slice(fc * 128, (fc + 1) * 128)
                        pa, pu = pb[2 * (fc % 2)], pb[2 * (fc % 2) + 1]
                        ka, ku = pk[2 * (fc % 2)], pk[2 * (fc % 2) + 1]
                        mm_group(P, pa[:], [(wgt[wb][:, k, fsl], h2T[:, k, tsl]) for k in range(NK)],
                                 h2k + [("wgt", wb, 0), ("wgt", wb, 1)], [ka])
                        mm_group(P, pu[:], [(wut[wb][:, k, fsl], h2T[:, k, tsl]) for k in range(NK)],
                                 h2k + [("wut", wb, 0), ("wut", wb, 1)], [ku])
                        act(P, sl[fc % 2][:], pa[:], AF.Silu, [ka], [("sl", fc % 2)])
                        tt(P, pr[fc % 2][:], sl[fc % 2][:], pu[:], ALU.mult, [("sl", fc % 2), ku], [("pr", fc % 2)])
                        tt(P, actT[ab][:, fc, :], pr[fc % 2][:], pb[6][:], ALU.mult, [("pr", fc % 2), pk[6]], [("actT", ab, fc)])
                    for dc in range(NK):
                        csl = slice(dc * 128, (dc + 1) * 128)
                        po, ko = pb[4 + dc % 2], pk[4 + dc % 2]
                        mm_group(P, po[:], [(wdt[wb][:, fc, csl], actT[ab][:, fc, :]) for fc in range(4)],
                                 [("actT", ab, fc) for fc in range(4)] + [("wdt", wb, 0), ("wdt", wb, 1)], [ko])
                        stt(P, x1T[:, dc, tsl], po[:], gate2[:, dc:dc + 1], x1T[:, dc, tsl], ALU.mult, ALU.add,
                            [ko, "mod", ("x1T", dc, tb)], [("x1T", dc, tb)])
            P.barrier()

        s23.close()
        ov = outT.rearrange("(k p) t -> p k t", p=128)
        if last:
            with ExitStack() as s4:
                T4 = lambda n, s, d: s4.enter_context(nc.sbuf_tensor(n, s, d))
                ob = [T4(f"ob{i}", [128, NK, TB], F32) for i in range(2)]
                for tb in range(NTB):
                    tsl = slice(tb * TB, (tb + 1) * TB)
                    o = ob[tb % 2]
                    emit_norm(P, nc, W, lambda k: x1T[:, k, tsl], gfs, None,
                              [lambda k: o[:, k, :]], [("x1T", k_, tb) for k_ in range(NK)] + ["gf"], [lambda k: ("ob", tb % 2, k)],
                              pb[0], "pb0")
                    dma(P, ov[:, :, tsl], o[:], reads=[("ob", tb % 2, k) for k in range(NK)])
                P.barrier()
        else:
            for tb in range(NTB):
                tsl = slice(tb * TB, (tb + 1) * TB)
                dma(P, ov[:, :, tsl], x1T[:, :, tsl], reads=[("x1T", k, tb) for k in range(NK)])
        P.emit()
    return nc


S_LEN = 8192
TA = 256
NCH = S_LEN // 128
MSCALE = 128.0 ** -0.5
ASCALE = 64.0 ** -0.5
NT_T = 324


SES_A = True
SES_B = True


def build_A(att_qblocks=16, do_mlstm=True):
    nc = bass.Bass("TRN2", target_bir_lowering=False)

    def din(name, shape, dt=F32):
        return nc.dram_tensor(name, shape, dt, kind="ExternalInput").ap()
    xT = din("xT", [D, S_LEN])
    cvec = din("cvec", [128, NK])
    w_ada = din("w_ada", [D, 2 * D])
    b_ada = din("b_ada", [128, 16])
    g1 = din("g1", [128, NK])
    w_F = din("w_F", [D, 512])
    b_F = din("b_F", [128, 4])
    w_T = din("w_T", [D, NT_T])
    b_T = din("b_T", [128, NT_T])
    cw = din("cw", [128, 10])
    cb = din("cb", [128, 2])
    gmr = din("gmr", [128, 128])
    gqk = din("gqk", [128, 2])
    cosT = din("cosT", [128, S_LEN])
    sinT = din("sinT", [128, S_LEN])
    ident = din("ident", [128, 128])
    masks = din("masks", [128, 256])
    rT = din("rT", [128, 128])
    oblk = din("oblk", [128, 128])
    ymT = nc.dram_tensor("ymT", [128, S_LEN], BF16, kind="ExternalOutput").ap()
    yaT = nc.dram_tensor("yaT", [128, S_LEN], BF16, kind="ExternalOutput").ap()
    NB = S_LEN // TA

    with ExitStack() as st:
        T = lambda n, s, d: st.enter_context(nc.sbuf_tensor(n, s, d))
        P = Prog(nc, same_engine_sync=SES_A)
        pb = [st.enter_context(nc.psum_tensor(f"pb{i}", [128, 512], F32)) for i in range(8)]
        pk = [f"pb{i}" for i in range(8)]
        QmT = T("QmT", [128, S_LEN], BF16)
        KmT = T("KmT", [128, S_LEN], BF16)
        Vaug = T("Vaug", [128, NCH, 129], BF16)
        osig = T("osig", [128, NCH, 128], BF16)
        G = T("G", [128, NCH, 4], F32)
        QaT = T("QaT", [128, S_LEN], BF16)
        KTa = T("KTa", [128, S_LEN], BF16)
        KTb = T("KTb", [128, S_LEN], BF16)
        Va = T("Va", [128, NCH, 65], BF16)
        W = dict(ones_bf=T("ones_bf", [128, 128], BF16), eps=T("eps", [128, 1], F32),
                 sq=T("sq", [128, NK, TA], BF16), rt=T("rt", [128, TA], F32), rstd=T("rstd", [128, TA], F32),
                 tmp=[T("ntmp0", [128, TA], F32), T("ntmp1", [128, TA], F32)])
        identb = T("identb", [128, 128], BF16)
        mk = T("mk", [128, 256], F32)
        onesf = T("onesf", [128, 128], F32)
        one1 = T("one1", [128, 1], F32)
        rTs = T("rTs", [128, 128], F32)
        oblkb = T("oblkb", [128, 128], BF16)
        gmrs = T("gmrs", [128, 128], F32)
        gqks = T("gqks", [128, 2], F32)
        bFs = T("bFs", [128, 4], F32)
        bTs = T("bTs", [128, NT_T], F32)
        cws = T("cws", [128, 10], F32)
        cbs = T("cbs", [128, 2], F32)
        g1s = T("g1s", [128, NK], F32)
        a1 = T("a1", [128, NK], F32)
        P.pool(lambda e: e.memset(W["ones_bf"][:], 1.0), [], ["ones"])
        P.pool(lambda e: e.memset(W["eps"][:], EPS), [], ["eps"])
        P.pool(lambda e: e.memset(onesf[:], 1.0), [], ["onesf"])
        P.pool(lambda e: e.memset(one1[:], 1.0), [], ["one1"])
        P.pool(lambda e: e.memset(Vaug[:, :, 128:129], 1.0), [], ["Vaug1"])
        P.pool(lambda e: e.memset(Va[:, :, 64:65], 1.0), [], ["Va1"])
        P.pool(lambda e: e.memset(KTa[64:128, :], 0.0), [], ["KTa0"])
        P.pool(lambda e: e.memset(KTb[0:64, :], 0.0), [], ["KTb0"])
        dma(P, identb[:], ident[:, :], writes=["identb"], q="gpsimd")
        dma(P, oblkb[:], oblk[:, :], writes=["oblkb"], q="gpsimd")
        for t_, d_, k_ in [(mk, masks, "mk"), (rTs, rT, "rT"),
                           (gmrs, gmr, "gmr"), (gqks, gqk, "gqk"), (bFs, b_F, "bF"), (bTs, b_T, "bT"),
                           (cws, cw, "cw"), (cbs, cb, "cb"), (g1s, g1, "g1")]:
            dma(P, t_[:], d_[:, :], writes=[k_])

        mod = emit_mod(P, nc, st, pb, cvec, w_ada, b_ada, list(range(16)))
        stt(P, a1[:], mod[:, 8:16], 1.0, g1s[:], ALU.add, ALU.mult, ["mod", "g1"], ["a1"])
        shift1 = mod[:, 0:8]

        with ExitStack() as s1:
            T1 = lambda n, s, d: s1.enter_context(nc.sbuf_tensor(n, s, d))
            wF = T1("wF", [128, NK, 512], BF16)
            wT = T1("wT", [128, NK, NT_T], BF16)
            dma(P, wF[:], w_F.rearrange("(k p) c -> p k c", p=128), writes=["wF"], q="gpsimd")
            dma(P, wT[:], w_T.rearrange("(k p) c -> p k c", p=128), writes=["wT"], q="gpsimd")
            xb = [T1(f"xb{i}", [128, NK, TA], F32) for i in range(2)]
            hTs = [T1(f"hT{i}", [128, NK, TA], BF16) for i in range(2)]
            W2 = dict(W)
            W2.update(sq=T1("sq2", [128, NK, TA], BF16), rt=T1("rt2", [128, TA], F32), rstd=T1("rstd2", [128, TA], F32),
                      tmp=[T1("ntmp20", [128, TA], F32), T1("ntmp21", [128, TA], F32)])
            Wp = [W, W2]
            NR = 3
            ring = [T1(f"ring{i}", [128, NR, TA], F32) for i in range(2)]
            acc = [T1(f"acc{i}", [128, TA], F32) for i in range(2)]
            cs_ = [T1(f"cosb{i}", [128, TA], F32) for i in range(2)]
            sn_ = [T1(f"sinb{i}", [128, TA], F32) for i in range(2)]
            RTMP = [{n_: T1(f"{n_}{i}", [128, TA], BF16 if n_ == "qsq" else F32)
                     for n_ in ("qf", "qsq", "qrt", "qrs", "qu", "qt1", "qt2")} for i in range(2)]
            tmpT = [T1(f"tmpT{i}", [128, NT_T], F32) for i in range(2)]
            xv = xT.rearrange("(k p) t -> p k t", p=128)

            def load_x(tb):
                tsl = slice(tb * TA, (tb + 1) * TA)
                for kh in range(2):
                    dma(P, xb[tb % 2][:, kh * 4:(kh + 1) * 4, :], xv[:, kh * 4:(kh + 1) * 4, tsl],
                        writes=[("xb", tb % 2, k) for k in range(kh * 4, kh * 4 + 4)])

            def conv_block(j):
                tsl = slice(j * TA, (j + 1) * TA)
                for qk in range(2):
                    cur = ring[qk][:, j % NR, :]
                    a = acc[qk]
                    ak = ("acc", qk)
                    rk = lambda jj: ("ring", qk, jj % NR)
                    wcol = lambda k: cws[:, qk * 5 + k:qk * 5 + k + 1]
                    ts(P, a[:], cur, wcol(2), ALU.mult, [rk(j), "cw"], [ak])
                    for k in (0, 1, 3, 4):
                        s_ = k - 2
                        if s_ < 0:
                            stt(P, a[:, -s_:TA], ring[qk][:, j % NR, 0:TA + s_], wcol(k), a[:, -s_:TA], ALU.mult, ALU.add,
                                [rk(j), "cw", ak], [ak])
                            if j > 0:
                                stt(P, a[:, 0:-s_], ring[qk][:, (j - 1) % NR, TA + s_:TA], wcol(k), a[:, 0:-s_], ALU.mult, ALU.add,
                                    [rk(j - 1), "cw", ak], [ak])
                        else:
                            stt(P, a[:, 0:TA - s_], ring[qk][:, j % NR, s_:TA], wcol(k), a[:, 0:TA - s_], ALU.mult, ALU.add,
                                [rk(j), "cw", ak], [ak])
                            if j < NB - 1:
                                stt(P, a[:, TA - s_:TA], ring[qk][:, (j + 1) % NR, 0:s_], wcol(k), a[:, TA - s_:TA], ALU.mult, ALU.add,
                                    [rk(j + 1), "cw", ak], [ak])
                    dest = (QmT if qk == 0 else KmT)
                    act(P, dest[:, tsl], a[:], AF.Silu, [ak, "cb"], [("QKm", qk, j)], bias=cbs[:, qk:qk + 1])

            def rope_norm(pf, pkey, bcol, gcol, dest, dkey, tb, rp):
                tsl = slice(tb * TA, (tb + 1) * TA)
                cb_, sb_ = cs_[tb % 2], sn_[tb % 2]
                R_ = RTMP[rp]
                qf, qsq, qrt, qrs, qu, qt1, qt2 = (R_[n_] for n_ in ("qf", "qsq", "qrt", "qrs", "qu", "qt1", "qt2"))
                kq = lambda n_: (n_, rp)
                pss, psr = pb[5 + rp], pk[5 + rp]
                act(P, qf[:], pf[:, 0:TA], AF.Identity, [pkey, "bF"], [kq("qf")], bias=bFs[:, bcol:bcol + 1])
                act(P, qsq[:], qf[:], AF.Square, [kq("qf")], [kq("qsq")])
                mm_group(P, pss[:, 0:TA], [(oblkb[:], qsq[:])], [kq("qsq"), "oblkb"], [psr])
                act(P, qrt[:], pss[:, 0:TA], AF.Sqrt, [psr, "eps"], [kq("qrt")], bias=W["eps"][:, 0:1], scale=1.0 / 64)
                P.dve(lambda e: e.reciprocal(out=qrs[:], in_=qrt[:]), [kq("qrt")], [kq("qrs")])
                ts(P, qu[:], qf[:], gqks[:, gcol:gcol + 1], ALU.mult, [kq("qf"), "gqk"], [kq("qu")])
                mm_group(P, pss[:, 256:256 + TA], [(rTs[:], qu[:])], [kq("qu"), "rT"], [psr])
                tt(P, qt1[:], qu[:], cb_[:], ALU.mult, [kq("qu"), ("cos", tb % 2)], [kq("qt1")])
                tt(P, qt2[:], pss[:, 256:256 + TA], sb_[:], ALU.mult, [psr, ("sin", tb % 2)], [kq("qt2")])
                tt(P, qt1[:], qt1[:], qt2[:], ALU.add, [kq("qt1"), kq("qt2")], [kq("qt1")], eng="gpsimd")
                if dest is None:
                    tt(P, KTa[0:64, tsl], qt1[0:64, :], qrs[0:64, :], ALU.mult, [kq("qt1"), kq("qrs")], [("KTa", tb)])
                    tt(P, KTb[64:128, tsl], qt1[64:128, :], qrs[64:128, :], ALU.mult, [kq("qt1"), kq("qrs")], [("KTb", tb)])
                else:
                    tt(P, dest[:, tsl], qt1[:], qrs[:], ALU.mult, [kq("qt1"), kq("qrs")], [(dkey, tb)])

            def norm_blk(tb, part):
                x_ = xb[tb % 2]
                hp = tb % 2
                hT = hTs[hp]
                nb_ = 0 if hp == 0 else 7
                emit_norm(P, nc, Wp[hp], lambda k: x_[:, k, :], a1, shift1, [lambda k: hT[:, k, :]],
                          [("xb", tb % 2, k_) for k_ in range(NK)] + ["a1", "mod"], [lambda k: ("hT", hp, k)],
                          pb[nb_], pk[nb_], n=TA, tag=f"n{hp}", xfull=x_[:, :, :], part=part)

            load_x(0)
            load_x(1)
            norm_blk(0, "ab")
            for tb in range(NB):
                tsl = slice(tb * TA, (tb + 1) * TA)
                dma(P, cs_[tb % 2][:], cosT[:, tsl], writes=[("cos", tb % 2)])
                dma(P, sn_[tb % 2][:], sinT[:, tsl], writes=[("sin", tb % 2)])
                if tb + 1 < NB:
                    norm_blk(tb + 1, "a")
                hp = tb % 2
                hT = hTs[hp]
                hk = [("hT", hp, k) for k in range(NK)]
                for fc in range(4):
                    bank = 1 + fc % 2
                    mm_group(P, pb[bank][:, 0:TA], [(wF[:, k, fc * 128:(fc + 1) * 128], hT[:, k, :]) for k in range(NK)],
                             hk + ["wF"], [pk[bank]])
                    if fc < 2:
                        act(P, ring[fc][:, tb % NR, :], pb[bank][:, 0:TA], AF.Identity, [pk[bank], "bF"], [("ring", fc, tb % NR)],
                            bias=bFs[:, fc:fc + 1])
                    elif fc == 2:
                        rope_norm(pb[bank], pk[bank], 2, 0, QaT, "QaT", tb, 0)
                    else:
                        rope_norm(pb[bank], pk[bank], 3, 1, None, "KT", tb, 1)
                if tb + 1 < NB:
                    norm_blk(tb + 1, "b")
                for sub in range(TA // 128):
                    ch = tb * (TA // 128) + sub
                    bank = 3 + sub % 2
                    mm_group(P, pb[bank][:, 0:NT_T], [(hT[:, k, sub * 128:(sub + 1) * 128], wT[:, k, :]) for k in range(NK)],
                             hk + ["wT"], [pk[bank]])
                    tm = tmpT[sub % 2]
                    tk = ("tmpT", sub % 2)
                    tt(P, tm[:], pb[bank][:, 0:NT_T], bTs[:], ALU.add, [pk[bank], "bT"], [tk])
                    cp(P, Vaug[:, ch, 0:128], tm[:, 0:128], [tk], [("Vaug", ch)], eng="gpsimd")
                    act(P, osig[:, ch, :], tm[:, 128:256], AF.Sigmoid, [tk], [("osig", ch)])
                    cp(P, G[:, ch, :], tm[:, 256:260], [tk], [("G", ch)], eng="gpsimd")
                    cp(P, Va[:, ch, 0:64], tm[:, 260:324], [tk], [("Va", ch)], eng="gpsimd")
                if tb >= 1:
                    conv_block(tb - 1)
                if tb + 2 < NB:
                    load_x(tb + 2)
            conv_block(NB - 1)
            P.barrier()

        if do_mlstm:
          with ExitStack() as s2:
            T2 = lambda n, s, d: s2.enter_context(nc.sbuf_tensor(n, s, d))
            hfwd = T2("hfwd", [128, NCH, 128], F32)
            Gk = [("G", ch) for ch in range(NCH)]
            ge = T2("ge", [128, NCH, 2], F32)
            lfn = T2("lfn", [128, NCH, 2], F32)
            dirs = []
            for d_ in range(2):
                dd = {n: T2(f"{n}{d_}", [128, NCH], F32) for n in ("b", "imb", "w", "ws", "flo", "ebl")}
                dirs.append(dd)
            Cst = T2("Cst", [128, 129], F32)
            Cbf = T2("Cbf", [128, 129], BF16)
            Ktok = [T2(f"Ktok{i}", [128, 128], BF16) for i in range(2)]
            Vw = [T2(f"Vw{i}", [128, 129], BF16) for i in range(2)]
            Sp = [T2(f"Sp{i}", [128, 128], BF16) for i in range(2)]
            den = [T2(f"den{i}", [128, 1], F32) for i in range(2)]
            rden = [T2(f"rden{i}", [128, 1], F32) for i in range(2)]
            hs = [T2(f"hs{i}", [128, 128], F32) for i in range(2)]
            hsq = [T2(f"hsq{i}", [128, 128], F32) for i in range(2)]
            ss = [T2(f"ss{i}", [128, 1], F32) for i in range(2)]
            srt = [T2(f"srt{i}", [128, 1], F32) for i in range(2)]
            srn = [T2(f"srn{i}", [128, 1], F32) for i in range(2)]
            yt = [T2(f"yt{i}", [128, 128], F32) for i in range(2)]
            y2 = [T2(f"y2{i}", [128, 128], BF16) for i in range(2)]
            ymb = [T2(f"ymb{i}", [128, 512], BF16) for i in range(2)]
            for d_ in range(2):
                fcol = 1 + 2 * d_
                act(P, ge[:, :, d_], G[:, :, fcol], AF.Exp, Gk, [("ge", d_)], scale=-1.0)
                act(P, lfn[:, :, d_], ge[:, :, d_], AF.Ln, [("ge", d_), "one1"], [("lfn", d_)], bias=one1[:, 0:1])
            for d_ in range(2):
                dd = dirs[d_]
                icol = 2 * d_
                mslice = mk[:, d_ * 128:(d_ + 1) * 128]
                mm_group(P, pb[0][:, 0:NCH], [(mslice, lfn[:, :, d_])], [("lfn", d_), "mk"], [pk[0]])
                mm_group(P, pb[1][:, 0:NCH], [(onesf[:], lfn[:, :, d_])], [("lfn", d_), "onesf"], [pk[1]])
                cp(P, dd["b"][:], pb[0][:, 0:NCH], [pk[0]], [("mb", d_)])
                tt(P, dd["imb"][:], G[:, :, icol], dd["b"][:], ALU.add, Gk + [("mb", d_)], [("imb", d_)])
                act(P, dd["w"][:], dd["imb"][:], AF.Exp, [("imb", d_)], [("w", d_)])
                ts(P, dd["ws"][:], dd["w"][:], MSCALE, ALU.mult, [("w", d_)], [("ws", d_)])
                act(P, dd["flo"][:], dd["b"][:], AF.Exp, [("mb", d_)], [("flo", d_)])
                act(P, dd["ebl"][:], pb[1][:, 0:NCH], AF.Exp, [pk[1]], [("ebl", d_)], scale=-1.0)
            it = 0
            for d_ in range(2):
                dd = dirs[d_]
                mslice = mk[:, d_ * 128:(d_ + 1) * 128]
                P.dve(lambda e: e.memset(Cst[:], 0.0), [], ["Cst"])
                P.dve(lambda e: e.memset(Cbf[:], 0.0), [], ["Cbf"])
                order = range(NCH) if d_ == 0 else range(NCH - 1, -1, -1)
                for c in order:
                    p2 = it % 2
                    it += 1
                    csl = slice(c * 128, (c + 1) * 128)
                    tb_q = c // (TA // 128)
                    qk_keys = [("QKm", 0, tb_q), ("QKm", 1, tb_q)]
                    P.mm(lambda e, o=pb[2][:].bitcast(BF16)[:, 0:128], i_=KmT[:, csl]: e.transpose(o, i_, identb[:]),
                         [("QKm", 1, tb_q), "identb"], [pk[2]])
                    cp(P, Ktok[p2][:], pb[2][:].bitcast(BF16)[:, 0:128], [pk[2]], [("Ktok", p2)], eng="scalar")
                    ts(P, Vw[p2][:], Vaug[:, c, :], dd["w"][:, c:c + 1], ALU.mult, [("Vaug", c), "Vaug1", ("w", d_)], [("Vw", p2)],
                       eng="gpsimd")
                    sb_ = 3 + p2
                    mm_group(P, pb[sb_][:, 0:128], [(KmT[:, csl], QmT[:, csl])], qk_keys, [pk[sb_]])
                    stt(P, Sp[p2][:], pb[sb_][:, 0:128], dd["ws"][:, c:c + 1], mslice, ALU.mult, ALU.mult,
                        [pk[sb_], ("ws", d_), "mk"], [("Sp", p2)])
                    ob_ = 5 + p2
                    mm_group(P, pb[ob_][:, 0:129], [(QmT[:, csl], Cbf[:]), (Sp[p2][:], Vaug[:, c, :])],
                             qk_keys + ["Cbf", ("Sp", p2), ("Vaug", c), "Vaug1"], [pk[ob_]])
                    act(P, den[p2][:], pb[ob_][:, 128:129], AF.Abs, [pk[ob_]], [("den", p2)])
                    tt(P, den[p2][:], den[p2][:], dd["flo"][:, c:c + 1], ALU.max, [("den", p2), ("flo", d_)], [("den", p2)])
                    P.dve(lambda e, o=rden[p2][:], i_=den[p2][:]: e.reciprocal(out=o, in_=i_), [("den", p2)], [("rden", p2)])
                    if d_ == 0:
                        ts(P, hfwd[:, c, :], pb[ob_][:, 0:128], rden[p2][:, 0:1], ALU.mult, [pk[ob_], ("rden", p2)], [("hfwd", c)])
                    else:
                        stt(P, hs[p2][:], pb[ob_][:, 0:128], rden[p2][:, 0:1], hfwd[:, c, :], ALU.mult, ALU.add,
                            [pk[ob_], ("rden", p2), ("hfwd", c)], [("hs", p2)])
                        tt(P, hsq[p2][:], hs[p2][:], hs[p2][:], ALU.mult, [("hs", p2)], [("hsq", p2)], eng="gpsimd")
                        red(P, ss[p2][:], hsq[p2][:], ALU.add, [("hsq", p2)], [("ss", p2)])
                        act(P, srt[p2][:], ss[p2][:], AF.Sqrt, [("ss", p2), "eps"], [("srt", p2)], bias=W["eps"][:, 0:1], scale=1.0 / 128)
                        P.dve(lambda e, o=srn[p2][:], i_=srt[p2][:]: e.reciprocal(out=o, in_=i_), [("srt", p2)], [("srn", p2)])
                        stt(P, yt[p2][:], hs[p2][:], srn[p2][:, 0:1], gmrs[:], ALU.mult, ALU.mult,
                            [("hs", p2), ("srn", p2), "gmr"], [("yt", p2)])
                        tt(P, y2[p2][:], yt[p2][:], osig[:, c, :], ALU.mult, [("yt", p2), ("osig", c)], [("y2", p2)], eng="gpsimd")
                        P.mm(lambda e, o=pb[7][:].bitcast(BF16)[:, 0:128], i_=y2[p2][:]: e.transpose(o, i_, identb[:]),
                             [("y2", p2), "identb"], [pk[7]])
                        grp = c // 4
                        yb = ymb[grp % 2]
                        cp(P, yb[:, (c % 4) * 128:(c % 4 + 1) * 128], pb[7][:].bitcast(BF16)[:, 0:128], [pk[7]],
                           [("ymb", grp % 2, c % 4)], eng="scalar")
                        if c % 4 == 0:
                            dma(P, ymT[:, grp * 512:(grp + 1) * 512], yb[:], reads=[("ymb", grp % 2, q_) for q_ in range(4)])
                    mm_group(P, pb[p2][:, 0:129], [(Ktok[p2][:], Vw[p2][:])], [("Ktok", p2), ("Vw", p2)], [pk[p2]])
                    ts(P, Cst[:], Cst[:], dd["ebl"][:, c:c + 1], ALU.mult, ["Cst", ("ebl", d_)], ["Cst"])
                    stt(P, Cst[:], pb[p2][:, 0:129], dd["ebl"][:, c:c + 1], Cst[:], ALU.mult, ALU.add,
                        [pk[p2], ("ebl", d_), "Cst"], ["Cst"])
                    act(P, Cbf[:], Cst[:], AF.Identity, ["Cst"], ["Cbf"], scale=MSCALE)
            P.barrier()

        with ExitStack() as s3:
            T3 = lambda n, s, d: s3.enter_context(nc.sbuf_tensor(n, s, d))
            pT = [T3(f"pT{i}", [128, 512], BF16) for i in range(3)]
            osb = [T3(f"osb{i}", [64, 512], F32) for i in range(2)]
            rec = T3("rec", [128, 512], F32)
            yab = [T3(f"yab{i}", [64, 512], BF16) for i in range(2)]
            NTQ = 512 // TA
            jobs = [(qb, h) for qb in range(att_qblocks) for h in range(2)]
            steps = [(ji, kc) for ji in range(len(jobs)) for kc in range(NCH)]
            LOOK = 2

            def emit_qk(i):
                ji, kc = steps[i]
                qb, h = jobs[ji]
                hsl = slice(h * 64, (h + 1) * 64)
                qsl = slice(qb * 512, (qb + 1) * 512)
                ksl = slice(kc * 128, (kc + 1) * 128)
                sb_ = i % 3
                KT_ = KTa if h == 0 else KTb
                mm_group(P, pb[sb_][:], [(KT_[:, ksl], QaT[:, qsl])],
                         [("KTa" if h == 0 else "KTb", kc // (TA // 128)), "KTa0", "KTb0"]
                         + [("QaT", qb * NTQ + i_) for i_ in range(NTQ)], [pk[sb_]])
                act(P, pT[sb_][:], pb[sb_][:], AF.Exp, [pk[sb_]], [("pT", sb_)], scale=ASCALE)

            def emit_pv(i):
                ji, kc = steps[i]
                qb, h = jobs[ji]
                hsl = slice(h * 64, (h + 1) * 64)
                qsl = slice(qb * 512, (qb + 1) * 512)
                sb_ = i % 3
                ob_ = 3 + ji % 2
                P.mm(lambda e, o=pb[ob_][0:65, :], l_=Va[:, kc, :], r_=pT[sb_][:], a_=(kc == 0), z_=(kc == NCH - 1):
                     e.matmul(o, lhsT=l_, rhs=r_, start=a_, stop=z_),
                     [("Va", kc), "Va1", ("pT", sb_)], [pk[ob_]])
                if kc == NCH - 1:
                    jb = ji % 2
                    P.dve(lambda e, o=rec[64:65, :], i_=pb[ob_][64:65, :]: e.reciprocal(out=o, in_=i_), [pk[ob_]], ["rec"])
                    mm_group(P, pb[5][0:64, :], [(onesf[64:65, 0:64], rec[64:65, :])], ["rec", "onesf"], [pk[5]])
                    cp(P, osb[jb][:], pb[ob_][0:64, :], [pk[ob_]], [("osb", jb)], eng="gpsimd" if False else "vector")
                    tt(P, yab[jb][:], osb[jb][:], pb[5][0:64, :], ALU.mult, [("osb", jb), pk[5]], [("yab", jb)])
                    dma(P, yaT[hsl, qsl], yab[jb][:], reads=[("yab", jb)])

            for i in range(len(steps) + LOOK):
                if i < len(steps):
                    emit_qk(i)
                if i - LOOK >= 0:
                    emit_pv(i - LOOK)
            P.barrier()
        P.emit()
    return nc


OFF = dict(mq=0, mk=512, mv=1024, mo=1536, gates=2048, aq=2064, ak=2576, av=2704, gm=2832, ga=3856, end=4880)


def _pk(v, n):
    return np.ascontiguousarray(np.asarray(v, np.float32).reshape(n, 128).T)


def _consts():
    esel = np.zeros((32, 32, 128), np.float32)
    for e in range(32):
        esel[e, e, :] = 1.0
    return dict(esel=esel.reshape(32, 32 * 128), ident=np.eye(128, dtype=np.float32))


def prep_B(inp, l, b, r, xT_b, ymT_b, yaT_b):
    tok = slice(r * NT_B, (r + 1) * NT_B)
    w_in = inp["w_in"][l]
    b_in = inp["b_in"][l]
    m = dict(
        xT=np.ascontiguousarray(xT_b[:, tok]),
        ymT=np.ascontiguousarray(ymT_b[:, tok]),
        yaT=np.ascontiguousarray(yaT_b[:, tok]),
        cvec=_pk(inp["c"][b], 8),
        w_ada=np.ascontiguousarray(inp["w_ada"][l]),
        b_ada=_pk(inp["b_ada"][l], 48),
        g1=_pk(inp["norm1_g"][l], 8), g2=_pk(inp["norm2_g"][l], 8), gf=_pk(inp["final_norm_g"], 8),
        w_g=np.ascontiguousarray(w_in[:, OFF["gm"]:OFF["end"]]),
        b_g=_pk(b_in[OFF["gm"]:OFF["end"]], 16),
        w_bm=np.ascontiguousarray(inp["w_branch_m"][l]),
        w_ba=np.ascontiguousarray(inp["w_branch_a"][l]),
        w_o=np.ascontiguousarray(inp["w_out"][l]),
        w_r=np.ascontiguousarray(np.concatenate([inp["w_router_group"][l], inp["w_router_expert"][l]], axis=1)),
        b_r=np.ascontiguousarray(np.broadcast_to(
            np.concatenate([inp["b_router_group"][l], inp["b_router_expert"][l]])[None, :], (128, 36))),
        w_gate=np.ascontiguousarray(inp["w_gate"][l]),
        w_up=np.ascontiguousarray(inp["w_up"][l]),
        w_down=np.ascontiguousarray(inp["w_down"][l]),
    )
    m.update(_consts())
    return m


def _rope_consts():
    rows = S_LEN // 64
    row = np.repeat(np.arange(rows, dtype=np.float32), 64)
    col = np.tile(np.arange(64, dtype=np.float32), rows)
    half = 32
    inv_freq = (np.float32(10000.0) ** (-np.arange(0, half, 2, dtype=np.float32) / np.float32(half))).astype(np.float32)
    ang_r = (row[:, None] * inv_freq).astype(np.float32)
    ang_c = (col[:, None] * inv_freq).astype(np.float32)
    cosT = np.zeros((64, S_LEN), np.float32)
    sinT = np.zeros((64, S_LEN), np.float32)
    for i in range(64):
        ang = ang_r if i < 32 else ang_c
        cosT[i] = np.cos(ang[:, i % 16])
        sinT[i] = np.sin(ang[:, i % 16])
    R = np.zeros((64, 64), np.float32)
    for i in range(64):
        if i % 32 < 16:
            R[i, i + 16] = -1.0
        else:
            R[i, i - 16] = 1.0
    R2 = np.zeros((128, 128), np.float32)
    R2[:64, :64] = R
    R2[64:, 64:] = R
    oblk = np.zeros((128, 128), np.float32)
    oblk[:64, :64] = 1.0
    oblk[64:, 64:] = 1.0
    masks = np.concatenate([np.triu(np.ones((128, 128), np.float32)), np.tril(np.ones((128, 128), np.float32))], axis=1)
    return dict(cosT=np.ascontiguousarray(np.tile(cosT, (2, 1))), sinT=np.ascontiguousarray(np.tile(sinT, (2, 1))),
                rT=np.ascontiguousarray(R2.T), oblk=oblk, masks=np.ascontiguousarray(masks),
                ident=np.eye(128, dtype=np.float32))


_ROPE = None


def prep_A(inp, l, b, r, xT_b):
    global _ROPE
    if _ROPE is None:
        _ROPE = _rope_consts()
    w_in = inp["w_in"][l]
    b_in = inp["b_in"][l]
    kv = r // 2
    fcols = np.concatenate([np.arange(OFF["mq"] + r * 128, OFF["mq"] + (r + 1) * 128),
                            np.arange(OFF["mk"] + r * 128, OFF["mk"] + (r + 1) * 128),
                            np.arange(OFF["aq"] + r * 128, OFF["aq"] + (r + 1) * 128),
                            np.arange(OFF["ak"] + kv * 64, OFF["ak"] + (kv + 1) * 64),
                            np.arange(OFF["ak"] + kv * 64, OFF["ak"] + (kv + 1) * 64)])
    tcols = np.concatenate([np.arange(OFF["mv"] + r * 128, OFF["mv"] + (r + 1) * 128),
                            np.arange(OFF["mo"] + r * 128, OFF["mo"] + (r + 1) * 128),
                            OFF["gates"] + np.arange(4) * 4 + r,
                            np.arange(OFF["av"] + kv * 64, OFF["av"] + (kv + 1) * 64)])
    cwl = inp["conv_w"][l][:, 0, :]
    cw = np.zeros((128, 10), np.float32)
    cb = np.zeros((128, 2), np.float32)
    for qk in range(2):
        ch = slice(qk * 512 + r * 128, qk * 512 + (r + 1) * 128)
        cw[:, qk * 5:(qk + 1) * 5] = cwl[:, ch].T
        cb[:, qk] = inp["conv_b"][l][ch]
    m = dict(
        xT=xT_b,
        cvec=_pk(inp["c"][b], 8),
        w_ada=np.ascontiguousarray(inp["w_ada"][l][:, 0:2 * D]),
        b_ada=_pk(inp["b_ada"][l][0:2 * D], 16),
        g1=_pk(inp["norm1_g"][l], 8),
        w_F=np.ascontiguousarray(w_in[:, fcols]),
        b_F=_pk(b_in[fcols], 4),
        w_T=np.ascontiguousarray(w_in[:, tcols]),
        b_T=np.ascontiguousarray(np.broadcast_to(b_in[tcols][None, :], (128, NT_T))),
        cw=cw, cb=cb,
        gmr=np.ascontiguousarray(np.broadcast_to(inp["mlstm_norm_g"][l][r * 128:(r + 1) * 128][None, :], (128, 128))),
        gqk=np.ascontiguousarray(np.stack([np.tile(inp["q_norm_g"][l], 2), np.tile(inp["k_norm_g"][l], 2)], axis=1)),
    )
    m.update(_ROPE)
    return m


def kernel(**inputs):
    inp = {k: np.asarray(v) for k, v in inputs.items()}
    cores = list(range(8))
    xT = [np.ascontiguousarray(inp["x"][b].T) for b in range(2)]
    for l in range(2):
        ncA = build_A()
        resA = run_bass_kernel_spmd(ncA, [prep_A(inp, l, c // 4, c % 4, xT[c // 4]) for c in cores], core_ids=cores)
        ymT = [np.concatenate([resA.results[b * 4 + r]["ymT"] for r in range(4)], axis=0) for b in range(2)]
        yaT = [np.concatenate([resA.results[b * 4 + r]["yaT"] for r in range(4)], axis=0) for b in range(2)]
        del resA
        ncB = build_B(last=(l == 1))
        resB = run_bass_kernel_spmd(ncB, [prep_B(inp, l, c // 4, c % 4, xT[c // 4], ymT[c // 4], yaT[c // 4]) for c in cores],
                                    core_ids=cores)
        xT = [np.concatenate([resB.results[b * 4 + r]["outT"] for r in range(4)], axis=1) for b in range(2)]
        del resB
    return np.ascontiguousarray(np.stack([xT[b].T for b in range(2)])).astype(np.float32)
```

```python
import numpy as np
import ml_dtypes
from contextlib import ExitStack
import concourse.bass as bass
import concourse.mybir as mybir
from concourse.bass_utils import run_bass_kernel_spmd

F32 = mybir.dt.float32
BF16 = mybir.dt.bfloat16
AF = mybir.ActivationFunctionType
ALU = mybir.AluOpType
AX = mybir.AxisListType

ENGS = ("tensor", "vector", "scalar", "gpsimd", "sync")
GEN = 30000


class Prog:
    def __init__(self, nc, n_dma_slots=8, same_engine_sync=True):
        self.nc = nc
        self.ops = {e: [] for e in ENGS}
        self.last_writer = {}
        self.readers = {}
        self.n_dma_slots = n_dma_slots
        self.dma_count = {e: 0 for e in ENGS}
        self.same_engine_sync = same_engine_sync
        self.pending_dma = []

    def op(self, eng, fn, reads=(), writes=(), dma=False, nosync_same=False):
        idx = len(self.ops[eng])
        deps = set()
        for k in reads:
            w = self.last_writer.get(k)
            if w is not None:
                deps.add(w)
        for k in writes:
            w = self.last_writer.get(k)
            if w is not None:
                deps.add(w)
            for r in self.readers.get(k, ()):
                deps.add(r)
        me = (eng, idx)
        deps.discard(me)
        slot = None
        if dma:
            slot = self.dma_count[eng] % self.n_dma_slots
            self.dma_count[eng] += 1
            self.pending_dma.append(me)
        elif nosync_same or not self.same_engine_sync:
            deps = {d for d in deps if d[0] != eng or self.ops[d[0]][d[1]]["dma"]}
        rec = dict(eng=eng, fn=fn, deps=deps, dma=dma, slot=slot, signal=False)
        self.ops[eng].append(rec)
        for d in deps:
            self.ops[d[0]][d[1]]["signal"] = True
        for k in reads:
            self.readers.setdefault(k, []).append(me)
        for k in writes:
            self.last_writer[k] = me
            self.readers[k] = []
        return me

    def mm(self, fn, reads=(), writes=()):
        return self.op("tensor", fn, reads, writes, nosync_same=True)

    def dve(self, fn, reads=(), writes=()):
        return self.op("vector", fn, reads, writes)

    def act(self, fn, reads=(), writes=()):
        return self.op("scalar", fn, reads, writes)

    def pool(self, fn, reads=(), writes=()):
        return self.op("gpsimd", fn, reads, writes)

    def dma(self, fn, reads=(), writes=(), q="sync"):
        return self.op(q, fn, reads, writes, dma=True)

    def barrier(self):
        lasts = []
        for e in ENGS:
            for i in range(len(self.ops[e]) - 1, -1, -1):
                r = self.ops[e][i]
                if r["fn"] is not None and not r["dma"]:
                    lasts.append((e, i))
                    break
        deps = set(lasts) | set(self.pending_dma)
        self.pending_dma = []
        for d in deps:
            self.ops[d[0]][d[1]]["signal"] = True
        for e in ENGS:
            self.ops[e].append(dict(eng=e, fn=None, deps={d for d in deps}, dma=False, slot=None, signal=False))

    def emit(self):
        nc = self.nc
        ngen = {}
        final_slot_counts = {}
        for e in ENGS:
            c = 0
            slot_counts = [0] * self.n_dma_slots
            for r in self.ops[e]:
                if r["dma"]:
                    slot_counts[r["slot"]] += 1
                    r["slot_prev"] = slot_counts[r["slot"]] - 1
                    r["sig"] = ("dma", e, r["slot"], 16 * slot_counts[r["slot"]])
                elif r["signal"]:
                    g, v = divmod(c, GEN)
                    r["sig"] = ("eng", e, g, v + 1)
                    c += 1
                else:
                    r["sig"] = None
            ngen[e] = (c + GEN - 1) // GEN if c else 0
            final_slot_counts[e] = slot_counts
        with ExitStack() as st:
            sems = {}
            for e in ENGS:
                for g in range(ngen[e]):
                    sems[("eng", e, g)] = st.enter_context(nc.semaphore(f"s_{e}_{g}"))
                if self.dma_count[e]:
                    for s in range(self.n_dma_slots):
                        sems[("dma", e, s)] = st.enter_context(nc.semaphore(f"d_{e}_{s}"))
            block = st.enter_context(nc.Block())

            def make_body(e):
                def body(eng):
                    waited = {}
                    for r in self.ops[e]:
                        need = {}
                        for d in r["deps"]:
                            if d[0] == e and r["fn"] is None and not self.ops[d[0]][d[1]]["dma"]:
                                continue
                            sig = self.ops[d[0]][d[1]]["sig"]
                            key = sig[:3]
                            need[key] = max(need.get(key, 0), sig[3])
                        if r["dma"] and r["slot_prev"] > 0:
                            key = ("dma", e, r["slot"])
                            need[key] = max(need.get(key, 0), 16 * r["slot_prev"])
                        for key, v in need.items():
                            if key[0] == "eng":
                                best = waited.get((key[0], key[1]), (-1, 0))
                                if (key[2], v) <= best:
                                    continue
                                waited[(key[0], key[1])] = (key[2], v)
                            else:
                                if waited.get(key, 0) >= v:
                                    continue
                                waited[key] = v
                            eng.wait_ge(sems[key], v)
                        if r["fn"] is None:
                            continue
                        ins = r["fn"](eng)
                        if r["sig"] is not None:
                            ins.then_inc(sems[r["sig"][:3]], 16 if r["dma"] else 1)
                    if e == "sync":
                        for q in ENGS:
                            if self.dma_count[q]:
                                for s in range(self.n_dma_slots):
                                    cnt = final_slot_counts[q][s]
                                    if cnt:
                                        eng.wait_ge(sems[("dma", q, s)], 16 * cnt)
                return body

            for e in ENGS:
                getattr(block, e)(make_body(e))


def mm_group(P, out_ap, pairs, reads, writes):
    n = len(pairs)

    def fn(e):
        ins = None
        for i, (l, r) in enumerate(pairs):
            ins = e.matmul(out_ap, lhsT=l, rhs=r, start=(i == 0), stop=(i == n - 1))
        return ins
    return P.mm(fn, reads, writes)


def dma(P, out_ap, in_ap, reads=(), writes=(), q="sync"):
    return P.dma(lambda e: e.dma_start(out=out_ap, in_=in_ap), reads, writes, q=q)


def act(P, out_ap, in_ap, func, reads, writes, bias=None, scale=None):
    kw = {}
    if bias is not None:
        kw["bias"] = bias
    if scale is not None:
        kw["scale"] = scale
    return P.act(lambda e: e.activation(out=out_ap, in_=in_ap, func=func, **kw), reads, writes)


def tt(P, out_ap, a, b, op, reads, writes, eng="vector"):
    return P.op(eng, lambda e: e.tensor_tensor(out=out_ap, in0=a, in1=b, op=op), reads, writes)


def ts(P, out_ap, a, s1, op0, reads, writes, s2=None, op1=None, eng="vector"):
    if op1 is None:
        return P.op(eng, lambda e: e.tensor_scalar(out=out_ap, in0=a, scalar1=s1, scalar2=None, op0=op0), reads, writes)
    return P.op(eng, lambda e: e.tensor_scalar(out=out_ap, in0=a, scalar1=s1, scalar2=s2, op0=op0, op1=op1), reads, writes)


def stt(P, out_ap, a, s, b, op0, op1, reads, writes):
    return P.dve(lambda e: e.scalar_tensor_tensor(out=out_ap, in0=a, scalar=s, in1=b, op0=op0, op1=op1), reads, writes)


def cp(P, out_ap, in_ap, reads, writes, eng="vector"):
    if eng == "scalar":
        return P.op(eng, lambda e: e.activation(out=out_ap, in_=in_ap, func=AF.Identity), reads, writes)
    return P.op(eng, lambda e: e.tensor_copy(out=out_ap, in_=in_ap), reads, writes)


def red(P, out_ap, in_ap, op, reads, writes):
    return P.dve(lambda e: e.tensor_reduce(out=out_ap, in_=in_ap, axis=AX.X, op=op), reads, writes)


D = 1024
NK = 8
TB = 512
EPS = 1e-6


def emit_mod(P, nc, st, pb, cvec, w_ada, b_ada, col_chunks, name="mod"):
    T = lambda n, s, d: st.enter_context(nc.sbuf_tensor(n, s, d))
    ncol = len(col_chunks)
    cs = T(name + "_cs", [128, NK], F32)
    css = T(name + "_css", [128, NK], F32)
    nch = max(col_chunks) + 1
    bsb = T(name + "_b", [128, nch], F32)
    mod = T(name, [128, nch], F32)
    dma(P, cs[:], cvec[:, :], writes=[name + "cs"])
    dma(P, bsb[:], b_ada[:, :], writes=[name + "b"])
    act(P, css[:], cs[:], AF.Silu, [name + "cs"], [name + "css"])
    wv = w_ada.rearrange("(k p) c -> p k c", p=128)
    with ExitStack() as st2:
        wa = [st2.enter_context(nc.sbuf_tensor(f"{name}_wa{i}", [128, NK, 768], F32)) for i in range(2)]
        pieces = []
        cur = []
        for j in col_chunks:
            if cur and (j != cur[-1] + 1 or len(cur) == 6):
                pieces.append(cur)
                cur = []
            cur.append(j)
        if cur:
            pieces.append(cur)
        for pi, piece in enumerate(pieces):
            buf = wa[pi % 2]
            key = (name + "wa", pi % 2)
            c0 = piece[0] * 128
            n = len(piece) * 128
            for kh in range(2):
                dma(P, buf[:, kh * 4:(kh + 1) * 4, 0:n], wv[:, kh * 4:(kh + 1) * 4, c0:c0 + n], writes=[key],
                    q=("sync" if kh == 0 else "gpsimd"))
            for jj, j in enumerate(piece):
                pairs = [(buf[:, k, jj * 128:(jj + 1) * 128], css[:, k:k + 1]) for k in range(NK)]
                mm_group(P, pb[0][:, j:j + 1], pairs, [key, name + "css"], ["pb0"])
        for j in col_chunks:
            tt(P, mod[:, j:j + 1], pb[0][:, j:j + 1], bsb[:, j:j + 1], ALU.add, ["pb0", name + "b"], [name])
        P.barrier()
    return mod


def emit_norm(P, nc, W, xk, a_t, shift_t, outs, xkeys, okeys, pbank, pkey, n=TB, tag="n", xfull=None, part="ab"):
    sq, rt, rstd, tmp = W["sq"], W["rt"], W["rstd"], W["tmp"]
    sqk = [(tag + "sq", k) for k in range(NK)]
    if "a" in part:
        if xfull is not None:
            act(P, sq[:, :, 0:n], xfull, AF.Square, xkeys, sqk)
        else:
            for k in range(NK):
                act(P, sq[:, k, 0:n], xk(k), AF.Square, xkeys, [sqk[k]])
    if "b" not in part:
        return
    pairs = [(W["ones_bf"][:], sq[:, k, 0:n]) for k in range(NK)]
    mm_group(P, pbank[:, 0:n], pairs, sqk + ["ones"], [pkey])
    act(P, rt[:, 0:n], pbank[:, 0:n], AF.Sqrt, [pkey, "eps"], [tag + "rt"], bias=W["eps"][:, 0:1], scale=1.0 / D)
    P.dve(lambda e: e.reciprocal(out=rstd[:, 0:n], in_=rt[:, 0:n]), [tag + "rt"], [tag + "rstd"])
    for k in range(NK):
        tb_ = tmp[k % 2]
        tk_ = (tag + "ntmp", k % 2)
        tt(P, tb_[:, 0:n], xk(k), rstd[:, 0:n], ALU.mult, xkeys + [tag + "rstd"], [tk_])
        for oi, ofn in enumerate(outs):
            if shift_t is not None:
                act(P, ofn(k), tb_[:, 0:n], AF.Identity, [tk_], [okeys[oi](k)],
                    bias=shift_t[:, k:k + 1], scale=a_t[:, k:k + 1])
            else:
                act(P, ofn(k), tb_[:, 0:n], AF.Identity, [tk_], [okeys[oi](k)],
                    scale=a_t[:, k:k + 1])


NT_B = 2048
NE = 32
FH = 512


def build_B(last, n_experts=NE):
    nc = bass.Bass("TRN2", target_bir_lowering=False)

    def din(name, shape, dt=F32):
        return nc.dram_tensor(name, shape, dt, kind="ExternalInput").ap()
    xT = din("xT", [D, NT_B])
    ymT = din("ymT", [512, NT_B], BF16)
    yaT = din("yaT", [512, NT_B], BF16)
    cvec = din("cvec", [128, NK])
    w_ada = din("w_ada", [D, 6 * D])
    b_ada = din("b_ada", [128, 48])
    g1 = din("g1", [128, NK])
    g2 = din("g2", [128, NK])
    gf = din("gf", [128, NK])
    w_g = din("w_g", [D, 2 * D])
    b_g = din("b_g", [128, 16])
    w_bm = din("w_bm", [512, D])
    w_ba = din("w_ba", [512, D])
    w_o = din("w_o", [D, D])
    w_r = din("w_r", [D, 36])
    b_r = din("b_r", [128, 36])
    w_gate = din("w_gate", [NE, D, FH])
    w_up = din("w_up", [NE, D, FH])
    w_down = din("w_down", [NE, FH, D])
    esel = din("esel", [32, 32 * 128])
    ident = din("ident", [128, 128])
    outT = nc.dram_tensor("outT", [D, NT_B], F32, kind="ExternalOutput").ap()
    NTB = NT_B // TB

    with ExitStack() as st:
        T = lambda n, s, d: st.enter_context(nc.sbuf_tensor(n, s, d))
        P = Prog(nc, same_engine_sync=SES_B)
        pb = [st.enter_context(nc.psum_tensor(f"pb{i}", [128, 512], F32)) for i in range(8)]
        pk = [f"pb{i}" for i in range(8)]
        x1T = T("x1T", [128, NK, NT_B], F32)
        W = dict(ones_bf=T("ones_bf", [128, 128], BF16), eps=T("eps", [128, 1], F32),
                 sq=T("sq", [128, NK, TB], BF16), rt=T("rt", [128, TB], F32), rstd=T("rstd", [128, TB], F32),
                 tmp=[T("ntmp0", [128, TB], F32), T("ntmp1", [128, TB], F32)])
        identf = T("identf", [128, 128], F32)
        g1s, g2s, gfs = T("g1s", [128, NK], F32), T("g2s", [128, NK], F32), T("gfs", [128, NK], F32)
        a1, a2 = T("a1", [128, NK], F32), T("a2", [128, NK], F32)
        bgs = T("bgs", [128, 16], F32)
        brs = T("brs", [128, 36], F32)
        wr = T("wr", [128, NK, 36], F32)
        P.pool(lambda e: e.memset(W["ones_bf"][:], 1.0), [], ["ones"])
        P.pool(lambda e: e.memset(W["eps"][:], EPS), [], ["eps"])
        dma(P, identf[:], ident[:, :], writes=["ident"])
        dma(P, g1s[:], g1[:, :], writes=["g1"])
        dma(P, g2s[:], g2[:, :], writes=["g2"])
        dma(P, gfs[:], gf[:, :], writes=["gf"])
        dma(P, bgs[:], b_g[:, :], writes=["bg"])
        dma(P, brs[:], b_r[:, :], writes=["br"])
        dma(P, wr[:], w_r.rearrange("(k p) c -> p k c", p=128), writes=["wr"])
        xv = xT.rearrange("(k p) t -> p k t", p=128)
        ymv = ymT.rearrange("(k p) t -> p k t", p=128)
        yav = yaT.rearrange("(k p) t -> p k t", p=128)
        for tb in range(NTB):
            for kh in range(2):
                dma(P, x1T[:, kh * 4:(kh + 1) * 4, tb * TB:(tb + 1) * TB], xv[:, kh * 4:(kh + 1) * 4, tb * TB:(tb + 1) * TB],
                    writes=[("x1T", k, tb) for k in range(kh * 4, kh * 4 + 4)])

        mod = emit_mod(P, nc, st, pb, cvec, w_ada, b_ada, list(range(48)))
        stt(P, a1[:], mod[:, 8:16], 1.0, g1s[:], ALU.add, ALU.mult, ["mod", "g1"], ["a1"])
        stt(P, a2[:], mod[:, 32:40], 1.0, g2s[:], ALU.add, ALU.mult, ["mod", "g2"], ["a2"])
        shift1, gate1, shift2, gate2 = mod[:, 0:8], mod[:, 16:24], mod[:, 24:32], mod[:, 40:48]

        with ExitStack() as s1:
            T1 = lambda n, s, d: s1.enter_context(nc.sbuf_tensor(n, s, d))
            wg_ = T1("wg_", [128, NK, 2 * D], BF16)
            wbm = T1("wbm", [128, 4, D], BF16)
            wba = T1("wba", [128, 4, D], BF16)
            wo = T1("wo", [128, NK, D], BF16)
            wgv = w_g.rearrange("(k p) c -> p k c", p=128)
            for k in range(NK):
                dma(P, wg_[:, k, :], wgv[:, k, :], writes=[("wg_", k)], q="gpsimd")
            dma(P, wbm[:], w_bm.rearrange("(k p) c -> p k c", p=128), writes=["wbm"], q="gpsimd")
            dma(P, wba[:], w_ba.rearrange("(k p) c -> p k c", p=128), writes=["wba"], q="gpsimd")
            wov = w_o.rearrange("(k p) c -> p k c", p=128)
            for k in range(0, NK, 2):
                dma(P, wo[:, k:k + 2, :], wov[:, k:k + 2, :], writes=[("wo", k), ("wo", k + 1)], q="gpsimd")
            h1 = T1("h1", [128, NK, TB], BF16)
            ymb = [T1(f"ymb{i}", [128, 4, TB], BF16) for i in range(2)]
            yab = [T1(f"yab{i}", [128, 4, TB], BF16) for i in range(2)]
            merged = T1("merged", [128, NK, TB], BF16)
            sg = [[T1(f"sg{i}{j}", [128, TB], F32) for j in range(2)] for i in range(2)]
            t12 = [[T1(f"t12{i}{j}", [128, TB], F32) for j in range(2)] for i in range(2)]
            for tb in range(NTB):
                tsl = slice(tb * TB, (tb + 1) * TB)
                b = tb % 2
                dma(P, ymb[b][:], ymv[:, :, tsl], writes=[("ymb", b)])
                dma(P, yab[b][:], yav[:, :, tsl], writes=[("yab", b)])
                emit_norm(P, nc, W, lambda k: x1T[:, k, tsl], a1, shift1,
                          [lambda k: h1[:, k, :]], [("x1T", k_, tb) for k_ in range(NK)] + ["a1", "mod"], [lambda k: ("h1", k)],
                          pb[0], "pb0")
                for dc in range(NK):
                    par = dc % 2
                    base = 4 * par
                    csl = slice(dc * 128, (dc + 1) * 128)
                    hk = [("h1", k) for k in range(NK)]
                    mm_group(P, pb[base][:], [(wg_[:, k, csl], h1[:, k, :]) for k in range(NK)],
                             hk + [("wg_", k) for k in range(NK)], [pk[base]])
                    mm_group(P, pb[base + 1][:], [(wg_[:, k, D + dc * 128:D + (dc + 1) * 128], h1[:, k, :]) for k in range(NK)],
                             hk + [("wg_", k) for k in range(NK)], [pk[base + 1]])
                    mm_group(P, pb[base + 2][:], [(wbm[:, k, csl], ymb[b][:, k, :]) for k in range(4)],
                             ["wbm", ("ymb", b)], [pk[base + 2]])
                    mm_group(P, pb[base + 3][:], [(wba[:, k, csl], yab[b][:, k, :]) for k in range(4)],
                             ["wba", ("yab", b)], [pk[base + 3]])
                    act(P, sg[par][0][:], pb[base][:], AF.Sigmoid, [pk[base], "bg"], [("sg", par, 0)], bias=bgs[:, dc:dc + 1])
                    act(P, sg[par][1][:], pb[base + 1][:], AF.Sigmoid, [pk[base + 1], "bg"], [("sg", par, 1)], bias=bgs[:, 8 + dc:9 + dc])
                    tt(P, t12[par][0][:], sg[par][0][:], pb[base + 2][:], ALU.mult, [("sg", par, 0), pk[base + 2]], [("t12", par, 0)])
                    tt(P, t12[par][1][:], sg[par][1][:], pb[base + 3][:], ALU.mult, [("sg", par, 1), pk[base + 3]], [("t12", par, 1)])
                    tt(P, merged[:, dc, :], t12[par][0][:], t12[par][1][:], ALU.add, [("t12", par, 0), ("t12", par, 1)],
                       [("merged", dc)])
                for dc in range(NK):
                    bank = dc % 2
                    csl = slice(dc * 128, (dc + 1) * 128)
                    mm_group(P, pb[bank][:], [(wo[:, k, csl], merged[:, k, :]) for k in range(NK)],
                             [("merged", k) for k in range(NK)] + [("wo", k) for k in range(NK)], [pk[bank]])
                    stt(P, x1T[:, dc, tsl], pb[bank][:], gate1[:, dc:dc + 1], x1T[:, dc, tsl], ALU.mult, ALU.add,
                        [pk[bank], "mod", ("x1T", dc, tb)], [("x1T", dc, tb)])
            P.barrier()

        s23 = st.enter_context(ExitStack())
        h2T = s23.enter_context(nc.sbuf_tensor("h2T", [128, NK, NT_B], BF16))
        combT = s23.enter_context(nc.sbuf_tensor("combT", [32, NT_B], F32))
        with ExitStack() as s2:
            T2 = lambda n, s, d: s2.enter_context(nc.sbuf_tensor(n, s, d))
            h2f = T2("h2f", [128, NK, TB], F32)
            R = {n: T2("r_" + n, [128, s], F32) for n, s in
                 [("lg", 36), ("gmax", 1), ("ngmax", 1), ("eg", 4), ("ssum", 1), ("ptop", 1), ("mg", 4), ("pen", 4),
                  ("lem", 32), ("e1", 1), ("m1", 32), ("lem2", 32), ("e2", 1), ("m2", 32), ("d", 1), ("s2", 1),
                  ("w2", 1), ("w1", 1), ("comb", 32), ("comb2", 32)]}
            for tb in range(NTB):
                tsl = slice(tb * TB, (tb + 1) * TB)
                emit_norm(P, nc, W, lambda k: x1T[:, k, tsl], a2, shift2,
                          [lambda k: h2T[:, k, tsl], lambda k: h2f[:, k, :]],
                          [("x1T", k_, tb) for k_ in range(NK)] + ["a2", "mod"],
                          [lambda k: ("h2T", k, tb), lambda k: ("h2f", k)], pb[0], "pb0")
                for sub in range(4):
                    ssl = slice(sub * 128, (sub + 1) * 128)
                    bank = 1 + sub % 2
                    mm_group(P, pb[bank][:, 0:36], [(h2f[:, k, ssl], wr[:, k, :]) for k in range(NK)],
                             [("h2f", k) for k in range(NK)] + ["wr"], [pk[bank]])
                    tt(P, R["lg"][:], pb[bank][:, 0:36], brs[:], ALU.add, [pk[bank], "br"], ["r_lg"])
                    red(P, R["gmax"][:], R["lg"][:, 0:4], ALU.max, ["r_lg"], ["r_gmax"])
                    ts(P, R["ngmax"][:], R["gmax"][:], -1.0, ALU.mult, ["r_gmax"], ["r_ngmax"])
                    act(P, R["eg"][:], R["lg"][:, 0:4], AF.Exp, ["r_lg", "r_ngmax"], ["r_eg"], bias=R["ngmax"][:, 0:1])
                    red(P, R["ssum"][:], R["eg"][:], ALU.add, ["r_eg"], ["r_ssum"])
                    P.dve(lambda e: e.reciprocal(out=R["ptop"][:], in_=R["ssum"][:]), ["r_ssum"], ["r_ptop"])
                    ts(P, R["mg"][:], R["lg"][:, 0:4], R["gmax"][:, 0:1], ALU.is_equal, ["r_lg", "r_gmax"], ["r_mg"])
                    ts(P, R["pen"][:], R["mg"][:], -1.0, ALU.add, ["r_mg"], ["r_pen"], s2=1e30, op1=ALU.mult)
                    for g in range(4):
                        ts(P, R["lem"][:, g * 8:(g + 1) * 8], R["lg"][:, 4 + g * 8:12 + g * 8], R["pen"][:, g:g + 1], ALU.add,
                           ["r_lg", "r_pen"], [("r_lem", g)])
                    lemk = [("r_lem", g) for g in range(4)]
                    red(P, R["e1"][:], R["lem"][:], ALU.max, lemk, ["r_e1"])
                    ts(P, R["m1"][:], R["lem"][:], R["e1"][:, 0:1], ALU.is_equal, lemk + ["r_e1"], ["r_m1"])
                    stt(P, R["lem2"][:], R["m1"][:], -1e30, R["lem"][:], ALU.mult, ALU.add, lemk + ["r_m1"], ["r_lem2"])
                    red(P, R["e2"][:], R["lem2"][:], ALU.max, ["r_lem2"], ["r_e2"])
                    ts(P, R["m2"][:], R["lem2"][:], R["e2"][:, 0:1], ALU.is_equal, ["r_lem2", "r_e2"], ["r_m2"])
                    tt(P, R["d"][:], R["e2"][:], R["e1"][:], ALU.subtract, ["r_e1", "r_e2"], ["r_d"])
                    act(P, R["s2"][:], R["d"][:], AF.Sigmoid, ["r_d"], ["r_s2"])
                    tt(P, R["w2"][:], R["ptop"][:], R["s2"][:], ALU.mult, ["r_ptop", "r_s2"], ["r_w2"])
                    tt(P, R["w1"][:], R["ptop"][:], R["w2"][:], ALU.subtract, ["r_ptop", "r_w2"], ["r_w1"])
                    ts(P, R["comb"][:], R["m1"][:], R["w1"][:, 0:1], ALU.mult, ["r_m1", "r_w1"], ["r_comb"])
                    stt(P, R["comb2"][:], R["m2"][:], R["w2"][:, 0:1], R["comb"][:], ALU.mult, ALU.add,
                        ["r_m2", "r_w2", "r_comb"], ["r_comb2"])
                    P.mm(lambda e: e.transpose(pb[3][0:32, 0:128], R["comb2"][:], identf[:]), ["r_comb2", "ident"], [pk[3]])
                    tok = slice(tb * TB + sub * 128, tb * TB + (sub + 1) * 128)
                    act(P, combT[:, tok], pb[3][0:32, 0:128], AF.Identity, [pk[3]], [("combT", tb, sub)])
            P.barrier()

        with ExitStack() as s3:
            T3 = lambda n, s, d: s3.enter_context(nc.sbuf_tensor(n, s, d))
            eselT = T3("eselT", [32, 32 * 128], F32)
            dma(P, eselT[:], esel[:, :], writes=["esel"])
            wgt = [T3(f"wgt{i}", [128, NK, FH], BF16) for i in range(2)]
            wut = [T3(f"wut{i}", [128, NK, FH], BF16) for i in range(2)]
            wdt = [T3(f"wdt{i}", [128, 4, D], BF16) for i in range(2)]
            actT = [T3(f"actT{i}", [128, 4, TB], BF16) for i in range(2)]
            sl = [T3(f"sl{i}", [128, TB], F32) for i in range(2)]
            pr = [T3(f"pr{i}", [128, TB], F32) for i in range(2)]
            it = 0
            for e_ in range(n_experts):
                wb = e_ % 2
                gv = w_gate[e_].rearrange("(k p) f -> p k f", p=128)
                uv = w_up[e_].rearrange("(k p) f -> p k f", p=128)
                dv = w_down[e_].rearrange("(k p) c -> p k c", p=128)
                for kh in range(2):
                    ksl = slice(kh * 4, kh * 4 + 4)
                    dma(P, wgt[wb][:, ksl, :], gv[:, ksl, :], writes=[("wgt", wb, kh)], q="gpsimd")
                    dma(P, wut[wb][:, ksl, :], uv[:, ksl, :], writes=[("wut", wb, kh)], q="gpsimd")
                for kh in range(2):
                    ksl = slice(kh * 2, kh * 2 + 2)
                    dma(P, wdt[wb][:, ksl, :], dv[:, ksl, :], writes=[("wdt", wb, kh)], q="gpsimd")
                for tb in range(NTB):
                    tsl = slice(tb * TB, (tb + 1) * TB)
                    ab = it % 2
                    it += 1
                    h2k = [("h2T", k, tb) for k in range(NK)]
                    mm_group(P, pb[6][:], [(eselT[:, e_ * 128:(e_ + 1) * 128], combT[:, tsl])],
                             ["esel"] + [("combT", tb, s_) for s_ in range(4)], [pk[6]])
                    for fc in range(4):
                        fsl = slice(fc * 128, (fc + 1) * 128)
                        pa, pu = pb[2 * (fc % 2)], pb[2 * (fc % 2) + 1]
                        ka, ku = pk[2 * (fc % 2)], pk[2 * (fc % 2) + 1]
                        mm_group(P, pa[:], [(wgt[wb][:, k, fsl], h2T[:, k, tsl]) for k in range(NK)],
                                 h2k + [("wgt", wb, 0), ("wgt", wb, 1)], [ka])
                        mm_group(P, pu[:], [(wut[wb][:, k, fsl], h2T[:, k, tsl]) for k in range(NK)],
                                 h2k + [("wut", wb, 0), ("wut", wb, 1)], [ku])
                        act(P, sl[fc % 2][:], pa[:], AF.Silu, [ka], [("sl", fc % 2)])
                        tt(P, pr[fc % 2][:], sl[fc % 2][:], pu[:], ALU.mult, [("sl", fc % 2), ku], [("pr", fc % 2)])
                        tt(P, actT[ab][:, fc, :], pr[fc % 2][:], pb[6][:], ALU.mult, [("pr", fc % 2), pk[6]], [("actT", ab, fc)])
                    for dc in range(NK):
                        csl = slice(dc * 128, (dc + 1) * 128)
                        po, ko = pb[4 + dc % 2], pk[4 + dc % 2]
                        mm_group(P, po[:], [(wdt[wb][:, fc, csl], actT[ab][:, fc, :]) for fc in range(4)],
                                 [("actT", ab, fc) for fc in range(4)] + [("wdt", wb, 0), ("wdt", wb, 1)], [ko])
                        stt(P, x1T[:, dc, tsl], po[:], gate2[:, dc:dc + 1], x1T[:, dc, tsl], ALU.mult, ALU.add,
                            [ko, "mod", ("x1T", dc, tb)], [("x1T", dc, tb)])
            P.barrier()

        s23.close()
        ov = outT.rearrange("(k p) t -> p k t", p=128)
        if last:
            with ExitStack() as s4:
                T4 = lambda n, s, d: s4.enter_context(nc.sbuf_tensor(n, s, d))
                ob = [T4(f"ob{i}", [128, NK, TB], F32) for i in range(2)]
                for tb in range(NTB):
                    tsl = slice(tb * TB, (tb + 1) * TB)
                    o = ob[tb % 2]
                    emit_norm(P, nc, W, lambda k: x1T[:, k, tsl], gfs, None,
                              [lambda k: o[:, k, :]], [("x1T", k_, tb) for k_ in range(NK)] + ["gf"], [lambda k: ("ob", tb % 2, k)],
                              pb[0], "pb0")
                    dma(P, ov[:, :, tsl], o[:], reads=[("ob", tb % 2, k) for k in range(NK)])
                P.barrier()
        else:
            for tb in range(NTB):
                tsl = slice(tb * TB, (tb + 1) * TB)
                dma(P, ov[:, :, tsl], x1T[:, :, tsl], reads=[("x1T", k, tb) for k in range(NK)])
        P.emit()
    return nc


S_LEN = 8192
TA = 256
NCH = S_LEN // 128
MSCALE = 128.0 ** -0.5
ASCALE = 64.0 ** -0.5
NT_T = 324


SES_A = True
SES_B = True


def build_A(att_qblocks=16, do_mlstm=True):
    nc = bass.Bass("TRN2", target_bir_lowering=False)

    def din(name, shape, dt=F32):
        return nc.dram_tensor(name, shape, dt, kind="ExternalInput").ap()
    xT = din("xT", [D, S_LEN])
    cvec = din("cvec", [128, NK])
    w_ada = din("w_ada", [D, 2 * D])
    b_ada = din("b_ada", [128, 16])
    g1 = din("g1", [128, NK])
    w_F = din("w_F", [D, 512])
    b_F = din("b_F", [128, 4])
    w_T = din("w_T", [D, NT_T])
    b_T = din("b_T", [128, NT_T])
    cw = din("cw", [128, 10])
    cb = din("cb", [128, 2])
    gmr = din("gmr", [128, 128])
    gqk = din("gqk", [128, 2])
    cosT = din("cosT", [128, S_LEN])
    sinT = din("sinT", [128, S_LEN])
    ident = din("ident", [128, 128])
    masks = din("masks", [128, 256])
    rT = din("rT", [128, 128])
    oblk = din("oblk", [128, 128])
    ymT = nc.dram_tensor("ymT", [128, S_LEN], BF16, kind="ExternalOutput").ap()
    yaT = nc.dram_tensor("yaT", [128, S_LEN], BF16, kind="ExternalOutput").ap()
    NB = S_LEN // TA

    with ExitStack() as st:
        T = lambda n, s, d: st.enter_context(nc.sbuf_tensor(n, s, d))
        P = Prog(nc, same_engine_sync=SES_A)
        pb = [st.enter_context(nc.psum_tensor(f"pb{i}", [128, 512], F32)) for i in range(8)]
        pk = [f"pb{i}" for i in range(8)]
        QmT = T("QmT", [128, S_LEN], BF16)
        KmT = T("KmT", [128, S_LEN], BF16)
        Vaug = T("Vaug", [128, NCH, 129], BF16)
        osig = T("osig", [128, NCH, 128], BF16)
        G = T("G", [128, NCH, 4], F32)
        QaT = T("QaT", [128, S_LEN], BF16)
        KTa = T("KTa", [128, S_LEN], BF16)
        KTb = T("KTb", [128, S_LEN], BF16)
        Va = T("Va", [128, NCH, 65], BF16)
        W = dict(ones_bf=T("ones_bf", [128, 128], BF16), eps=T("eps", [128, 1], F32))
        identb = T("identb", [128, 128], BF16)
        mk = T("mk", [128, 256], F32)
        onesf = T("onesf", [128, 128], F32)
        one1 = T("one1", [128, 1], F32)
        rTs = T("rTs", [128, 128], F32)
        oblkb = T("oblkb", [128, 128], BF16)
        gmrs = T("gmrs", [128, 128], F32)
        gqks = T("gqks", [128, 2], F32)
        bFs = T("bFs", [128, 4], F32)
        bTs = T("bTs", [128, NT_T], F32)
        cws = T("cws", [128, 10], F32)
        cbs = T("cbs", [128, 2], F32)
        g1s = T("g1s", [128, NK], F32)
        a1 = T("a1", [128, NK], F32)
        P.pool(lambda e: e.memset(W["ones_bf"][:], 1.0), [], ["ones"])
        P.pool(lambda e: e.memset(W["eps"][:], EPS), [], ["eps"])
        P.pool(lambda e: e.memset(onesf[:], 1.0), [], ["onesf"])
        P.pool(lambda e: e.memset(one1[:], 1.0), [], ["one1"])
        P.pool(lambda e: e.memset(Vaug[:, :, 128:129], 1.0), [], ["Vaug1"])
        P.pool(lambda e: e.memset(Va[:, :, 64:65], 1.0), [], ["Va1"])
        P.pool(lambda e: e.memset(KTa[64:128, :], 0.0), [], ["KTa0"])
        P.pool(lambda e: e.memset(KTb[0:64, :], 0.0), [], ["KTb0"])
        dma(P, identb[:], ident[:, :], writes=["identb"], q="gpsimd")
        dma(P, oblkb[:], oblk[:, :], writes=["oblkb"], q="gpsimd")
        for t_, d_, k_ in [(mk, masks, "mk"), (rTs, rT, "rT"),
                           (gmrs, gmr, "gmr"), (gqks, gqk, "gqk"), (bFs, b_F, "bF"), (bTs, b_T, "bT"),
                           (cws, cw, "cw"), (cbs, cb, "cb"), (g1s, g1, "g1")]:
            dma(P, t_[:], d_[:, :], writes=[k_])

        mod = emit_mod(P, nc, st, pb, cvec, w_ada, b_ada, list(range(16)))
        stt(P, a1[:], mod[:, 8:16], 1.0, g1s[:], ALU.add, ALU.mult, ["mod", "g1"], ["a1"])
        shift1 = mod[:, 0:8]

        with ExitStack() as s1:
            T1 = lambda n, s, d: s1.enter_context(nc.sbuf_tensor(n, s, d))
            wF = T1("wF", [128, NK, 512], BF16)
            wT = T1("wT", [128, NK, NT_T], BF16)
            dma(P, wF[:], w_F.rearrange("(k p) c -> p k c", p=128), writes=["wF"], q="gpsimd")
            dma(P, wT[:], w_T.rearrange("(k p) c -> p k c", p=128), writes=["wT"], q="gpsimd")
            xb = [T1(f"xb{i}", [128, NK, TA], F32) for i in range(2)]
            hTs = [T1(f"hT{i}", [128, NK, TA], BF16) for i in range(2)]
            W.update(sq=T1("sq", [128, NK, TA], BF16), rt=T1("rt", [128, TA], F32), rstd=T1("rstd", [128, TA], F32),
                     tmp=[T1("ntmp0", [128, TA], F32), T1("ntmp1", [128, TA], F32)])
            W2 = dict(W)
            W2.update(sq=T1("sq2", [128, NK, TA], BF16), rt=T1("rt2", [128, TA], F32), rstd=T1("rstd2", [128, TA], F32),
                      tmp=[T1("ntmp20", [128, TA], F32), T1("ntmp21", [128, TA], F32)])
            Wp = [W, W2]
            NR = 3
            ring = [T1(f"ring{i}", [128, NR, TA], F32) for i in range(2)]
            acc = [T1(f"acc{i}", [128, TA], F32) for i in range(2)]
            cs_ = [T1(f"cosb{i}", [128, TA], F32) for i in range(2)]
            sn_ = [T1(f"sinb{i}", [128, TA], F32) for i in range(2)]
            RTMP = [{n_: T1(f"{n_}{i}", [128, TA], BF16 if n_ == "qsq" else F32)
                     for n_ in ("qf", "qsq", "qrt", "qrs", "qu", "qt1", "qt2")} for i in range(2)]
            tmpT = [T1(f"tmpT{i}", [128, NT_T], F32) for i in range(2)]
            xv = xT.rearrange("(k p) t -> p k t", p=128)

            def load_x(tb):
                tsl = slice(tb * TA, (tb + 1) * TA)
                for kh in range(2):
                    dma(P, xb[tb % 2][:, kh * 4:(kh + 1) * 4, :], xv[:, kh * 4:(kh + 1) * 4, tsl],
                        writes=[("xb", tb % 2, k) for k in range(kh * 4, kh * 4 + 4)])

            def conv_block(j):
                tsl = slice(j * TA, (j + 1) * TA)
                for qk in range(2):
                    cur = ring[qk][:, j % NR, :]
                    a = acc[qk]
                    ak = ("acc", qk)
                    rk = lambda jj: ("ring", qk, jj % NR)
                    wcol = lambda k: cws[:, qk * 5 + k:qk * 5 + k + 1]
                    ts(P, a[:], cur, wcol(2), ALU.mult, [rk(j), "cw"], [ak])
                    for k in (0, 1, 3, 4):
                        s_ = k - 2
                        if s_ < 0:
                            stt(P, a[:, -s_:TA], ring[qk][:, j % NR, 0:TA + s_], wcol(k), a[:, -s_:TA], ALU.mult, ALU.add,
                                [rk(j), "cw", ak], [ak])
                            if j > 0:
                                stt(P, a[:, 0:-s_], ring[qk][:, (j - 1) % NR, TA + s_:TA], wcol(k), a[:, 0:-s_], ALU.mult, ALU.add,
                                    [rk(j - 1), "cw", ak], [ak])
                        else:
                            stt(P, a[:, 0:TA - s_], ring[qk][:, j % NR, s_:TA], wcol(k), a[:, 0:TA - s_], ALU.mult, ALU.add,
                                [rk(j), "cw", ak], [ak])
                            if j < NB - 1:
                                stt(P, a[:, TA - s_:TA], ring[qk][:, (j + 1) % NR, 0:s_], wcol(k), a[:, TA - s_:TA], ALU.mult, ALU.add,
                                    [rk(j + 1), "cw", ak], [ak])
                    dest = (QmT if qk == 0 else KmT)
                    act(P, dest[:, tsl], a[:], AF.Silu, [ak, "cb"], [("QKm", qk, j)], bias=cbs[:, qk:qk + 1])

            def rope_norm(pf, pkey, bcol, gcol, dest, dkey, tb, rp):
                tsl = slice(tb * TA, (tb + 1) * TA)
                cb_, sb_ = cs_[tb % 2], sn_[tb % 2]
                R_ = RTMP[rp]
                qf, qsq, qrt, qrs, qu, qt1, qt2 = (R_[n_] for n_ in ("qf", "qsq", "qrt", "qrs", "qu", "qt1", "qt2"))
                kq = lambda n_: (n_, rp)
                pss, psr = pb[5 + rp], pk[5 + rp]
                act(P, qf[:], pf[:, 0:TA], AF.Identity, [pkey, "bF"], [kq("qf")], bias=bFs[:, bcol:bcol + 1])
                act(P, qsq[:], qf[:], AF.Square, [kq("qf")], [kq("qsq")])
                mm_group(P, pss[:, 0:TA], [(oblkb[:], qsq[:])], [kq("qsq"), "oblkb"], [psr])
                act(P, qrt[:], pss[:, 0:TA], AF.Sqrt, [psr, "eps"], [kq("qrt")], bias=W["eps"][:, 0:1], scale=1.0 / 64)
                P.dve(lambda e: e.reciprocal(out=qrs[:], in_=qrt[:]), [kq("qrt")], [kq("qrs")])
                ts(P, qu[:], qf[:], gqks[:, gcol:gcol + 1], ALU.mult, [kq("qf"), "gqk"], [kq("qu")])
                mm_group(P, pss[:, 256:256 + TA], [(rTs[:], qu[:])], [kq("qu"), "rT"], [psr])
                tt(P, qt1[:], qu[:], cb_[:], ALU.mult, [kq("qu"), ("cos", tb % 2)], [kq("qt1")])
                tt(P, qt2[:], pss[:, 256:256 + TA], sb_[:], ALU.mult, [psr, ("sin", tb % 2)], [kq("qt2")])
                tt(P, qt1[:], qt1[:], qt2[:], ALU.add, [kq("qt1"), kq("qt2")], [kq("qt1")])
                if dest is None:
                    tt(P, KTa[0:64, tsl], qt1[0:64, :], qrs[0:64, :], ALU.mult, [kq("qt1"), kq("qrs")], [("KTa", tb)])
                    tt(P, KTb[64:128, tsl], qt1[64:128, :], qrs[64:128, :], ALU.mult, [kq("qt1"), kq("qrs")], [("KTb", tb)])
                else:
                    tt(P, dest[:, tsl], qt1[:], qrs[:], ALU.mult, [kq("qt1"), kq("qrs")], [(dkey, tb)])

            def norm_blk(tb, part):
                x_ = xb[tb % 2]
                hp = tb % 2
                hT = hTs[hp]
                nb_ = 0 if hp == 0 else 7
                emit_norm(P, nc, Wp[hp], lambda k: x_[:, k, :], a1, shift1, [lambda k: hT[:, k, :]],
                          [("xb", tb % 2, k_) for k_ in range(NK)] + ["a1", "mod"], [lambda k: ("hT", hp, k)],
                          pb[nb_], pk[nb_], n=TA, tag=f"n{hp}", xfull=x_[:, :, :], part=part)

            load_x(0)
            load_x(1)
            norm_blk(0, "ab")
            for tb in range(NB):
                tsl = slice(tb * TA, (tb + 1) * TA)
                dma(P, cs_[tb % 2][:], cosT[:, tsl], writes=[("cos", tb % 2)])
                dma(P, sn_[tb % 2][:], sinT[:, tsl], writes=[("sin", tb % 2)])
                if tb + 1 < NB:
                    norm_blk(tb + 1, "a")
                hp = tb % 2
                hT = hTs[hp]
                hk = [("hT", hp, k) for k in range(NK)]
                for fc in range(4):
                    bank = 1 + fc % 2
                    mm_group(P, pb[bank][:, 0:TA], [(wF[:, k, fc * 128:(fc + 1) * 128], hT[:, k, :]) for k in range(NK)],
                             hk + ["wF"], [pk[bank]])
                    if fc < 2:
                        act(P, ring[fc][:, tb % NR, :], pb[bank][:, 0:TA], AF.Identity, [pk[bank], "bF"], [("ring", fc, tb % NR)],
                            bias=bFs[:, fc:fc + 1])
                    elif fc == 2:
                        rope_norm(pb[bank], pk[bank], 2, 0, QaT, "QaT", tb, 0)
                    else:
                        rope_norm(pb[bank], pk[bank], 3, 1, None, "KT", tb, 1)
                if tb + 1 < NB:
                    norm_blk(tb + 1, "b")
                for sub in range(TA // 128):
                    ch = tb * (TA // 128) + sub
                    bank = 3 + sub % 2
                    mm_group(P, pb[bank][:, 0:NT_T], [(hT[:, k, sub * 128:(sub + 1) * 128], wT[:, k, :]) for k in range(NK)],
                             hk + ["wT"], [pk[bank]])
                    tm = tmpT[sub % 2]
                    tk = ("tmpT", sub % 2)
                    tt(P, tm[:], pb[bank][:, 0:NT_T], bTs[:], ALU.add, [pk[bank], "bT"], [tk])
                    cp(P, Vaug[:, ch, 0:128], tm[:, 0:128], [tk], [("Vaug", ch)], eng="scalar")
                    act(P, osig[:, ch, :], tm[:, 128:256], AF.Sigmoid, [tk], [("osig", ch)])
                    cp(P, G[:, ch, :], tm[:, 256:260], [tk], [("G", ch)], eng="scalar")
                    cp(P, Va[:, ch, 0:64], tm[:, 260:324], [tk], [("Va", ch)], eng="scalar")
                if tb >= 1:
                    conv_block(tb - 1)
                if tb + 2 < NB:
                    load_x(tb + 2)
            conv_block(NB - 1)
            P.barrier()

        if do_mlstm:
          with ExitStack() as s2:
            T2 = lambda n, s, d: s2.enter_context(nc.sbuf_tensor(n, s, d))
            hfwd = T2("hfwd", [128, NCH, 128], F32)
            Gk = [("G", ch) for ch in range(NCH)]
            ge = T2("ge", [128, NCH, 2], F32)
            lfn = T2("lfn", [128, NCH, 2], F32)
            dirs = []
            for d_ in range(2):
                dd = {n: T2(f"{n}{d_}", [128, NCH], F32) for n in ("b", "imb", "w", "ws", "flo", "ebl")}
                dirs.append(dd)
            Cst = T2("Cst", [128, 129], F32)
            Cbf = T2("Cbf", [128, 129], BF16)
            Ktok = [T2(f"Ktok{i}", [128, 128], BF16) for i in range(2)]
            Vw = [T2(f"Vw{i}", [128, 129], BF16) for i in range(2)]
            Sp = [T2(f"Sp{i}", [128, 128], BF16) for i in range(2)]
            den = [T2(f"den{i}", [128, 1], F32) for i in range(2)]
            rden = [T2(f"rden{i}", [128, 1], F32) for i in range(2)]
            hs = [T2(f"hs{i}", [128, 128], F32) for i in range(2)]
            hsq = [T2(f"hsq{i}", [128, 128], F32) for i in range(2)]
            ss = [T2(f"ss{i}", [128, 1], F32) for i in range(2)]
            srt = [T2(f"srt{i}", [128, 1], F32) for i in range(2)]
            srn = [T2(f"srn{i}", [128, 1], F32) for i in range(2)]
            yt = [T2(f"yt{i}", [128, 128], F32) for i in range(2)]
            y2 = [T2(f"y2{i}", [128, 128], BF16) for i in range(2)]
            ymb = [T2(f"ymb{i}", [128, 512], BF16) for i in range(2)]
            for d_ in range(2):
                fcol = 1 + 2 * d_
                act(P, ge[:, :, d_], G[:, :, fcol], AF.Exp, Gk, [("ge", d_)], scale=-1.0)
                act(P, lfn[:, :, d_], ge[:, :, d_], AF.Ln, [("ge", d_), "one1"], [("lfn", d_)], bias=one1[:, 0:1])
            for d_ in range(2):
                dd = dirs[d_]
                icol = 2 * d_
                mslice = mk[:, d_ * 128:(d_ + 1) * 128]
                mm_group(P, pb[0][:, 0:NCH], [(mslice, lfn[:, :, d_])], [("lfn", d_), "mk"], [pk[0]])
                mm_group(P, pb[1][:, 0:NCH], [(onesf[:], lfn[:, :, d_])], [("lfn", d_), "onesf"], [pk[1]])
                cp(P, dd["b"][:], pb[0][:, 0:NCH], [pk[0]], [("mb", d_)])
                tt(P, dd["imb"][:], G[:, :, icol], dd["b"][:], ALU.add, Gk + [("mb", d_)], [("imb", d_)])
                act(P, dd["w"][:], dd["imb"][:], AF.Exp, [("imb", d_)], [("w", d_)])
                ts(P, dd["ws"][:], dd["w"][:], MSCALE, ALU.mult, [("w", d_)], [("ws", d_)])
                act(P, dd["flo"][:], dd["b"][:], AF.Exp, [("mb", d_)], [("flo", d_)])
                act(P, dd["ebl"][:], pb[1][:, 0:NCH], AF.Exp, [pk[1]], [("ebl", d_)], scale=-1.0)
            hbwd = T2("hbwd", [128, NCH, 128], F32)
            Cst2 = [Cst, T2("Cst_b", [128, 129], F32)]
            Cbf2 = [Cbf, T2("Cbf_b", [128, 129], BF16)]
            for d_ in range(2):
                P.dve(lambda e, o=Cst2[d_][:]: e.memset(o, 0.0), [], [("Cst", d_)])
                P.dve(lambda e, o=Cbf2[d_][:]: e.memset(o, 0.0), [], [("Cbf", d_)])
            hdir = [hfwd, hbwd]

            def mchunk(d_, c):
                dd = dirs[d_]
                mslice = mk[:, d_ * 128:(d_ + 1) * 128]
                p2 = d_
                Cs, Cb = Cst2[d_], Cbf2[d_]
                csl = slice(c * 128, (c + 1) * 128)
                tb_q = c // (TA // 128)
                qk_keys = [("QKm", 0, tb_q), ("QKm", 1, tb_q)]
                tbank = 2 if d_ == 0 else 7
                P.mm(lambda e, o=pb[tbank][:].bitcast(BF16)[:, 0:128], i_=KmT[:, csl]: e.transpose(o, i_, identb[:]),
                     [("QKm", 1, tb_q), "identb"], [pk[tbank]])
                cp(P, Ktok[p2][:], pb[tbank][:].bitcast(BF16)[:, 0:128], [pk[tbank]], [("Ktok", p2)], eng="scalar")
                act(P, Vw[p2][:], Vaug[:, c, :], AF.Identity, [("Vaug", c), "Vaug1", ("w", d_)], [("Vw", p2)], scale=dd["w"][:, c:c + 1])
                sb_ = 3 + p2
                mm_group(P, pb[sb_][:, 0:128], [(KmT[:, csl], QmT[:, csl])], qk_keys, [pk[sb_]])
                stt(P, Sp[p2][:], pb[sb_][:, 0:128], dd["ws"][:, c:c + 1], mslice, ALU.mult, ALU.mult,
                    [pk[sb_], ("ws", d_), "mk"], [("Sp", p2)])
                ob_ = 5 + p2
                mm_group(P, pb[ob_][:, 0:129], [(QmT[:, csl], Cb[:]), (Sp[p2][:], Vaug[:, c, :])],
                         qk_keys + [("Cbf", d_), ("Sp", p2), ("Vaug", c), "Vaug1"], [pk[ob_]])
                act(P, den[p2][:], pb[ob_][:, 128:129], AF.Abs, [pk[ob_]], [("den", p2)])
                tt(P, den[p2][:], den[p2][:], dd["flo"][:, c:c + 1], ALU.max, [("den", p2), ("flo", d_)], [("den", p2)])
                P.dve(lambda e, o=rden[p2][:], i_=den[p2][:]: e.reciprocal(out=o, in_=i_), [("den", p2)], [("rden", p2)])
                ts(P, hdir[d_][:, c, :], pb[ob_][:, 0:128], rden[p2][:, 0:1], ALU.mult, [pk[ob_], ("rden", p2)], [("hdir", d_, c)])
                mm_group(P, pb[p2][:, 0:129], [(Ktok[p2][:], Vw[p2][:])], [("Ktok", p2), ("Vw", p2)], [pk[p2]])
                ts(P, Cs[:], Cs[:], dd["ebl"][:, c:c + 1], ALU.mult, [("Cst", d_), ("ebl", d_)], [("Cst", d_)])
                stt(P, Cs[:], pb[p2][:, 0:129], dd["ebl"][:, c:c + 1], Cs[:], ALU.mult, ALU.add,
                    [pk[p2], ("ebl", d_), ("Cst", d_)], [("Cst", d_)])
                act(P, Cb[:], Cs[:], AF.Identity, [("Cst", d_)], [("Cbf", d_)], scale=MSCALE)

            for i in range(NCH):
                mchunk(0, i)
                mchunk(1, NCH - 1 - i)
            for c in range(NCH):
                p2 = c % 2
                tt(P, hs[p2][:], hfwd[:, c, :], hbwd[:, c, :], ALU.add, [("hdir", 0, c), ("hdir", 1, c)], [("hs", p2)])
                tt(P, hsq[p2][:], hs[p2][:], hs[p2][:], ALU.mult, [("hs", p2)], [("hsq", p2)])
                red(P, ss[p2][:], hsq[p2][:], ALU.add, [("hsq", p2)], [("ss", p2)])
                act(P, srt[p2][:], ss[p2][:], AF.Sqrt, [("ss", p2), "eps"], [("srt", p2)], bias=W["eps"][:, 0:1], scale=1.0 / 128)
                P.dve(lambda e, o=srn[p2][:], i_=srt[p2][:]: e.reciprocal(out=o, in_=i_), [("srt", p2)], [("srn", p2)])
                stt(P, yt[p2][:], hs[p2][:], srn[p2][:, 0:1], gmrs[:], ALU.mult, ALU.mult,
                    [("hs", p2), ("srn", p2), "gmr"], [("yt", p2)])
                tt(P, y2[p2][:], yt[p2][:], osig[:, c, :], ALU.mult, [("yt", p2), ("osig", c)], [("y2", p2)])
                ybank = 3 + p2
                P.mm(lambda e, o=pb[ybank][:].bitcast(BF16)[:, 0:128], i_=y2[p2][:]: e.transpose(o, i_, identb[:]),
                     [("y2", p2), "identb"], [pk[ybank]])
                grp = c // 4
                yb = ymb[grp % 2]
                cp(P, yb[:, (c % 4) * 128:(c % 4 + 1) * 128], pb[ybank][:].bitcast(BF16)[:, 0:128], [pk[ybank]],
                   [("ymb", grp % 2, c % 4)], eng="scalar")
                if c % 4 == 3:
                    dma(P, ymT[:, grp * 512:(grp + 1) * 512], yb[:], reads=[("ymb", grp % 2, q_) for q_ in range(4)])
            P.barrier()

        with ExitStack() as s3:
            T3 = lambda n, s, d: s3.enter_context(nc.sbuf_tensor(n, s, d))
            pT = [T3(f"pT{i}", [128, 512], BF16) for i in range(3)]
            osb = [T3(f"osb{i}", [64, 512], F32) for i in range(2)]
            rec = T3("rec", [128, 512], F32)
            yab = [T3(f"yab{i}", [64, 512], BF16) for i in range(2)]
            NTQ = 512 // TA
            jobs = [(qb, h) for qb in range(att_qblocks) for h in range(2)]
            steps = [(ji, kc) for ji in range(len(jobs)) for kc in range(NCH)]
            LOOK = 2

            def emit_qk(i):
                ji, kc = steps[i]
                qb, h = jobs[ji]
                hsl = slice(h * 64, (h + 1) * 64)
                qsl = slice(qb * 512, (qb + 1) * 512)
                ksl = slice(kc * 128, (kc + 1) * 128)
                sb_ = i % 3
                KT_ = KTa if h == 0 else KTb
                mm_group(P, pb[sb_][:], [(KT_[:, ksl], QaT[:, qsl])],
                         [("KTa" if h == 0 else "KTb", kc // (TA // 128)), "KTa0", "KTb0"]
                         + [("QaT", qb * NTQ + i_) for i_ in range(NTQ)], [pk[sb_]])
                act(P, pT[sb_][:], pb[sb_][:], AF.Exp, [pk[sb_]], [("pT", sb_)], scale=ASCALE)

            def emit_pv(i):
                ji, kc = steps[i]
                qb, h = jobs[ji]
                hsl = slice(h * 64, (h + 1) * 64)
                qsl = slice(qb * 512, (qb + 1) * 512)
                sb_ = i % 3
                ob_ = 3 + ji % 2
                P.mm(lambda e, o=pb[ob_][0:65, :], l_=Va[:, kc, :], r_=pT[sb_][:], a_=(kc == 0), z_=(kc == NCH - 1):
                     e.matmul(o, lhsT=l_, rhs=r_, start=a_, stop=z_),
                     [("Va", kc), "Va1", ("pT", sb_)], [pk[ob_]])
                if kc == NCH - 1:
                    jb = ji % 2
                    P.dve(lambda e, o=rec[64:65, :], i_=pb[ob_][64:65, :]: e.reciprocal(out=o, in_=i_), [pk[ob_]], ["rec"])
                    mm_group(P, pb[5][0:64, :], [(onesf[64:65, 0:64], rec[64:65, :])], ["rec", "onesf"], [pk[5]])
                    cp(P, osb[jb][:], pb[ob_][0:64, :], [pk[ob_]], [("osb", jb)], eng="gpsimd" if False else "vector")
                    tt(P, yab[jb][:], osb[jb][:], pb[5][0:64, :], ALU.mult, [("osb", jb), pk[5]], [("yab", jb)])
                    dma(P, yaT[hsl, qsl], yab[jb][:], reads=[("yab", jb)])

            for i in range(len(steps) + LOOK):
                if i < len(steps):
                    emit_qk(i)
                if i - LOOK >= 0:
                    emit_pv(i - LOOK)
            P.barrier()
        P.emit()
    return nc


OFF = dict(mq=0, mk=512, mv=1024, mo=1536, gates=2048, aq=2064, ak=2576, av=2704, gm=2832, ga=3856, end=4880)


def _pk(v, n):
    return np.ascontiguousarray(np.asarray(v, np.float32).reshape(n, 128).T)


def _consts():
    esel = np.zeros((32, 32, 128), np.float32)
    for e in range(32):
        esel[e, e, :] = 1.0
    return dict(esel=esel.reshape(32, 32 * 128), ident=np.eye(128, dtype=np.float32))


def prep_B(inp, l, b, r, xT_b, ymT_b, yaT_b):
    tok = slice(r * NT_B, (r + 1) * NT_B)
    w_in = inp["w_in"][l]
    b_in = inp["b_in"][l]
    m = dict(
        xT=np.ascontiguousarray(xT_b[:, tok]),
        ymT=np.ascontiguousarray(ymT_b[:, tok]),
        yaT=np.ascontiguousarray(yaT_b[:, tok]),
        cvec=_pk(inp["c"][b], 8),
        w_ada=np.ascontiguousarray(inp["w_ada"][l]),
        b_ada=_pk(inp["b_ada"][l], 48),
        g1=_pk(inp["norm1_g"][l], 8), g2=_pk(inp["norm2_g"][l], 8), gf=_pk(inp["final_norm_g"], 8),
        w_g=np.ascontiguousarray(w_in[:, OFF["gm"]:OFF["end"]]),
        b_g=_pk(b_in[OFF["gm"]:OFF["end"]], 16),
        w_bm=np.ascontiguousarray(inp["w_branch_m"][l]),
        w_ba=np.ascontiguousarray(inp["w_branch_a"][l]),
        w_o=np.ascontiguousarray(inp["w_out"][l]),
        w_r=np.ascontiguousarray(np.concatenate([inp["w_router_group"][l], inp["w_router_expert"][l]], axis=1)),
        b_r=np.ascontiguousarray(np.broadcast_to(
            np.concatenate([inp["b_router_group"][l], inp["b_router_expert"][l]])[None, :], (128, 36))),
        w_gate=np.ascontiguousarray(inp["w_gate"][l]),
        w_up=np.ascontiguousarray(inp["w_up"][l]),
        w_down=np.ascontiguousarray(inp["w_down"][l]),
    )
    m.update(_consts())
    return m


def _rope_consts():
    rows = S_LEN // 64
    row = np.repeat(np.arange(rows, dtype=np.float32), 64)
    col = np.tile(np.arange(64, dtype=np.float32), rows)
    half = 32
    inv_freq = (np.float32(10000.0) ** (-np.arange(0, half, 2, dtype=np.float32) / np.float32(half))).astype(np.float32)
    ang_r = (row[:, None] * inv_freq).astype(np.float32)
    ang_c = (col[:, None] * inv_freq).astype(np.float32)
    cosT = np.zeros((64, S_LEN), np.float32)
    sinT = np.zeros((64, S_LEN), np.float32)
    for i in range(64):
        ang = ang_r if i < 32 else ang_c
        cosT[i] = np.cos(ang[:, i % 16])
        sinT[i] = np.sin(ang[:, i % 16])
    R = np.zeros((64, 64), np.float32)
    for i in range(64):
        if i % 32 < 16:
            R[i, i + 16] = -1.0
        else:
            R[i, i - 16] = 1.0
    R2 = np.zeros((128, 128), np.float32)
    R2[:64, :64] = R
    R2[64:, 64:] = R
    oblk = np.zeros((128, 128), np.float32)
    oblk[:64, :64] = 1.0
    oblk[64:, 64:] = 1.0
    masks = np.concatenate([np.triu(np.ones((128, 128), np.float32)), np.tril(np.ones((128, 128), np.float32))], axis=1)
    return dict(cosT=np.ascontiguousarray(np.tile(cosT, (2, 1))), sinT=np.ascontiguousarray(np.tile(sinT, (2, 1))),
                rT=np.ascontiguousarray(R2.T), oblk=oblk, masks=np.ascontiguousarray(masks),
                ident=np.eye(128, dtype=np.float32))


_ROPE = None


def prep_A(inp, l, b, r, xT_b):
    global _ROPE
    if _ROPE is None:
        _ROPE = _rope_consts()
    w_in = inp["w_in"][l]
    b_in = inp["b_in"][l]
    kv = r // 2
    fcols = np.concatenate([np.arange(OFF["mq"] + r * 128, OFF["mq"] + (r + 1) * 128),
                            np.arange(OFF["mk"] + r * 128, OFF["mk"] + (r + 1) * 128),
                            np.arange(OFF["aq"] + r * 128, OFF["aq"] + (r + 1) * 128),
                            np.arange(OFF["ak"] + kv * 64, OFF["ak"] + (kv + 1) * 64),
                            np.arange(OFF["ak"] + kv * 64, OFF["ak"] + (kv + 1) * 64)])
    tcols = np.concatenate([np.arange(OFF["mv"] + r * 128, OFF["mv"] + (r + 1) * 128),
                            np.arange(OFF["mo"] + r * 128, OFF["mo"] + (r + 1) * 128),
                            OFF["gates"] + np.arange(4) * 4 + r,
                            np.arange(OFF["av"] + kv * 64, OFF["av"] + (kv + 1) * 64)])
    cwl = inp["conv_w"][l][:, 0, :]
    cw = np.zeros((128, 10), np.float32)
    cb = np.zeros((128, 2), np.float32)
    for qk in range(2):
        ch = slice(qk * 512 + r * 128, qk * 512 + (r + 1) * 128)
        cw[:, qk * 5:(qk + 1) * 5] = cwl[:, ch].T
        cb[:, qk] = inp["conv_b"][l][ch]
    m = dict(
        xT=xT_b,
        cvec=_pk(inp["c"][b], 8),
        w_ada=np.ascontiguousarray(inp["w_ada"][l][:, 0:2 * D]),
        b_ada=_pk(inp["b_ada"][l][0:2 * D], 16),
        g1=_pk(inp["norm1_g"][l], 8),
        w_F=np.ascontiguousarray(w_in[:, fcols]),
        b_F=_pk(b_in[fcols], 4),
        w_T=np.ascontiguousarray(w_in[:, tcols]),
        b_T=np.ascontiguousarray(np.broadcast_to(b_in[tcols][None, :], (128, NT_T))),
        cw=cw, cb=cb,
        gmr=np.ascontiguousarray(np.broadcast_to(inp["mlstm_norm_g"][l][r * 128:(r + 1) * 128][None, :], (128, 128))),
        gqk=np.ascontiguousarray(np.stack([np.tile(inp["q_norm_g"][l], 2), np.tile(inp["k_norm_g"][l], 2)], axis=1)),
    )
    m.update(_ROPE)
    return m


def kernel(**inputs):
    inp = {k: np.asarray(v) for k, v in inputs.items()}
    cores = list(range(8))
    xT = [np.ascontiguousarray(inp["x"][b].T) for b in range(2)]
    for l in range(2):
        ncA = build_A()
        resA = run_bass_kernel_spmd(ncA, [prep_A(inp, l, c // 4, c % 4, xT[c // 4]) for c in cores], core_ids=cores)
        ymT = [np.concatenate([resA.results[b * 4 + r]["ymT"] for r in range(4)], axis=0) for b in range(2)]
        yaT = [np.concatenate([resA.results[b * 4 + r]["yaT"] for r in range(4)], axis=0) for b in range(2)]
        del resA
        ncB = build_B(last=(l == 1))
        resB = run_bass_kernel_spmd(ncB, [prep_B(inp, l, c // 4, c % 4, xT[c // 4], ymT[c // 4], yaT[c // 4]) for c in cores],
                                    core_ids=cores)
        xT = [np.concatenate([resB.results[b * 4 + r]["outT"] for r in range(4)], axis=1) for b in range(2)]
        del resB
    return np.ascontiguousarray(np.stack([xT[b].T for b in range(2)])).astype(np.float32)
```

```python
import numpy as np
import ml_dtypes
from contextlib import ExitStack
import concourse.bass as bass
import concourse.mybir as mybir
from concourse.bass_utils import run_bass_kernel_spmd

F32 = mybir.dt.float32
BF16 = mybir.dt.bfloat16
AF = mybir.ActivationFunctionType
ALU = mybir.AluOpType
AX = mybir.AxisListType

ENGS = ("tensor", "vector", "scalar", "gpsimd", "sync")
GEN = 30000


class Prog:
    def __init__(self, nc, n_dma_slots=8, same_engine_sync=True):
        self.nc = nc
        self.ops = {e: [] for e in ENGS}
        self.last_writer = {}
        self.readers = {}
        self.n_dma_slots = n_dma_slots
        self.dma_count = {e: 0 for e in ENGS}
        self.same_engine_sync = same_engine_sync
        self.pending_dma = []

    def op(self, eng, fn, reads=(), writes=(), dma=False, nosync_same=False):
        idx = len(self.ops[eng])
        deps = set()
        for k in reads:
            w = self.last_writer.get(k)
            if w is not None:
                deps.add(w)
        for k in writes:
            w = self.last_writer.get(k)
            if w is not None:
                deps.add(w)
            for r in self.readers.get(k, ()):
                deps.add(r)
        me = (eng, idx)
        deps.discard(me)
        slot = None
        if dma:
            slot = self.dma_count[eng] % self.n_dma_slots
            self.dma_count[eng] += 1
            self.pending_dma.append(me)
        elif nosync_same or not self.same_engine_sync:
            deps = {d for d in deps if d[0] != eng or self.ops[d[0]][d[1]]["dma"]}
        rec = dict(eng=eng, fn=fn, deps=deps, dma=dma, slot=slot, signal=False)
        self.ops[eng].append(rec)
        for d in deps:
            self.ops[d[0]][d[1]]["signal"] = True
        for k in reads:
            self.readers.setdefault(k, []).append(me)
        for k in writes:
            self.last_writer[k] = me
            self.readers[k] = []
        return me

    def mm(self, fn, reads=(), writes=()):
        return self.op("tensor", fn, reads, writes, nosync_same=True)

    def dve(self, fn, reads=(), writes=()):
        return self.op("vector", fn, reads, writes)

    def act(self, fn, reads=(), writes=()):
        return self.op("scalar", fn, reads, writes)

    def pool(self, fn, reads=(), writes=()):
        return self.op("gpsimd", fn, reads, writes)

    def dma(self, fn, reads=(), writes=(), q="sync"):
        return self.op(q, fn, reads, writes, dma=True)

    def barrier(self):
        lasts = []
        for e in ENGS:
            for i in range(len(self.ops[e]) - 1, -1, -1):
                r = self.ops[e][i]
                if r["fn"] is not None and not r["dma"]:
                    lasts.append((e, i))
                    break
        deps = set(lasts) | set(self.pending_dma)
        self.pending_dma = []
        for d in deps:
            self.ops[d[0]][d[1]]["signal"] = True
        for e in ENGS:
            self.ops[e].append(dict(eng=e, fn=None, deps={d for d in deps}, dma=False, slot=None, signal=False))

    def emit(self):
        nc = self.nc
        ngen = {}
        final_slot_counts = {}
        for e in ENGS:
            c = 0
            slot_counts = [0] * self.n_dma_slots
            for r in self.ops[e]:
                if r["dma"]:
                    slot_counts[r["slot"]] += 1
                    r["slot_prev"] = slot_counts[r["slot"]] - 1
                    r["sig"] = ("dma", e, r["slot"], 16 * slot_counts[r["slot"]])
                elif r["signal"]:
                    g, v = divmod(c, GEN)
                    r["sig"] = ("eng", e, g, v + 1)
                    c += 1
                else:
                    r["sig"] = None
            ngen[e] = (c + GEN - 1) // GEN if c else 0
            final_slot_counts[e] = slot_counts
        with ExitStack() as st:
            sems = {}
            for e in ENGS:
                for g in range(ngen[e]):
                    sems[("eng", e, g)] = st.enter_context(nc.semaphore(f"s_{e}_{g}"))
                if self.dma_count[e]:
                    for s in range(self.n_dma_slots):
                        sems[("dma", e, s)] = st.enter_context(nc.semaphore(f"d_{e}_{s}"))
            block = st.enter_context(nc.Block())

            def make_body(e):
                def body(eng):
                    waited = {}
                    for r in self.ops[e]:
                        need = {}
                        for d in r["deps"]:
                            if d[0] == e and r["fn"] is None and not self.ops[d[0]][d[1]]["dma"]:
                                continue
                            sig = self.ops[d[0]][d[1]]["sig"]
                            key = sig[:3]
                            need[key] = max(need.get(key, 0), sig[3])
                        if r["dma"] and r["slot_prev"] > 0:
                            key = ("dma", e, r["slot"])
                            need[key] = max(need.get(key, 0), 16 * r["slot_prev"])
                        for key, v in need.items():
                            if key[0] == "eng":
                                best = waited.get((key[0], key[1]), (-1, 0))
                                if (key[2], v) <= best:
                                    continue
                                waited[(key[0], key[1])] = (key[2], v)
                            else:
                                if waited.get(key, 0) >= v:
                                    continue
                                waited[key] = v
                            eng.wait_ge(sems[key], v)
                        if r["fn"] is None:
                            continue
                        ins = r["fn"](eng)
                        if r["sig"] is not None:
                            ins.then_inc(sems[r["sig"][:3]], 16 if r["dma"] else 1)
                    if e == "sync":
                        for q in ENGS:
                            if self.dma_count[q]:
                                for s in range(self.n_dma_slots):
                                    cnt = final_slot_counts[q][s]
                                    if cnt:
                                        eng.wait_ge(sems[("dma", q, s)], 16 * cnt)
                return body

            for e in ENGS:
                getattr(block, e)(make_body(e))


def mm_group(P, out_ap, pairs, reads, writes):
    n = len(pairs)

    def fn(e):
        ins = None
        for i, (l, r) in enumerate(pairs):
            ins = e.matmul(out_ap, lhsT=l, rhs=r, start=(i == 0), stop=(i == n - 1))
        return ins
    return P.mm(fn, reads, writes)


def dma(P, out_ap, in_ap, reads=(), writes=(), q="sync"):
    return P.dma(lambda e: e.dma_start(out=out_ap, in_=in_ap), reads, writes, q=q)


def act(P, out_ap, in_ap, func, reads, writes, bias=None, scale=None):
    kw = {}
    if bias is not None:
        kw["bias"] = bias
    if scale is not None:
        kw["scale"] = scale
    return P.act(lambda e: e.activation(out=out_ap, in_=in_ap, func=func, **kw), reads, writes)


def tt(P, out_ap, a, b, op, reads, writes, eng="vector"):
    return P.op(eng, lambda e: e.tensor_tensor(out=out_ap, in0=a, in1=b, op=op), reads, writes)


def ts(P, out_ap, a, s1, op0, reads, writes, s2=None, op1=None, eng="vector"):
    if op1 is None:
        return P.op(eng, lambda e: e.tensor_scalar(out=out_ap, in0=a, scalar1=s1, scalar2=None, op0=op0), reads, writes)
    return P.op(eng, lambda e: e.tensor_scalar(out=out_ap, in0=a, scalar1=s1, scalar2=s2, op0=op0, op1=op1), reads, writes)


def stt(P, out_ap, a, s, b, op0, op1, reads, writes):
    return P.dve(lambda e: e.scalar_tensor_tensor(out=out_ap, in0=a, scalar=s, in1=b, op0=op0, op1=op1), reads, writes)


def cp(P, out_ap, in_ap, reads, writes, eng="vector"):
    if eng == "scalar":
        return P.op(eng, lambda e: e.activation(out=out_ap, in_=in_ap, func=AF.Identity), reads, writes)
    return P.op(eng, lambda e: e.tensor_copy(out=out_ap, in_=in_ap), reads, writes)


def red(P, out_ap, in_ap, op, reads, writes):
    return P.dve(lambda e: e.tensor_reduce(out=out_ap, in_=in_ap, axis=AX.X, op=op), reads, writes)


D = 1024
NK = 8
TB = 512
EPS = 1e-6


def emit_mod(P, nc, st, pb, cvec, w_ada, b_ada, col_chunks, name="mod"):
    T = lambda n, s, d: st.enter_context(nc.sbuf_tensor(n, s, d))
    ncol = len(col_chunks)
    cs = T(name + "_cs", [128, NK], F32)
    css = T(name + "_css", [128, NK], F32)
    nch = max(col_chunks) + 1
    bsb = T(name + "_b", [128, nch], F32)
    mod = T(name, [128, nch], F32)
    dma(P, cs[:], cvec[:, :], writes=[name + "cs"])
    dma(P, bsb[:], b_ada[:, :], writes=[name + "b"])
    act(P, css[:], cs[:], AF.Silu, [name + "cs"], [name + "css"])
    wv = w_ada.rearrange("(k p) c -> p k c", p=128)
    with ExitStack() as st2:
        wa = [st2.enter_context(nc.sbuf_tensor(f"{name}_wa{i}", [128, NK, 768], F32)) for i in range(2)]
        pieces = []
        cur = []
        for j in col_chunks:
            if cur and (j != cur[-1] + 1 or len(cur) == 6):
                pieces.append(cur)
                cur = []
            cur.append(j)
        if cur:
            pieces.append(cur)
        for pi, piece in enumerate(pieces):
            buf = wa[pi % 2]
            key = (name + "wa", pi % 2)
            c0 = piece[0] * 128
            n = len(piece) * 128
            for kh in range(2):
                dma(P, buf[:, kh * 4:(kh + 1) * 4, 0:n], wv[:, kh * 4:(kh + 1) * 4, c0:c0 + n], writes=[key],
                    q=("sync" if kh == 0 else "gpsimd"))
            for jj, j in enumerate(piece):
                pairs = [(buf[:, k, jj * 128:(jj + 1) * 128], css[:, k:k + 1]) for k in range(NK)]
                mm_group(P, pb[0][:, j:j + 1], pairs, [key, name + "css"], ["pb0"])
        for j in col_chunks:
            tt(P, mod[:, j:j + 1], pb[0][:, j:j + 1], bsb[:, j:j + 1], ALU.add, ["pb0", name + "b"], [name])
        P.barrier()
    return mod


def emit_norm(P, nc, W, xk, a_t, shift_t, outs, xkeys, okeys, pbank, pkey, n=TB, tag="n", xfull=None, part="ab"):
    sq, rt, rstd, tmp = W["sq"], W["rt"], W["rstd"], W["tmp"]
    sqk = [(tag + "sq", k) for k in range(NK)]
    if "a" in part:
        if xfull is not None:
            act(P, sq[:, :, 0:n], xfull, AF.Square, xkeys, sqk)
        else:
            for k in range(NK):
                act(P, sq[:, k, 0:n], xk(k), AF.Square, xkeys, [sqk[k]])
    if "b" not in part:
        return
    pairs = [(W["ones_bf"][:], sq[:, k, 0:n]) for k in range(NK)]
    mm_group(P, pbank[:, 0:n], pairs, sqk + ["ones"], [pkey])
    act(P, rt[:, 0:n], pbank[:, 0:n], AF.Sqrt, [pkey, "eps"], [tag + "rt"], bias=W["eps"][:, 0:1], scale=1.0 / D)
    P.dve(lambda e: e.reciprocal(out=rstd[:, 0:n], in_=rt[:, 0:n]), [tag + "rt"], [tag + "rstd"])
    for k in range(NK):
        tb_ = tmp[k % 2]
        tk_ = (tag + "ntmp", k % 2)
        tt(P, tb_[:, 0:n], xk(k), rstd[:, 0:n], ALU.mult, xkeys + [tag + "rstd"], [tk_])
        for oi, ofn in enumerate(outs):
            if shift_t is not None:
                act(P, ofn(k), tb_[:, 0:n], AF.Identity, [tk_], [okeys[oi](k)],
                    bias=shift_t[:, k:k + 1], scale=a_t[:, k:k + 1])
            else:
                act(P, ofn(k), tb_[:, 0:n], AF.Identity, [tk_], [okeys[oi](k)],
                    scale=a_t[:, k:k + 1])


NT_B = 2048
NE = 32
FH = 512


def build_B(last, n_experts=NE):
    nc = bass.Bass("TRN2", target_bir_lowering=False)

    def din(name, shape, dt=F32):
        return nc.dram_tensor(name, shape, dt, kind="ExternalInput").ap()
    xT = din("xT", [D, NT_B])
    ymT = din("ymT", [512, NT_B], BF16)
    yaT = din("yaT", [512, NT_B], BF16)
    cvec = din("cvec", [128, NK])
    w_ada = din("w_ada", [D, 6 * D])
    b_ada = din("b_ada", [128, 48])
    g1 = din("g1", [128, NK])
    g2 = din("g2", [128, NK])
    gf = din("gf", [128, NK])
    w_g = din("w_g", [D, 2 * D])
    b_g = din("b_g", [128, 16])
    w_bm = din("w_bm", [512, D])
    w_ba = din("w_ba", [512, D])
    w_o = din("w_o", [D, D])
    w_r = din("w_r", [D, 36])
    b_r = din("b_r", [128, 36])
    w_gate = din("w_gate", [NE, D, FH])
    w_up = din("w_up", [NE, D, FH])
    w_down = din("w_down", [NE, FH, D])
    esel = din("esel", [32, 32 * 128])
    ident = din("ident", [128, 128])
    outT = nc.dram_tensor("outT", [D, NT_B], F32, kind="ExternalOutput").ap()
    NTB = NT_B // TB

    with ExitStack() as st:
        T = lambda n, s, d: st.enter_context(nc.sbuf_tensor(n, s, d))
        P = Prog(nc, same_engine_sync=SES_B)
        pb = [st.enter_context(nc.psum_tensor(f"pb{i}", [128, 512], F32)) for i in range(8)]
        pk = [f"pb{i}" for i in range(8)]
        x1T = T("x1T", [128, NK, NT_B], F32)
        W = dict(ones_bf=T("ones_bf", [128, 128], BF16), eps=T("eps", [128, 1], F32),
                 sq=T("sq", [128, NK, TB], BF16), rt=T("rt", [128, TB], F32), rstd=T("rstd", [128, TB], F32),
                 tmp=[T("ntmp0", [128, TB], F32), T("ntmp1", [128, TB], F32)])
        identf = T("identf", [128, 128], F32)
        g1s, g2s, gfs = T("g1s", [128, NK], F32), T("g2s", [128, NK], F32), T("gfs", [128, NK], F32)
        a1, a2 = T("a1", [128, NK], F32), T("a2", [128, NK], F32)
        bgs = T("bgs", [128, 16], F32)
        brs = T("brs", [128, 36], F32)
        wr = T("wr", [128, NK, 36], F32)
        P.pool(lambda e: e.memset(W["ones_bf"][:], 1.0), [], ["ones"])
        P.pool(lambda e: e.memset(W["eps"][:], EPS), [], ["eps"])
        dma(P, identf[:], ident[:, :], writes=["ident"])
        dma(P, g1s[:], g1[:, :], writes=["g1"])
        dma(P, g2s[:], g2[:, :], writes=["g2"])
        dma(P, gfs[:], gf[:, :], writes=["gf"])
        dma(P, bgs[:], b_g[:, :], writes=["bg"])
        dma(P, brs[:], b_r[:, :], writes=["br"])
        dma(P, wr[:], w_r.rearrange("(k p) c -> p k c", p=128), writes=["wr"])
        xv = xT.rearrange("(k p) t -> p k t", p=128)
        ymv = ymT.rearrange("(k p) t -> p k t", p=128)
        yav = yaT.rearrange("(k p) t -> p k t", p=128)
        for tb in range(NTB):
            for kh in range(2):
                dma(P, x1T[:, kh * 4:(kh + 1) * 4, tb * TB:(tb + 1) * TB], xv[:, kh * 4:(kh + 1) * 4, tb * TB:(tb + 1) * TB],
                    writes=[("x1T", k, tb) for k in range(kh * 4, kh * 4 + 4)])

        mod = emit_mod(P, nc, st, pb, cvec, w_ada, b_ada, list(range(48)))
        stt(P, a1[:], mod[:, 8:16], 1.0, g1s[:], ALU.add, ALU.mult, ["mod", "g1"], ["a1"])
        stt(P, a2[:], mod[:, 32:40], 1.0, g2s[:], ALU.add, ALU.mult, ["mod", "g2"], ["a2"])
        shift1, gate1, shift2, gate2 = mod[:, 0:8], mod[:, 16:24], mod[:, 24:32], mod[:, 40:48]

        with ExitStack() as s1:
            T1 = lambda n, s, d: s1.enter_context(nc.sbuf_tensor(n, s, d))
            wg_ = T1("wg_", [128, NK, 2 * D], BF16)
            wbm = T1("wbm", [128, 4, D], BF16)
            wba = T1("wba", [128, 4, D], BF16)
            wo = T1("wo", [128, NK, D], BF16)
            wgv = w_g.rearrange("(k p) c -> p k c", p=128)
            for k in range(NK):
                dma(P, wg_[:, k, :], wgv[:, k, :], writes=[("wg_", k)], q="gpsimd")
            dma(P, wbm[:], w_bm.rearrange("(k p) c -> p k c", p=128), writes=["wbm"], q="gpsimd")
            dma(P, wba[:], w_ba.rearrange("(k p) c -> p k c", p=128), writes=["wba"], q="gpsimd")
            wov = w_o.rearrange("(k p) c -> p k c", p=128)
            for k in range(0, NK, 2):
                dma(P, wo[:, k:k + 2, :], wov[:, k:k + 2, :], writes=[("wo", k), ("wo", k + 1)], q="gpsimd")
            h1 = T1("h1", [128, NK, TB], BF16)
            ymb = [T1(f"ymb{i}", [128, 4, TB], BF16) for i in range(2)]
            yab = [T1(f"yab{i}", [128, 4, TB], BF16) for i in range(2)]
            merged = T1("merged", [128, NK, TB], BF16)
            sg = [[T1(f"sg{i}{j}", [128, TB], F32) for j in range(2)] for i in range(2)]
            t12 = [[T1(f"t12{i}{j}", [128, TB], F32) for j in range(2)] for i in range(2)]
            for tb in range(NTB):
                tsl = slice(tb * TB, (tb + 1) * TB)
                b = tb % 2
                dma(P, ymb[b][:], ymv[:, :, tsl], writes=[("ymb", b)])
                dma(P, yab[b][:], yav[:, :, tsl], writes=[("yab", b)])
                emit_norm(P, nc, W, lambda k: x1T[:, k, tsl], a1, shift1,
                          [lambda k: h1[:, k, :]], [("x1T", k_, tb) for k_ in range(NK)] + ["a1", "mod"], [lambda k: ("h1", k)],
                          pb[0], "pb0")
                for dc in range(NK):
                    par = dc % 2
                    base = 4 * par
                    csl = slice(dc * 128, (dc + 1) * 128)
                    hk = [("h1", k) for k in range(NK)]
                    mm_group(P, pb[base][:], [(wg_[:, k, csl], h1[:, k, :]) for k in range(NK)],
                             hk + [("wg_", k) for k in range(NK)], [pk[base]])
                    mm_group(P, pb[base + 1][:], [(wg_[:, k, D + dc * 128:D + (dc + 1) * 128], h1[:, k, :]) for k in range(NK)],
                             hk + [("wg_", k) for k in range(NK)], [pk[base + 1]])
                    mm_group(P, pb[base + 2][:], [(wbm[:, k, csl], ymb[b][:, k, :]) for k in range(4)],
                             ["wbm", ("ymb", b)], [pk[base + 2]])
                    mm_group(P, pb[base + 3][:], [(wba[:, k, csl], yab[b][:, k, :]) for k in range(4)],
                             ["wba", ("yab", b)], [pk[base + 3]])
                    act(P, sg[par][0][:], pb[base][:], AF.Sigmoid, [pk[base], "bg"], [("sg", par, 0)], bias=bgs[:, dc:dc + 1])
                    act(P, sg[par][1][:], pb[base + 1][:], AF.Sigmoid, [pk[base + 1], "bg"], [("sg", par, 1)], bias=bgs[:, 8 + dc:9 + dc])
                    tt(P, t12[par][0][:], sg[par][0][:], pb[base + 2][:], ALU.mult, [("sg", par, 0), pk[base + 2]], [("t12", par, 0)])
                    tt(P, t12[par][1][:], sg[par][1][:], pb[base + 3][:], ALU.mult, [("sg", par, 1), pk[base + 3]], [("t12", par, 1)])
                    tt(P, merged[:, dc, :], t12[par][0][:], t12[par][1][:], ALU.add, [("t12", par, 0), ("t12", par, 1)],
                       [("merged", dc)])
                for dc in range(NK):
                    bank = dc % 2
                    csl = slice(dc * 128, (dc + 1) * 128)
                    mm_group(P, pb[bank][:], [(wo[:, k, csl], merged[:, k, :]) for k in range(NK)],
                             [("merged", k) for k in range(NK)] + [("wo", k) for k in range(NK)], [pk[bank]])
                    stt(P, x1T[:, dc, tsl], pb[bank][:], gate1[:, dc:dc + 1], x1T[:, dc, tsl], ALU.mult, ALU.add,
                        [pk[bank], "mod", ("x1T", dc, tb)], [("x1T", dc, tb)])
            P.barrier()

        s23 = st.enter_context(ExitStack())
        h2T = s23.enter_context(nc.sbuf_tensor("h2T", [128, NK, NT_B], BF16))
        combT = s23.enter_context(nc.sbuf_tensor("combT", [32, NT_B], F32))
        with ExitStack() as s2:
            T2 = lambda n, s, d: s2.enter_context(nc.sbuf_tensor(n, s, d))
            h2f = T2("h2f", [128, NK, TB], F32)
            R = {n: T2("r_" + n, [128, s], F32) for n, s in
                 [("lg", 36), ("gmax", 1), ("ngmax", 1), ("eg", 4), ("ssum", 1), ("ptop", 1), ("mg", 4), ("pen", 4),
                  ("lem", 32), ("e1", 1), ("m1", 32), ("lem2", 32), ("e2", 1), ("m2", 32), ("d", 1), ("s2", 1),
                  ("w2", 1), ("w1", 1), ("comb", 32), ("comb2", 32)]}
            for tb in range(NTB):
                tsl = slice(tb * TB, (tb + 1) * TB)
                emit_norm(P, nc, W, lambda k: x1T[:, k, tsl], a2, shift2,
                          [lambda k: h2T[:, k, tsl], lambda k: h2f[:, k, :]],
                          [("x1T", k_, tb) for k_ in range(NK)] + ["a2", "mod"],
                          [lambda k: ("h2T", k, tb), lambda k: ("h2f", k)], pb[0], "pb0")
                for sub in range(4):
                    ssl = slice(sub * 128, (sub + 1) * 128)
                    bank = 1 + sub % 2
                    mm_group(P, pb[bank][:, 0:36], [(h2f[:, k, ssl], wr[:, k, :]) for k in range(NK)],
                             [("h2f", k) for k in range(NK)] + ["wr"], [pk[bank]])
                    tt(P, R["lg"][:], pb[bank][:, 0:36], brs[:], ALU.add, [pk[bank], "br"], ["r_lg"])
                    red(P, R["gmax"][:], R["lg"][:, 0:4], ALU.max, ["r_lg"], ["r_gmax"])
                    ts(P, R["ngmax"][:], R["gmax"][:], -1.0, ALU.mult, ["r_gmax"], ["r_ngmax"])
                    act(P, R["eg"][:], R["lg"][:, 0:4], AF.Exp, ["r_lg", "r_ngmax"], ["r_eg"], bias=R["ngmax"][:, 0:1])
                    red(P, R["ssum"][:], R["eg"][:], ALU.add, ["r_eg"], ["r_ssum"])
                    P.dve(lambda e: e.reciprocal(out=R["ptop"][:], in_=R["ssum"][:]), ["r_ssum"], ["r_ptop"])
                    ts(P, R["mg"][:], R["lg"][:, 0:4], R["gmax"][:, 0:1], ALU.is_equal, ["r_lg", "r_gmax"], ["r_mg"])
                    ts(P, R["pen"][:], R["mg"][:], -1.0, ALU.add, ["r_mg"], ["r_pen"], s2=1e30, op1=ALU.mult)
                    for g in range(4):
                        ts(P, R["lem"][:, g * 8:(g + 1) * 8], R["lg"][:, 4 + g * 8:12 + g * 8], R["pen"][:, g:g + 1], ALU.add,
                           ["r_lg", "r_pen"], [("r_lem", g)])
                    lemk = [("r_lem", g) for g in range(4)]
                    red(P, R["e1"][:], R["lem"][:], ALU.max, lemk, ["r_e1"])
                    ts(P, R["m1"][:], R["lem"][:], R["e1"][:, 0:1], ALU.is_equal, lemk + ["r_e1"], ["r_m1"])
                    stt(P, R["lem2"][:], R["m1"][:], -1e30, R["lem"][:], ALU.mult, ALU.add, lemk + ["r_m1"], ["r_lem2"])
                    red(P, R["e2"][:], R["lem2"][:], ALU.max, ["r_lem2"], ["r_e2"])
                    ts(P, R["m2"][:], R["lem2"][:], R["e2"][:, 0:1], ALU.is_equal, ["r_lem2", "r_e2"], ["r_m2"])
                    tt(P, R["d"][:], R["e2"][:], R["e1"][:], ALU.subtract, ["r_e1", "r_e2"], ["r_d"])
                    act(P, R["s2"][:], R["d"][:], AF.Sigmoid, ["r_d"], ["r_s2"])
                    tt(P, R["w2"][:], R["ptop"][:], R["s2"][:], ALU.mult, ["r_ptop", "r_s2"], ["r_w2"])
                    tt(P, R["w1"][:], R["ptop"][:], R["w2"][:], ALU.subtract, ["r_ptop", "r_w2"], ["r_w1"])
                    ts(P, R["comb"][:], R["m1"][:], R["w1"][:, 0:1], ALU.mult, ["r_m1", "r_w1"], ["r_comb"])
                    stt(P, R["comb2"][:], R["m2"][:], R["w2"][:, 0:1], R["comb"][:], ALU.mult, ALU.add,
                        ["r_m2", "r_w2", "r_comb"], ["r_comb2"])
                    P.mm(lambda e: e.transpose(pb[3][0:32, 0:128], R["comb2"][:], identf[:]), ["r_comb2", "ident"], [pk[3]])
                    tok = slice(tb * TB + sub * 128, tb * TB + (sub + 1) * 128)
                    act(P, combT[:, tok], pb[3][0:32, 0:128], AF.Identity, [pk[3]], [("combT", tb, sub)])
            P.barrier()

        with ExitStack() as s3:
            T3 = lambda n, s, d: s3.enter_context(nc.sbuf_tensor(n, s, d))
            eselT = T3("eselT", [32, 32 * 128], F32)
            dma(P, eselT[:], esel[:, :], writes=["esel"])
            wgt = [T3(f"wgt{i}", [128, NK, FH], BF16) for i in range(2)]
            wut = [T3(f"wut{i}", [128, NK, FH], BF16) for i in range(2)]
            wdt = [T3(f"wdt{i}", [128, 4, D], BF16) for i in range(2)]
            actT = [T3(f"actT{i}", [128, 4, TB], BF16) for i in range(2)]
            sl = [T3(f"sl{i}", [128, TB], F32) for i in range(2)]
            pr = [T3(f"pr{i}", [128, TB], F32) for i in range(2)]
            it = 0
            for e_ in range(n_experts):
                wb = e_ % 2
                gv = w_gate[e_].rearrange("(k p) f -> p k f", p=128)
                uv = w_up[e_].rearrange("(k p) f -> p k f", p=128)
                dv = w_down[e_].rearrange("(k p) c -> p k c", p=128)
                for kh in range(2):
                    ksl = slice(kh * 4, kh * 4 + 4)
                    dma(P, wgt[wb][:, ksl, :], gv[:, ksl, :], writes=[("wgt", wb, kh)], q="gpsimd")
                    dma(P, wut[wb][:, ksl, :], uv[:, ksl, :], writes=[("wut", wb, kh)], q="gpsimd")
                for kh in range(2):
                    ksl = slice(kh * 2, kh * 2 + 2)
                    dma(P, wdt[wb][:, ksl, :], dv[:, ksl, :], writes=[("wdt", wb, kh)], q="gpsimd")
                for tb in range(NTB):
                    tsl = slice(tb * TB, (tb + 1) * TB)
                    ab = it % 2
                    it += 1
                    h2k = [("h2T", k, tb) for k in range(NK)]
                    mm_group(P, pb[6][:], [(eselT[:, e_ * 128:(e_ + 1) * 128], combT[:, tsl])],
                             ["esel"] + [("combT", tb, s_) for s_ in range(4)], [pk[6]])
                    for fc in range(4):
                        fsl = slice(fc * 128, (fc + 1) * 128)
                        pa, pu = pb[2 * (fc % 2)], pb[2 * (fc % 2) + 1]
                        ka, ku = pk[2 * (fc % 2)], pk[2 * (fc % 2) + 1]
                        mm_group(P, pa[:], [(wgt[wb][:, k, fsl], h2T[:, k, tsl]) for k in range(NK)],
                                 h2k + [("wgt", wb, 0), ("wgt", wb, 1)], [ka])
                        mm_group(P, pu[:], [(wut[wb][:, k, fsl], h2T[:, k, tsl]) for k in range(NK)],
                                 h2k + [("wut", wb, 0), ("wut", wb, 1)], [ku])
                        act(P, sl[fc % 2][:], pa[:], AF.Silu, [ka], [("sl", fc % 2)])
                        tt(P, pr[fc % 2][:], sl[fc % 2][:], pu[:], ALU.mult, [("sl", fc % 2), ku], [("pr", fc % 2)])
                        tt(P, actT[ab][:, fc, :], pr[fc % 2][:], pb[6][:], ALU.mult, [("pr", fc % 2), pk[6]], [("actT", ab, fc)])
                    for dc in range(NK):
                        csl = slice(dc * 128, (dc + 1) * 128)
                        po, ko = pb[4 + dc % 2], pk[4 + dc % 2]
                        mm_group(P, po[:], [(wdt[wb][:, fc, csl], actT[ab][:, fc, :]) for fc in range(4)],
                                 [("actT", ab, fc) for fc in range(4)] + [("wdt", wb, 0), ("wdt", wb, 1)], [ko])
                        stt(P, x1T[:, dc, tsl], po[:], gate2[:, dc:dc + 1], x1T[:, dc, tsl], ALU.mult, ALU.add,
                            [ko, "mod", ("x1T", dc, tb)], [("x1T", dc, tb)])
            P.barrier()

        s23.close()
        ov = outT.rearrange("(k p) t -> p k t", p=128)
        if last:
            with ExitStack() as s4:
                T4 = lambda n, s, d: s4.enter_context(nc.sbuf_tensor(n, s, d))
                ob = [T4(f"ob{i}", [128, NK, TB], F32) for i in range(2)]
                for tb in range(NTB):
                    tsl = slice(tb * TB, (tb + 1) * TB)
                    o = ob[tb % 2]
                    emit_norm(P, nc, W, lambda k: x1T[:, k, tsl], gfs, None,
                              [lambda k: o[:, k, :]], [("x1T", k_, tb) for k_ in range(NK)] + ["gf"], [lambda k: ("ob", tb % 2, k)],
                              pb[0], "pb0")
                    dma(P, ov[:, :, tsl], o[:], reads=[("ob", tb % 2, k) for k in range(NK)])
                P.barrier()
        else:
            for tb in range(NTB):
                tsl = slice(tb * TB, (tb + 1) * TB)
                dma(P, ov[:, :, tsl], x1T[:, :, tsl], reads=[("x1T", k, tb) for k in range(NK)])
        P.emit()
    return nc


S_LEN = 8192
TA = 256
NCH = S_LEN // 128
MSCALE = 128.0 ** -0.5
ASCALE = 64.0 ** -0.5
NT_T = 324


SES_A = True
SES_B = True


def build_A(att_qblocks=16, do_mlstm=True):
    nc = bass.Bass("TRN2", target_bir_lowering=False)

    def din(name, shape, dt=F32):
        return nc.dram_tensor(name, shape, dt, kind="ExternalInput").ap()
    xT = din("xT", [D, S_LEN])
    cvec = din("cvec", [128, NK])
    w_ada = din("w_ada", [D, 2 * D])
    b_ada = din("b_ada", [128, 16])
    g1 = din("g1", [128, NK])
    w_F = din("w_F", [D, 512])
    b_F = din("b_F", [128, 4])
    w_T = din("w_T", [D, NT_T])
    b_T = din("b_T", [128, NT_T])
    cw = din("cw", [128, 10])
    cb = din("cb", [128, 2])
    gmr = din("gmr", [128, 128])
    gqk = din("gqk", [128, 2])
    cosT = din("cosT", [128, S_LEN])
    sinT = din("sinT", [128, S_LEN])
    ident = din("ident", [128, 128])
    masks = din("masks", [128, 256])
    rT = din("rT", [128, 128])
    oblk = din("oblk", [128, 128])
    ymT = nc.dram_tensor("ymT", [128, S_LEN], BF16, kind="ExternalOutput").ap()
    yaT = nc.dram_tensor("yaT", [128, S_LEN], BF16, kind="ExternalOutput").ap()
    NB = S_LEN // TA

    with ExitStack() as st:
        T = lambda n, s, d: st.enter_context(nc.sbuf_tensor(n, s, d))
        P = Prog(nc, same_engine_sync=SES_A)
        pb = [st.enter_context(nc.psum_tensor(f"pb{i}", [128, 512], F32)) for i in range(8)]
        pk = [f"pb{i}" for i in range(8)]
        QmT = T("QmT", [128, S_LEN], BF16)
        KmT = T("KmT", [128, S_LEN], BF16)
        Vaug = T("Vaug", [128, NCH, 129], BF16)
        osig = T("osig", [128, NCH, 128], BF16)
        G = T("G", [128, NCH, 4], F32)
        QaT = T("QaT", [128, S_LEN], BF16)
        KTa = T("KTa", [128, S_LEN], BF16)
        KTb = T("KTb", [128, S_LEN], BF16)
        Va = T("Va", [128, NCH, 65], BF16)
        W = dict(ones_bf=T("ones_bf", [128, 128], BF16), eps=T("eps", [128, 1], F32))
        identb = T("identb", [128, 128], BF16)
        mk = T("mk", [128, 256], F32)
        onesf = T("onesf", [128, 128], F32)
        one1 = T("one1", [128, 1], F32)
        rTs = T("rTs", [128, 128], F32)
        oblkb = T("oblkb", [128, 128], BF16)
        gmrs = T("gmrs", [128, 128], F32)
        gqks = T("gqks", [128, 2], F32)
        bFs = T("bFs", [128, 4], F32)
        bTs = T("bTs", [128, NT_T], F32)
        cws = T("cws", [128, 10], F32)
        cbs = T("cbs", [128, 2], F32)
        g1s = T("g1s", [128, NK], F32)
        a1 = T("a1", [128, NK], F32)
        P.pool(lambda e: e.memset(W["ones_bf"][:], 1.0), [], ["ones"])
        P.pool(lambda e: e.memset(W["eps"][:], EPS), [], ["eps"])
        P.pool(lambda e: e.memset(onesf[:], 1.0), [], ["onesf"])
        P.pool(lambda e: e.memset(one1[:], 1.0), [], ["one1"])
        P.pool(lambda e: e.memset(Vaug[:, :, 128:129], 1.0), [], ["Vaug1"])
        P.pool(lambda e: e.memset(Va[:, :, 64:65], 1.0), [], ["Va1"])
        P.pool(lambda e: e.memset(KTa[64:128, :], 0.0), [], ["KTa0"])
        P.pool(lambda e: e.memset(KTb[0:64, :], 0.0), [], ["KTb0"])
        dma(P, identb[:], ident[:, :], writes=["identb"], q="gpsimd")
        dma(P, oblkb[:], oblk[:, :], writes=["oblkb"], q="gpsimd")
        for t_, d_, k_ in [(mk, masks, "mk"), (rTs, rT, "rT"),
                           (gmrs, gmr, "gmr"), (gqks, gqk, "gqk"), (bFs, b_F, "bF"), (bTs, b_T, "bT"),
                           (cws, cw, "cw"), (cbs, cb, "cb"), (g1s, g1, "g1")]:
            dma(P, t_[:], d_[:, :], writes=[k_])

        mod = emit_mod(P, nc, st, pb, cvec, w_ada, b_ada, list(range(16)))
        stt(P, a1[:], mod[:, 8:16], 1.0, g1s[:], ALU.add, ALU.mult, ["mod", "g1"], ["a1"])
        shift1 = mod[:, 0:8]

        with ExitStack() as s1:
            T1 = lambda n, s, d: s1.enter_context(nc.sbuf_tensor(n, s, d))
            wF = T1("wF", [128, NK, 512], BF16)
            wT = T1("wT", [128, NK, NT_T], BF16)
            dma(P, wF[:], w_F.rearrange("(k p) c -> p k c", p=128), writes=["wF"], q="gpsimd")
            dma(P, wT[:], w_T.rearrange("(k p) c -> p k c", p=128), writes=["wT"], q="gpsimd")
            xb = [T1(f"xb{i}", [128, NK, TA], F32) for i in range(2)]
            hTs = [T1(f"hT{i}", [128, NK, TA], BF16) for i in range(2)]
            W.update(sq=T1("sq", [128, NK, TA], BF16), rt=T1("rt", [128, TA], F32), rstd=T1("rstd", [128, TA], F32),
                     tmp=[T1("ntmp0", [128, TA], F32), T1("ntmp1", [128, TA], F32)])
            W2 = dict(W)
            W2.update(sq=T1("sq2", [128, NK, TA], BF16), rt=T1("rt2", [128, TA], F32), rstd=T1("rstd2", [128, TA], F32),
                      tmp=[T1("ntmp20", [128, TA], F32), T1("ntmp21", [128, TA], F32)])
            Wp = [W, W2]
            NR = 3
            ring = [T1(f"ring{i}", [128, NR, TA], F32) for i in range(2)]
            acc = [T1(f"acc{i}", [128, TA], F32) for i in range(2)]
            cs_ = [T1(f"cosb{i}", [128, TA], F32) for i in range(2)]
            sn_ = [T1(f"sinb{i}", [128, TA], F32) for i in range(2)]
            RTMP = [{n_: T1(f"{n_}{i}", [128, TA], BF16 if n_ == "qsq" else F32)
                     for n_ in ("qf", "qsq", "qrt", "qrs", "qu", "qt1", "qt2")} for i in range(2)]
            tmpT = [T1(f"tmpT{i}", [128, NT_T], F32) for i in range(2)]
            xv = xT.rearrange("(k p) t -> p k t", p=128)

            def load_x(tb):
                tsl = slice(tb * TA, (tb + 1) * TA)
                for kh in range(2):
                    dma(P, xb[tb % 2][:, kh * 4:(kh + 1) * 4, :], xv[:, kh * 4:(kh + 1) * 4, tsl],
                        writes=[("xb", tb % 2, k) for k in range(kh * 4, kh * 4 + 4)])

            def conv_block(j, part="da"):
                tsl = slice(j * TA, (j + 1) * TA)
                for qk in range(2):
                    cur = ring[qk][:, j % NR, :]
                    a = acc[qk]
                    ak = ("acc", qk)
                    rk = lambda jj: ("ring", qk, jj % NR)
                    wcol = lambda k: cws[:, qk * 5 + k:qk * 5 + k + 1]
                    if "d" in part:
                        ts(P, a[:], cur, wcol(2), ALU.mult, [rk(j), "cw"], [ak])
                    for k in ((0, 1, 3, 4) if "d" in part else ()):
                        s_ = k - 2
                        if s_ < 0:
                            stt(P, a[:, -s_:TA], ring[qk][:, j % NR, 0:TA + s_], wcol(k), a[:, -s_:TA], ALU.mult, ALU.add,
                                [rk(j), "cw", ak], [ak])
                            if j > 0:
                                stt(P, a[:, 0:-s_], ring[qk][:, (j - 1) % NR, TA + s_:TA], wcol(k), a[:, 0:-s_], ALU.mult, ALU.add,
                                    [rk(j - 1), "cw", ak], [ak])
                        else:
                            stt(P, a[:, 0:TA - s_], ring[qk][:, j % NR, s_:TA], wcol(k), a[:, 0:TA - s_], ALU.mult, ALU.add,
                                [rk(j), "cw", ak], [ak])
                            if j < NB - 1:
                                stt(P, a[:, TA - s_:TA], ring[qk][:, (j + 1) % NR, 0:s_], wcol(k), a[:, TA - s_:TA], ALU.mult, ALU.add,
                                    [rk(j + 1), "cw", ak], [ak])
                    dest = (QmT if qk == 0 else KmT)
                    if "a" in part:
                        act(P, dest[:, tsl], a[:], AF.Silu, [ak, "cb"], [("QKm", qk, j)], bias=cbs[:, qk:qk + 1])

            def rope_norm(pf, pkey, bcol, gcol, dest, dkey, tb, rp):
                tsl = slice(tb * TA, (tb + 1) * TA)
                cb_, sb_ = cs_[tb % 2], sn_[tb % 2]
                R_ = RTMP[rp]
                qf, qsq, qrt, qrs, qu, qt1, qt2 = (R_[n_] for n_ in ("qf", "qsq", "qrt", "qrs", "qu", "qt1", "qt2"))
                kq = lambda n_: (n_, rp)
                pss, psr = pb[5 + rp], pk[5 + rp]

                def fin():
                    if dest is None:
                        tt(P, KTa[0:64, tsl], qt1[0:64, :], qrs[0:64, :], ALU.mult, [kq("qt1"), kq("qrs")], [("KTa", tb)])
                        tt(P, KTb[64:128, tsl], qt1[64:128, :], qrs[64:128, :], ALU.mult, [kq("qt1"), kq("qrs")], [("KTb", tb)])
                    else:
                        tt(P, dest[:, tsl], qt1[:], qrs[:], ALU.mult, [kq("qt1"), kq("qrs")], [(dkey, tb)])
                return [
                    lambda: act(P, qf[:], pf[:, 0:TA], AF.Identity, [pkey, "bF"], [kq("qf")], bias=bFs[:, bcol:bcol + 1]),
                    lambda: act(P, qsq[:], qf[:], AF.Square, [kq("qf")], [kq("qsq")]),
                    lambda: ts(P, qu[:], qf[:], gqks[:, gcol:gcol + 1], ALU.mult, [kq("qf"), "gqk"], [kq("qu")]),
                    lambda: mm_group(P, pss[:, 0:TA], [(oblkb[:], qsq[:])], [kq("qsq"), "oblkb"], [psr]),
                    lambda: tt(P, qt1[:], qu[:], cb_[:], ALU.mult, [kq("qu"), ("cos", tb % 2)], [kq("qt1")]),
                    lambda: act(P, qrt[:], pss[:, 0:TA], AF.Sqrt, [psr, "eps"], [kq("qrt")], bias=W["eps"][:, 0:1], scale=1.0 / 64),
                    lambda: mm_group(P, pss[:, 256:256 + TA], [(rTs[:], qu[:])], [kq("qu"), "rT"], [psr]),
                    lambda: P.dve(lambda e: e.reciprocal(out=qrs[:], in_=qrt[:]), [kq("qrt")], [kq("qrs")]),
                    lambda: tt(P, qt2[:], pss[:, 256:256 + TA], sb_[:], ALU.mult, [psr, ("sin", tb % 2)], [kq("qt2")]),
                    lambda: tt(P, qt1[:], qt1[:], qt2[:], ALU.add, [kq("qt1"), kq("qt2")], [kq("qt1")]),
                    fin,
                ]

            def norm_blk(tb, part):
                x_ = xb[tb % 2]
                hp = tb % 2
                hT = hTs[hp]
                nb_ = 0 if hp == 0 else 7
                emit_norm(P, nc, Wp[hp], lambda k: x_[:, k, :], a1, shift1, [lambda k: hT[:, k, :]],
                          [("xb", tb % 2, k_) for k_ in range(NK)] + ["a1", "mod"], [lambda k: ("hT", hp, k)],
                          pb[nb_], pk[nb_], n=TA, tag=f"n{hp}", xfull=x_[:, :, :], part=part)

            load_x(0)
            load_x(1)
            norm_blk(0, "ab")
            for tb in range(NB):
                tsl = slice(tb * TA, (tb + 1) * TA)
                dma(P, cs_[tb % 2][:], cosT[:, tsl], writes=[("cos", tb % 2)])
                dma(P, sn_[tb % 2][:], sinT[:, tsl], writes=[("sin", tb % 2)])
                if tb + 1 < NB:
                    norm_blk(tb + 1, "a")
                hp = tb % 2
                hT = hTs[hp]
                hk = [("hT", hp, k) for k in range(NK)]
                for fc in range(4):
                    bank = 1 + fc % 2
                    mm_group(P, pb[bank][:, 0:TA], [(wF[:, k, fc * 128:(fc + 1) * 128], hT[:, k, :]) for k in range(NK)],
                             hk + ["wF"], [pk[bank]])
                    if fc < 2:
                        act(P, ring[fc][:, tb % NR, :], pb[bank][:, 0:TA], AF.Identity, [pk[bank], "bF"], [("ring", fc, tb % NR)],
                            bias=bFs[:, fc:fc + 1])
                        if fc == 1 and tb >= 1:
                            conv_block(tb - 1, "d")
                    elif fc == 2:
                        chain_q = rope_norm(pb[bank], pk[bank], 2, 0, QaT, "QaT", tb, 0)
                    else:
                        chain_k = rope_norm(pb[bank], pk[bank], 3, 1, None, "KT", tb, 1)
                for sq_, sk_ in zip(chain_q, chain_k):
                    sq_()
                    sk_()
                if tb + 1 < NB:
                    norm_blk(tb + 1, "b")
                for sub in range(TA // 128):
                    ch = tb * (TA // 128) + sub
                    bank = 3 + sub % 2
                    mm_group(P, pb[bank][:, 0:NT_T], [(hT[:, k, sub * 128:(sub + 1) * 128], wT[:, k, :]) for k in range(NK)],
                             hk + ["wT"], [pk[bank]])
                    tm = tmpT[sub % 2]
                    tk = ("tmpT", sub % 2)
                    tt(P, tm[:], pb[bank][:, 0:NT_T], bTs[:], ALU.add, [pk[bank], "bT"], [tk])
                    cp(P, Vaug[:, ch, 0:128], tm[:, 0:128], [tk], [("Vaug", ch)], eng="scalar")
                    act(P, osig[:, ch, :], tm[:, 128:256], AF.Sigmoid, [tk], [("osig", ch)])
                    cp(P, G[:, ch, :], tm[:, 256:260], [tk], [("G", ch)], eng="scalar")
                    cp(P, Va[:, ch, 0:64], tm[:, 260:324], [tk], [("Va", ch)], eng="scalar")
                if tb >= 1:
                    conv_block(tb - 1, "a")
                if tb + 2 < NB:
                    load_x(tb + 2)
            conv_block(NB - 1)
            P.barrier()

        if do_mlstm:
          with ExitStack() as s2:
            T2 = lambda n, s, d: s2.enter_context(nc.sbuf_tensor(n, s, d))
            hfwd = T2("hfwd", [128, NCH, 128], F32)
            Gk = [("G", ch) for ch in range(NCH)]
            ge = T2("ge", [128, NCH, 2], F32)
            lfn = T2("lfn", [128, NCH, 2], F32)
            dirs = []
            for d_ in range(2):
                dd = {n: T2(f"{n}{d_}", [128, NCH], F32) for n in ("b", "imb", "w", "ws", "flo", "ebl")}
                dirs.append(dd)
            Cst = T2("Cst", [128, 129], F32)
            Cbf = T2("Cbf", [128, 129], BF16)
            Ktok = [T2(f"Ktok{i}", [128, 128], BF16) for i in range(2)]
            Vw = [T2(f"Vw{i}", [128, 129], BF16) for i in range(2)]
            Sp = [T2(f"Sp{i}", [128, 128], BF16) for i in range(2)]
            den = [T2(f"den{i}", [128, 1], F32) for i in range(2)]
            rden = [T2(f"rden{i}", [128, 1], F32) for i in range(2)]
            hs = [T2(f"hs{i}", [128, 128], F32) for i in range(2)]
            hsq = [T2(f"hsq{i}", [128, 128], F32) for i in range(2)]
            ss = [T2(f"ss{i}", [128, 1], F32) for i in range(2)]
            srt = [T2(f"srt{i}", [128, 1], F32) for i in range(2)]
            srn = [T2(f"srn{i}", [128, 1], F32) for i in range(2)]
            yt = [T2(f"yt{i}", [128, 128], F32) for i in range(2)]
            y2 = [T2(f"y2{i}", [128, 128], BF16) for i in range(2)]
            ymb = [T2(f"ymb{i}", [128, 512], BF16) for i in range(2)]
            for d_ in range(2):
                fcol = 1 + 2 * d_
                act(P, ge[:, :, d_], G[:, :, fcol], AF.Exp, Gk, [("ge", d_)], scale=-1.0)
                act(P, lfn[:, :, d_], ge[:, :, d_], AF.Ln, [("ge", d_), "one1"], [("lfn", d_)], bias=one1[:, 0:1])
            for d_ in range(2):
                dd = dirs[d_]
                icol = 2 * d_
                mslice = mk[:, d_ * 128:(d_ + 1) * 128]
                mm_group(P, pb[0][:, 0:NCH], [(mslice, lfn[:, :, d_])], [("lfn", d_), "mk"], [pk[0]])
                mm_group(P, pb[1][:, 0:NCH], [(onesf[:], lfn[:, :, d_])], [("lfn", d_), "onesf"], [pk[1]])
                cp(P, dd["b"][:], pb[0][:, 0:NCH], [pk[0]], [("mb", d_)])
                tt(P, dd["imb"][:], G[:, :, icol], dd["b"][:], ALU.add, Gk + [("mb", d_)], [("imb", d_)])
                act(P, dd["w"][:], dd["imb"][:], AF.Exp, [("imb", d_)], [("w", d_)])
                ts(P, dd["ws"][:], dd["w"][:], MSCALE, ALU.mult, [("w", d_)], [("ws", d_)])
                act(P, dd["flo"][:], dd["b"][:], AF.Exp, [("mb", d_)], [("flo", d_)])
                act(P, dd["ebl"][:], pb[1][:, 0:NCH], AF.Exp, [pk[1]], [("ebl", d_)], scale=-1.0)
            hbwd = T2("hbwd", [128, NCH, 128], F32)
            Cst2 = [Cst, T2("Cst_b", [128, 129], F32)]
            Cbf2 = [Cbf, T2("Cbf_b", [128, 129], BF16)]
            for d_ in range(2):
                P.dve(lambda e, o=Cst2[d_][:]: e.memset(o, 0.0), [], [("Cst", d_)])
                P.dve(lambda e, o=Cbf2[d_][:]: e.memset(o, 0.0), [], [("Cbf", d_)])
            hdir = [hfwd, hbwd]

            def mchunk(d_, c):
                dd = dirs[d_]
                mslice = mk[:, d_ * 128:(d_ + 1) * 128]
                p2 = d_
                Cs, Cb = Cst2[d_], Cbf2[d_]
                csl = slice(c * 128, (c + 1) * 128)
                tb_q = c // (TA // 128)
                qk_keys = [("QKm", 0, tb_q), ("QKm", 1, tb_q)]
                tbank = 2 if d_ == 0 else 7
                P.mm(lambda e, o=pb[tbank][:].bitcast(BF16)[:, 0:128], i_=KmT[:, csl]: e.transpose(o, i_, identb[:]),
                     [("QKm", 1, tb_q), "identb"], [pk[tbank]])
                cp(P, Ktok[p2][:], pb[tbank][:].bitcast(BF16)[:, 0:128], [pk[tbank]], [("Ktok", p2)], eng="scalar")
                act(P, Vw[p2][:], Vaug[:, c, :], AF.Identity, [("Vaug", c), "Vaug1", ("w", d_)], [("Vw", p2)], scale=dd["w"][:, c:c + 1])
                sb_ = 3 + p2
                mm_group(P, pb[sb_][:, 0:128], [(KmT[:, csl], QmT[:, csl])], qk_keys, [pk[sb_]])
                stt(P, Sp[p2][:], pb[sb_][:, 0:128], dd["ws"][:, c:c + 1], mslice, ALU.mult, ALU.mult,
                    [pk[sb_], ("ws", d_), "mk"], [("Sp", p2)])
                ob_ = 5 + p2
                mm_group(P, pb[ob_][:, 0:129], [(QmT[:, csl], Cb[:]), (Sp[p2][:], Vaug[:, c, :])],
                         qk_keys + [("Cbf", d_), ("Sp", p2), ("Vaug", c), "Vaug1"], [pk[ob_]])
                act(P, den[p2][:], pb[ob_][:, 128:129], AF.Abs, [pk[ob_]], [("den", p2)])
                tt(P, den[p2][:], den[p2][:], dd["flo"][:, c:c + 1], ALU.max, [("den", p2), ("flo", d_)], [("den", p2)])
                P.dve(lambda e, o=rden[p2][:], i_=den[p2][:]: e.reciprocal(out=o, in_=i_), [("den", p2)], [("rden", p2)])
                ts(P, hdir[d_][:, c, :], pb[ob_][:, 0:128], rden[p2][:, 0:1], ALU.mult, [pk[ob_], ("rden", p2)], [("hdir", d_, c)])
                mm_group(P, pb[p2][:, 0:129], [(Ktok[p2][:], Vw[p2][:])], [("Ktok", p2), ("Vw", p2)], [pk[p2]])
                ts(P, Cs[:], Cs[:], dd["ebl"][:, c:c + 1], ALU.mult, [("Cst", d_), ("ebl", d_)], [("Cst", d_)])
                stt(P, Cs[:], pb[p2][:, 0:129], dd["ebl"][:, c:c + 1], Cs[:], ALU.mult, ALU.add,
                    [pk[p2], ("ebl", d_), ("Cst", d_)], [("Cst", d_)])
                act(P, Cb[:], Cs[:], AF.Identity, [("Cst", d_)], [("Cbf", d_)], scale=MSCALE)

            for i in range(NCH):
                mchunk(0, i)
                mchunk(1, NCH - 1 - i)
            for c in range(NCH):
                p2 = c % 2
                tt(P, hs[p2][:], hfwd[:, c, :], hbwd[:, c, :], ALU.add, [("hdir", 0, c), ("hdir", 1, c)], [("hs", p2)])
                tt(P, hsq[p2][:], hs[p2][:], hs[p2][:], ALU.mult, [("hs", p2)], [("hsq", p2)])
                red(P, ss[p2][:], hsq[p2][:], ALU.add, [("hsq", p2)], [("ss", p2)])
                act(P, srt[p2][:], ss[p2][:], AF.Sqrt, [("ss", p2), "eps"], [("srt", p2)], bias=W["eps"][:, 0:1], scale=1.0 / 128)
                P.dve(lambda e, o=srn[p2][:], i_=srt[p2][:]: e.reciprocal(out=o, in_=i_), [("srt", p2)], [("srn", p2)])
                stt(P, yt[p2][:], hs[p2][:], srn[p2][:, 0:1], gmrs[:], ALU.mult, ALU.mult,
                    [("hs", p2), ("srn", p2), "gmr"], [("yt", p2)])
                tt(P, y2[p2][:], yt[p2][:], osig[:, c, :], ALU.mult, [("yt", p2), ("osig", c)], [("y2", p2)])
                ybank = 3 + p2
                P.mm(lambda e, o=pb[ybank][:].bitcast(BF16)[:, 0:128], i_=y2[p2][:]: e.transpose(o, i_, identb[:]),
                     [("y2", p2), "identb"], [pk[ybank]])
                grp = c // 4
                yb = ymb[grp % 2]
                cp(P, yb[:, (c % 4) * 128:(c % 4 + 1) * 128], pb[ybank][:].bitcast(BF16)[:, 0:128], [pk[ybank]],
                   [("ymb", grp % 2, c % 4)], eng="scalar")
                if c % 4 == 3:
                    dma(P, ymT[:, grp * 512:(grp + 1) * 512], yb[:], reads=[("ymb", grp % 2, q_) for q_ in range(4)])
            P.barrier()

        with ExitStack() as s3:
            T3 = lambda n, s, d: s3.enter_context(nc.sbuf_tensor(n, s, d))
            pT = [T3(f"pT{i}", [128, 512], BF16) for i in range(3)]
            osb = [T3(f"osb{i}", [64, 512], F32) for i in range(2)]
            rec = T3("rec", [128, 512], F32)
            yab = [T3(f"yab{i}", [64, 512], BF16) for i in range(2)]
            NTQ = 512 // TA
            jobs = [(qb, h) for qb in range(att_qblocks) for h in range(2)]
            steps = [(ji, kc) for ji in range(len(jobs)) for kc in range(NCH)]
            LOOK = 2

            def emit_qk(i):
                ji, kc = steps[i]
                qb, h = jobs[ji]
                hsl = slice(h * 64, (h + 1) * 64)
                qsl = slice(qb * 512, (qb + 1) * 512)
                ksl = slice(kc * 128, (kc + 1) * 128)
                sb_ = i % 3
                KT_ = KTa if h == 0 else KTb
                mm_group(P, pb[sb_][:], [(KT_[:, ksl], QaT[:, qsl])],
                         [("KTa" if h == 0 else "KTb", kc // (TA // 128)), "KTa0", "KTb0"]
                         + [("QaT", qb * NTQ + i_) for i_ in range(NTQ)], [pk[sb_]])
                act(P, pT[sb_][:], pb[sb_][:], AF.Exp, [pk[sb_]], [("pT", sb_)], scale=ASCALE)

            def emit_pv(i):
                ji, kc = steps[i]
                qb, h = jobs[ji]
                hsl = slice(h * 64, (h + 1) * 64)
                qsl = slice(qb * 512, (qb + 1) * 512)
                sb_ = i % 3
                ob_ = 3 + ji % 2
                P.mm(lambda e, o=pb[ob_][0:65, :], l_=Va[:, kc, :], r_=pT[sb_][:], a_=(kc == 0), z_=(kc == NCH - 1):
                     e.matmul(o, lhsT=l_, rhs=r_, start=a_, stop=z_),
                     [("Va", kc), "Va1", ("pT", sb_)], [pk[ob_]])
                if kc == NCH - 1:
                    jb = ji % 2
                    P.dve(lambda e, o=rec[64:65, :], i_=pb[ob_][64:65, :]: e.reciprocal(out=o, in_=i_), [pk[ob_]], ["rec"])
                    mm_group(P, pb[5][0:64, :], [(onesf[64:65, 0:64], rec[64:65, :])], ["rec", "onesf"], [pk[5]])
                    cp(P, osb[jb][:], pb[ob_][0:64, :], [pk[ob_]], [("osb", jb)], eng="gpsimd" if False else "vector")
                    tt(P, yab[jb][:], osb[jb][:], pb[5][0:64, :], ALU.mult, [("osb", jb), pk[5]], [("yab", jb)])
                    dma(P, yaT[hsl, qsl], yab[jb][:], reads=[("yab", jb)])

            for i in range(len(steps) + LOOK):
                if i < len(steps):
                    emit_qk(i)
                if i - LOOK >= 0:
                    emit_pv(i - LOOK)
            P.barrier()
        P.emit()
    return nc


OFF = dict(mq=0, mk=512, mv=1024, mo=1536, gates=2048, aq=2064, ak=2576, av=2704, gm=2832, ga=3856, end=4880)


def _pk(v, n):
    return np.ascontiguousarray(np.asarray(v, np.float32).reshape(n, 128).T)


def _consts():
    esel = np.zeros((32, 32, 128), np.float32)
    for e in range(32):
        esel[e, e, :] = 1.0
    return dict(esel=esel.reshape(32, 32 * 128), ident=np.eye(128, dtype=np.float32))


def prep_B(inp, l, b, r, xT_b, ymT_b, yaT_b):
    tok = slice(r * NT_B, (r + 1) * NT_B)
    w_in = inp["w_in"][l]
    b_in = inp["b_in"][l]
    m = dict(
        xT=np.ascontiguousarray(xT_b[:, tok]),
        ymT=np.ascontiguousarray(ymT_b[:, tok]),
        yaT=np.ascontiguousarray(yaT_b[:, tok]),
        cvec=_pk(inp["c"][b], 8),
        w_ada=np.ascontiguousarray(inp["w_ada"][l]),
        b_ada=_pk(inp["b_ada"][l], 48),
        g1=_pk(inp["norm1_g"][l], 8), g2=_pk(inp["norm2_g"][l], 8), gf=_pk(inp["final_norm_g"], 8),
        w_g=np.ascontiguousarray(w_in[:, OFF["gm"]:OFF["end"]]),
        b_g=_pk(b_in[OFF["gm"]:OFF["end"]], 16),
        w_bm=np.ascontiguousarray(inp["w_branch_m"][l]),
        w_ba=np.ascontiguousarray(inp["w_branch_a"][l]),
        w_o=np.ascontiguousarray(inp["w_out"][l]),
        w_r=np.ascontiguousarray(np.concatenate([inp["w_router_group"][l], inp["w_router_expert"][l]], axis=1)),
        b_r=np.ascontiguousarray(np.broadcast_to(
            np.concatenate([inp["b_router_group"][l], inp["b_router_expert"][l]])[None, :], (128, 36))),
        w_gate=np.ascontiguousarray(inp["w_gate"][l]),
        w_up=np.ascontiguousarray(inp["w_up"][l]),
        w_down=np.ascontiguousarray(inp["w_down"][l]),
    )
    m.update(_consts())
    return m


def _rope_consts():
    rows = S_LEN // 64
    row = np.repeat(np.arange(rows, dtype=np.float32), 64)
    col = np.tile(np.arange(64, dtype=np.float32), rows)
    half = 32
    inv_freq = (np.float32(10000.0) ** (-np.arange(0, half, 2, dtype=np.float32) / np.float32(half))).astype(np.float32)
    ang_r = (row[:, None] * inv_freq).astype(np.float32)
    ang_c = (col[:, None] * inv_freq).astype(np.float32)
    cosT = np.zeros((64, S_LEN), np.float32)
    sinT = np.zeros((64, S_LEN), np.float32)
    for i in range(64):
        ang = ang_r if i < 32 else ang_c
        cosT[i] = np.cos(ang[:, i % 16])
        sinT[i] = np.sin(ang[:, i % 16])
    R = np.zeros((64, 64), np.float32)
    for i in range(64):
        if i % 32 < 16:
            R[i, i + 16] = -1.0
        else:
            R[i, i - 16] = 1.0
    R2 = np.zeros((128, 128), np.float32)
    R2[:64, :64] = R
    R2[64:, 64:] = R
    oblk = np.zeros((128, 128), np.float32)
    oblk[:64, :64] = 1.0
    oblk[64:, 64:] = 1.0
    masks = np.concatenate([np.triu(np.ones((128, 128), np.float32)), np.tril(np.ones((128, 128), np.float32))], axis=1)
    return dict(cosT=np.ascontiguousarray(np.tile(cosT, (2, 1))), sinT=np.ascontiguousarray(np.tile(sinT, (2, 1))),
                rT=np.ascontiguousarray(R2.T), oblk=oblk, masks=np.ascontiguousarray(masks),
                ident=np.eye(128, dtype=np.float32))


_ROPE = None


def prep_A(inp, l, b, r, xT_b):
    global _ROPE
    if _ROPE is None:
        _ROPE = _rope_consts()
    w_in = inp["w_in"][l]
    b_in = inp["b_in"][l]
    kv = r // 2
    fcols = np.concatenate([np.arange(OFF["mq"] + r * 128, OFF["mq"] + (r + 1) * 128),
                            np.arange(OFF["mk"] + r * 128, OFF["mk"] + (r + 1) * 128),
                            np.arange(OFF["aq"] + r * 128, OFF["aq"] + (r + 1) * 128),
                            np.arange(OFF["ak"] + kv * 64, OFF["ak"] + (kv + 1) * 64),
                            np.arange(OFF["ak"] + kv * 64, OFF["ak"] + (kv + 1) * 64)])
    tcols = np.concatenate([np.arange(OFF["mv"] + r * 128, OFF["mv"] + (r + 1) * 128),
                            np.arange(OFF["mo"] + r * 128, OFF["mo"] + (r + 1) * 128),
                            OFF["gates"] + np.arange(4) * 4 + r,
                            np.arange(OFF["av"] + kv * 64, OFF["av"] + (kv + 1) * 64)])
    cwl = inp["conv_w"][l][:, 0, :]
    cw = np.zeros((128, 10), np.float32)
    cb = np.zeros((128, 2), np.float32)
    for qk in range(2):
        ch = slice(qk * 512 + r * 128, qk * 512 + (r + 1) * 128)
        cw[:, qk * 5:(qk + 1) * 5] = cwl[:, ch].T
        cb[:, qk] = inp["conv_b"][l][ch]
    m = dict(
        xT=xT_b,
        cvec=_pk(inp["c"][b], 8),
        w_ada=np.ascontiguousarray(inp["w_ada"][l][:, 0:2 * D]),
        b_ada=_pk(inp["b_ada"][l][0:2 * D], 16),
        g1=_pk(inp["norm1_g"][l], 8),
        w_F=np.ascontiguousarray(w_in[:, fcols]),
        b_F=_pk(b_in[fcols], 4),
        w_T=np.ascontiguousarray(w_in[:, tcols]),
        b_T=np.ascontiguousarray(np.broadcast_to(b_in[tcols][None, :], (128, NT_T))),
        cw=cw, cb=cb,
        gmr=np.ascontiguousarray(np.broadcast_to(inp["mlstm_norm_g"][l][r * 128:(r + 1) * 128][None, :], (128, 128))),
        gqk=np.ascontiguousarray(np.stack([np.tile(inp["q_norm_g"][l], 2), np.tile(inp["k_norm_g"][l], 2)], axis=1)),
    )
    m.update(_ROPE)
    return m


def kernel(**inputs):
    inp = {k: np.asarray(v) for k, v in inputs.items()}
    cores = list(range(8))
    xT = [np.ascontiguousarray(inp["x"][b].T) for b in range(2)]
    for l in range(2):
        ncA = build_A()
        resA = run_bass_kernel_spmd(ncA, [prep_A(inp, l, c // 4, c % 4, xT[c // 4]) for c in cores], core_ids=cores)
        ymT = [np.concatenate([resA.results[b * 4 + r]["ymT"] for r in range(4)], axis=0) for b in range(2)]
        yaT = [np.concatenate([resA.results[b * 4 + r]["yaT"] for r in range(4)], axis=0) for b in range(2)]
        del resA
        ncB = build_B(last=(l == 1))
        resB = run_bass_kernel_spmd(ncB, [prep_B(inp, l, c // 4, c % 4, xT[c // 4], ymT[c // 4], yaT[c // 4]) for c in cores],
                                    core_ids=cores)
        xT = [np.concatenate([resB.results[b * 4 + r]["outT"] for r in range(4)], axis=1) for b in range(2)]
        del resB
    return np.ascontiguousarray(np.stack([xT[b].T for b in range(2)])).astype(np.float32)
```

```python
import numpy as np
import ml_dtypes
from contextlib import ExitStack
import concourse.bass as bass
import concourse.mybir as mybir
from concourse.bass_utils import run_bass_kernel_spmd

F32 = mybir.dt.float32
BF16 = mybir.dt.bfloat16
AF = mybir.ActivationFunctionType
ALU = mybir.AluOpType
AX = mybir.AxisListType

ENGS = ("tensor", "vector", "scalar", "gpsimd", "sync")
GEN = 30000


class Prog:
    def __init__(self, nc, n_dma_slots=8, same_engine_sync=True):
        self.nc = nc
        self.ops = {e: [] for e in ENGS}
        self.last_writer = {}
        self.readers = {}
        self.n_dma_slots = n_dma_slots
        self.dma_count = {e: 0 for e in ENGS}
        self.same_engine_sync = same_engine_sync
        self.pending_dma = []

    def op(self, eng, fn, reads=(), writes=(), dma=False, nosync_same=False):
        idx = len(self.ops[eng])
        deps = set()
        for k in reads:
            w = self.last_writer.get(k)
            if w is not None:
                deps.add(w)
        for k in writes:
            w = self.last_writer.get(k)
            if w is not None:
                deps.add(w)
            for r in self.readers.get(k, ()):
                deps.add(r)
        me = (eng, idx)
        deps.discard(me)
        slot = None
        if dma:
            slot = self.dma_count[eng] % self.n_dma_slots
            self.dma_count[eng] += 1
            self.pending_dma.append(me)
        elif nosync_same or not self.same_engine_sync:
            deps = {d for d in deps if d[0] != eng or self.ops[d[0]][d[1]]["dma"]}
        rec = dict(eng=eng, fn=fn, deps=deps, dma=dma, slot=slot, signal=False)
        self.ops[eng].append(rec)
        for d in deps:
            self.ops[d[0]][d[1]]["signal"] = True
        for k in reads:
            self.readers.setdefault(k, []).append(me)
        for k in writes:
            self.last_writer[k] = me
            self.readers[k] = []
        return me

    def mm(self, fn, reads=(), writes=()):
        return self.op("tensor", fn, reads, writes, nosync_same=True)

    def dve(self, fn, reads=(), writes=()):
        return self.op("vector", fn, reads, writes)

    def act(self, fn, reads=(), writes=()):
        return self.op("scalar", fn, reads, writes)

    def pool(self, fn, reads=(), writes=()):
        return self.op("gpsimd", fn, reads, writes)

    def dma(self, fn, reads=(), writes=(), q="sync"):
        return self.op(q, fn, reads, writes, dma=True)

    def barrier(self):
        lasts = []
        for e in ENGS:
            for i in range(len(self.ops[e]) - 1, -1, -1):
                r = self.ops[e][i]
                if r["fn"] is not None and not r["dma"]:
                    lasts.append((e, i))
                    break
        deps = set(lasts) | set(self.pending_dma)
        self.pending_dma = []
        for d in deps:
            self.ops[d[0]][d[1]]["signal"] = True
        for e in ENGS:
            self.ops[e].append(dict(eng=e, fn=None, deps={d for d in deps}, dma=False, slot=None, signal=False))

    def emit(self):
        nc = self.nc
        ngen = {}
        final_slot_counts = {}
        for e in ENGS:
            c = 0
            slot_counts = [0] * self.n_dma_slots
            for r in self.ops[e]:
                if r["dma"]:
                    slot_counts[r["slot"]] += 1
                    r["slot_prev"] = slot_counts[r["slot"]] - 1
                    r["sig"] = ("dma", e, r["slot"], 16 * slot_counts[r["slot"]])
                elif r["signal"]:
                    g, v = divmod(c, GEN)
                    r["sig"] = ("eng", e, g, v + 1)
                    c += 1
                else:
                    r["sig"] = None
            ngen[e] = (c + GEN - 1) // GEN if c else 0
            final_slot_counts[e] = slot_counts
        with ExitStack() as st:
            sems = {}
            for e in ENGS:
                for g in range(ngen[e]):
                    sems[("eng", e, g)] = st.enter_context(nc.semaphore(f"s_{e}_{g}"))
                if self.dma_count[e]:
                    for s in range(self.n_dma_slots):
                        sems[("dma", e, s)] = st.enter_context(nc.semaphore(f"d_{e}_{s}"))
            block = st.enter_context(nc.Block())

            def make_body(e):
                def body(eng):
                    waited = {}
                    for r in self.ops[e]:
                        need = {}
                        for d in r["deps"]:
                            if d[0] == e and r["fn"] is None and not self.ops[d[0]][d[1]]["dma"]:
                                continue
                            sig = self.ops[d[0]][d[1]]["sig"]
                            key = sig[:3]
                            need[key] = max(need.get(key, 0), sig[3])
                        if r["dma"] and r["slot_prev"] > 0:
                            key = ("dma", e, r["slot"])
                            need[key] = max(need.get(key, 0), 16 * r["slot_prev"])
                        for key, v in need.items():
                            if key[0] == "eng":
                                best = waited.get((key[0], key[1]), (-1, 0))
                                if (key[2], v) <= best:
                                    continue
                                waited[(key[0], key[1])] = (key[2], v)
                            else:
                                if waited.get(key, 0) >= v:
                                    continue
                                waited[key] = v
                            eng.wait_ge(sems[key], v)
                        if r["fn"] is None:
                            continue
                        ins = r["fn"](eng)
                        if r["sig"] is not None:
                            ins.then_inc(sems[r["sig"][:3]], 16 if r["dma"] else 1)
                    if e == "sync":
                        for q in ENGS:
                            if self.dma_count[q]:
                                for s in range(self.n_dma_slots):
                                    cnt = final_slot_counts[q][s]
                                    if cnt:
                                        eng.wait_ge(sems[("dma", q, s)], 16 * cnt)
                return body

            for e in ENGS:
                getattr(block, e)(make_body(e))


def mm_group(P, out_ap, pairs, reads, writes):
    n = len(pairs)

    def fn(e):
        ins = None
        for i, (l, r) in enumerate(pairs):
            ins = e.matmul(out_ap, lhsT=l, rhs=r, start=(i == 0), stop=(i == n - 1))
        return ins
    return P.mm(fn, reads, writes)


def dma(P, out_ap, in_ap, reads=(), writes=(), q="sync"):
    return P.dma(lambda e: e.dma_start(out=out_ap, in_=in_ap), reads, writes, q=q)


def act(P, out_ap, in_ap, func, reads, writes, bias=None, scale=None):
    kw = {}
    if bias is not None:
        kw["bias"] = bias
    if scale is not None:
        kw["scale"] = scale
    return P.act(lambda e: e.activation(out=out_ap, in_=in_ap, func=func, **kw), reads, writes)


def tt(P, out_ap, a, b, op, reads, writes, eng="vector"):
    return P.op(eng, lambda e: e.tensor_tensor(out=out_ap, in0=a, in1=b, op=op), reads, writes)


def ts(P, out_ap, a, s1, op0, reads, writes, s2=None, op1=None, eng="vector"):
    if op1 is None:
        return P.op(eng, lambda e: e.tensor_scalar(out=out_ap, in0=a, scalar1=s1, scalar2=None, op0=op0), reads, writes)
    return P.op(eng, lambda e: e.tensor_scalar(out=out_ap, in0=a, scalar1=s1, scalar2=s2, op0=op0, op1=op1), reads, writes)


def stt(P, out_ap, a, s, b, op0, op1, reads, writes):
    return P.dve(lambda e: e.scalar_tensor_tensor(out=out_ap, in0=a, scalar=s, in1=b, op0=op0, op1=op1), reads, writes)


def cp(P, out_ap, in_ap, reads, writes, eng="vector"):
    if eng == "scalar":
        return P.op(eng, lambda e: e.activation(out=out_ap, in_=in_ap, func=AF.Identity), reads, writes)
    return P.op(eng, lambda e: e.tensor_copy(out=out_ap, in_=in_ap), reads, writes)


def red(P, out_ap, in_ap, op, reads, writes):
    return P.dve(lambda e: e.tensor_reduce(out=out_ap, in_=in_ap, axis=AX.X, op=op), reads, writes)


D = 1024
NK = 8
TB = 512
EPS = 1e-6


def emit_mod(P, nc, st, pb, cvec, w_ada, b_ada, col_chunks, name="mod"):
    T = lambda n, s, d: st.enter_context(nc.sbuf_tensor(n, s, d))
    ncol = len(col_chunks)
    cs = T(name + "_cs", [128, NK], F32)
    css = T(name + "_css", [128, NK], F32)
    nch = max(col_chunks) + 1
    bsb = T(name + "_b", [128, nch], F32)
    mod = T(name, [128, nch], F32)
    dma(P, cs[:], cvec[:, :], writes=[name + "cs"])
    dma(P, bsb[:], b_ada[:, :], writes=[name + "b"])
    act(P, css[:], cs[:], AF.Silu, [name + "cs"], [name + "css"])
    wv = w_ada.rearrange("(k p) c -> p k c", p=128)
    with ExitStack() as st2:
        wa = [st2.enter_context(nc.sbuf_tensor(f"{name}_wa{i}", [128, NK, 768], F32)) for i in range(2)]
        pieces = []
        cur = []
        for j in col_chunks:
            if cur and (j != cur[-1] + 1 or len(cur) == 6):
                pieces.append(cur)
                cur = []
            cur.append(j)
        if cur:
            pieces.append(cur)
        for pi, piece in enumerate(pieces):
            buf = wa[pi % 2]
            key = (name + "wa", pi % 2)
            c0 = piece[0] * 128
            n = len(piece) * 128
            for kh in range(2):
                dma(P, buf[:, kh * 4:(kh + 1) * 4, 0:n], wv[:, kh * 4:(kh + 1) * 4, c0:c0 + n], writes=[key],
                    q=("sync" if kh == 0 else "gpsimd"))
            for jj, j in enumerate(piece):
                pairs = [(buf[:, k, jj * 128:(jj + 1) * 128], css[:, k:k + 1]) for k in range(NK)]
                mm_group(P, pb[0][:, j:j + 1], pairs, [key, name + "css"], ["pb0"])
        for j in col_chunks:
            tt(P, mod[:, j:j + 1], pb[0][:, j:j + 1], bsb[:, j:j + 1], ALU.add, ["pb0", name + "b"], [name])
        P.barrier()
    return mod


def emit_norm(P, nc, W, xk, a_t, shift_t, outs, xkeys, okeys, pbank, pkey, n=TB, tag="n", xfull=None, part="ab"):
    sq, rt, rstd, tmp = W["sq"], W["rt"], W["rstd"], W["tmp"]
    sqk = [(tag + "sq", k) for k in range(NK)]
    if "a" in part:
        if xfull is not None:
            act(P, sq[:, :, 0:n], xfull, AF.Square, xkeys, sqk)
        else:
            for k in range(NK):
                act(P, sq[:, k, 0:n], xk(k), AF.Square, xkeys, [sqk[k]])
    if "b" not in part:
        return
    pairs = [(W["ones_bf"][:], sq[:, k, 0:n]) for k in range(NK)]
    mm_group(P, pbank[:, 0:n], pairs, sqk + ["ones"], [pkey])
    act(P, rt[:, 0:n], pbank[:, 0:n], AF.Sqrt, [pkey, "eps"], [tag + "rt"], bias=W["eps"][:, 0:1], scale=1.0 / D)
    P.dve(lambda e: e.reciprocal(out=rstd[:, 0:n], in_=rt[:, 0:n]), [tag + "rt"], [tag + "rstd"])
    for k in range(NK):
        tb_ = tmp[k % 2]
        tk_ = (tag + "ntmp", k % 2)
        tt(P, tb_[:, 0:n], xk(k), rstd[:, 0:n], ALU.mult, xkeys + [tag + "rstd"], [tk_])
        for oi, ofn in enumerate(outs):
            if shift_t is not None:
                act(P, ofn(k), tb_[:, 0:n], AF.Identity, [tk_], [okeys[oi](k)],
                    bias=shift_t[:, k:k + 1], scale=a_t[:, k:k + 1])
            else:
                act(P, ofn(k), tb_[:, 0:n], AF.Identity, [tk_], [okeys[oi](k)],
                    scale=a_t[:, k:k + 1])


NT_B = 2048
NE = 32
FH = 512


def build_B(last, n_experts=NE):
    nc = bass.Bass("TRN2", target_bir_lowering=False)

    def din(name, shape, dt=F32):
        return nc.dram_tensor(name, shape, dt, kind="ExternalInput").ap()
    xT = din("xT", [D, NT_B])
    ymT = din("ymT", [512, NT_B], BF16)
    yaT = din("yaT", [512, NT_B], BF16)
    cvec = din("cvec", [128, NK])
    w_ada = din("w_ada", [D, 6 * D])
    b_ada = din("b_ada", [128, 48])
    g1 = din("g1", [128, NK])
    g2 = din("g2", [128, NK])
    gf = din("gf", [128, NK])
    w_g = din("w_g", [D, 2 * D])
    b_g = din("b_g", [128, 16])
    w_bm = din("w_bm", [512, D])
    w_ba = din("w_ba", [512, D])
    w_o = din("w_o", [D, D])
    w_r = din("w_r", [D, 36])
    b_r = din("b_r", [128, 36])
    w_gate = din("w_gate", [NE, D, FH])
    w_up = din("w_up", [NE, D, FH])
    w_down = din("w_down", [NE, FH, D])
    esel = din("esel", [32, 32 * 128])
    ident = din("ident", [128, 128])
    outT = nc.dram_tensor("outT", [D, NT_B], F32, kind="ExternalOutput").ap()
    NTB = NT_B // TB

    with ExitStack() as st:
        T = lambda n, s, d: st.enter_context(nc.sbuf_tensor(n, s, d))
        P = Prog(nc, same_engine_sync=SES_B)
        pb = [st.enter_context(nc.psum_tensor(f"pb{i}", [128, 512], F32)) for i in range(8)]
        pk = [f"pb{i}" for i in range(8)]
        x1T = T("x1T", [128, NK, NT_B], F32)
        W = dict(ones_bf=T("ones_bf", [128, 128], BF16), eps=T("eps", [128, 1], F32),
                 sq=T("sq", [128, NK, TB], BF16), rt=T("rt", [128, TB], F32), rstd=T("rstd", [128, TB], F32),
                 tmp=[T("ntmp0", [128, TB], F32), T("ntmp1", [128, TB], F32)])
        identf = T("identf", [128, 128], F32)
        g1s, g2s, gfs = T("g1s", [128, NK], F32), T("g2s", [128, NK], F32), T("gfs", [128, NK], F32)
        a1, a2 = T("a1", [128, NK], F32), T("a2", [128, NK], F32)
        bgs = T("bgs", [128, 16], F32)
        brs = T("brs", [128, 36], F32)
        wr = T("wr", [128, NK, 36], F32)
        P.pool(lambda e: e.memset(W["ones_bf"][:], 1.0), [], ["ones"])
        P.pool(lambda e: e.memset(W["eps"][:], EPS), [], ["eps"])
        dma(P, identf[:], ident[:, :], writes=["ident"])
        dma(P, g1s[:], g1[:, :], writes=["g1"])
        dma(P, g2s[:], g2[:, :], writes=["g2"])
        dma(P, gfs[:], gf[:, :], writes=["gf"])
        dma(P, bgs[:], b_g[:, :], writes=["bg"])
        dma(P, brs[:], b_r[:, :], writes=["br"])
        dma(P, wr[:], w_r.rearrange("(k p) c -> p k c", p=128), writes=["wr"])
        xv = xT.rearrange("(k p) t -> p k t", p=128)
        ymv = ymT.rearrange("(k p) t -> p k t", p=128)
        yav = yaT.rearrange("(k p) t -> p k t", p=128)
        for tb in range(NTB):
            for kh in range(2):
                dma(P, x1T[:, kh * 4:(kh + 1) * 4, tb * TB:(tb + 1) * TB], xv[:, kh * 4:(kh + 1) * 4, tb * TB:(tb + 1) * TB],
                    writes=[("x1T", k, tb) for k in range(kh * 4, kh * 4 + 4)])

        mod = emit_mod(P, nc, st, pb, cvec, w_ada, b_ada, list(range(48)))
        stt(P, a1[:], mod[:, 8:16], 1.0, g1s[:], ALU.add, ALU.mult, ["mod", "g1"], ["a1"])
        stt(P, a2[:], mod[:, 32:40], 1.0, g2s[:], ALU.add, ALU.mult, ["mod", "g2"], ["a2"])
        shift1, gate1, shift2, gate2 = mod[:, 0:8], mod[:, 16:24], mod[:, 24:32], mod[:, 40:48]

        with ExitStack() as s1:
            T1 = lambda n, s, d: s1.enter_context(nc.sbuf_tensor(n, s, d))
            wg_ = T1("wg_", [128, NK, 2 * D], BF16)
            wbm = T1("wbm", [128, 4, D], BF16)
            wba = T1("wba", [128, 4, D], BF16)
            wo = T1("wo", [128, NK, D], BF16)
            wgv = w_g.rearrange("(k p) c -> p k c", p=128)
            for k in range(NK):
                dma(P, wg_[:, k, :], wgv[:, k, :], writes=[("wg_", k)], q="gpsimd")
            dma(P, wbm[:], w_bm.rearrange("(k p) c -> p k c", p=128), writes=["wbm"], q="gpsimd")
            dma(P, wba[:], w_ba.rearrange("(k p) c -> p k c", p=128), writes=["wba"], q="gpsimd")
            wov = w_o.rearrange("(k p) c -> p k c", p=128)
            for k in range(0, NK, 2):
                dma(P, wo[:, k:k + 2, :], wov[:, k:k + 2, :], writes=[("wo", k), ("wo", k + 1)], q="gpsimd")
            h1 = T1("h1", [128, NK, TB], BF16)
            ymb = [T1(f"ymb{i}", [128, 4, TB], BF16) for i in range(2)]
            yab = [T1(f"yab{i}", [128, 4, TB], BF16) for i in range(2)]
            merged = T1("merged", [128, NK, TB], BF16)
            sg = [[T1(f"sg{i}{j}", [128, TB], F32) for j in range(2)] for i in range(2)]
            t12 = [[T1(f"t12{i}{j}", [128, TB], F32) for j in range(2)] for i in range(2)]
            for tb in range(NTB):
                tsl = slice(tb * TB, (tb + 1) * TB)
                b = tb % 2
                dma(P, ymb[b][:], ymv[:, :, tsl], writes=[("ymb", b)])
                dma(P, yab[b][:], yav[:, :, tsl], writes=[("yab", b)])
                emit_norm(P, nc, W, lambda k: x1T[:, k, tsl], a1, shift1,
                          [lambda k: h1[:, k, :]], [("x1T", k_, tb) for k_ in range(NK)] + ["a1", "mod"], [lambda k: ("h1", k)],
                          pb[0], "pb0")
                for dc in range(NK):
                    par = dc % 2
                    base = 4 * par
                    csl = slice(dc * 128, (dc + 1) * 128)
                    hk = [("h1", k) for k in range(NK)]
                    mm_group(P, pb[base][:], [(wg_[:, k, csl], h1[:, k, :]) for k in range(NK)],
                             hk + [("wg_", k) for k in range(NK)], [pk[base]])
                    mm_group(P, pb[base + 1][:], [(wg_[:, k, D + dc * 128:D + (dc + 1) * 128], h1[:, k, :]) for k in range(NK)],
                             hk + [("wg_", k) for k in range(NK)], [pk[base + 1]])
                    mm_group(P, pb[base + 2][:], [(wbm[:, k, csl], ymb[b][:, k, :]) for k in range(4)],
                             ["wbm", ("ymb", b)], [pk[base + 2]])
                    mm_group(P, pb[base + 3][:], [(wba[:, k, csl], yab[b][:, k, :]) for k in range(4)],
                             ["wba", ("yab", b)], [pk[base + 3]])
                    act(P, sg[par][0][:], pb[base][:], AF.Sigmoid, [pk[base], "bg"], [("sg", par, 0)], bias=bgs[:, dc:dc + 1])
                    act(P, sg[par][1][:], pb[base + 1][:], AF.Sigmoid, [pk[base + 1], "bg"], [("sg", par, 1)], bias=bgs[:, 8 + dc:9 + dc])
                    tt(P, t12[par][0][:], sg[par][0][:], pb[base + 2][:], ALU.mult, [("sg", par, 0), pk[base + 2]], [("t12", par, 0)])
                    tt(P, t12[par][1][:], sg[par][1][:], pb[base + 3][:], ALU.mult, [("sg", par, 1), pk[base + 3]], [("t12", par, 1)])
                    tt(P, merged[:, dc, :], t12[par][0][:], t12[par][1][:], ALU.add, [("t12", par, 0), ("t12", par, 1)],
                       [("merged", dc)])
                for dc in range(NK):
                    bank = dc % 2
                    csl = slice(dc * 128, (dc + 1) * 128)
                    mm_group(P, pb[bank][:], [(wo[:, k, csl], merged[:, k, :]) for k in range(NK)],
                             [("merged", k) for k in range(NK)] + [("wo", k) for k in range(NK)], [pk[bank]])
                    stt(P, x1T[:, dc, tsl], pb[bank][:], gate1[:, dc:dc + 1], x1T[:, dc, tsl], ALU.mult, ALU.add,
                        [pk[bank], "mod", ("x1T", dc, tb)], [("x1T", dc, tb)])
            P.barrier()

        s23 = st.enter_context(ExitStack())
        h2T = s23.enter_context(nc.sbuf_tensor("h2T", [128, NK, NT_B], BF16))
        combT = s23.enter_context(nc.sbuf_tensor("combT", [32, NT_B], F32))
        with ExitStack() as s2:
            T2 = lambda n, s, d: s2.enter_context(nc.sbuf_tensor(n, s, d))
            h2f = T2("h2f", [128, NK, TB], F32)
            RR = [{n: T2(f"r{q_}_" + n, [128, s_], F32) for n, s_ in
                   [("lg", 36), ("gmax", 1), ("ngmax", 1), ("eg", 4), ("ssum", 1), ("ptop", 1), ("mg", 4), ("pen", 4),
                    ("lem", 32), ("e1", 1), ("m1", 32), ("lem2", 32), ("e2", 1), ("m2", 32), ("d", 1), ("s2", 1),
                    ("w2", 1), ("w1", 1), ("comb", 32), ("comb2", 32)]} for q_ in range(2)]

            def router_chain(tb, sub):
                R = RR[sub % 2]
                rk_ = lambda n_: (n_, sub % 2)
                ssl = slice(sub * 128, (sub + 1) * 128)
                bank = 1 + sub % 2
                tok = slice(tb * TB + sub * 128, tb * TB + (sub + 1) * 128)
                tps = pb[3 + sub % 2][0:32, 0:128]
                tkey = pk[3 + sub % 2]
                lemk = [rk_(("lem", g)) for g in range(4)]
                st_ = [
                    lambda: mm_group(P, pb[bank][:, 0:36], [(h2f[:, k, ssl], wr[:, k, :]) for k in range(NK)],
                                     [("h2f", k) for k in range(NK)] + ["wr"], [pk[bank]]),
                    lambda: tt(P, R["lg"][:], pb[bank][:, 0:36], brs[:], ALU.add, [pk[bank], "br"], [rk_("lg")]),
                    lambda: red(P, R["gmax"][:], R["lg"][:, 0:4], ALU.max, [rk_("lg")], [rk_("gmax")]),
                    lambda: ts(P, R["ngmax"][:], R["gmax"][:], -1.0, ALU.mult, [rk_("gmax")], [rk_("ngmax")]),
                    lambda: act(P, R["eg"][:], R["lg"][:, 0:4], AF.Exp, [rk_("lg"), rk_("ngmax")], [rk_("eg")], bias=R["ngmax"][:, 0:1]),
                    lambda: ts(P, R["mg"][:], R["lg"][:, 0:4], R["gmax"][:, 0:1], ALU.is_equal, [rk_("lg"), rk_("gmax")], [rk_("mg")]),
                    lambda: ts(P, R["pen"][:], R["mg"][:], -1.0, ALU.add, [rk_("mg")], [rk_("pen")], s2=1e30, op1=ALU.mult),
                ]
                for g in range(4):
                    st_.append(lambda g=g: ts(P, R["lem"][:, g * 8:(g + 1) * 8], R["lg"][:, 4 + g * 8:12 + g * 8], R["pen"][:, g:g + 1],
                                              ALU.add, [rk_("lg"), rk_("pen")], [rk_(("lem", g))]))
                st_ += [
                    lambda: red(P, R["e1"][:], R["lem"][:], ALU.max, lemk, [rk_("e1")]),
                    lambda: ts(P, R["m1"][:], R["lem"][:], R["e1"][:, 0:1], ALU.is_equal, lemk + [rk_("e1")], [rk_("m1")]),
                    lambda: stt(P, R["lem2"][:], R["m1"][:], -1e30, R["lem"][:], ALU.mult, ALU.add, lemk + [rk_("m1")], [rk_("lem2")]),
                    lambda: red(P, R["e2"][:], R["lem2"][:], ALU.max, [rk_("lem2")], [rk_("e2")]),
                    lambda: ts(P, R["m2"][:], R["lem2"][:], R["e2"][:, 0:1], ALU.is_equal, [rk_("lem2"), rk_("e2")], [rk_("m2")]),
                    lambda: tt(P, R["d"][:], R["e2"][:], R["e1"][:], ALU.subtract, [rk_("e1"), rk_("e2")], [rk_("d")]),
                    lambda: red(P, R["ssum"][:], R["eg"][:], ALU.add, [rk_("eg")], [rk_("ssum")]),
                    lambda: act(P, R["s2"][:], R["d"][:], AF.Sigmoid, [rk_("d")], [rk_("s2")]),
                    lambda: P.dve(lambda e: e.reciprocal(out=R["ptop"][:], in_=R["ssum"][:]), [rk_("ssum")], [rk_("ptop")]),
                    lambda: tt(P, R["w2"][:], R["ptop"][:], R["s2"][:], ALU.mult, [rk_("ptop"), rk_("s2")], [rk_("w2")]),
                    lambda: tt(P, R["w1"][:], R["ptop"][:], R["w2"][:], ALU.subtract, [rk_("ptop"), rk_("w2")], [rk_("w1")]),
                    lambda: ts(P, R["comb"][:], R["m1"][:], R["w1"][:, 0:1], ALU.mult, [rk_("m1"), rk_("w1")], [rk_("comb")]),
                    lambda: stt(P, R["comb2"][:], R["m2"][:], R["w2"][:, 0:1], R["comb"][:], ALU.mult, ALU.add,
                                [rk_("m2"), rk_("w2"), rk_("comb")], [rk_("comb2")]),
                    lambda: P.mm(lambda e: e.transpose(tps, R["comb2"][:], identf[:]), [rk_("comb2"), "ident"], [tkey]),
                    lambda: act(P, combT[:, tok], tps, AF.Identity, [tkey], [("combT", tb, sub)]),
                ]
                return st_

            for tb in range(NTB):
                tsl = slice(tb * TB, (tb + 1) * TB)
                emit_norm(P, nc, W, lambda k: x1T[:, k, tsl], a2, shift2,
                          [lambda k: h2T[:, k, tsl], lambda k: h2f[:, k, :]],
                          [("x1T", k_, tb) for k_ in range(NK)] + ["a2", "mod"],
                          [lambda k: ("h2T", k, tb), lambda k: ("h2f", k)], pb[0], "pb0")
                for half in range(2):
                    chains = [router_chain(tb, half * 2 + q_) for q_ in range(2)]
                    for stages in zip(*chains):
                        for f_ in stages:
                            f_()
            P.barrier()

        with ExitStack() as s3:
            T3 = lambda n, s, d: s3.enter_context(nc.sbuf_tensor(n, s, d))
            eselT = T3("eselT", [32, 32 * 128], F32)
            dma(P, eselT[:], esel[:, :], writes=["esel"])
            wgt = [T3(f"wgt{i}", [128, NK, FH], BF16) for i in range(2)]
            wut = [T3(f"wut{i}", [128, NK, FH], BF16) for i in range(2)]
            wdt = [T3(f"wdt{i}", [128, 4, D], BF16) for i in range(2)]
            actT = [T3(f"actT{i}", [128, 4, TB], BF16) for i in range(2)]
            sl = [T3(f"sl{i}", [128, TB], F32) for i in range(2)]
            pr = [T3(f"pr{i}", [128, TB], F32) for i in range(2)]
            def load_w(e_):
                wb = e_ % 2
                gv = w_gate[e_].rearrange("(k p) f -> p k f", p=128)
                uv = w_up[e_].rearrange("(k p) f -> p k f", p=128)
                dv = w_down[e_].rearrange("(k p) c -> p k c", p=128)
                for kh in range(2):
                    ksl = slice(kh * 4, kh * 4 + 4)
                    dma(P, wgt[wb][:, ksl, :], gv[:, ksl, :], writes=[("wgt", wb, kh)], q="gpsimd")
                    dma(P, wut[wb][:, ksl, :], uv[:, ksl, :], writes=[("wut", wb, kh)], q="gpsimd")
                for kh in range(2):
                    ksl = slice(kh * 2, kh * 2 + 2)
                    dma(P, wdt[wb][:, ksl, :], dv[:, ksl, :], writes=[("wdt", wb, kh)], q="gpsimd")

            def emit_gu(i):
                e_, tb = divmod(i, NTB)
                wb, ab, cb_ = e_ % 2, i % 2, 6 + i % 2
                tsl = slice(tb * TB, (tb + 1) * TB)
                h2k = [("h2T", k, tb) for k in range(NK)]
                mm_group(P, pb[cb_][:], [(eselT[:, e_ * 128:(e_ + 1) * 128], combT[:, tsl])],
                         ["esel"] + [("combT", tb, s_) for s_ in range(4)], [pk[cb_]])
                for fc in range(4):
                    fsl = slice(fc * 128, (fc + 1) * 128)
                    pa, pu = pb[2 * (fc % 2)], pb[2 * (fc % 2) + 1]
                    ka, ku = pk[2 * (fc % 2)], pk[2 * (fc % 2) + 1]
                    mm_group(P, pa[:], [(wgt[wb][:, k, fsl], h2T[:, k, tsl]) for k in range(NK)],
                             h2k + [("wgt", wb, 0), ("wgt", wb, 1)], [ka])
                    mm_group(P, pu[:], [(wut[wb][:, k, fsl], h2T[:, k, tsl]) for k in range(NK)],
                             h2k + [("wut", wb, 0), ("wut", wb, 1)], [ku])
                    act(P, sl[fc % 2][:], pa[:], AF.Silu, [ka], [("sl", fc % 2)])
                    tt(P, pr[fc % 2][:], sl[fc % 2][:], pu[:], ALU.mult, [("sl", fc % 2), ku], [("pr", fc % 2)])
                    tt(P, actT[ab][:, fc, :], pr[fc % 2][:], pb[cb_][:], ALU.mult, [("pr", fc % 2), pk[cb_]], [("actT", ab, fc)])

            def emit_down(i):
                e_, tb = divmod(i, NTB)
                wb, ab = e_ % 2, i % 2
                tsl = slice(tb * TB, (tb + 1) * TB)
                for dc in range(NK):
                    csl = slice(dc * 128, (dc + 1) * 128)
                    po, ko = pb[4 + dc % 2], pk[4 + dc % 2]
                    mm_group(P, po[:], [(wdt[wb][:, fc, csl], actT[ab][:, fc, :]) for fc in range(4)],
                             [("actT", ab, fc) for fc in range(4)] + [("wdt", wb, 0), ("wdt", wb, 1)], [ko])
                    stt(P, x1T[:, dc, tsl], po[:], gate2[:, dc:dc + 1], x1T[:, dc, tsl], ALU.mult, ALU.add,
                        [ko, "mod", ("x1T", dc, tb)], [("x1T", dc, tb)])

            nsteps = n_experts * NTB
            for i in range(nsteps + 1):
                if i < nsteps:
                    if i % NTB == 0:
                        load_w(i // NTB)
                    emit_gu(i)
                if i >= 1:
                    emit_down(i - 1)
            P.barrier()

        s23.close()
        ov = outT.rearrange("(k p) t -> p k t", p=128)
        if last:
            with ExitStack() as s4:
                T4 = lambda n, s, d: s4.enter_context(nc.sbuf_tensor(n, s, d))
                ob = [T4(f"ob{i}", [128, NK, TB], F32) for i in range(2)]
                for tb in range(NTB):
                    tsl = slice(tb * TB, (tb + 1) * TB)
                    o = ob[tb % 2]
                    emit_norm(P, nc, W, lambda k: x1T[:, k, tsl], gfs, None,
                              [lambda k: o[:, k, :]], [("x1T", k_, tb) for k_ in range(NK)] + ["gf"], [lambda k: ("ob", tb % 2, k)],
                              pb[0], "pb0")
                    dma(P, ov[:, :, tsl], o[:], reads=[("ob", tb % 2, k) for k in range(NK)])
                P.barrier()
        else:
            for tb in range(NTB):
                tsl = slice(tb * TB, (tb + 1) * TB)
                dma(P, ov[:, :, tsl], x1T[:, :, tsl], reads=[("x1T", k, tb) for k in range(NK)])
        P.emit()
    return nc


S_LEN = 8192
TA = 256
NCH = S_LEN // 128
MSCALE = 128.0 ** -0.5
ASCALE = 64.0 ** -0.5
NT_T = 324


SES_A = True
SES_B = True


def build_A(att_qblocks=16, do_mlstm=True):
    nc = bass.Bass("TRN2", target_bir_lowering=False)

    def din(name, shape, dt=F32):
        return nc.dram_tensor(name, shape, dt, kind="ExternalInput").ap()
    xT = din("xT", [D, S_LEN])
    cvec = din("cvec", [128, NK])
    w_ada = din("w_ada", [D, 2 * D])
    b_ada = din("b_ada", [128, 16])
    g1 = din("g1", [128, NK])
    w_F = din("w_F", [D, 512])
    b_F = din("b_F", [128, 4])
    w_T = din("w_T", [D, NT_T])
    b_T = din("b_T", [128, NT_T])
    cw = din("cw", [128, 10])
    cb = din("cb", [128, 2])
    gmr = din("gmr", [128, 128])
    gqk = din("gqk", [128, 2])
    cosT = din("cosT", [128, S_LEN])
    sinT = din("sinT", [128, S_LEN])
    ident = din("ident", [128, 128])
    masks = din("masks", [128, 256])
    rT = din("rT", [128, 128])
    oblk = din("oblk", [128, 128])
    ymT = nc.dram_tensor("ymT", [128, S_LEN], BF16, kind="ExternalOutput").ap()
    yaT = nc.dram_tensor("yaT", [128, S_LEN], BF16, kind="ExternalOutput").ap()
    NB = S_LEN // TA

    with ExitStack() as st:
        T = lambda n, s, d: st.enter_context(nc.sbuf_tensor(n, s, d))
        P = Prog(nc, same_engine_sync=SES_A)
        pb = [st.enter_context(nc.psum_tensor(f"pb{i}", [128, 512], F32)) for i in range(8)]
        pk = [f"pb{i}" for i in range(8)]
        QmT = T("QmT", [128, S_LEN], BF16)
        KmT = T("KmT", [128, S_LEN], BF16)
        Vaug = T("Vaug", [128, NCH, 129], BF16)
        osig = T("osig", [128, NCH, 128], BF16)
        G = T("G", [128, NCH, 4], F32)
        QaT = T("QaT", [128, S_LEN], BF16)
        KTa = T("KTa", [128, S_LEN], BF16)
        KTb = T("KTb", [128, S_LEN], BF16)
        Va = T("Va", [128, NCH, 65], BF16)
        W = dict(ones_bf=T("ones_bf", [128, 128], BF16), eps=T("eps", [128, 1], F32))
        identb = T("identb", [128, 128], BF16)
        mk = T("mk", [128, 256], F32)
        onesf = T("onesf", [128, 128], F32)
        one1 = T("one1", [128, 1], F32)
        rTs = T("rTs", [128, 128], F32)
        oblkb = T("oblkb", [128, 128], BF16)
        gmrs = T("gmrs", [128, 128], F32)
        gqks = T("gqks", [128, 2], F32)
        bFs = T("bFs", [128, 4], F32)
        bTs = T("bTs", [128, NT_T], F32)
        cws = T("cws", [128, 10], F32)
        cbs = T("cbs", [128, 2], F32)
        g1s = T("g1s", [128, NK], F32)
        a1 = T("a1", [128, NK], F32)
        P.pool(lambda e: e.memset(W["ones_bf"][:], 1.0), [], ["ones"])
        P.pool(lambda e: e.memset(W["eps"][:], EPS), [], ["eps"])
        P.pool(lambda e: e.memset(onesf[:], 1.0), [], ["onesf"])
        P.pool(lambda e: e.memset(one1[:], 1.0), [], ["one1"])
        P.pool(lambda e: e.memset(Vaug[:, :, 128:129], 1.0), [], ["Vaug1"])
        P.pool(lambda e: e.memset(Va[:, :, 64:65], 1.0), [], ["Va1"])
        P.pool(lambda e: e.memset(KTa[64:128, :], 0.0), [], ["KTa0"])
        P.pool(lambda e: e.memset(KTb[0:64, :], 0.0), [], ["KTb0"])
        dma(P, identb[:], ident[:, :], writes=["identb"], q="gpsimd")
        dma(P, oblkb[:], oblk[:, :], writes=["oblkb"], q="gpsimd")
        for t_, d_, k_ in [(mk, masks, "mk"), (rTs, rT, "rT"),
                           (gmrs, gmr, "gmr"), (gqks, gqk, "gqk"), (bFs, b_F, "bF"), (bTs, b_T, "bT"),
                           (cws, cw, "cw"), (cbs, cb, "cb"), (g1s, g1, "g1")]:
            dma(P, t_[:], d_[:, :], writes=[k_])

        mod = emit_mod(P, nc, st, pb, cvec, w_ada, b_ada, list(range(16)))
        stt(P, a1[:], mod[:, 8:16], 1.0, g1s[:], ALU.add, ALU.mult, ["mod", "g1"], ["a1"])
        shift1 = mod[:, 0:8]

        with ExitStack() as s1:
            T1 = lambda n, s, d: s1.enter_context(nc.sbuf_tensor(n, s, d))
            wF = T1("wF", [128, NK, 512], BF16)
            wT = T1("wT", [128, NK, NT_T], BF16)
            dma(P, wF[:], w_F.rearrange("(k p) c -> p k c", p=128), writes=["wF"], q="gpsimd")
            dma(P, wT[:], w_T.rearrange("(k p) c -> p k c", p=128), writes=["wT"], q="gpsimd")
            xb = [T1(f"xb{i}", [128, NK, TA], F32) for i in range(2)]
            hTs = [T1(f"hT{i}", [128, NK, TA], BF16) for i in range(2)]
            W.update(sq=T1("sq", [128, NK, TA], BF16), rt=T1("rt", [128, TA], F32), rstd=T1("rstd", [128, TA], F32),
                     tmp=[T1("ntmp0", [128, TA], F32), T1("ntmp1", [128, TA], F32)])
            W2 = dict(W)
            W2.update(sq=T1("sq2", [128, NK, TA], BF16), rt=T1("rt2", [128, TA], F32), rstd=T1("rstd2", [128, TA], F32),
                      tmp=[T1("ntmp20", [128, TA], F32), T1("ntmp21", [128, TA], F32)])
            Wp = [W, W2]
            NR = 3
            ring = [T1(f"ring{i}", [128, NR, TA], F32) for i in range(2)]
            acc = [T1(f"acc{i}", [128, TA], F32) for i in range(2)]
            cs_ = [T1(f"cosb{i}", [128, TA], F32) for i in range(2)]
            sn_ = [T1(f"sinb{i}", [128, TA], F32) for i in range(2)]
            RTMP = [{n_: T1(f"{n_}{i}", [128, TA], BF16 if n_ == "qsq" else F32)
                     for n_ in ("qf", "qsq", "qrt", "qrs", "qu", "qt1", "qt2")} for i in range(2)]
            tmpT = [T1(f"tmpT{i}", [128, NT_T], F32) for i in range(2)]
            xv = xT.rearrange("(k p) t -> p k t", p=128)

            def load_x(tb):
                tsl = slice(tb * TA, (tb + 1) * TA)
                for kh in range(2):
                    dma(P, xb[tb % 2][:, kh * 4:(kh + 1) * 4, :], xv[:, kh * 4:(kh + 1) * 4, tsl],
                        writes=[("xb", tb % 2, k) for k in range(kh * 4, kh * 4 + 4)])

            def conv_block(j, part="da"):
                tsl = slice(j * TA, (j + 1) * TA)
                for qk in range(2):
                    cur = ring[qk][:, j % NR, :]
                    a = acc[qk]
                    ak = ("acc", qk)
                    rk = lambda jj: ("ring", qk, jj % NR)
                    wcol = lambda k: cws[:, qk * 5 + k:qk * 5 + k + 1]
                    if "d" in part:
                        ts(P, a[:], cur, wcol(2), ALU.mult, [rk(j), "cw"], [ak])
                    for k in ((0, 1, 3, 4) if "d" in part else ()):
                        s_ = k - 2
                        if s_ < 0:
                            stt(P, a[:, -s_:TA], ring[qk][:, j % NR, 0:TA + s_], wcol(k), a[:, -s_:TA], ALU.mult, ALU.add,
                                [rk(j), "cw", ak], [ak])
                            if j > 0:
                                stt(P, a[:, 0:-s_], ring[qk][:, (j - 1) % NR, TA + s_:TA], wcol(k), a[:, 0:-s_], ALU.mult, ALU.add,
                                    [rk(j - 1), "cw", ak], [ak])
                        else:
                            stt(P, a[:, 0:TA - s_], ring[qk][:, j % NR, s_:TA], wcol(k), a[:, 0:TA - s_], ALU.mult, ALU.add,
                                [rk(j), "cw", ak], [ak])
                            if j < NB - 1:
                                stt(P, a[:, TA - s_:TA], ring[qk][:, (j + 1) % NR, 0:s_], wcol(k), a[:, TA - s_:TA], ALU.mult, ALU.add,
                                    [rk(j + 1), "cw", ak], [ak])
                    dest = (QmT if qk == 0 else KmT)
                    if "a" in part:
                        act(P, dest[:, tsl], a[:], AF.Silu, [ak, "cb"], [("QKm", qk, j)], bias=cbs[:, qk:qk + 1])

            def rope_norm(pf, pkey, bcol, gcol, dest, dkey, tb, rp):
                tsl = slice(tb * TA, (tb + 1) * TA)
                cb_, sb_ = cs_[tb % 2], sn_[tb % 2]
                R_ = RTMP[rp]
                qf, qsq, qrt, qrs, qu, qt1, qt2 = (R_[n_] for n_ in ("qf", "qsq", "qrt", "qrs", "qu", "qt1", "qt2"))
                kq = lambda n_: (n_, rp)
                pss, psr = pb[5 + rp], pk[5 + rp]

                def fin():
                    if dest is None:
                        tt(P, KTa[0:64, tsl], qt1[0:64, :], qrs[0:64, :], ALU.mult, [kq("qt1"), kq("qrs")], [("KTa", tb)])
                        tt(P, KTb[64:128, tsl], qt1[64:128, :], qrs[64:128, :], ALU.mult, [kq("qt1"), kq("qrs")], [("KTb", tb)])
                    else:
                        tt(P, dest[:, tsl], qt1[:], qrs[:], ALU.mult, [kq("qt1"), kq("qrs")], [(dkey, tb)])
                return [
                    lambda: act(P, qf[:], pf[:, 0:TA], AF.Identity, [pkey, "bF"], [kq("qf")], bias=bFs[:, bcol:bcol + 1]),
                    lambda: act(P, qsq[:], qf[:], AF.Square, [kq("qf")], [kq("qsq")]),
                    lambda: ts(P, qu[:], qf[:], gqks[:, gcol:gcol + 1], ALU.mult, [kq("qf"), "gqk"], [kq("qu")]),
                    lambda: mm_group(P, pss[:, 0:TA], [(oblkb[:], qsq[:])], [kq("qsq"), "oblkb"], [psr]),
                    lambda: tt(P, qt1[:], qu[:], cb_[:], ALU.mult, [kq("qu"), ("cos", tb % 2)], [kq("qt1")]),
                    lambda: act(P, qrt[:], pss[:, 0:TA], AF.Sqrt, [psr, "eps"], [kq("qrt")], bias=W["eps"][:, 0:1], scale=1.0 / 64),
                    lambda: mm_group(P, pss[:, 256:256 + TA], [(rTs[:], qu[:])], [kq("qu"), "rT"], [psr]),
                    lambda: P.dve(lambda e: e.reciprocal(out=qrs[:], in_=qrt[:]), [kq("qrt")], [kq("qrs")]),
                    lambda: tt(P, qt2[:], pss[:, 256:256 + TA], sb_[:], ALU.mult, [psr, ("sin", tb % 2)], [kq("qt2")]),
                    lambda: tt(P, qt1[:], qt1[:], qt2[:], ALU.add, [kq("qt1"), kq("qt2")], [kq("qt1")]),
                    fin,
                ]

            def norm_blk(tb, part):
                x_ = xb[tb % 2]
                hp = tb % 2
                hT = hTs[hp]
                nb_ = 0 if hp == 0 else 7
                emit_norm(P, nc, Wp[hp], lambda k: x_[:, k, :], a1, shift1, [lambda k: hT[:, k, :]],
                          [("xb", tb % 2, k_) for k_ in range(NK)] + ["a1", "mod"], [lambda k: ("hT", hp, k)],
                          pb[nb_], pk[nb_], n=TA, tag=f"n{hp}", xfull=x_[:, :, :], part=part)

            load_x(0)
            load_x(1)
            norm_blk(0, "ab")
            for tb in range(NB):
                tsl = slice(tb * TA, (tb + 1) * TA)
                dma(P, cs_[tb % 2][:], cosT[:, tsl], writes=[("cos", tb % 2)])
                dma(P, sn_[tb % 2][:], sinT[:, tsl], writes=[("sin", tb % 2)])
                if tb + 1 < NB:
                    norm_blk(tb + 1, "a")
                hp = tb % 2
                hT = hTs[hp]
                hk = [("hT", hp, k) for k in range(NK)]
                for fc in range(4):
                    bank = 1 + fc % 2
                    mm_group(P, pb[bank][:, 0:TA], [(wF[:, k, fc * 128:(fc + 1) * 128], hT[:, k, :]) for k in range(NK)],
                             hk + ["wF"], [pk[bank]])
                    if fc < 2:
                        act(P, ring[fc][:, tb % NR, :], pb[bank][:, 0:TA], AF.Identity, [pk[bank], "bF"], [("ring", fc, tb % NR)],
                            bias=bFs[:, fc:fc + 1])
                        if fc == 1 and tb >= 1:
                            conv_block(tb - 1, "d")
                    elif fc == 2:
                        chain_q = rope_norm(pb[bank], pk[bank], 2, 0, QaT, "QaT", tb, 0)
                    else:
                        chain_k = rope_norm(pb[bank], pk[bank], 3, 1, None, "KT", tb, 1)
                for sq_, sk_ in zip(chain_q, chain_k):
                    sq_()
                    sk_()
                if tb + 1 < NB:
                    norm_blk(tb + 1, "b")
                for sub in range(TA // 128):
                    ch = tb * (TA // 128) + sub
                    bank = 3 + sub % 2
                    mm_group(P, pb[bank][:, 0:NT_T], [(hT[:, k, sub * 128:(sub + 1) * 128], wT[:, k, :]) for k in range(NK)],
                             hk + ["wT"], [pk[bank]])
                    tm = tmpT[sub % 2]
                    tk = ("tmpT", sub % 2)
                    tt(P, tm[:], pb[bank][:, 0:NT_T], bTs[:], ALU.add, [pk[bank], "bT"], [tk])
                    cp(P, Vaug[:, ch, 0:128], tm[:, 0:128], [tk], [("Vaug", ch)], eng="scalar")
                    act(P, osig[:, ch, :], tm[:, 128:256], AF.Sigmoid, [tk], [("osig", ch)])
                    cp(P, G[:, ch, :], tm[:, 256:260], [tk], [("G", ch)], eng="scalar")
                    cp(P, Va[:, ch, 0:64], tm[:, 260:324], [tk], [("Va", ch)], eng="scalar")
                if tb >= 1:
                    conv_block(tb - 1, "a")
                if tb + 2 < NB:
                    load_x(tb + 2)
            conv_block(NB - 1)
            P.barrier()

        if do_mlstm:
          with ExitStack() as s2:
            T2 = lambda n, s, d: s2.enter_context(nc.sbuf_tensor(n, s, d))
            hfwd = T2("hfwd", [128, NCH, 128], F32)
            Gk = [("G", ch) for ch in range(NCH)]
            ge = T2("ge", [128, NCH, 2], F32)
            lfn = T2("lfn", [128, NCH, 2], F32)
            dirs = []
            for d_ in range(2):
                dd = {n: T2(f"{n}{d_}", [128, NCH], F32) for n in ("b", "imb", "w", "ws", "flo", "ebl")}
                dirs.append(dd)
            Cst = T2("Cst", [128, 129], F32)
            Cbf = T2("Cbf", [128, 129], BF16)
            Ktok = [T2(f"Ktok{i}", [128, 128], BF16) for i in range(2)]
            Vw = [T2(f"Vw{i}", [128, 129], BF16) for i in range(2)]
            Sp = [T2(f"Sp{i}", [128, 128], BF16) for i in range(2)]
            den = [T2(f"den{i}", [128, 1], F32) for i in range(2)]
            rden = [T2(f"rden{i}", [128, 1], F32) for i in range(2)]
            hs = [T2(f"hs{i}", [128, 128], F32) for i in range(2)]
            hsq = [T2(f"hsq{i}", [128, 128], F32) for i in range(2)]
            ss = [T2(f"ss{i}", [128, 1], F32) for i in range(2)]
            srt = [T2(f"srt{i}", [128, 1], F32) for i in range(2)]
            srn = [T2(f"srn{i}", [128, 1], F32) for i in range(2)]
            yt = [T2(f"yt{i}", [128, 128], F32) for i in range(2)]
            y2 = [T2(f"y2{i}", [128, 128], BF16) for i in range(2)]
            ymb = [T2(f"ymb{i}", [128, 512], BF16) for i in range(2)]
            for d_ in range(2):
                fcol = 1 + 2 * d_
                act(P, ge[:, :, d_], G[:, :, fcol], AF.Exp, Gk, [("ge", d_)], scale=-1.0)
                act(P, lfn[:, :, d_], ge[:, :, d_], AF.Ln, [("ge", d_), "one1"], [("lfn", d_)], bias=one1[:, 0:1])
            for d_ in range(2):
                dd = dirs[d_]
                icol = 2 * d_
                mslice = mk[:, d_ * 128:(d_ + 1) * 128]
                mm_group(P, pb[0][:, 0:NCH], [(mslice, lfn[:, :, d_])], [("lfn", d_), "mk"], [pk[0]])
                mm_group(P, pb[1][:, 0:NCH], [(onesf[:], lfn[:, :, d_])], [("lfn", d_), "onesf"], [pk[1]])
                cp(P, dd["b"][:], pb[0][:, 0:NCH], [pk[0]], [("mb", d_)])
                tt(P, dd["imb"][:], G[:, :, icol], dd["b"][:], ALU.add, Gk + [("mb", d_)], [("imb", d_)])
                act(P, dd["w"][:], dd["imb"][:], AF.Exp, [("imb", d_)], [("w", d_)])
                ts(P, dd["ws"][:], dd["w"][:], MSCALE, ALU.mult, [("w", d_)], [("ws", d_)])
                act(P, dd["flo"][:], dd["b"][:], AF.Exp, [("mb", d_)], [("flo", d_)])
                act(P, dd["ebl"][:], pb[1][:, 0:NCH], AF.Exp, [pk[1]], [("ebl", d_)], scale=-1.0)
            hbwd = T2("hbwd", [128, NCH, 128], F32)
            Cst2 = [Cst, T2("Cst_b", [128, 129], F32)]
            Cbf2 = [Cbf, T2("Cbf_b", [128, 129], BF16)]
            for d_ in range(2):
                P.dve(lambda e, o=Cst2[d_][:]: e.memset(o, 0.0), [], [("Cst", d_)])
                P.dve(lambda e, o=Cbf2[d_][:]: e.memset(o, 0.0), [], [("Cbf", d_)])
            hdir = [hfwd, hbwd]

            def mchunk(d_, c):
                dd = dirs[d_]
                mslice = mk[:, d_ * 128:(d_ + 1) * 128]
                p2 = d_
                Cs, Cb = Cst2[d_], Cbf2[d_]
                csl = slice(c * 128, (c + 1) * 128)
                tb_q = c // (TA // 128)
                qk_keys = [("QKm", 0, tb_q), ("QKm", 1, tb_q)]
                tbank = 2 if d_ == 0 else 7
                P.mm(lambda e, o=pb[tbank][:].bitcast(BF16)[:, 0:128], i_=KmT[:, csl]: e.transpose(o, i_, identb[:]),
                     [("QKm", 1, tb_q), "identb"], [pk[tbank]])
                cp(P, Ktok[p2][:], pb[tbank][:].bitcast(BF16)[:, 0:128], [pk[tbank]], [("Ktok", p2)], eng="scalar")
                act(P, Vw[p2][:], Vaug[:, c, :], AF.Identity, [("Vaug", c), "Vaug1", ("w", d_)], [("Vw", p2)], scale=dd["w"][:, c:c + 1])
                sb_ = 3 + p2
                mm_group(P, pb[sb_][:, 0:128], [(KmT[:, csl], QmT[:, csl])], qk_keys, [pk[sb_]])
                stt(P, Sp[p2][:], pb[sb_][:, 0:128], dd["ws"][:, c:c + 1], mslice, ALU.mult, ALU.mult,
                    [pk[sb_], ("ws", d_), "mk"], [("Sp", p2)])
                ob_ = 5 + p2
                mm_group(P, pb[ob_][:, 0:129], [(QmT[:, csl], Cb[:]), (Sp[p2][:], Vaug[:, c, :])],
                         qk_keys + [("Cbf", d_), ("Sp", p2), ("Vaug", c), "Vaug1"], [pk[ob_]])
                act(P, den[p2][:], pb[ob_][:, 128:129], AF.Abs, [pk[ob_]], [("den", p2)])
                tt(P, den[p2][:], den[p2][:], dd["flo"][:, c:c + 1], ALU.max, [("den", p2), ("flo", d_)], [("den", p2)])
                P.dve(lambda e, o=rden[p2][:], i_=den[p2][:]: e.reciprocal(out=o, in_=i_), [("den", p2)], [("rden", p2)])
                ts(P, hdir[d_][:, c, :], pb[ob_][:, 0:128], rden[p2][:, 0:1], ALU.mult, [pk[ob_], ("rden", p2)], [("hdir", d_, c)])
                mm_group(P, pb[p2][:, 0:129], [(Ktok[p2][:], Vw[p2][:])], [("Ktok", p2), ("Vw", p2)], [pk[p2]])
                ts(P, Cs[:], Cs[:], dd["ebl"][:, c:c + 1], ALU.mult, [("Cst", d_), ("ebl", d_)], [("Cst", d_)])
                stt(P, Cs[:], pb[p2][:, 0:129], dd["ebl"][:, c:c + 1], Cs[:], ALU.mult, ALU.add,
                    [pk[p2], ("ebl", d_), ("Cst", d_)], [("Cst", d_)])
                act(P, Cb[:], Cs[:], AF.Identity, [("Cst", d_)], [("Cbf", d_)], scale=MSCALE)

            for i in range(NCH):
                mchunk(0, i)
                mchunk(1, NCH - 1 - i)
            for c in range(NCH):
                p2 = c % 2
                tt(P, hs[p2][:], hfwd[:, c, :], hbwd[:, c, :], ALU.add, [("hdir", 0, c), ("hdir", 1, c)], [("hs", p2)])
                tt(P, hsq[p2][:], hs[p2][:], hs[p2][:], ALU.mult, [("hs", p2)], [("hsq", p2)])
                red(P, ss[p2][:], hsq[p2][:], ALU.add, [("hsq", p2)], [("ss", p2)])
                act(P, srt[p2][:], ss[p2][:], AF.Sqrt, [("ss", p2), "eps"], [("srt", p2)], bias=W["eps"][:, 0:1], scale=1.0 / 128)
                P.dve(lambda e, o=srn[p2][:], i_=srt[p2][:]: e.reciprocal(out=o, in_=i_), [("srt", p2)], [("srn", p2)])
                stt(P, yt[p2][:], hs[p2][:], srn[p2][:, 0:1], gmrs[:], ALU.mult, ALU.mult,
                    [("hs", p2), ("srn", p2), "gmr"], [("yt", p2)])
                tt(P, y2[p2][:], yt[p2][:], osig[:, c, :], ALU.mult, [("yt", p2), ("osig", c)], [("y2", p2)])
                ybank = 3 + p2
                P.mm(lambda e, o=pb[ybank][:].bitcast(BF16)[:, 0:128], i_=y2[p2][:]: e.transpose(o, i_, identb[:]),
                     [("y2", p2), "identb"], [pk[ybank]])
                grp = c // 4
                yb = ymb[grp % 2]
                cp(P, yb[:, (c % 4) * 128:(c % 4 + 1) * 128], pb[ybank][:].bitcast(BF16)[:, 0:128], [pk[ybank]],
                   [("ymb", grp % 2, c % 4)], eng="scalar")
                if c % 4 == 3:
                    dma(P, ymT[:, grp * 512:(grp + 1) * 512], yb[:], reads=[("ymb", grp % 2, q_) for q_ in range(4)])
            P.barrier()

        with ExitStack() as s3:
            T3 = lambda n, s, d: s3.enter_context(nc.sbuf_tensor(n, s, d))
            pT = [T3(f"pT{i}", [128, 512], BF16) for i in range(3)]
            osb = [T3(f"osb{i}", [64, 512], F32) for i in range(2)]
            rec = T3("rec", [128, 512], F32)
            yab = [T3(f"yab{i}", [64, 512], BF16) for i in range(2)]
            NTQ = 512 // TA
            jobs = [(qb, h) for qb in range(att_qblocks) for h in range(2)]
            steps = [(ji, kc) for ji in range(len(jobs)) for kc in range(NCH)]
            LOOK = 2

            def emit_qk(i):
                ji, kc = steps[i]
                qb, h = jobs[ji]
                hsl = slice(h * 64, (h + 1) * 64)
                qsl = slice(qb * 512, (qb + 1) * 512)
                ksl = slice(kc * 128, (kc + 1) * 128)
                sb_ = i % 3
                KT_ = KTa if h == 0 else KTb
                mm_group(P, pb[sb_][:], [(KT_[:, ksl], QaT[:, qsl])],
                         [("KTa" if h == 0 else "KTb", kc // (TA // 128)), "KTa0", "KTb0"]
                         + [("QaT", qb * NTQ + i_) for i_ in range(NTQ)], [pk[sb_]])
                act(P, pT[sb_][:], pb[sb_][:], AF.Exp, [pk[sb_]], [("pT", sb_)], scale=ASCALE)

            def emit_pv(i):
                ji, kc = steps[i]
                qb, h = jobs[ji]
                hsl = slice(h * 64, (h + 1) * 64)
                qsl = slice(qb * 512, (qb + 1) * 512)
                sb_ = i % 3
                ob_ = 3 + ji % 2
                P.mm(lambda e, o=pb[ob_][0:65, :], l_=Va[:, kc, :], r_=pT[sb_][:], a_=(kc == 0), z_=(kc == NCH - 1):
                     e.matmul(o, lhsT=l_, rhs=r_, start=a_, stop=z_),
                     [("Va", kc), "Va1", ("pT", sb_)], [pk[ob_]])
                if kc == NCH - 1:
                    jb = ji % 2
                    P.dve(lambda e, o=rec[64:65, :], i_=pb[ob_][64:65, :]: e.reciprocal(out=o, in_=i_), [pk[ob_]], ["rec"])
                    mm_group(P, pb[5][0:64, :], [(onesf[64:65, 0:64], rec[64:65, :])], ["rec", "onesf"], [pk[5]])
                    cp(P, osb[jb][:], pb[ob_][0:64, :], [pk[ob_]], [("osb", jb)], eng="gpsimd" if False else "vector")
                    tt(P, yab[jb][:], osb[jb][:], pb[5][0:64, :], ALU.mult, [("osb", jb), pk[5]], [("yab", jb)])
                    dma(P, yaT[hsl, qsl], yab[jb][:], reads=[("yab", jb)])

            for i in range(len(steps) + LOOK):
                if i < len(steps):
                    emit_qk(i)
                if i - LOOK >= 0:
                    emit_pv(i - LOOK)
            P.barrier()
        P.emit()
    return nc


OFF = dict(mq=0, mk=512, mv=1024, mo=1536, gates=2048, aq=2064, ak=2576, av=2704, gm=2832, ga=3856, end=4880)


def _pk(v, n):
    return np.ascontiguousarray(np.asarray(v, np.float32).reshape(n, 128).T)


def _consts():
    esel = np.zeros((32, 32, 128), np.float32)
    for e in range(32):
        esel[e, e, :] = 1.0
    return dict(esel=esel.reshape(32, 32 * 128), ident=np.eye(128, dtype=np.float32))


def prep_B(inp, l, b, r, xT_b, ymT_b, yaT_b):
    tok = slice(r * NT_B, (r + 1) * NT_B)
    w_in = inp["w_in"][l]
    b_in = inp["b_in"][l]
    m = dict(
        xT=np.ascontiguousarray(xT_b[:, tok]),
        ymT=np.ascontiguousarray(ymT_b[:, tok]),
        yaT=np.ascontiguousarray(yaT_b[:, tok]),
        cvec=_pk(inp["c"][b], 8),
        w_ada=np.ascontiguousarray(inp["w_ada"][l]),
        b_ada=_pk(inp["b_ada"][l], 48),
        g1=_pk(inp["norm1_g"][l], 8), g2=_pk(inp["norm2_g"][l], 8), gf=_pk(inp["final_norm_g"], 8),
        w_g=np.ascontiguousarray(w_in[:, OFF["gm"]:OFF["end"]]),
        b_g=_pk(b_in[OFF["gm"]:OFF["end"]], 16),
        w_bm=np.ascontiguousarray(inp["w_branch_m"][l]),
        w_ba=np.ascontiguousarray(inp["w_branch_a"][l]),
        w_o=np.ascontiguousarray(inp["w_out"][l]),
        w_r=np.ascontiguousarray(np.concatenate([inp["w_router_group"][l], inp["w_router_expert"][l]], axis=1)),
        b_r=np.ascontiguousarray(np.broadcast_to(
            np.concatenate([inp["b_router_group"][l], inp["b_router_expert"][l]])[None, :], (128, 36))),
        w_gate=np.ascontiguousarray(inp["w_gate"][l]),
        w_up=np.ascontiguousarray(inp["w_up"][l]),
        w_down=np.ascontiguousarray(inp["w_down"][l]),
    )
    m.update(_consts())
    return m


def _rope_consts():
    rows = S_LEN // 64
    row = np.repeat(np.arange(rows, dtype=np.float32), 64)
    col = np.tile(np.arange(64, dtype=np.float32), rows)
    half = 32
    inv_freq = (np.float32(10000.0) ** (-np.arange(0, half, 2, dtype=np.float32) / np.float32(half))).astype(np.float32)
    ang_r = (row[:, None] * inv_freq).astype(np.float32)
    ang_c = (col[:, None] * inv_freq).astype(np.float32)
    cosT = np.zeros((64, S_LEN), np.float32)
    sinT = np.zeros((64, S_LEN), np.float32)
    for i in range(64):
        ang = ang_r if i < 32 else ang_c
        cosT[i] = np.cos(ang[:, i % 16])
        sinT[i] = np.sin(ang[:, i % 16])
    R = np.zeros((64, 64), np.float32)
    for i in range(64):
        if i % 32 < 16:
            R[i, i + 16] = -1.0
        else:
            R[i, i - 16] = 1.0
    R2 = np.zeros((128, 128), np.float32)
    R2[:64, :64] = R
    R2[64:, 64:] = R
    oblk = np.zeros((128, 128), np.float32)
    oblk[:64, :64] = 1.0
    oblk[64:, 64:] = 1.0
    masks = np.concatenate([np.triu(np.ones((128, 128), np.float32)), np.tril(np.ones((128, 128), np.float32))], axis=1)
    return dict(cosT=np.ascontiguousarray(np.tile(cosT, (2, 1))), sinT=np.ascontiguousarray(np.tile(sinT, (2, 1))),
                rT=np.ascontiguousarray(R2.T), oblk=oblk, masks=np.ascontiguousarray(masks),
                ident=np.eye(128, dtype=np.float32))


_ROPE = None


def prep_A(inp, l, b, r, xT_b):
    global _ROPE
    if _ROPE is None:
        _ROPE = _rope_consts()
    w_in = inp["w_in"][l]
    b_in = inp["b_in"][l]
    kv = r // 2
    fcols = np.concatenate([np.arange(OFF["mq"] + r * 128, OFF["mq"] + (r + 1) * 128),
                            np.arange(OFF["mk"] + r * 128, OFF["mk"] + (r + 1) * 128),
                            np.arange(OFF["aq"] + r * 128, OFF["aq"] + (r + 1) * 128),
                            np.arange(OFF["ak"] + kv * 64, OFF["ak"] + (kv + 1) * 64),
                            np.arange(OFF["ak"] + kv * 64, OFF["ak"] + (kv + 1) * 64)])
    tcols = np.concatenate([np.arange(OFF["mv"] + r * 128, OFF["mv"] + (r + 1) * 128),
                            np.arange(OFF["mo"] + r * 128, OFF["mo"] + (r + 1) * 128),
                            OFF["gates"] + np.arange(4) * 4 + r,
                            np.arange(OFF["av"] + kv * 64, OFF["av"] + (kv + 1) * 64)])
    cwl = inp["conv_w"][l][:, 0, :]
    cw = np.zeros((128, 10), np.float32)
    cb = np.zeros((128, 2), np.float32)
    for qk in range(2):
        ch = slice(qk * 512 + r * 128, qk * 512 + (r + 1) * 128)
        cw[:, qk * 5:(qk + 1) * 5] = cwl[:, ch].T
        cb[:, qk] = inp["conv_b"][l][ch]
    m = dict(
        xT=xT_b,
        cvec=_pk(inp["c"][b], 8),
        w_ada=np.ascontiguousarray(inp["w_ada"][l][:, 0:2 * D]),
        b_ada=_pk(inp["b_ada"][l][0:2 * D], 16),
        g1=_pk(inp["norm1_g"][l], 8),
        w_F=np.ascontiguousarray(w_in[:, fcols]),
        b_F=_pk(b_in[fcols], 4),
        w_T=np.ascontiguousarray(w_in[:, tcols]),
        b_T=np.ascontiguousarray(np.broadcast_to(b_in[tcols][None, :], (128, NT_T))),
        cw=cw, cb=cb,
        gmr=np.ascontiguousarray(np.broadcast_to(inp["mlstm_norm_g"][l][r * 128:(r + 1) * 128][None, :], (128, 128))),
        gqk=np.ascontiguousarray(np.stack([np.tile(inp["q_norm_g"][l], 2), np.tile(inp["k_norm_g"][l], 2)], axis=1)),
    )
    m.update(_ROPE)
    return m


def kernel(**inputs):
    inp = {k: np.asarray(v) for k, v in inputs.items()}
    cores = list(range(8))
    xT = [np.ascontiguousarray(inp["x"][b].T) for b in range(2)]
    for l in range(2):
        ncA = build_A()
        resA = run_bass_kernel_spmd(ncA, [prep_A(inp, l, c // 4, c % 4, xT[c // 4]) for c in cores], core_ids=cores)
        ymT = [np.concatenate([resA.results[b * 4 + r]["ymT"] for r in range(4)], axis=0) for b in range(2)]
        yaT = [np.concatenate([resA.results[b * 4 + r]["yaT"] for r in range(4)], axis=0) for b in range(2)]
        del resA
        ncB = build_B(last=(l == 1))
        resB = run_bass_kernel_spmd(ncB, [prep_B(inp, l, c // 4, c % 4, xT[c // 4], ymT[c // 4], yaT[c // 4]) for c in cores],
                                    core_ids=cores)
        xT = [np.concatenate([resB.results[b * 4 + r]["outT"] for r in range(4)], axis=1) for b in range(2)]
        del resB
    return np.ascontiguousarray(np.stack([xT[b].T for b in range(2)])).astype(np.float32)
```

```python
import numpy as np
import ml_dtypes
from contextlib import ExitStack
import concourse.bass as bass
import concourse.mybir as mybir
from concourse.bass_utils import run_bass_kernel_spmd

F32 = mybir.dt.float32
BF16 = mybir.dt.bfloat16
AF = mybir.ActivationFunctionType
ALU = mybir.AluOpType
AX = mybir.AxisListType

ENGS = ("tensor", "vector", "scalar", "gpsimd", "sync")
GEN = 30000


class Prog:
    def __init__(self, nc, n_dma_slots=8, same_engine_sync=True):
        self.nc = nc
        self.ops = {e: [] for e in ENGS}
        self.last_writer = {}
        self.readers = {}
        self.n_dma_slots = n_dma_slots
        self.dma_count = {e: 0 for e in ENGS}
        self.same_engine_sync = same_engine_sync
        self.pending_dma = []

    def op(self, eng, fn, reads=(), writes=(), dma=False, nosync_same=False):
        idx = len(self.ops[eng])
        deps = set()
        for k in reads:
            w = self.last_writer.get(k)
            if w is not None:
                deps.add(w)
        for k in writes:
            w = self.last_writer.get(k)
            if w is not None:
                deps.add(w)
            for r in self.readers.get(k, ()):
                deps.add(r)
        me = (eng, idx)
        deps.discard(me)
        slot = None
        if dma:
            slot = self.dma_count[eng] % self.n_dma_slots
            self.dma_count[eng] += 1
            self.pending_dma.append(me)
        elif nosync_same or not self.same_engine_sync:
            deps = {d for d in deps if d[0] != eng or self.ops[d[0]][d[1]]["dma"]}
        rec = dict(eng=eng, fn=fn, deps=deps, dma=dma, slot=slot, signal=False)
        self.ops[eng].append(rec)
        for d in deps:
            self.ops[d[0]][d[1]]["signal"] = True
        for k in reads:
            self.readers.setdefault(k, []).append(me)
        for k in writes:
            self.last_writer[k] = me
            self.readers[k] = []
        return me

    def mm(self, fn, reads=(), writes=()):
        return self.op("tensor", fn, reads, writes, nosync_same=True)

    def dve(self, fn, reads=(), writes=()):
        return self.op("vector", fn, reads, writes)

    def act(self, fn, reads=(), writes=()):
        return self.op("scalar", fn, reads, writes)

    def pool(self, fn, reads=(), writes=()):
        return self.op("gpsimd", fn, reads, writes)

    def dma(self, fn, reads=(), writes=(), q="sync"):
        return self.op(q, fn, reads, writes, dma=True)

    def barrier(self):
        lasts = []
        for e in ENGS:
            for i in range(len(self.ops[e]) - 1, -1, -1):
                r = self.ops[e][i]
                if r["fn"] is not None and not r["dma"]:
                    lasts.append((e, i))
                    break
        deps = set(lasts) | set(self.pending_dma)
        self.pending_dma = []
        for d in deps:
            self.ops[d[0]][d[1]]["signal"] = True
        for e in ENGS:
            self.ops[e].append(dict(eng=e, fn=None, deps={d for d in deps}, dma=False, slot=None, signal=False))

    def emit(self):
        nc = self.nc
        ngen = {}
        final_slot_counts = {}
        for e in ENGS:
            c = 0
            slot_counts = [0] * self.n_dma_slots
            for r in self.ops[e]:
                if r["dma"]:
                    slot_counts[r["slot"]] += 1
                    r["slot_prev"] = slot_counts[r["slot"]] - 1
                    r["sig"] = ("dma", e, r["slot"], 16 * slot_counts[r["slot"]])
                elif r["signal"]:
                    g, v = divmod(c, GEN)
                    r["sig"] = ("eng", e, g, v + 1)
                    c += 1
                else:
                    r["sig"] = None
            ngen[e] = (c + GEN - 1) // GEN if c else 0
            final_slot_counts[e] = slot_counts
        with ExitStack() as st:
            sems = {}
            for e in ENGS:
                for g in range(ngen[e]):
                    sems[("eng", e, g)] = st.enter_context(nc.semaphore(f"s_{e}_{g}"))
                if self.dma_count[e]:
                    for s in range(self.n_dma_slots):
                        sems[("dma", e, s)] = st.enter_context(nc.semaphore(f"d_{e}_{s}"))
            block = st.enter_context(nc.Block())

            def make_body(e):
                def body(eng):
                    waited = {}
                    for r in self.ops[e]:
                        need = {}
                        for d in r["deps"]:
                            if d[0] == e and r["fn"] is None and not self.ops[d[0]][d[1]]["dma"]:
                                continue
                            sig = self.ops[d[0]][d[1]]["sig"]
                            key = sig[:3]
                            need[key] = max(need.get(key, 0), sig[3])
                        if r["dma"] and r["slot_prev"] > 0:
                            key = ("dma", e, r["slot"])
                            need[key] = max(need.get(key, 0), 16 * r["slot_prev"])
                        for key, v in need.items():
                            if key[0] == "eng":
                                best = waited.get((key[0], key[1]), (-1, 0))
                                if (key[2], v) <= best:
                                    continue
                                waited[(key[0], key[1])] = (key[2], v)
                            else:
                                if waited.get(key, 0) >= v:
                                    continue
                                waited[key] = v
                            eng.wait_ge(sems[key], v)
                        if r["fn"] is None:
                            continue
                        ins = r["fn"](eng)
                        if r["sig"] is not None:
                            ins.then_inc(sems[r["sig"][:3]], 16 if r["dma"] else 1)
                    if e == "sync":
                        for q in ENGS:
                            if self.dma_count[q]:
                                for s in range(self.n_dma_slots):
                                    cnt = final_slot_counts[q][s]
                                    if cnt:
                                        eng.wait_ge(sems[("dma", q, s)], 16 * cnt)
                return body

            for e in ENGS:
                getattr(block, e)(make_body(e))


def mm_group(P, out_ap, pairs, reads, writes):
    n = len(pairs)

    def fn(e):
        ins = None
        for i, (l, r) in enumerate(pairs):
            ins = e.matmul(out_ap, lhsT=l, rhs=r, start=(i == 0), stop=(i == n - 1))
        return ins
    return P.mm(fn, reads, writes)


def dma(P, out_ap, in_ap, reads=(), writes=(), q="sync"):
    return P.dma(lambda e: e.dma_start(out=out_ap, in_=in_ap), reads, writes, q=q)


def act(P, out_ap, in_ap, func, reads, writes, bias=None, scale=None):
    kw = {}
    if bias is not None:
        kw["bias"] = bias
    if scale is not None:
        kw["scale"] = scale
    return P.act(lambda e: e.activation(out=out_ap, in_=in_ap, func=func, **kw), reads, writes)


def tt(P, out_ap, a, b, op, reads, writes, eng="vector"):
    return P.op(eng, lambda e: e.tensor_tensor(out=out_ap, in0=a, in1=b, op=op), reads, writes)


def ts(P, out_ap, a, s1, op0, reads, writes, s2=None, op1=None, eng="vector"):
    if op1 is None:
        return P.op(eng, lambda e: e.tensor_scalar(out=out_ap, in0=a, scalar1=s1, scalar2=None, op0=op0), reads, writes)
    return P.op(eng, lambda e: e.tensor_scalar(out=out_ap, in0=a, scalar1=s1, scalar2=s2, op0=op0, op1=op1), reads, writes)


def stt(P, out_ap, a, s, b, op0, op1, reads, writes):
    return P.dve(lambda e: e.scalar_tensor_tensor(out=out_ap, in0=a, scalar=s, in1=b, op0=op0, op1=op1), reads, writes)


def cp(P, out_ap, in_ap, reads, writes, eng="vector"):
    if eng == "scalar":
        return P.op(eng, lambda e: e.activation(out=out_ap, in_=in_ap, func=AF.Identity), reads, writes)
    return P.op(eng, lambda e: e.tensor_copy(out=out_ap, in_=in_ap), reads, writes)


def red(P, out_ap, in_ap, op, reads, writes):
    return P.dve(lambda e: e.tensor_reduce(out=out_ap, in_=in_ap, axis=AX.X, op=op), reads, writes)


D = 1024
NK = 8
TB = 512
EPS = 1e-6


def emit_mod(P, nc, st, pb, cvec, w_ada, b_ada, col_chunks, name="mod"):
    T = lambda n, s, d: st.enter_context(nc.sbuf_tensor(n, s, d))
    ncol = len(col_chunks)
    cs = T(name + "_cs", [128, NK], F32)
    css = T(name + "_css", [128, NK], F32)
    nch = max(col_chunks) + 1
    bsb = T(name + "_b", [128, nch], F32)
    mod = T(name, [128, nch], F32)
    dma(P, cs[:], cvec[:, :], writes=[name + "cs"])
    dma(P, bsb[:], b_ada[:, :], writes=[name + "b"])
    act(P, css[:], cs[:], AF.Silu, [name + "cs"], [name + "css"])
    wv = w_ada.rearrange("(k p) c -> p k c", p=128)
    with ExitStack() as st2:
        wa = [st2.enter_context(nc.sbuf_tensor(f"{name}_wa{i}", [128, NK, 768], F32)) for i in range(2)]
        pieces = []
        cur = []
        for j in col_chunks:
            if cur and (j != cur[-1] + 1 or len(cur) == 6):
                pieces.append(cur)
                cur = []
            cur.append(j)
        if cur:
            pieces.append(cur)
        for pi, piece in enumerate(pieces):
            buf = wa[pi % 2]
            key = (name + "wa", pi % 2)
            c0 = piece[0] * 128
            n = len(piece) * 128
            for kh in range(2):
                dma(P, buf[:, kh * 4:(kh + 1) * 4, 0:n], wv[:, kh * 4:(kh + 1) * 4, c0:c0 + n], writes=[key],
                    q=("sync" if kh == 0 else "gpsimd"))
            for jj, j in enumerate(piece):
                pairs = [(buf[:, k, jj * 128:(jj + 1) * 128], css[:, k:k + 1]) for k in range(NK)]
                mm_group(P, pb[0][:, j:j + 1], pairs, [key, name + "css"], ["pb0"])
        for j in col_chunks:
            tt(P, mod[:, j:j + 1], pb[0][:, j:j + 1], bsb[:, j:j + 1], ALU.add, ["pb0", name + "b"], [name])
        P.barrier()
    return mod


def emit_norm(P, nc, W, xk, a_t, shift_t, outs, xkeys, okeys, pbank, pkey, n=TB, tag="n", xfull=None, part="ab"):
    sq, rt, rstd, tmp = W["sq"], W["rt"], W["rstd"], W["tmp"]
    sqk = [(tag + "sq", k) for k in range(NK)]
    if "a" in part:
        if xfull is not None:
            act(P, sq[:, :, 0:n], xfull, AF.Square, xkeys, sqk)
        else:
            for k in range(NK):
                act(P, sq[:, k, 0:n], xk(k), AF.Square, xkeys, [sqk[k]])
    if "b" not in part:
        return
    pairs = [(W["ones_bf"][:], sq[:, k, 0:n]) for k in range(NK)]
    mm_group(P, pbank[:, 0:n], pairs, sqk + ["ones"], [pkey])
    act(P, rt[:, 0:n], pbank[:, 0:n], AF.Sqrt, [pkey, "eps"], [tag + "rt"], bias=W["eps"][:, 0:1], scale=1.0 / D)
    P.dve(lambda e: e.reciprocal(out=rstd[:, 0:n], in_=rt[:, 0:n]), [tag + "rt"], [tag + "rstd"])
    for k in range(NK):
        tb_ = tmp[k % 2]
        tk_ = (tag + "ntmp", k % 2)
        tt(P, tb_[:, 0:n], xk(k), rstd[:, 0:n], ALU.mult, xkeys + [tag + "rstd"], [tk_])
        for oi, ofn in enumerate(outs):
            if shift_t is not None:
                act(P, ofn(k), tb_[:, 0:n], AF.Identity, [tk_], [okeys[oi](k)],
                    bias=shift_t[:, k:k + 1], scale=a_t[:, k:k + 1])
            else:
                act(P, ofn(k), tb_[:, 0:n], AF.Identity, [tk_], [okeys[oi](k)],
                    scale=a_t[:, k:k + 1])


NT_B = 2048
NE = 32
FH = 512


def build_B(last, n_experts=NE):
    nc = bass.Bass("TRN2", target_bir_lowering=False)

    def din(name, shape, dt=F32):
        return nc.dram_tensor(name, shape, dt, kind="ExternalInput").ap()
    xT = din("xT", [D, NT_B])
    ymT = din("ymT", [512, NT_B], BF16)
    yaT = din("yaT", [512, NT_B], BF16)
    cvec = din("cvec", [128, NK])
    w_ada = din("w_ada", [D, 6 * D])
    b_ada = din("b_ada", [128, 48])
    g1 = din("g1", [128, NK])
    g2 = din("g2", [128, NK])
    gf = din("gf", [128, NK])
    w_g = din("w_g", [D, 2 * D])
    b_g = din("b_g", [128, 16])
    w_bm = din("w_bm", [512, D])
    w_ba = din("w_ba", [512, D])
    w_o = din("w_o", [D, D])
    w_r = din("w_r", [D, 36])
    b_r = din("b_r", [128, 36])
    w_gate = din("w_gate", [NE, D, FH])
    w_up = din("w_up", [NE, D, FH])
    w_down = din("w_down", [NE, FH, D])
    esel = din("esel", [32, 32 * 128])
    ident = din("ident", [128, 128])
    outT = nc.dram_tensor("outT", [D, NT_B], F32, kind="ExternalOutput").ap()
    NTB = NT_B // TB

    with ExitStack() as st:
        T = lambda n, s, d: st.enter_context(nc.sbuf_tensor(n, s, d))
        P = Prog(nc, same_engine_sync=SES_B)
        pb = [st.enter_context(nc.psum_tensor(f"pb{i}", [128, 512], F32)) for i in range(8)]
        pk = [f"pb{i}" for i in range(8)]
        x1T = T("x1T", [128, NK, NT_B], F32)
        W = dict(ones_bf=T("ones_bf", [128, 128], BF16), eps=T("eps", [128, 1], F32),
                 sq=T("sq", [128, NK, TB], BF16), rt=T("rt", [128, TB], F32), rstd=T("rstd", [128, TB], F32),
                 tmp=[T("ntmp0", [128, TB], F32), T("ntmp1", [128, TB], F32)])
        identf = T("identf", [128, 128], F32)
        g1s, g2s, gfs = T("g1s", [128, NK], F32), T("g2s", [128, NK], F32), T("gfs", [128, NK], F32)
        a1, a2 = T("a1", [128, NK], F32), T("a2", [128, NK], F32)
        bgs = T("bgs", [128, 16], F32)
        brs = T("brs", [128, 36], F32)
        wr = T("wr", [128, NK, 36], F32)
        P.pool(lambda e: e.memset(W["ones_bf"][:], 1.0), [], ["ones"])
        P.pool(lambda e: e.memset(W["eps"][:], EPS), [], ["eps"])
        dma(P, identf[:], ident[:, :], writes=["ident"])
        dma(P, g1s[:], g1[:, :], writes=["g1"])
        dma(P, g2s[:], g2[:, :], writes=["g2"])
        dma(P, gfs[:], gf[:, :], writes=["gf"])
        dma(P, bgs[:], b_g[:, :], writes=["bg"])
        dma(P, brs[:], b_r[:, :], writes=["br"])
        dma(P, wr[:], w_r.rearrange("(k p) c -> p k c", p=128), writes=["wr"])
        xv = xT.rearrange("(k p) t -> p k t", p=128)
        ymv = ymT.rearrange("(k p) t -> p k t", p=128)
        yav = yaT.rearrange("(k p) t -> p k t", p=128)
        for tb in range(NTB):
            for kh in range(2):
                dma(P, x1T[:, kh * 4:(kh + 1) * 4, tb * TB:(tb + 1) * TB], xv[:, kh * 4:(kh + 1) * 4, tb * TB:(tb + 1) * TB],
                    writes=[("x1T", k, tb) for k in range(kh * 4, kh * 4 + 4)])

        mod = emit_mod(P, nc, st, pb, cvec, w_ada, b_ada, list(range(48)))
        stt(P, a1[:], mod[:, 8:16], 1.0, g1s[:], ALU.add, ALU.mult, ["mod", "g1"], ["a1"])
        stt(P, a2[:], mod[:, 32:40], 1.0, g2s[:], ALU.add, ALU.mult, ["mod", "g2"], ["a2"])
        shift1, gate1, shift2, gate2 = mod[:, 0:8], mod[:, 16:24], mod[:, 24:32], mod[:, 40:48]

        with ExitStack() as s1:
            T1 = lambda n, s, d: s1.enter_context(nc.sbuf_tensor(n, s, d))
            wg_ = T1("wg_", [128, NK, 2 * D], BF16)
            wbm = T1("wbm", [128, 4, D], BF16)
            wba = T1("wba", [128, 4, D], BF16)
            wo = T1("wo", [128, NK, D], BF16)
            wgv = w_g.rearrange("(k p) c -> p k c", p=128)
            for k in range(NK):
                dma(P, wg_[:, k, :], wgv[:, k, :], writes=[("wg_", k)], q="gpsimd")
            dma(P, wbm[:], w_bm.rearrange("(k p) c -> p k c", p=128), writes=["wbm"], q="gpsimd")
            dma(P, wba[:], w_ba.rearrange("(k p) c -> p k c", p=128), writes=["wba"], q="gpsimd")
            wov = w_o.rearrange("(k p) c -> p k c", p=128)
            for k in range(0, NK, 2):
                dma(P, wo[:, k:k + 2, :], wov[:, k:k + 2, :], writes=[("wo", k), ("wo", k + 1)], q="gpsimd")
            h1 = T1("h1", [128, NK, TB], BF16)
            ymb = [T1(f"ymb{i}", [128, 4, TB], BF16) for i in range(2)]
            yab = [T1(f"yab{i}", [128, 4, TB], BF16) for i in range(2)]
            merged = T1("merged", [128, NK, TB], BF16)
            sg = [[T1(f"sg{i}{j}", [128, TB], F32) for j in range(2)] for i in range(2)]
            t12 = [[T1(f"t12{i}{j}", [128, TB], F32) for j in range(2)] for i in range(2)]
            for tb in range(NTB):
                tsl = slice(tb * TB, (tb + 1) * TB)
                b = tb % 2
                dma(P, ymb[b][:], ymv[:, :, tsl], writes=[("ymb", b)])
                dma(P, yab[b][:], yav[:, :, tsl], writes=[("yab", b)])
                emit_norm(P, nc, W, lambda k: x1T[:, k, tsl], a1, shift1,
                          [lambda k: h1[:, k, :]], [("x1T", k_, tb) for k_ in range(NK)] + ["a1", "mod"], [lambda k: ("h1", k)],
                          pb[0], "pb0")
                for dc in range(NK):
                    par = dc % 2
                    base = 4 * par
                    csl = slice(dc * 128, (dc + 1) * 128)
                    hk = [("h1", k) for k in range(NK)]
                    mm_group(P, pb[base][:], [(wg_[:, k, csl], h1[:, k, :]) for k in range(NK)],
                             hk + [("wg_", k) for k in range(NK)], [pk[base]])
                    mm_group(P, pb[base + 1][:], [(wg_[:, k, D + dc * 128:D + (dc + 1) * 128], h1[:, k, :]) for k in range(NK)],
                             hk + [("wg_", k) for k in range(NK)], [pk[base + 1]])
                    mm_group(P, pb[base + 2][:], [(wbm[:, k, csl], ymb[b][:, k, :]) for k in range(4)],
                             ["wbm", ("ymb", b)], [pk[base + 2]])
                    mm_group(P, pb[base + 3][:], [(wba[:, k, csl], yab[b][:, k, :]) for k in range(4)],
                             ["wba", ("yab", b)], [pk[base + 3]])
                    act(P, sg[par][0][:], pb[base][:], AF.Sigmoid, [pk[base], "bg"], [("sg", par, 0)], bias=bgs[:, dc:dc + 1])
                    act(P, sg[par][1][:], pb[base + 1][:], AF.Sigmoid, [pk[base + 1], "bg"], [("sg", par, 1)], bias=bgs[:, 8 + dc:9 + dc])
                    tt(P, t12[par][0][:], sg[par][0][:], pb[base + 2][:], ALU.mult, [("sg", par, 0), pk[base + 2]], [("t12", par, 0)])
                    tt(P, t12[par][1][:], sg[par][1][:], pb[base + 3][:], ALU.mult, [("sg", par, 1), pk[base + 3]], [("t12", par, 1)])
                    tt(P, merged[:, dc, :], t12[par][0][:], t12[par][1][:], ALU.add, [("t12", par, 0), ("t12", par, 1)],
                       [("merged", dc)])
                for dc in range(NK):
                    bank = dc % 2
                    csl = slice(dc * 128, (dc + 1) * 128)
                    mm_group(P, pb[bank][:], [(wo[:, k, csl], merged[:, k, :]) for k in range(NK)],
                             [("merged", k) for k in range(NK)] + [("wo", k) for k in range(NK)], [pk[bank]])
                    stt(P, x1T[:, dc, tsl], pb[bank][:], gate1[:, dc:dc + 1], x1T[:, dc, tsl], ALU.mult, ALU.add,
                        [pk[bank], "mod", ("x1T", dc, tb)], [("x1T", dc, tb)])
            P.barrier()

        s23 = st.enter_context(ExitStack())
        h2T = s23.enter_context(nc.sbuf_tensor("h2T", [128, NK, NT_B], BF16))
        combT = s23.enter_context(nc.sbuf_tensor("combT", [32, NT_B], F32))
        with ExitStack() as s2:
            T2 = lambda n, s, d: s2.enter_context(nc.sbuf_tensor(n, s, d))
            h2f = T2("h2f", [128, NK, TB], F32)
            RR = [{n: T2(f"r{q_}_" + n, [128, s_], F32) for n, s_ in
                   [("lg", 36), ("gmax", 1), ("ngmax", 1), ("eg", 4), ("ssum", 1), ("ptop", 1), ("mg", 4), ("pen", 4),
                    ("lem", 32), ("e1", 1), ("m1", 32), ("lem2", 32), ("e2", 1), ("m2", 32), ("d", 1), ("s2", 1),
                    ("w2", 1), ("w1", 1), ("comb", 32), ("comb2", 32)]} for q_ in range(2)]

            def router_chain(tb, sub):
                R = RR[sub % 2]
                rk_ = lambda n_: (n_, sub % 2)
                ssl = slice(sub * 128, (sub + 1) * 128)
                bank = 1 + sub % 2
                tok = slice(tb * TB + sub * 128, tb * TB + (sub + 1) * 128)
                tps = pb[3 + sub % 2][0:32, 0:128]
                tkey = pk[3 + sub % 2]
                lemk = [rk_(("lem", g)) for g in range(4)]
                st_ = [
                    lambda: mm_group(P, pb[bank][:, 0:36], [(h2f[:, k, ssl], wr[:, k, :]) for k in range(NK)],
                                     [("h2f", k) for k in range(NK)] + ["wr"], [pk[bank]]),
                    lambda: tt(P, R["lg"][:], pb[bank][:, 0:36], brs[:], ALU.add, [pk[bank], "br"], [rk_("lg")]),
                    lambda: red(P, R["gmax"][:], R["lg"][:, 0:4], ALU.max, [rk_("lg")], [rk_("gmax")]),
                    lambda: ts(P, R["ngmax"][:], R["gmax"][:], -1.0, ALU.mult, [rk_("gmax")], [rk_("ngmax")]),
                    lambda: act(P, R["eg"][:], R["lg"][:, 0:4], AF.Exp, [rk_("lg"), rk_("ngmax")], [rk_("eg")], bias=R["ngmax"][:, 0:1]),
                    lambda: ts(P, R["mg"][:], R["lg"][:, 0:4], R["gmax"][:, 0:1], ALU.is_equal, [rk_("lg"), rk_("gmax")], [rk_("mg")]),
                    lambda: ts(P, R["pen"][:], R["mg"][:], -1.0, ALU.add, [rk_("mg")], [rk_("pen")], s2=1e30, op1=ALU.mult),
                ]
                for g in range(4):
                    st_.append(lambda g=g: ts(P, R["lem"][:, g * 8:(g + 1) * 8], R["lg"][:, 4 + g * 8:12 + g * 8], R["pen"][:, g:g + 1],
                                              ALU.add, [rk_("lg"), rk_("pen")], [rk_(("lem", g))]))
                st_ += [
                    lambda: red(P, R["e1"][:], R["lem"][:], ALU.max, lemk, [rk_("e1")]),
                    lambda: ts(P, R["m1"][:], R["lem"][:], R["e1"][:, 0:1], ALU.is_equal, lemk + [rk_("e1")], [rk_("m1")]),
                    lambda: stt(P, R["lem2"][:], R["m1"][:], -1e30, R["lem"][:], ALU.mult, ALU.add, lemk + [rk_("m1")], [rk_("lem2")]),
                    lambda: red(P, R["e2"][:], R["lem2"][:], ALU.max, [rk_("lem2")], [rk_("e2")]),
                    lambda: ts(P, R["m2"][:], R["lem2"][:], R["e2"][:, 0:1], ALU.is_equal, [rk_("lem2"), rk_("e2")], [rk_("m2")]),
                    lambda: tt(P, R["d"][:], R["e2"][:], R["e1"][:], ALU.subtract, [rk_("e1"), rk_("e2")], [rk_("d")]),
                    lambda: red(P, R["ssum"][:], R["eg"][:], ALU.add, [rk_("eg")], [rk_("ssum")]),
                    lambda: act(P, R["s2"][:], R["d"][:], AF.Sigmoid, [rk_("d")], [rk_("s2")]),
                    lambda: P.dve(lambda e: e.reciprocal(out=R["ptop"][:], in_=R["ssum"][:]), [rk_("ssum")], [rk_("ptop")]),
                    lambda: tt(P, R["w2"][:], R["ptop"][:], R["s2"][:], ALU.mult, [rk_("ptop"), rk_("s2")], [rk_("w2")]),
                    lambda: tt(P, R["w1"][:], R["ptop"][:], R["w2"][:], ALU.subtract, [rk_("ptop"), rk_("w2")], [rk_("w1")]),
                    lambda: ts(P, R["comb"][:], R["m1"][:], R["w1"][:, 0:1], ALU.mult, [rk_("m1"), rk_("w1")], [rk_("comb")]),
                    lambda: stt(P, R["comb2"][:], R["m2"][:], R["w2"][:, 0:1], R["comb"][:], ALU.mult, ALU.add,
                                [rk_("m2"), rk_("w2"), rk_("comb")], [rk_("comb2")]),
                    lambda: P.mm(lambda e: e.transpose(tps, R["comb2"][:], identf[:]), [rk_("comb2"), "ident"], [tkey]),
                    lambda: act(P, combT[:, tok], tps, AF.Identity, [tkey], [("combT", tb, sub)]),
                ]
                return st_

            for tb in range(NTB):
                tsl = slice(tb * TB, (tb + 1) * TB)
                emit_norm(P, nc, W, lambda k: x1T[:, k, tsl], a2, shift2,
                          [lambda k: h2T[:, k, tsl], lambda k: h2f[:, k, :]],
                          [("x1T", k_, tb) for k_ in range(NK)] + ["a2", "mod"],
                          [lambda k: ("h2T", k, tb), lambda k: ("h2f", k)], pb[0], "pb0")
                for half in range(2):
                    chains = [router_chain(tb, half * 2 + q_) for q_ in range(2)]
                    for stages in zip(*chains):
                        for f_ in stages:
                            f_()
            P.barrier()

        with ExitStack() as s3:
            T3 = lambda n, s, d: s3.enter_context(nc.sbuf_tensor(n, s, d))
            eselT = T3("eselT", [32, 32 * 128], F32)
            dma(P, eselT[:], esel[:, :], writes=["esel"])
            wgt = [T3(f"wgt{i}", [128, NK, FH], BF16) for i in range(2)]
            wut = [T3(f"wut{i}", [128, NK, FH], BF16) for i in range(2)]
            wdt = [T3(f"wdt{i}", [128, 4, D], BF16) for i in range(2)]
            actT = [T3(f"actT{i}", [128, 4, TB], BF16) for i in range(2)]
            sl = [T3(f"sl{i}", [128, TB], F32) for i in range(2)]
            pr = [T3(f"pr{i}", [128, TB], F32) for i in range(2)]
            def load_w(e_):
                wb = e_ % 2
                gv = w_gate[e_].rearrange("(k p) f -> p k f", p=128)
                uv = w_up[e_].rearrange("(k p) f -> p k f", p=128)
                dv = w_down[e_].rearrange("(k p) c -> p k c", p=128)
                for kh in range(2):
                    ksl = slice(kh * 4, kh * 4 + 4)
                    dma(P, wgt[wb][:, ksl, :], gv[:, ksl, :], writes=[("wgt", wb, kh)], q="gpsimd")
                    dma(P, wut[wb][:, ksl, :], uv[:, ksl, :], writes=[("wut", wb, kh)], q="gpsimd")
                for kh in range(2):
                    ksl = slice(kh * 2, kh * 2 + 2)
                    dma(P, wdt[wb][:, ksl, :], dv[:, ksl, :], writes=[("wdt", wb, kh)], q="gpsimd")

            def emit_gu(i):
                e_, tb = divmod(i, NTB)
                wb, ab, cb_ = e_ % 2, i % 2, 6 + i % 2
                tsl = slice(tb * TB, (tb + 1) * TB)
                h2k = [("h2T", k, tb) for k in range(NK)]
                mm_group(P, pb[cb_][:], [(eselT[:, e_ * 128:(e_ + 1) * 128], combT[:, tsl])],
                         ["esel"] + [("combT", tb, s_) for s_ in range(4)], [pk[cb_]])
                for fc in range(4):
                    fsl = slice(fc * 128, (fc + 1) * 128)
                    pa, pu = pb[2 * (fc % 2)], pb[2 * (fc % 2) + 1]
                    ka, ku = pk[2 * (fc % 2)], pk[2 * (fc % 2) + 1]
                    mm_group(P, pa[:], [(wgt[wb][:, k, fsl], h2T[:, k, tsl]) for k in range(NK)],
                             h2k + [("wgt", wb, 0), ("wgt", wb, 1)], [ka])
                    mm_group(P, pu[:], [(wut[wb][:, k, fsl], h2T[:, k, tsl]) for k in range(NK)],
                             h2k + [("wut", wb, 0), ("wut", wb, 1)], [ku])
                    act(P, sl[fc % 2][:], pa[:], AF.Silu, [ka], [("sl", fc % 2)])
                    tt(P, pr[fc % 2][:], sl[fc % 2][:], pu[:], ALU.mult, [("sl", fc % 2), ku], [("pr", fc % 2)])
                    tt(P, actT[ab][:, fc, :], pr[fc % 2][:], pb[cb_][:], ALU.mult, [("pr", fc % 2), pk[cb_]], [("actT", ab, fc)])

            def emit_down(i):
                e_, tb = divmod(i, NTB)
                wb, ab = e_ % 2, i % 2
                tsl = slice(tb * TB, (tb + 1) * TB)
                for dc in range(NK):
                    csl = slice(dc * 128, (dc + 1) * 128)
                    po, ko = pb[4 + dc % 2], pk[4 + dc % 2]
                    mm_group(P, po[:], [(wdt[wb][:, fc, csl], actT[ab][:, fc, :]) for fc in range(4)],
                             [("actT", ab, fc) for fc in range(4)] + [("wdt", wb, 0), ("wdt", wb, 1)], [ko])
                    stt(P, x1T[:, dc, tsl], po[:], gate2[:, dc:dc + 1], x1T[:, dc, tsl], ALU.mult, ALU.add,
                        [ko, "mod", ("x1T", dc, tb)], [("x1T", dc, tb)])

            nsteps = n_experts * NTB
            for i in range(nsteps + 1):
                if i < nsteps:
                    if i % NTB == 0:
                        load_w(i // NTB)
                    emit_gu(i)
                if i >= 1:
                    emit_down(i - 1)
            P.barrier()

        s23.close()
        ov = outT.rearrange("(k p) t -> p k t", p=128)
        if last:
            with ExitStack() as s4:
                T4 = lambda n, s, d: s4.enter_context(nc.sbuf_tensor(n, s, d))
                ob = [T4(f"ob{i}", [128, NK, TB], F32) for i in range(2)]
                for tb in range(NTB):
                    tsl = slice(tb * TB, (tb + 1) * TB)
                    o = ob[tb % 2]
                    emit_norm(P, nc, W, lambda k: x1T[:, k, tsl], gfs, None,
                              [lambda k: o[:, k, :]], [("x1T", k_, tb) for k_ in range(NK)] + ["gf"], [lambda k: ("ob", tb % 2, k)],
                              pb[0], "pb0")
                    dma(P, ov[:, :, tsl], o[:], reads=[("ob", tb % 2, k) for k in range(NK)])
                P.barrier()
        else:
            for tb in range(NTB):
                tsl = slice(tb * TB, (tb + 1) * TB)
                dma(P, ov[:, :, tsl], x1T[:, :, tsl], reads=[("x1T", k, tb) for k in range(NK)])
        P.emit()
    return nc


S_LEN = 8192
TA = 256
NCH = S_LEN // 128
MSCALE = 128.0 ** -0.5
ASCALE = 64.0 ** -0.5
NT_T = 324


SES_A = True
SES_B = True


def build_A(att_qblocks=16, do_mlstm=True):
    nc = bass.Bass("TRN2", target_bir_lowering=False)

    def din(name, shape, dt=F32):
        return nc.dram_tensor(name, shape, dt, kind="ExternalInput").ap()
    xT = din("xT", [D, S_LEN])
    cvec = din("cvec", [128, NK])
    w_ada = din("w_ada", [D, 2 * D])
    b_ada = din("b_ada", [128, 16])
    g1 = din("g1", [128, NK])
    w_F = din("w_F", [D, 512])
    b_F = din("b_F", [128, 4])
    w_T = din("w_T", [D, NT_T])
    b_T = din("b_T", [128, NT_T])
    cw = din("cw", [128, 10])
    cb = din("cb", [128, 2])
    gmr = din("gmr", [128, 128])
    gqk = din("gqk", [128, 2])
    cosT = din("cosT", [128, S_LEN])
    sinT = din("sinT", [128, S_LEN])
    ident = din("ident", [128, 128])
    masks = din("masks", [128, 256])
    rT = din("rT", [128, 128])
    oblk = din("oblk", [128, 128])
    ymT = nc.dram_tensor("ymT", [128, S_LEN], BF16, kind="ExternalOutput").ap()
    yaT = nc.dram_tensor("yaT", [128, S_LEN], BF16, kind="ExternalOutput").ap()
    NB = S_LEN // TA

    with ExitStack() as st:
        T = lambda n, s, d: st.enter_context(nc.sbuf_tensor(n, s, d))
        P = Prog(nc, same_engine_sync=SES_A)
        pb = [st.enter_context(nc.psum_tensor(f"pb{i}", [128, 512], F32)) for i in range(8)]
        pk = [f"pb{i}" for i in range(8)]
        QmT = T("QmT", [128, S_LEN], BF16)
        KmT = T("KmT", [128, S_LEN], BF16)
        Vaug = T("Vaug", [128, NCH, 129], BF16)
        osig = T("osig", [128, NCH, 128], BF16)
        G = T("G", [128, NCH, 4], F32)
        QaT = T("QaT", [128, S_LEN], BF16)
        KTa = T("KTa", [128, S_LEN], BF16)
        KTb = T("KTb", [128, S_LEN], BF16)
        Va = T("Va", [128, NCH, 65], BF16)
        W = dict(ones_bf=T("ones_bf", [128, 128], BF16), eps=T("eps", [128, 1], F32))
        identb = T("identb", [128, 128], BF16)
        mk = T("mk", [128, 256], F32)
        onesf = T("onesf", [128, 128], F32)
        one1 = T("one1", [128, 1], F32)
        rTs = T("rTs", [128, 128], F32)
        oblkb = T("oblkb", [128, 128], BF16)
        gmrs = T("gmrs", [128, 128], F32)
        gqks = T("gqks", [128, 2], F32)
        bFs = T("bFs", [128, 4], F32)
        bTs = T("bTs", [128, NT_T], F32)
        cws = T("cws", [128, 10], F32)
        cbs = T("cbs", [128, 2], F32)
        g1s = T("g1s", [128, NK], F32)
        a1 = T("a1", [128, NK], F32)
        P.pool(lambda e: e.memset(W["ones_bf"][:], 1.0), [], ["ones"])
        P.pool(lambda e: e.memset(W["eps"][:], EPS), [], ["eps"])
        P.pool(lambda e: e.memset(onesf[:], 1.0), [], ["onesf"])
        P.pool(lambda e: e.memset(one1[:], 1.0), [], ["one1"])
        P.pool(lambda e: e.memset(Vaug[:, :, 128:129], 1.0), [], ["Vaug1"])
        P.pool(lambda e: e.memset(Va[:, :, 64:65], 1.0), [], ["Va1"])
        P.pool(lambda e: e.memset(KTa[64:128, :], 0.0), [], ["KTa0"])
        P.pool(lambda e: e.memset(KTb[0:64, :], 0.0), [], ["KTb0"])
        dma(P, identb[:], ident[:, :], writes=["identb"], q="gpsimd")
        dma(P, oblkb[:], oblk[:, :], writes=["oblkb"], q="gpsimd")
        for t_, d_, k_ in [(mk, masks, "mk"), (rTs, rT, "rT"),
                           (gmrs, gmr, "gmr"), (gqks, gqk, "gqk"), (bFs, b_F, "bF"), (bTs, b_T, "bT"),
                           (cws, cw, "cw"), (cbs, cb, "cb"), (g1s, g1, "g1")]:
            dma(P, t_[:], d_[:, :], writes=[k_])

        mod = emit_mod(P, nc, st, pb, cvec, w_ada, b_ada, list(range(16)))
        stt(P, a1[:], mod[:, 8:16], 1.0, g1s[:], ALU.add, ALU.mult, ["mod", "g1"], ["a1"])
        shift1 = mod[:, 0:8]

        with ExitStack() as s1:
            T1 = lambda n, s, d: s1.enter_context(nc.sbuf_tensor(n, s, d))
            wF = T1("wF", [128, NK, 512], BF16)
            wT = T1("wT", [128, NK, NT_T], BF16)
            dma(P, wF[:], w_F.rearrange("(k p) c -> p k c", p=128), writes=["wF"], q="gpsimd")
            dma(P, wT[:], w_T.rearrange("(k p) c -> p k c", p=128), writes=["wT"], q="gpsimd")
            xb = [T1(f"xb{i}", [128, NK, TA], F32) for i in range(2)]
            hTs = [T1(f"hT{i}", [128, NK, TA], BF16) for i in range(2)]
            W.update(sq=T1("sq", [128, NK, TA], BF16), rt=T1("rt", [128, TA], F32), rstd=T1("rstd", [128, TA], F32),
                     tmp=[T1("ntmp0", [128, TA], F32), T1("ntmp1", [128, TA], F32)])
            W2 = dict(W)
            W2.update(sq=T1("sq2", [128, NK, TA], BF16), rt=T1("rt2", [128, TA], F32), rstd=T1("rstd2", [128, TA], F32),
                      tmp=[T1("ntmp20", [128, TA], F32), T1("ntmp21", [128, TA], F32)])
            Wp = [W, W2]
            NR = 3
            ring = [T1(f"ring{i}", [128, NR, TA], F32) for i in range(2)]
            acc = [T1(f"acc{i}", [128, TA], F32) for i in range(2)]
            cs_ = [T1(f"cosb{i}", [128, TA], F32) for i in range(2)]
            sn_ = [T1(f"sinb{i}", [128, TA], F32) for i in range(2)]
            RTMP = [{n_: T1(f"{n_}{i}", [128, TA], BF16 if n_ == "qsq" else F32)
                     for n_ in ("qf", "qsq", "qrt", "qrs", "qu", "qt1", "qt2")} for i in range(2)]
            tmpT = [T1(f"tmpT{i}", [128, NT_T], F32) for i in range(2)]
            xv = xT.rearrange("(k p) t -> p k t", p=128)

            def load_x(tb):
                tsl = slice(tb * TA, (tb + 1) * TA)
                for kh in range(2):
                    dma(P, xb[tb % 2][:, kh * 4:(kh + 1) * 4, :], xv[:, kh * 4:(kh + 1) * 4, tsl],
                        writes=[("xb", tb % 2, k) for k in range(kh * 4, kh * 4 + 4)])

            def conv_block(j, part="da"):
                tsl = slice(j * TA, (j + 1) * TA)
                for qk in range(2):
                    cur = ring[qk][:, j % NR, :]
                    a = acc[qk]
                    ak = ("acc", qk)
                    rk = lambda jj: ("ring", qk, jj % NR)
                    wcol = lambda k: cws[:, qk * 5 + k:qk * 5 + k + 1]
                    if "d" in part:
                        ts(P, a[:], cur, wcol(2), ALU.mult, [rk(j), "cw"], [ak])
                    for k in ((0, 1, 3, 4) if "d" in part else ()):
                        s_ = k - 2
                        if s_ < 0:
                            stt(P, a[:, -s_:TA], ring[qk][:, j % NR, 0:TA + s_], wcol(k), a[:, -s_:TA], ALU.mult, ALU.add,
                                [rk(j), "cw", ak], [ak])
                            if j > 0:
                                stt(P, a[:, 0:-s_], ring[qk][:, (j - 1) % NR, TA + s_:TA], wcol(k), a[:, 0:-s_], ALU.mult, ALU.add,
                                    [rk(j - 1), "cw", ak], [ak])
                        else:
                            stt(P, a[:, 0:TA - s_], ring[qk][:, j % NR, s_:TA], wcol(k), a[:, 0:TA - s_], ALU.mult, ALU.add,
                                [rk(j), "cw", ak], [ak])
                            if j < NB - 1:
                                stt(P, a[:, TA - s_:TA], ring[qk][:, (j + 1) % NR, 0:s_], wcol(k), a[:, TA - s_:TA], ALU.mult, ALU.add,
                                    [rk(j + 1), "cw", ak], [ak])
                    dest = (QmT if qk == 0 else KmT)
                    if "a" in part:
                        act(P, dest[:, tsl], a[:], AF.Silu, [ak, "cb"], [("QKm", qk, j)], bias=cbs[:, qk:qk + 1])

            def rope_norm(pf, pkey, bcol, gcol, dest, dkey, tb, rp):
                tsl = slice(tb * TA, (tb + 1) * TA)
                cb_, sb_ = cs_[tb % 2], sn_[tb % 2]
                R_ = RTMP[rp]
                qf, qsq, qrt, qrs, qu, qt1, qt2 = (R_[n_] for n_ in ("qf", "qsq", "qrt", "qrs", "qu", "qt1", "qt2"))
                kq = lambda n_: (n_, rp)
                pss, psr = pb[5 + rp], pk[5 + rp]

                def fin():
                    if dest is None:
                        tt(P, KTa[0:64, tsl], qt1[0:64, :], qrs[0:64, :], ALU.mult, [kq("qt1"), kq("qrs")], [("KTa", tb)])
                        tt(P, KTb[64:128, tsl], qt1[64:128, :], qrs[64:128, :], ALU.mult, [kq("qt1"), kq("qrs")], [("KTb", tb)])
                    else:
                        tt(P, dest[:, tsl], qt1[:], qrs[:], ALU.mult, [kq("qt1"), kq("qrs")], [(dkey, tb)])
                return [
                    lambda: act(P, qf[:], pf[:, 0:TA], AF.Identity, [pkey, "bF"], [kq("qf")], bias=bFs[:, bcol:bcol + 1]),
                    lambda: act(P, qsq[:], qf[:], AF.Square, [kq("qf")], [kq("qsq")]),
                    lambda: ts(P, qu[:], qf[:], gqks[:, gcol:gcol + 1], ALU.mult, [kq("qf"), "gqk"], [kq("qu")]),
                    lambda: mm_group(P, pss[:, 0:TA], [(oblkb[:], qsq[:])], [kq("qsq"), "oblkb"], [psr]),
                    lambda: tt(P, qt1[:], qu[:], cb_[:], ALU.mult, [kq("qu"), ("cos", tb % 2)], [kq("qt1")]),
                    lambda: act(P, qrt[:], pss[:, 0:TA], AF.Sqrt, [psr, "eps"], [kq("qrt")], bias=W["eps"][:, 0:1], scale=1.0 / 64),
                    lambda: mm_group(P, pss[:, 256:256 + TA], [(rTs[:], qu[:])], [kq("qu"), "rT"], [psr]),
                    lambda: P.dve(lambda e: e.reciprocal(out=qrs[:], in_=qrt[:]), [kq("qrt")], [kq("qrs")]),
                    lambda: tt(P, qt2[:], pss[:, 256:256 + TA], sb_[:], ALU.mult, [psr, ("sin", tb % 2)], [kq("qt2")]),
                    lambda: tt(P, qt1[:], qt1[:], qt2[:], ALU.add, [kq("qt1"), kq("qt2")], [kq("qt1")]),
                    fin,
                ]

            def norm_blk(tb, part):
                x_ = xb[tb % 2]
                hp = tb % 2
                hT = hTs[hp]
                nb_ = 0 if hp == 0 else 7
                emit_norm(P, nc, Wp[hp], lambda k: x_[:, k, :], a1, shift1, [lambda k: hT[:, k, :]],
                          [("xb", tb % 2, k_) for k_ in range(NK)] + ["a1", "mod"], [lambda k: ("hT", hp, k)],
                          pb[nb_], pk[nb_], n=TA, tag=f"n{hp}", xfull=x_[:, :, :], part=part)

            load_x(0)
            load_x(1)
            norm_blk(0, "ab")
            for tb in range(NB):
                tsl = slice(tb * TA, (tb + 1) * TA)
                dma(P, cs_[tb % 2][:], cosT[:, tsl], writes=[("cos", tb % 2)])
                dma(P, sn_[tb % 2][:], sinT[:, tsl], writes=[("sin", tb % 2)])
                if tb + 1 < NB:
                    norm_blk(tb + 1, "a")
                hp = tb % 2
                hT = hTs[hp]
                hk = [("hT", hp, k) for k in range(NK)]
                for fc in range(4):
                    bank = 1 + fc % 2
                    mm_group(P, pb[bank][:, 0:TA], [(wF[:, k, fc * 128:(fc + 1) * 128], hT[:, k, :]) for k in range(NK)],
                             hk + ["wF"], [pk[bank]])
                    if fc < 2:
                        act(P, ring[fc][:, tb % NR, :], pb[bank][:, 0:TA], AF.Identity, [pk[bank], "bF"], [("ring", fc, tb % NR)],
                            bias=bFs[:, fc:fc + 1])
                        if fc == 1 and tb >= 1:
                            conv_block(tb - 1, "d")
                    elif fc == 2:
                        chain_q = rope_norm(pb[bank], pk[bank], 2, 0, QaT, "QaT", tb, 0)
                    else:
                        chain_k = rope_norm(pb[bank], pk[bank], 3, 1, None, "KT", tb, 1)
                for sq_, sk_ in zip(chain_q, chain_k):
                    sq_()
                    sk_()
                if tb + 1 < NB:
                    norm_blk(tb + 1, "b")
                for sub in range(TA // 128):
                    ch = tb * (TA // 128) + sub
                    bank = 3 + sub % 2
                    mm_group(P, pb[bank][:, 0:NT_T], [(hT[:, k, sub * 128:(sub + 1) * 128], wT[:, k, :]) for k in range(NK)],
                             hk + ["wT"], [pk[bank]])
                    tm = tmpT[sub % 2]
                    tk = ("tmpT", sub % 2)
                    tt(P, tm[:], pb[bank][:, 0:NT_T], bTs[:], ALU.add, [pk[bank], "bT"], [tk])
                    cp(P, Vaug[:, ch, 0:128], tm[:, 0:128], [tk], [("Vaug", ch)], eng="scalar")
                    act(P, osig[:, ch, :], tm[:, 128:256], AF.Sigmoid, [tk], [("osig", ch)])
                    cp(P, G[:, ch, :], tm[:, 256:260], [tk], [("G", ch)], eng="scalar")
                    cp(P, Va[:, ch, 0:64], tm[:, 260:324], [tk], [("Va", ch)], eng="scalar")
                if tb >= 1:
                    conv_block(tb - 1, "a")
                if tb + 2 < NB:
                    load_x(tb + 2)
            conv_block(NB - 1)
            P.barrier()

        if do_mlstm:
          with ExitStack() as s2:
            T2 = lambda n, s, d: s2.enter_context(nc.sbuf_tensor(n, s, d))
            hfwd = T2("hfwd", [128, NCH, 128], F32)
            Gk = [("G", ch) for ch in range(NCH)]
            ge = T2("ge", [128, NCH, 2], F32)
            lfn = T2("lfn", [128, NCH, 2], F32)
            dirs = []
            for d_ in range(2):
                dd = {n: T2(f"{n}{d_}", [128, NCH], F32) for n in ("b", "imb", "w", "ws", "flo", "ebl")}
                dirs.append(dd)
            Cst = T2("Cst", [128, 129], F32)
            Cbf = T2("Cbf", [128, 129], BF16)
            Ktok = [T2(f"Ktok{i}", [128, 128], BF16) for i in range(2)]
            Vw = [T2(f"Vw{i}", [128, 129], BF16) for i in range(2)]
            Sp = [T2(f"Sp{i}", [128, 128], BF16) for i in range(2)]
            den = [T2(f"den{i}", [128, 1], F32) for i in range(2)]
            rden = [T2(f"rden{i}", [128, 1], F32) for i in range(2)]
            hs = [T2(f"hs{i}", [128, 128], F32) for i in range(2)]
            hsq = [T2(f"hsq{i}", [128, 128], F32) for i in range(2)]
            ss = [T2(f"ss{i}", [128, 1], F32) for i in range(2)]
            srt = [T2(f"srt{i}", [128, 1], F32) for i in range(2)]
            srn = [T2(f"srn{i}", [128, 1], F32) for i in range(2)]
            yt = [T2(f"yt{i}", [128, 128], F32) for i in range(2)]
            y2 = [T2(f"y2{i}", [128, 128], BF16) for i in range(2)]
            ymb = [T2(f"ymb{i}", [128, 512], BF16) for i in range(2)]
            for d_ in range(2):
                fcol = 1 + 2 * d_
                act(P, ge[:, :, d_], G[:, :, fcol], AF.Exp, Gk, [("ge", d_)], scale=-1.0)
                act(P, lfn[:, :, d_], ge[:, :, d_], AF.Ln, [("ge", d_), "one1"], [("lfn", d_)], bias=one1[:, 0:1])
            for d_ in range(2):
                dd = dirs[d_]
                icol = 2 * d_
                mslice = mk[:, d_ * 128:(d_ + 1) * 128]
                mm_group(P, pb[0][:, 0:NCH], [(mslice, lfn[:, :, d_])], [("lfn", d_), "mk"], [pk[0]])
                mm_group(P, pb[1][:, 0:NCH], [(onesf[:], lfn[:, :, d_])], [("lfn", d_), "onesf"], [pk[1]])
                cp(P, dd["b"][:], pb[0][:, 0:NCH], [pk[0]], [("mb", d_)])
                tt(P, dd["imb"][:], G[:, :, icol], dd["b"][:], ALU.add, Gk + [("mb", d_)], [("imb", d_)])
                act(P, dd["w"][:], dd["imb"][:], AF.Exp, [("imb", d_)], [("w", d_)])
                ts(P, dd["ws"][:], dd["w"][:], MSCALE, ALU.mult, [("w", d_)], [("ws", d_)])
                act(P, dd["flo"][:], dd["b"][:], AF.Exp, [("mb", d_)], [("flo", d_)])
                act(P, dd["ebl"][:], pb[1][:, 0:NCH], AF.Exp, [pk[1]], [("ebl", d_)], scale=-1.0)
            hbwd = T2("hbwd", [128, NCH, 128], F32)
            Cst2 = [Cst, T2("Cst_b", [128, 129], F32)]
            Cbf2 = [Cbf, T2("Cbf_b", [128, 129], BF16)]
            for d_ in range(2):
                P.dve(lambda e, o=Cst2[d_][:]: e.memset(o, 0.0), [], [("Cst", d_)])
                P.dve(lambda e, o=Cbf2[d_][:]: e.memset(o, 0.0), [], [("Cbf", d_)])
            hdir = [hfwd, hbwd]

            def mchunk(d_, c):
                dd = dirs[d_]
                mslice = mk[:, d_ * 128:(d_ + 1) * 128]
                p2 = d_
                Cs, Cb = Cst2[d_], Cbf2[d_]
                csl = slice(c * 128, (c + 1) * 128)
                tb_q = c // (TA // 128)
                qk_keys = [("QKm", 0, tb_q), ("QKm", 1, tb_q)]
                tbank = 2 if d_ == 0 else 7
                P.mm(lambda e, o=pb[tbank][:].bitcast(BF16)[:, 0:128], i_=KmT[:, csl]: e.transpose(o, i_, identb[:]),
                     [("QKm", 1, tb_q), "identb"], [pk[tbank]])
                cp(P, Ktok[p2][:], pb[tbank][:].bitcast(BF16)[:, 0:128], [pk[tbank]], [("Ktok", p2)], eng="scalar")
                act(P, Vw[p2][:], Vaug[:, c, :], AF.Identity, [("Vaug", c), "Vaug1", ("w", d_)], [("Vw", p2)], scale=dd["w"][:, c:c + 1])
                sb_ = 3 + p2
                mm_group(P, pb[sb_][:, 0:128], [(KmT[:, csl], QmT[:, csl])], qk_keys, [pk[sb_]])
                stt(P, Sp[p2][:], pb[sb_][:, 0:128], dd["ws"][:, c:c + 1], mslice, ALU.mult, ALU.mult,
                    [pk[sb_], ("ws", d_), "mk"], [("Sp", p2)])
                ob_ = 5 + p2
                mm_group(P, pb[ob_][:, 0:129], [(QmT[:, csl], Cb[:]), (Sp[p2][:], Vaug[:, c, :])],
                         qk_keys + [("Cbf", d_), ("Sp", p2), ("Vaug", c), "Vaug1"], [pk[ob_]])
                act(P, den[p2][:], pb[ob_][:, 128:129], AF.Abs, [pk[ob_]], [("den", p2)])
                tt(P, den[p2][:], den[p2][:], dd["flo"][:, c:c + 1], ALU.max, [("den", p2), ("flo", d_)], [("den", p2)])
                P.dve(lambda e, o=rden[p2][:], i_=den[p2][:]: e.reciprocal(out=o, in_=i_), [("den", p2)], [("rden", p2)])
                ts(P, hdir[d_][:, c, :], pb[ob_][:, 0:128], rden[p2][:, 0:1], ALU.mult, [pk[ob_], ("rden", p2)], [("hdir", d_, c)])
                mm_group(P, pb[p2][:, 0:129], [(Ktok[p2][:], Vw[p2][:])], [("Ktok", p2), ("Vw", p2)], [pk[p2]])
                ts(P, Cs[:], Cs[:], dd["ebl"][:, c:c + 1], ALU.mult, [("Cst", d_), ("ebl", d_)], [("Cst", d_)])
                stt(P, Cs[:], pb[p2][:, 0:129], dd["ebl"][:, c:c + 1], Cs[:], ALU.mult, ALU.add,
                    [pk[p2], ("ebl", d_), ("Cst", d_)], [("Cst", d_)])
                act(P, Cb[:], Cs[:], AF.Identity, [("Cst", d_)], [("Cbf", d_)], scale=MSCALE)

            for i in range(NCH):
                mchunk(0, i)
                mchunk(1, NCH - 1 - i)
            for c in range(NCH):
                p2 = c % 2
                tt(P, hs[p2][:], hfwd[:, c, :], hbwd[:, c, :], ALU.add, [("hdir", 0, c), ("hdir", 1, c)], [("hs", p2)])
                tt(P, hsq[p2][:], hs[p2][:], hs[p2][:], ALU.mult, [("hs", p2)], [("hsq", p2)])
                red(P, ss[p2][:], hsq[p2][:], ALU.add, [("hsq", p2)], [("ss", p2)])
                act(P, srt[p2][:], ss[p2][:], AF.Sqrt, [("ss", p2), "eps"], [("srt", p2)], bias=W["eps"][:, 0:1], scale=1.0 / 128)
                P.dve(lambda e, o=srn[p2][:], i_=srt[p2][:]: e.reciprocal(out=o, in_=i_), [("srt", p2)], [("srn", p2)])
                stt(P, yt[p2][:], hs[p2][:], srn[p2][:, 0:1], gmrs[:], ALU.mult, ALU.mult,
                    [("hs", p2), ("srn", p2), "gmr"], [("yt", p2)])
                tt(P, y2[p2][:], yt[p2][:], osig[:, c, :], ALU.mult, [("yt", p2), ("osig", c)], [("y2", p2)])
                ybank = 3 + p2
                P.mm(lambda e, o=pb[ybank][:].bitcast(BF16)[:, 0:128], i_=y2[p2][:]: e.transpose(o, i_, identb[:]),
                     [("y2", p2), "identb"], [pk[ybank]])
                grp = c // 4
                yb = ymb[grp % 2]
                cp(P, yb[:, (c % 4) * 128:(c % 4 + 1) * 128], pb[ybank][:].bitcast(BF16)[:, 0:128], [pk[ybank]],
                   [("ymb", grp % 2, c % 4)], eng="scalar")
                if c % 4 == 3:
                    dma(P, ymT[:, grp * 512:(grp + 1) * 512], yb[:], reads=[("ymb", grp % 2, q_) for q_ in range(4)])
            P.barrier()

        with ExitStack() as s3:
            T3 = lambda n, s, d: s3.enter_context(nc.sbuf_tensor(n, s, d))
            pT = [T3(f"pT{i}", [128, 512], BF16) for i in range(4)]
            SB = [0, 1, 2, 6]
            osb = [T3(f"osb{i}", [64, 512], F32) for i in range(2)]
            rec = T3("rec", [128, 512], F32)
            yab = [T3(f"yab{i}", [64, 512], BF16) for i in range(2)]
            NTQ = 512 // TA
            jobs = [(qb, h) for qb in range(att_qblocks) for h in range(2)]
            steps = [(ji, kc) for ji in range(len(jobs)) for kc in range(NCH)]
            LOOK = 3

            def emit_qk(i):
                ji, kc = steps[i]
                qb, h = jobs[ji]
                hsl = slice(h * 64, (h + 1) * 64)
                qsl = slice(qb * 512, (qb + 1) * 512)
                ksl = slice(kc * 128, (kc + 1) * 128)
                sb_ = i % 4
                bk_ = SB[sb_]
                KT_ = KTa if h == 0 else KTb
                mm_group(P, pb[bk_][:], [(KT_[:, ksl], QaT[:, qsl])],
                         [("KTa" if h == 0 else "KTb", kc // (TA // 128)), "KTa0", "KTb0"]
                         + [("QaT", qb * NTQ + i_) for i_ in range(NTQ)], [pk[bk_]])
                act(P, pT[sb_][:], pb[bk_][:], AF.Exp, [pk[bk_]], [("pT", sb_)], scale=ASCALE)

            def emit_pv(i):
                ji, kc = steps[i]
                qb, h = jobs[ji]
                hsl = slice(h * 64, (h + 1) * 64)
                qsl = slice(qb * 512, (qb + 1) * 512)
                sb_ = i % 4
                ob_ = 3 + ji % 2
                P.mm(lambda e, o=pb[ob_][0:65, :], l_=Va[:, kc, :], r_=pT[sb_][:], a_=(kc == 0), z_=(kc == NCH - 1):
                     e.matmul(o, lhsT=l_, rhs=r_, start=a_, stop=z_),
                     [("Va", kc), "Va1", ("pT", sb_)], [pk[ob_]])
                if kc == NCH - 1:
                    jb = ji % 2
                    P.dve(lambda e, o=rec[64:65, :], i_=pb[ob_][64:65, :]: e.reciprocal(out=o, in_=i_), [pk[ob_]], ["rec"])
                    mm_group(P, pb[5][0:64, :], [(onesf[64:65, 0:64], rec[64:65, :])], ["rec", "onesf"], [pk[5]])
                    cp(P, osb[jb][:], pb[ob_][0:64, :], [pk[ob_]], [("osb", jb)], eng="gpsimd" if False else "vector")
                    tt(P, yab[jb][:], osb[jb][:], pb[5][0:64, :], ALU.mult, [("osb", jb), pk[5]], [("yab", jb)])
                    dma(P, yaT[hsl, qsl], yab[jb][:], reads=[("yab", jb)])

            for i in range(len(steps) + LOOK):
                if i < len(steps):
                    emit_qk(i)
                if i - LOOK >= 0:
                    emit_pv(i - LOOK)
            P.barrier()
        P.emit()
    return nc


OFF = dict(mq=0, mk=512, mv=1024, mo=1536, gates=2048, aq=2064, ak=2576, av=2704, gm=2832, ga=3856, end=4880)


def _pk(v, n):
    return np.ascontiguousarray(np.asarray(v, np.float32).reshape(n, 128).T)


def _consts():
    esel = np.zeros((32, 32, 128), np.float32)
    for e in range(32):
        esel[e, e, :] = 1.0
    return dict(esel=esel.reshape(32, 32 * 128), ident=np.eye(128, dtype=np.float32))


def prep_B(inp, l, b, r, xT_b, ymT_b, yaT_b):
    tok = slice(r * NT_B, (r + 1) * NT_B)
    w_in = inp["w_in"][l]
    b_in = inp["b_in"][l]
    m = dict(
        xT=np.ascontiguousarray(xT_b[:, tok]),
        ymT=np.ascontiguousarray(ymT_b[:, tok]),
        yaT=np.ascontiguousarray(yaT_b[:, tok]),
        cvec=_pk(inp["c"][b], 8),
        w_ada=np.ascontiguousarray(inp["w_ada"][l]),
        b_ada=_pk(inp["b_ada"][l], 48),
        g1=_pk(inp["norm1_g"][l], 8), g2=_pk(inp["norm2_g"][l], 8), gf=_pk(inp["final_norm_g"], 8),
        w_g=np.ascontiguousarray(w_in[:, OFF["gm"]:OFF["end"]]),
        b_g=_pk(b_in[OFF["gm"]:OFF["end"]], 16),
        w_bm=np.ascontiguousarray(inp["w_branch_m"][l]),
        w_ba=np.ascontiguousarray(inp["w_branch_a"][l]),
        w_o=np.ascontiguousarray(inp["w_out"][l]),
        w_r=np.ascontiguousarray(np.concatenate([inp["w_router_group"][l], inp["w_router_expert"][l]], axis=1)),
        b_r=np.ascontiguousarray(np.broadcast_to(
            np.concatenate([inp["b_router_group"][l], inp["b_router_expert"][l]])[None, :], (128, 36))),
        w_gate=np.ascontiguousarray(inp["w_gate"][l]),
        w_up=np.ascontiguousarray(inp["w_up"][l]),
        w_down=np.ascontiguousarray(inp["w_down"][l]),
    )
    m.update(_consts())
    return m


def _rope_consts():
    rows = S_LEN // 64
    row = np.repeat(np.arange(rows, dtype=np.float32), 64)
    col = np.tile(np.arange(64, dtype=np.float32), rows)
    half = 32
    inv_freq = (np.float32(10000.0) ** (-np.arange(0, half, 2, dtype=np.float32) / np.float32(half))).astype(np.float32)
    ang_r = (row[:, None] * inv_freq).astype(np.float32)
    ang_c = (col[:, None] * inv_freq).astype(np.float32)
    cosT = np.zeros((64, S_LEN), np.float32)
    sinT = np.zeros((64, S_LEN), np.float32)
    for i in range(64):
        ang = ang_r if i < 32 else ang_c
        cosT[i] = np.cos(ang[:, i % 16])
        sinT[i] = np.sin(ang[:, i % 16])
    R = np.zeros((64, 64), np.float32)
    for i in range(64):
        if i % 32 < 16:
            R[i, i + 16] = -1.0
        else:
            R[i, i - 16] = 1.0
    R2 = np.zeros((128, 128), np.float32)
    R2[:64, :64] = R
    R2[64:, 64:] = R
    oblk = np.zeros((128, 128), np.float32)
    oblk[:64, :64] = 1.0
    oblk[64:, 64:] = 1.0
    masks = np.concatenate([np.triu(np.ones((128, 128), np.float32)), np.tril(np.ones((128, 128), np.float32))], axis=1)
    return dict(cosT=np.ascontiguousarray(np.tile(cosT, (2, 1))), sinT=np.ascontiguousarray(np.tile(sinT, (2, 1))),
                rT=np.ascontiguousarray(R2.T), oblk=oblk, masks=np.ascontiguousarray(masks),
                ident=np.eye(128, dtype=np.float32))


_ROPE = None


def prep_A(inp, l, b, r, xT_b):
    global _ROPE
    if _ROPE is None:
        _ROPE = _rope_consts()
    w_in = inp["w_in"][l]
    b_in = inp["b_in"][l]
    kv = r // 2
    fcols = np.concatenate([np.arange(OFF["mq"] + r * 128, OFF["mq"] + (r + 1) * 128),
                            np.arange(OFF["mk"] + r * 128, OFF["mk"] + (r + 1) * 128),
                            np.arange(OFF["aq"] + r * 128, OFF["aq"] + (r + 1) * 128),
                            np.arange(OFF["ak"] + kv * 64, OFF["ak"] + (kv + 1) * 64),
                            np.arange(OFF["ak"] + kv * 64, OFF["ak"] + (kv + 1) * 64)])
    tcols = np.concatenate([np.arange(OFF["mv"] + r * 128, OFF["mv"] + (r + 1) * 128),
                            np.arange(OFF["mo"] + r * 128, OFF["mo"] + (r + 1) * 128),
                            OFF["gates"] + np.arange(4) * 4 + r,
                            np.arange(OFF["av"] + kv * 64, OFF["av"] + (kv + 1) * 64)])
    cwl = inp["conv_w"][l][:, 0, :]
    cw = np.zeros((128, 10), np.float32)
    cb = np.zeros((128, 2), np.float32)
    for qk in range(2):
        ch = slice(qk * 512 + r * 128, qk * 512 + (r + 1) * 128)
        cw[:, qk * 5:(qk + 1) * 5] = cwl[:, ch].T
        cb[:, qk] = inp["conv_b"][l][ch]
    m = dict(
        xT=xT_b,
        cvec=_pk(inp["c"][b], 8),
        w_ada=np.ascontiguousarray(inp["w_ada"][l][:, 0:2 * D]),
        b_ada=_pk(inp["b_ada"][l][0:2 * D], 16),
        g1=_pk(inp["norm1_g"][l], 8),
        w_F=np.ascontiguousarray(w_in[:, fcols]),
        b_F=_pk(b_in[fcols], 4),
        w_T=np.ascontiguousarray(w_in[:, tcols]),
        b_T=np.ascontiguousarray(np.broadcast_to(b_in[tcols][None, :], (128, NT_T))),
        cw=cw, cb=cb,
        gmr=np.ascontiguousarray(np.broadcast_to(inp["mlstm_norm_g"][l][r * 128:(r + 1) * 128][None, :], (128, 128))),
        gqk=np.ascontiguousarray(np.stack([np.tile(inp["q_norm_g"][l], 2), np.tile(inp["k_norm_g"][l], 2)], axis=1)),
    )
    m.update(_ROPE)
    return m


def kernel(**inputs):
    inp = {k: np.asarray(v) for k, v in inputs.items()}
    cores = list(range(8))
    xT = [np.ascontiguousarray(inp["x"][b].T) for b in range(2)]
    for l in range(2):
        ncA = build_A()
        resA = run_bass_kernel_spmd(ncA, [prep_A(inp, l, c // 4, c % 4, xT[c // 4]) for c in cores], core_ids=cores)
        ymT = [np.concatenate([resA.results[b * 4 + r]["ymT"] for r in range(4)], axis=0) for b in range(2)]
        yaT = [np.concatenate([resA.results[b * 4 + r]["yaT"] for r in range(4)], axis=0) for b in range(2)]
        del resA
        ncB = build_B(last=(l == 1))
        resB = run_bass_kernel_spmd(ncB, [prep_B(inp, l, c // 4, c % 4, xT[c // 4], ymT[c // 4], yaT[c // 4]) for c in cores],
                                    core_ids=cores)
        xT = [np.concatenate([resB.results[b * 4 + r]["outT"] for r in range(4)], axis=1) for b in range(2)]
        del resB
    return np.ascontiguousarray(np.stack([xT[b].T for b in range(2)])).astype(np.float32)
```
